# Optimizing a Trainium2 kernel written in Bass

```python
import math
import jax, jax.numpy as jnp
from jax import lax
import numpy as np

D_MODEL = 2048
BATCH = 1
SEQ = 8192
DEPTH = 4

HEAD_DIM = 64
SWA_Q_HEADS = D_MODEL // 128
SWA_KV_HEADS = SWA_Q_HEADS // 4
WINDOW = 128
SB_HEADS = D_MODEL // 128
DIFF_HEADS = D_MODEL // 256
DIFF_DIM = HEAD_DIM
BLOCK_Q = 128
BRANCH_WIDTH = SWA_Q_HEADS * HEAD_DIM
A_KV = SWA_KV_HEADS * HEAD_DIM
N_BRANCHES = 3
IN_SPLITS = (BRANCH_WIDTH, A_KV, A_KV, BRANCH_WIDTH, BRANCH_WIDTH, BRANCH_WIDTH,
             BRANCH_WIDTH, BRANCH_WIDTH, BRANCH_WIDTH)
D_IN = 7 * BRANCH_WIDTH + 2 * A_KV
N_GROUPS = 8
EXPERTS_PER_GROUP = 8
N_EXPERTS = N_GROUPS * EXPERTS_PER_GROUP
TOP_K_EXPERT = 2
D_EXPERT = 3 * D_MODEL // 16
DISPATCH_BLOCK = 128
ALPHA = (2.0 * DEPTH) ** 0.25
BETA = (8.0 * DEPTH) ** -0.25
LN_EPS = 1e-5
RMS_EPS = 1e-5

kernel_name = "hybrid_gated_swa_stickbreak_diffattn_hmoe"


def layer_norm(x, g, b):
    xf = x.astype(jnp.float32)
    mu = jnp.mean(xf, axis=-1, keepdims=True)
    var = jnp.mean(jnp.square(xf - mu), axis=-1, keepdims=True)
    y = (xf - mu) * lax.rsqrt(var + LN_EPS)
    return (y * g.astype(jnp.float32) + b.astype(jnp.float32)).astype(x.dtype)


def rms_norm(x, g):
    xf = x.astype(jnp.float32)
    y = xf * lax.rsqrt(jnp.mean(jnp.square(xf), axis=-1, keepdims=True) + RMS_EPS)
    return (y * g.astype(jnp.float32)).astype(x.dtype)


def alibi_slopes(n_heads):
    return jnp.exp2(-8.0 * jnp.arange(1, n_heads + 1, dtype=jnp.float32) / n_heads)


def swa_sink_attention(q, k, v, sinks):
    B_, S_ = q.shape[0], q.shape[1]
    nb = S_ // BLOCK_Q
    G = SWA_Q_HEADS // SWA_KV_HEADS
    qb = q.reshape(B_, nb, BLOCK_Q, SWA_KV_HEADS, G, HEAD_DIM)
    pad = ((0, 0), (BLOCK_Q, 0), (0, 0), (0, 0))
    kp = jnp.pad(k, pad).reshape(B_, nb + 1, BLOCK_Q, SWA_KV_HEADS, HEAD_DIM)
    vp = jnp.pad(v, pad).reshape(B_, nb + 1, BLOCK_Q, SWA_KV_HEADS, HEAD_DIM)
    kb = jnp.concatenate([kp[:, :-1], kp[:, 1:]], axis=2)
    vb = jnp.concatenate([vp[:, :-1], vp[:, 1:]], axis=2)
    s = jnp.einsum('bnqhgd,bnkhd->bnhgqk', qb, kb).astype(jnp.float32) / math.sqrt(HEAD_DIM)
    qi = jnp.arange(BLOCK_Q)[:, None]
    ki = jnp.arange(2 * BLOCK_Q)[None, :]
    dist = qi + BLOCK_Q - ki
    key_pos = jnp.arange(nb)[:, None] * BLOCK_Q - BLOCK_Q + jnp.arange(2 * BLOCK_Q)[None, :]
    valid = ((dist >= 0) & (dist < WINDOW))[None] & (key_pos >= 0)[:, None, :]
    slopes = alibi_slopes(SWA_Q_HEADS).reshape(SWA_KV_HEADS, G)
    s = s - slopes[:, :, None, None] * dist.astype(jnp.float32)
    s = jnp.where(valid[None, :, None, None], s, -jnp.inf)
    sink = jnp.broadcast_to(sinks.astype(jnp.float32).reshape(SWA_KV_HEADS, G)[None, None, :, :, None, None],
                            s.shape[:-1] + (1,))
    p = jax.nn.softmax(jnp.concatenate([s, sink], axis=-1), axis=-1)[..., :-1]
    o = jnp.einsum('bnhgqk,bnkhd->bnqhgd', p.astype(v.dtype), vb)
    return o.reshape(B_, S_, SWA_Q_HEADS * HEAD_DIM)


def stick_breaking_attention(q, k, v):
    B_, S_, H, Dh = q.shape
    nb = S_ // BLOCK_Q
    qb = jnp.moveaxis(q.reshape(B_, nb, BLOCK_Q, H, Dh), 1, 0)
    key_pos = jnp.arange(S_)
    scale = 1.0 / math.sqrt(Dh)

    def one_block(args):
        q_blk, n = args
        z = jnp.einsum('bqhd,bkhd->bhqk', q_blk, k).astype(jnp.float32) * scale
        q_pos = n * BLOCK_Q + jnp.arange(BLOCK_Q)
        causal = key_pos[None, :] < q_pos[:, None]
        log_rem = jnp.where(causal, jax.nn.log_sigmoid(-z), 0.0)
        after = lax.cumsum(log_rem, axis=3, reverse=True) - log_rem
        w = jnp.where(causal, jnp.exp(jax.nn.log_sigmoid(z) + after), 0.0)
        return jnp.einsum('bhqk,bkhd->bqhd', w.astype(v.dtype), v)

    o = lax.map(one_block, (qb, jnp.arange(nb)))
    return jnp.moveaxis(o, 0, 1).reshape(B_, S_, H * Dh)


def diff_attention(q, k, v, lam, lambda_init, subln_g):
    B_, S_, H = q.shape[0], q.shape[1], q.shape[2]
    nb = S_ // BLOCK_Q
    qb = jnp.moveaxis(q.reshape(B_, nb, BLOCK_Q, H, 2, DIFF_DIM), 1, 0)
    key_pos = jnp.arange(S_)
    slopes = alibi_slopes(H)
    scale = 1.0 / math.sqrt(DIFF_DIM)

    def one_block(args):
        q_blk, n = args
        s = jnp.einsum('bqhmd,bkhmd->bhmqk', q_blk, k).astype(jnp.float32) * scale
        q_pos = n * BLOCK_Q + jnp.arange(BLOCK_Q)
        dist = (q_pos[:, None] - key_pos[None, :]).astype(jnp.float32)
        s = s - slopes[:, None, None, None] * dist
        s = jnp.where(dist >= 0, s, -jnp.inf)
        p = jax.nn.softmax(s, axis=-1)
        a = p[:, :, 0] - lam * p[:, :, 1]
        return jnp.einsum('bhqk,bkhd->bqhd', a.astype(v.dtype), v)

    o = jnp.moveaxis(lax.map(one_block, (qb, jnp.arange(nb))), 0, 1).reshape(B_, S_, H, 2 * DIFF_DIM)
    o = rms_norm(o, subln_g) * (1.0 - lambda_init)
    return o.reshape(B_, S_, H * 2 * DIFF_DIM)


def mixer_sublayer(h, w_in, w_gate, b_gate, sinks, lq1, lk1, lq2, lk2, subln_g, w_branch, w_out,
                   lambda_init):
    B_, S_, _ = h.shape
    proj = h @ w_in
    offs = []
    acc = 0
    for n in IN_SPLITS[:-1]:
        acc += n
        offs.append(acc)
    qa, ka, va, qb, kb, vb, qc, kc, vc = jnp.split(proj, offs, axis=-1)
    o_a = swa_sink_attention(qa.reshape(B_, S_, SWA_Q_HEADS, HEAD_DIM),
                             ka.reshape(B_, S_, SWA_KV_HEADS, HEAD_DIM),
                             va.reshape(B_, S_, SWA_KV_HEADS, HEAD_DIM), sinks)
    o_b = stick_breaking_attention(qb.reshape(B_, S_, SB_HEADS, HEAD_DIM),
                                   kb.reshape(B_, S_, SB_HEADS, HEAD_DIM),
                                   vb.reshape(B_, S_, SB_HEADS, HEAD_DIM))
    f32 = jnp.float32
    lam = (jnp.exp(jnp.sum(lq1.astype(f32) * lk1.astype(f32)))
           - jnp.exp(jnp.sum(lq2.astype(f32) * lk2.astype(f32))) + lambda_init)
    o_c = diff_attention(qc.reshape(B_, S_, DIFF_HEADS, 2, DIFF_DIM),
                         kc.reshape(B_, S_, DIFF_HEADS, 2, DIFF_DIM),
                         vc.reshape(B_, S_, DIFF_HEADS, 2 * DIFF_DIM), lam, lambda_init, subln_g)
    branches = jnp.stack([o_a, o_b, o_c], axis=2)
    y = jnp.einsum('bsnf,nfd->bsnd', branches, w_branch)
    gates = jax.nn.sigmoid(h @ w_gate + b_gate).reshape(B_, S_, N_BRANCHES, D_MODEL)
    return jnp.einsum('bsnd,de->bse', gates * y, w_out)


def hierarchical_moe(h, w_rg, b_rg, w_re, b_re, w_g, w_u, w_d):
    B_, S_, D = h.shape
    T = B_ * S_
    xf = h.reshape(T, D)
    group_logits = (xf @ w_rg + b_rg).astype(jnp.float32)
    group_prob = jax.nn.softmax(group_logits, axis=-1)
    g_top = jnp.argmax(group_logits, axis=-1)
    p_group = jnp.take_along_axis(group_prob, g_top[:, None], axis=-1)
    exp_logits = (xf @ w_re + b_re).astype(jnp.float32).reshape(T, N_GROUPS, EXPERTS_PER_GROUP)
    in_group = jnp.take_along_axis(exp_logits, g_top[:, None, None], axis=1)[:, 0]
    top_val, top_idx = lax.top_k(in_group, TOP_K_EXPERT)
    w_top = jax.nn.softmax(top_val, axis=-1) * p_group
    expert_id = g_top[:, None].astype(jnp.int32) * EXPERTS_PER_GROUP + top_idx.astype(jnp.int32)

    A = T * TOP_K_EXPERT
    DB = DISPATCH_BLOCK
    flat_e = expert_id.reshape(A)
    flat_tok = jnp.repeat(jnp.arange(T, dtype=jnp.int32), TOP_K_EXPERT)
    flat_w = w_top.reshape(A)
    counts = jnp.zeros((N_EXPERTS,), jnp.int32).at[flat_e].add(1)
    padded = (counts + DB - 1) // DB * DB
    start = jnp.cumsum(counts) - counts
    pend = jnp.cumsum(padded)
    pstart = pend - padded
    order = jnp.argsort(flat_e, stable=True)
    se = flat_e[order]
    dest = pstart[se] + (jnp.arange(A, dtype=jnp.int32) - start[se])
    P = ((A + DB - 1) // DB) * DB + N_EXPERTS * DB
    nblk = P // DB
    buf_tok = jnp.full((P,), T, jnp.int32).at[dest].set(flat_tok[order])
    buf_w = jnp.zeros((P,), jnp.float32).at[dest].set(flat_w[order])
    block_e = jnp.minimum(jnp.searchsorted(pend, jnp.arange(nblk, dtype=jnp.int32) * DB, side='right'),
                          N_EXPERTS - 1)
    x_pad = jnp.concatenate([xf, jnp.zeros((1, D), xf.dtype)], axis=0)
    xb = x_pad[buf_tok].reshape(nblk, DB, D)

    def expert_block(args):
        xblk, e = args
        return (jax.nn.silu(xblk @ w_g[e]) * (xblk @ w_u[e])) @ w_d[e]

    yb = lax.map(expert_block, (xb, block_e)).reshape(P, D)
    y = jnp.zeros((T + 1, D), yb.dtype).at[buf_tok].add(yb * buf_w[:, None].astype(yb.dtype))
    return y[:T].reshape(B_, S_, D)


def setup_inputs(seed: int = 0) -> dict:
    key = jax.random.key(seed)
    ks = jax.random.split(key, 26)
    f32 = jnp.float32

    def nrm(k, shape, scale):
        return jax.random.normal(k, shape, f32) * scale

    D, L = D_MODEL, DEPTH
    return {
        "x": nrm(ks[0], (BATCH, SEQ, D), 1.0),
        "c": nrm(ks[1], (BATCH, D), 1.0),
        "w_ada": nrm(ks[2], (L, D, 6 * D), 0.5 * D ** -0.5),
        "b_ada": nrm(ks[3], (L, 6 * D), 0.01),
        "w_in": nrm(ks[4], (L, D, D_IN), D ** -0.5),
        "w_branch_gate": nrm(ks[5], (L, D, N_BRANCHES * D), D ** -0.5),
        "b_branch_gate": nrm(ks[6], (L, N_BRANCHES * D), 0.01),
        "attn_sinks": nrm(ks[7], (L, SWA_Q_HEADS), 0.5),
        "lambda_q1": nrm(ks[8], (L, DIFF_DIM), 0.1),
        "lambda_k1": nrm(ks[9], (L, DIFF_DIM), 0.1),
        "lambda_q2": nrm(ks[10], (L, DIFF_DIM), 0.1),
        "lambda_k2": nrm(ks[11], (L, DIFF_DIM), 0.1),
        "subln_g": 1.0 + nrm(ks[12], (L, 2 * DIFF_DIM), 0.02),
        "w_branch": nrm(ks[13], (L, N_BRANCHES, BRANCH_WIDTH, D), BRANCH_WIDTH ** -0.5 * BETA),
        "w_out": nrm(ks[14], (L, D, D), D ** -0.5 * BETA),
        "ln1_g": 1.0 + nrm(ks[15], (L, D), 0.02),
        "ln1_b": nrm(ks[16], (L, D), 0.02),
        "w_router_group": nrm(ks[17], (L, D, N_GROUPS), D ** -0.5),
        "b_router_group": nrm(ks[18], (L, N_GROUPS), 0.01),
        "w_router_expert": nrm(ks[19], (L, D, N_EXPERTS), D ** -0.5),
        "b_router_expert": nrm(ks[20], (L, N_EXPERTS), 0.01),
        "w_exp_gate": nrm(ks[21], (L, N_EXPERTS, D, D_EXPERT), D ** -0.5),
        "w_exp_up": nrm(ks[22], (L, N_EXPERTS, D, D_EXPERT), D ** -0.5),
        "w_exp_down": nrm(ks[23], (L, N_EXPERTS, D_EXPERT, D), D_EXPERT ** -0.5 * BETA),
        "ln2_g": 1.0 + nrm(ks[24], (L, D), 0.02),
        "ln2_b": nrm(ks[25], (L, D), 0.02),
    }


def reference(x, c, w_ada, b_ada, w_in, w_branch_gate, b_branch_gate, attn_sinks,
              lambda_q1, lambda_k1, lambda_q2, lambda_k2, subln_g, w_branch, w_out,
              ln1_g, ln1_b, w_router_group, b_router_group, w_router_expert, b_router_expert,
              w_exp_gate, w_exp_up, w_exp_down, ln2_g, ln2_b):
    for l in range(DEPTH):
        lambda_init = 0.8 - 0.6 * math.exp(-0.3 * l)
        mod = (c @ w_ada[l] + b_ada[l])[:, None, :]
        sh1, sc1, g1, sh2, sc2, g2 = jnp.split(mod, 6, axis=-1)
        h = x * (1.0 + sc1) + sh1
        y = mixer_sublayer(h, w_in[l], w_branch_gate[l], b_branch_gate[l], attn_sinks[l],
                           lambda_q1[l], lambda_k1[l], lambda_q2[l], lambda_k2[l], subln_g[l],
                           w_branch[l], w_out[l], lambda_init)
        x = layer_norm(ALPHA * x + g1 * y, ln1_g[l], ln1_b[l])
        h = x * (1.0 + sc2) + sh2
        y = hierarchical_moe(h, w_router_group[l], b_router_group[l], w_router_expert[l],
                             b_router_expert[l], w_exp_gate[l], w_exp_up[l], w_exp_down[l])
        x = layer_norm(ALPHA * x + g2 * y, ln2_g[l], ln2_b[l])
    return x
```

```python
import math
from contextlib import ExitStack
import numpy as np
import ml_dtypes
import concourse.bass as bass
import concourse.mybir as mybir
from concourse.bass_utils import run_bass_kernel_spmd

F32 = mybir.dt.float32
BF16 = mybir.dt.bfloat16
AF = mybir.ActivationFunctionType
ALU = mybir.AluOpType
AX = mybir.AxisListType

D = 2048
SEQ = 8192
DEPTH = 4
NCORE = 8
DC = D // 128
ALPHA = (2.0 * DEPTH) ** 0.25
LN_EPS = 1e-5
RMS_EPS = 1e-5
SCALE = 1.0 / 8.0
NEG = -30000.0


class Tk:
    __slots__ = ("w", "r")

    def __init__(self):
        self.w = None
        self.r = []


class Prog:
    ENG = ("pe", "act", "dve", "pool", "sp")
    NDMA = 40

    def __init__(self, nc, st):
        self.nc = nc
        self.st = st
        self.ops = {e: [] for e in self.ENG}
        self.sem = {e: st.enter_context(nc.semaphore("s_" + e)) for e in self.ENG}
        self.cnt = {e: 0 for e in self.ENG}
        self.seen = {e: {} for e in self.ENG}
        self.dsem = [st.enter_context(nc.semaphore("d%d" % i)) for i in range(self.NDMA)]
        self.dcnt = [0] * self.NDMA
        self.dnext = 0
        self.outs = []
        self.meta = {e: [] for e in self.ENG}

    def _need(self, e, ev, waits, raw):
        if ev is None:
            return
        key, val, sem = ev
        if key == e and (e == "pe" or not raw):
            return
        if self.seen[e].get(key, 0) >= val:
            return
        self.seen[e][key] = val
        waits.append((sem, val))

    def _deps(self, e, reads, writes):
        waits = []
        for t in reads:
            self._need(e, t.w, waits, True)
        for t in writes:
            self._need(e, t.w, waits, False)
            for r in t.r:
                self._need(e, r, waits, False)
        return waits

    def op(self, e, fn, reads=(), writes=()):
        waits = self._deps(e, reads, writes)
        self.cnt[e] += 1
        sem = self.sem[e]
        ev = (e, self.cnt[e], sem)

        def run(eng, waits=waits, fn=fn, sem=sem):
            for s, v in waits:
                eng.wait_ge(s, v)
            fn(eng).then_inc(sem, 1)

        self.ops[e].append(run)
        self.meta[e].append(([(id(s_), v) for s_, v in waits], (id(sem), 1)))
        for t in reads:
            t.r.append(ev)
        for t in writes:
            t.w = ev
            t.r = []

    def dma(self, q, out, in_, reads=(), writes=(), is_out=False, slow=False):
        waits = self._deps(q, reads, writes)
        i = self.dnext
        self.dnext = (self.dnext + 1) % self.NDMA
        sem = self.dsem[i]
        key = "d%d" % i
        prev = self.dcnt[i]
        if prev and self.seen[q].get(key, 0) < prev:
            self.seen[q][key] = prev
            waits.append((sem, prev))
        self.dcnt[i] += 16
        ev = (key, self.dcnt[i], sem)

        def run(eng, waits=waits, sem=sem, out=out, in_=in_, slow=slow):
            for s, v in waits:
                eng.wait_ge(s, v)
            if slow:
                eng.dma_start(out=out, in_=in_, allow_slow_non_contiguous=True).then_inc(sem, 16)
            else:
                eng.dma_start(out=out, in_=in_).then_inc(sem, 16)

        self.ops[q].append(run)
        self.meta[q].append(([(id(s_), v) for s_, v in waits], (id(sem), 16)))
        for t in reads:
            t.r.append(ev)
        for t in writes:
            t.w = ev
            t.r = []
        if is_out:
            self.outs.append(ev)

    def check_deadlock(self):
        cnt = {}
        pos = {e: 0 for e in self.ENG}
        progress = True
        while progress:
            progress = False
            for e in self.ENG:
                while pos[e] < len(self.meta[e]):
                    waits, (sk, inc) = self.meta[e][pos[e]]
                    if all(cnt.get(k, 0) >= v for k, v in waits):
                        cnt[sk] = cnt.get(sk, 0) + inc
                        pos[e] += 1
                        progress = True
                    else:
                        break
        stuck = {e: (pos[e], len(self.meta[e])) for e in self.ENG if pos[e] < len(self.meta[e])}
        return stuck

    def finish(self):
        waits = []
        for ev in self.outs:
            self._need("sp", ev, waits, True)

        def run(eng, waits=waits):
            for s, v in waits:
                eng.wait_ge(s, v)

        self.ops["sp"].append(run)
        nc = self.nc
        with nc.Block() as blk:
            @blk.tensor
            def _(e):
                for f in self.ops["pe"]:
                    f(e)

            @blk.scalar
            def _(e):
                for f in self.ops["act"]:
                    f(e)

            @blk.vector
            def _(e):
                for f in self.ops["dve"]:
                    f(e)

            @blk.gpsimd
            def _(e):
                for f in self.ops["pool"]:
                    f(e)

            @blk.sync
            def _(e):
                for f in self.ops["sp"]:
                    f(e)


def _sb(nc, st, name, shape, dt):
    return st.enter_context(nc.sbuf_tensor("sb_" + name, shape, dt))


def _ps(nc, st, name, shape, dt):
    return st.enter_context(nc.psum_tensor("ps_" + name, shape, dt))


P0_COLS = 6 * D // NCORE
P0_HALF = 768


def build_phase0():
    nc = bass.Bass("TRN2", target_bir_lowering=False)
    c = nc.dram_tensor("c", [1, D], F32, kind="ExternalInput").ap()
    w = nc.dram_tensor("w", [DEPTH, D, P0_COLS], F32, kind="ExternalInput").ap()
    b = nc.dram_tensor("b", [DEPTH, P0_COLS], F32, kind="ExternalInput").ap()
    addc = nc.dram_tensor("addc", [1, P0_COLS], F32, kind="ExternalInput").ap()
    out = nc.dram_tensor("mod", [DEPTH, P0_COLS], F32, kind="ExternalOutput").ap()
    with ExitStack() as st:
        p = Prog(nc, st)
        cT = _sb(nc, st, "cT", [128, DC], F32)
        bt = _sb(nc, st, "bt", [1, DEPTH * P0_COLS], F32)
        at = _sb(nc, st, "at", [1, P0_COLS], F32)
        res = _sb(nc, st, "res", [1, DEPTH * P0_COLS], F32)
        wb = [_sb(nc, st, "wb%d" % i, [128, DC, P0_HALF], F32) for i in range(2)]
        ps = [_ps(nc, st, "ps%d" % i, [128, 512], F32) for i in range(4)]
        t_c, t_b, t_a, t_res = Tk(), Tk(), Tk(), Tk()
        t_w = [Tk(), Tk()]
        t_ps = [Tk() for _ in range(4)]
        p.dma("sp", cT[:], c[0, :].rearrange("(k p) -> p k", p=128), writes=[t_c], slow=True)
        p.dma("sp", bt[:], b.rearrange("l n -> (l n)")[None, :], writes=[t_b])
        p.dma("sp", at[:], addc[:, :], writes=[t_a])
        u = 0
        pi = 0
        for l in range(DEPTH):
            for hf in range(2):
                buf = u % 2
                q = "sp" if u % 2 == 0 else "pool"
                p.dma(q, wb[buf][:], w[l, :, hf * P0_HALF:(hf + 1) * P0_HALF].rearrange("(k p) n -> p k n", p=128),
                      writes=[t_w[buf]])
                for n0 in range(0, P0_HALF, 384):
                    pt = ps[pi % 4]
                    tk = t_ps[pi % 4]
                    pi += 1
                    for k in range(DC):
                        p.op("pe", lambda e, pt=pt, k=k, buf=buf, n0=n0: e.matmul(
                            pt[0:1, 0:384], cT[:, k:k + 1], wb[buf][:, k, n0:n0 + 384], start=(k == 0), stop=(k == DC - 1)),
                            reads=[t_c, t_w[buf]], writes=[tk])
                    o0 = l * P0_COLS + hf * P0_HALF + n0
                    a0 = hf * P0_HALF + n0
                    p.op("dve", lambda e, pt=pt, o0=o0: e.tensor_tensor(
                        out=res[0:1, o0:o0 + 384], in0=pt[0:1, 0:384], in1=bt[0:1, o0:o0 + 384], op=ALU.add),
                        reads=[tk, t_b], writes=[t_res])
                    p.op("dve", lambda e, o0=o0, a0=a0: e.tensor_tensor(
                        out=res[0:1, o0:o0 + 384], in0=res[0:1, o0:o0 + 384], in1=at[0:1, a0:a0 + 384], op=ALU.add),
                        reads=[t_res, t_a], writes=[t_res])
                u += 1
        p.dma("sp", out.rearrange("l n -> (l n)")[None, :], res[:], reads=[t_res], is_out=True)
        p.finish()
    return nc


def run_phase0(c, w_ada, b_ada):
    nc = build_phase0()
    addfull = np.zeros((1, 6 * D), np.float32)
    addfull[0, D:2 * D] = 1.0
    addfull[0, 4 * D:5 * D] = 1.0
    maps = []
    for i in range(NCORE):
        sl = slice(i * P0_COLS, (i + 1) * P0_COLS)
        maps.append({"c": np.ascontiguousarray(c, np.float32),
                     "w": np.ascontiguousarray(w_ada[:, :, sl]),
                     "b": np.ascontiguousarray(b_ada[:, sl]),
                     "addc": np.ascontiguousarray(addfull[:, sl])})
    res = run_bass_kernel_spmd(nc, maps, core_ids=list(range(NCORE)))
    return np.concatenate([r["mod"] for r in res.results], axis=1)


def pipeline(n_iter, stages, skew=1):
    for step in range(n_iter + (len(stages) - 1) * skew):
        for k, f in enumerate(stages):
            i = step - k * skew
            if 0 <= i < n_iter:
                f(i)


class Rot:
    def __init__(self, bufs):
        self.bufs = bufs
        self.tks = [Tk() for _ in bufs]
        self.i = 0

    def next(self):
        j = self.i % len(self.bufs)
        self.i += 1
        return self.bufs[j], self.tks[j]


def _bf(a):
    return np.asarray(a, np.float32).astype(ml_dtypes.bfloat16)


A_WT = 768
A_WV = 320
VW = 322
A_SM = 2 + 4 * 64 + 128 + 2


def phaseA_consts(core, S):
    NQ = S // 128
    sl_swa = [2.0 ** (-8.0 * (h + 1) / 16.0) for h in (2 * core, 2 * core + 1)]
    sl_d = 2.0 ** (-8.0 * (core + 1) / 8.0)
    p = np.arange(128)[:, None].astype(np.float64)
    t = np.arange(128)[None, :].astype(np.float64)
    swab = np.zeros((128, 2, 2, 128), np.float32)
    for hh in range(2):
        d0 = 128 + t - p
        swab[:, hh, 0, :] = np.where(d0 < 128, -sl_swa[hh] * d0, NEG)
        d1 = t - p
        swab[:, hh, 1, :] = np.where(d1 >= 0, -sl_swa[hh] * d1, NEG)
    nj = NQ + 3
    jj = np.arange(nj)[None, :] - 3
    dbias = (sl_d * p - sl_d * 128.0 * jj).astype(np.float32)
    ident = np.eye(128, dtype=np.float32)
    cf = np.concatenate([swab.reshape(128, 512), dbias, ident], axis=1)
    tri = (p >= t).astype(np.float32)
    ones = np.ones((128, 128), np.float32)
    strict = (p < t).astype(np.float32)
    incl = (p <= t).astype(np.float32)
    tl = np.arange(512).astype(np.float64)
    v = (-8.0 * sl_d * tl)
    hi = v.astype(np.float32).astype(ml_dtypes.bfloat16)
    lo = (v - hi.astype(np.float64)).astype(np.float32).astype(ml_dtypes.bfloat16)
    qaug = np.zeros((128, 512), np.float32)
    qaug[0] = hi.astype(np.float32)
    qaug[1] = lo.astype(np.float32)
    cb = _bf(np.concatenate([tri, ones, strict, incl, qaug], axis=1))
    return np.ascontiguousarray(cf), np.ascontiguousarray(cb)


def build_phaseA(S):
    NQ = S // 128
    NG = S // 512
    NJ = NQ + 3
    NCF = 512 + NJ + 128
    nc = bass.Bass("TRN2", target_bir_lowering=False)
    x = nc.dram_tensor("x", [S, D], F32, kind="ExternalInput").ap()
    mod = nc.dram_tensor("mod", [2, D], F32, kind="ExternalInput").ap()
    w_t = nc.dram_tensor("w_t", [D, A_WT], F32, kind="ExternalInput").ap()
    w_v = nc.dram_tensor("w_v", [D, A_WV], F32, kind="ExternalInput").ap()
    cf_d = nc.dram_tensor("cf", [128, NCF], F32, kind="ExternalInput").ap()
    cb_d = nc.dram_tensor("cb", [128, 1024], BF16, kind="ExternalInput").ap()
    sm_d = nc.dram_tensor("sm", [1, A_SM], F32, kind="ExternalInput").ap()
    o_d = nc.dram_tensor("o", [S, 384], F32, kind="ExternalOutput").ap()
    with ExitStack() as st:
        p = Prog(nc, st)
        sb = lambda name, shape, dt: _sb(nc, st, name, shape, dt)
        wT = sb("wT", [128, DC, A_WT], BF16)
        wV = sb("wV", [128, DC, A_WV], BF16)
        KA = sb("KA", [128, S], BF16)
        KB = sb("KB", [128, S], BF16)
        KC = sb("KC", [128, S], BF16)
        V = sb("V", [128, NQ, VW], BF16)
        xs = sb("xs", [128, 2, D], F32)
        hT = sb("hT", [128, DC, 512], BF16)
        QA = sb("QA", [128, 512], BF16)
        QB = sb("QB", [128, 512], BF16)
        QC = sb("QC", [128, 512], BF16)
        cf = sb("cfs", [128, NCF], F32)
        cb = sb("cbs", [128, 1024], BF16)
        sm = sb("sms", [128, A_SM], F32)
        sclT = sb("sclT", [128, DC], F32)
        shT = sb("shT", [128, DC], F32)
        misc = sb("misc", [128, 16], F32)
        gsub = sb("gsub", [128, 128], F32)
        junk = sb("junk", [128, 128], F32)
        ost = [sb("ost%d" % i, [128, 4, 384], F32) for i in range(2)]
        t_ost = [Tk(), Tk()]
        banks = [_ps(nc, st, "bk%d" % i, [128, 512], F32) for i in range(8)]
        scr = Rot(banks[0:3])
        SBACC, t_sbacc = banks[3], Tk()
        DACC = banks[4:8]
        t_dacc = [Tk() for _ in range(4)]
        e_r = Rot([sb("e%d" % i, [128, 512], F32) for i in range(3)])
        L_r = Rot([sb("L%d" % i, [128, 512], BF16) for i in range(3)])
        X_r = Rot([sb("X%d" % i, [128, 512], F32) for i in range(2)])
        W_r = Rot([sb("W%d" % i, [128, 512], BF16) for i in range(3)])
        E_r = Rot([sb("E%d" % i, [128, 512], BF16) for i in range(3)])
        Lacc = sb("Lacc", [128, 512], F32)
        Lab_r = Rot([sb("Lab%d" % i, [128, 512], BF16) for i in range(2)])
        st_r = Rot([sb("stmp%d" % i, [128, 128], F32) for i in range(2)])
        sE_r = Rot([sb("sE%d" % i, [128, 128], BF16) for i in range(2)])
        fin_r = Rot([sb("fin%d" % i, [128, 8], F32) for i in range(2)])
        da_r = Rot([sb("da%d" % i, [128, 128], F32) for i in range(2)])
        dd_r = Rot([sb("dd%d" % i, [128, 128], F32) for i in range(2)])

        t_c = Tk()
        t_k = [Tk() for _ in range(NG)]
        t_xs, t_hT, t_q = Tk(), Tk(), Tk()
        t_lacc = Tk()
        t_misc = Tk()

        swab = lambda hh, kind: cf[:, (hh * 2 + kind) * 128:(hh * 2 + kind + 1) * 128]
        dbias = lambda j: cf[:, 512 + j:512 + j + 1]
        ident = cf[:, 512 + NJ:512 + NJ + 128]
        tri = cb[:, 0:128]
        ones = cb[:, 128:256]
        strict = cb[:, 256:384]
        incl = cb[:, 384:512]
        qaug = cb[:, 512:1024]

        p.dma("sp", cf[:], cf_d[:, :], writes=[t_c])
        p.dma("sp", cb[:], cb_d[:, :], writes=[t_c])
        p.dma("sp", sm[:], sm_d[0, :].partition_broadcast(128), writes=[t_c])
        p.dma("sp", sclT[:], mod[0, :].rearrange("(k p) -> p k", p=128), writes=[t_c], slow=True)
        p.dma("sp", shT[:], mod[1, :].rearrange("(k p) -> p k", p=128), writes=[t_c], slow=True)
        for k in range(DC):
            p.dma("pool", wT[:, k, :], w_t[k * 128:(k + 1) * 128, :], writes=[t_c])
            p.dma("pool", wV[:, k, :], w_v[k * 128:(k + 1) * 128, :], writes=[t_c])
        p.op("pool", lambda e: e.memset(V[:, :, 64:65], 1.0), writes=[t_c])
        p.op("pool", lambda e: e.memset(V[:, :, 321:322], 1.0), writes=[t_c])
        p.op("act", lambda e: e.activation(out=misc[:, 0:2], in_=sm[:, 0:2], func=AF.Exp), reads=[t_c], writes=[t_misc])
        p.op("dve", lambda e: e.tensor_tensor(out=junk[:, 0:64], in0=sm[:, 2:66], in1=sm[:, 66:130], op=ALU.mult),
             reads=[t_c], writes=[t_misc])
        p.op("dve", lambda e: e.reduce_sum(out=misc[:, 2:3], in_=junk[:, 0:64], axis=AX.X), reads=[t_misc], writes=[t_misc])
        p.op("dve", lambda e: e.tensor_tensor(out=junk[:, 64:128], in0=sm[:, 130:194], in1=sm[:, 194:258], op=ALU.mult),
             reads=[t_c], writes=[t_misc])
        p.op("dve", lambda e: e.reduce_sum(out=misc[:, 3:4], in_=junk[:, 64:128], axis=AX.X), reads=[t_misc], writes=[t_misc])
        p.op("act", lambda e: e.activation(out=misc[:, 5:7], in_=misc[:, 2:4], func=AF.Exp), reads=[t_misc], writes=[t_misc])
        p.op("dve", lambda e: e.tensor_tensor(out=misc[:, 7:8], in0=misc[:, 6:7], in1=misc[:, 5:6], op=ALU.subtract),
             reads=[t_misc], writes=[t_misc])
        p.op("dve", lambda e: e.tensor_tensor(out=misc[:, 4:5], in0=misc[:, 7:8], in1=sm[:, 386:387], op=ALU.subtract),
             reads=[t_misc, t_c], writes=[t_misc])
        p.op("dve", lambda e: e.tensor_scalar(out=gsub[:], in0=sm[:, 258:386], scalar1=sm[:, 387:388], scalar2=None,
                                              op0=ALU.mult), reads=[t_c], writes=[t_misc])

        for g in range(NG):
            for half in range(2):
                r0 = g * 512 + half * 256
                p.dma("sp", xs[:], x[r0:r0 + 256, :].rearrange("(t q) d -> q t d", q=128), writes=[t_xs])
                for c in range(DC):
                    bk, tk = scr.next()
                    for t in range(2):
                        p.op("pe", lambda e, bk=bk, t=t, c=c: e.transpose(
                            bk[:, t * 128:(t + 1) * 128], xs[:, t, c * 128:(c + 1) * 128], ident),
                            reads=[t_xs, t_c], writes=[tk])
                    p.op("act", lambda e, bk=bk, c=c, half=half: e.activation(
                        out=hT[:, c, half * 256:(half + 1) * 256], in_=bk[:, 0:256], func=AF.Identity,
                        bias=shT[:, c:c + 1], scale=sclT[:, c:c + 1]), reads=[tk, t_c], writes=[t_hT])
            dests = [QA[:, :], KA[:, g * 512:(g + 1) * 512], QB[:, :], KB[:, g * 512:(g + 1) * 512],
                     QC[:, :], KC[:, g * 512:(g + 1) * 512]]
            for m in range(6):
                bk, tk = scr.next()
                for k in range(DC):
                    p.op("pe", lambda e, bk=bk, k=k, m=m: e.matmul(
                        bk[:, :], wT[:, k, m * 128:(m + 1) * 128], hT[:, k, :], start=(k == 0), stop=(k == DC - 1)),
                        reads=[t_c, t_hT], writes=[tk])
                wtk = t_q if m % 2 == 0 else t_k[g]
                p.op("dve", lambda e, bk=bk, m=m, dests=dests: e.tensor_copy(out=dests[m], in_=bk[:, :]),
                     reads=[tk], writes=[wtk])
            for t in range(4):
                bk, tk = scr.next()
                for k in range(DC):
                    p.op("pe", lambda e, bk=bk, k=k, t=t: e.matmul(
                        bk[:, 0:A_WV], hT[:, k, t * 128:(t + 1) * 128], wV[:, k, :], start=(k == 0), stop=(k == DC - 1)),
                        reads=[t_c, t_hT], writes=[tk])
                qb = g * 4 + t
                p.op("dve", lambda e, bk=bk, qb=qb: e.tensor_copy(out=V[:, qb, 0:64], in_=bk[:, 0:64]),
                     reads=[tk], writes=[t_k[g]])
                p.op("dve", lambda e, bk=bk, qb=qb: e.tensor_copy(out=V[:, qb, 65:321], in_=bk[:, 64:320]),
                     reads=[tk], writes=[t_k[g]])

            o_t, o_tk = ost[g % 2], t_ost[g % 2]

            for hh in range(2):
                for qi in range(4):
                    i = g * 4 + qi
                    kbs = [kb for kb in (i - 1, i) if kb >= 0]
                    abk, atk = scr.next()
                    ps_l = []
                    for kb in kbs:
                        kind = 1 if kb == i else 0
                        bk, tk = scr.next()
                        kg = t_k[kb // 4]
                        p.op("pe", lambda e, bk=bk, kb=kb, hh=hh, qi=qi: e.matmul(
                            bk[:, 0:128], KA[64 * hh:64 * hh + 64, kb * 128:(kb + 1) * 128],
                            QA[64 * hh:64 * hh + 64, qi * 128:(qi + 1) * 128], start=True, stop=True),
                            reads=[kg, t_q], writes=[tk])
                        tm, ttk = st_r.next()
                        p.op("dve", lambda e, bk=bk, tm=tm, hh=hh, kind=kind: e.scalar_tensor_tensor(
                            out=tm[:], in0=bk[:, 0:128], scalar=SCALE, in1=swab(hh, kind), op0=ALU.mult, op1=ALU.add),
                            reads=[tk, t_c], writes=[ttk])
                        sE, setk = sE_r.next()
                        p.op("act", lambda e, tm=tm, sE=sE: e.activation(out=sE[:], in_=tm[:], func=AF.Exp),
                             reads=[ttk], writes=[setk])
                        ps_l.append((sE, setk, kb, kg))
                    for n, (sE, setk, kb, kg) in enumerate(ps_l):
                        p.op("pe", lambda e, abk=abk, sE=sE, kb=kb, n=n, ln=len(ps_l): e.matmul(
                            abk[:, 0:65], sE[:], V[:, kb, 0:65], start=(n == 0), stop=(n == ln - 1)),
                            reads=[setk, kg, t_c], writes=[atk])
                    fb, ftk = fin_r.next()
                    p.op("dve", lambda e, fb=fb, abk=abk, hh=hh: e.tensor_tensor(
                        out=fb[:, 0:1], in0=abk[:, 64:65], in1=misc[:, hh:hh + 1], op=ALU.add),
                        reads=[atk, t_misc], writes=[ftk])
                    p.op("dve", lambda e, fb=fb: e.reciprocal(out=fb[:, 1:2], in_=fb[:, 0:1]), reads=[ftk], writes=[ftk])
                    p.op("dve", lambda e, fb=fb, abk=abk, hh=hh, qi=qi, o_t=o_t: e.tensor_scalar(
                        out=o_t[:, qi, 64 * hh:64 * hh + 64], in0=abk[:, 0:64], scalar1=fb[:, 1:2], scalar2=None,
                        op0=ALU.mult), reads=[atk, ftk], writes=[o_tk])

            its = [(hh, kb) for hh in range(2) for kb in range(g * 4 + 3, -1, -1)]
            stt = {}

            def sb_a(n):
                hh, kb = its[n]
                qs = max(0, kb - 4 * g) * 128
                first = (kb == g * 4 + 3)
                kg = t_k[kb // 4]
                bk, tk = scr.next()
                p.op("pe", lambda e: e.matmul(bk[:, qs:512], KB[64 * hh:64 * hh + 64, kb * 128:(kb + 1) * 128],
                                              QB[64 * hh:64 * hh + 64, qs:512], start=True, stop=True),
                     reads=[kg, t_q], writes=[tk])
                eb, etk = e_r.next()
                p.op("act", lambda e: e.activation(out=eb[:, qs:512], in_=bk[:, qs:512], func=AF.Exp, scale=SCALE),
                     reads=[tk], writes=[etk])
                Lb, Ltk = L_r.next()
                p.op("act", lambda e: e.activation(out=Lb[:, qs:512], in_=eb[:, qs:512], func=AF.Ln, bias=1.0, scale=1.0),
                     reads=[etk], writes=[Ltk])
                if kb >= 4 * g:
                    p.op("pool", lambda e: e.tensor_tensor(out=Lb[:, qs:qs + 128], in0=Lb[:, qs:qs + 128], in1=strict,
                                                           op=ALU.mult), reads=[Ltk, t_c], writes=[Ltk])
                if first:
                    p.op("pool", lambda e: e.memset(Lacc[:], 0.0), writes=[t_lacc])
                stt[n] = dict(hh=hh, kb=kb, qs=qs, first=first, kg=kg, eb=eb, etk=etk, Lb=Lb, Ltk=Ltk)

            def sb_b(n):
                s = stt[n]
                hh, kb, qs, first = s["hh"], s["kb"], s["qs"], s["first"]
                eb, etk, Lb, Ltk = s["eb"], s["etk"], s["Lb"], s["Ltk"]
                cbk, ctk = scr.next()
                p.op("pe", lambda e: e.matmul(cbk[:, qs:512], tri, Lb[:, qs:512], start=True, stop=first),
                     reads=[Ltk, t_c], writes=[ctk])
                if not first:
                    plab, plabtk = s_prev_lab[0]
                    p.op("pe", lambda e: e.matmul(cbk[:, qs:512], ones, plab[:, qs:512], start=False, stop=True),
                         reads=[plabtk, t_c], writes=[ctk])
                Xb, Xtk = X_r.next()
                p.op("act", lambda e: e.activation(out=Xb[:, qs:512], in_=cbk[:, qs:512], func=AF.Exp, scale=-1.0),
                     reads=[ctk], writes=[Xtk])
                Wb, Wtk = W_r.next()
                p.op("dve", lambda e: e.tensor_tensor(out=Wb[:, qs:512], in0=eb[:, qs:512], in1=Xb[:, qs:512], op=ALU.mult),
                     reads=[etk, Xtk], writes=[Wtk])
                if kb >= 4 * g:
                    p.op("pool", lambda e: e.tensor_tensor(out=Wb[:, qs:qs + 128], in0=Wb[:, qs:qs + 128], in1=strict,
                                                           op=ALU.mult), reads=[Wtk, t_c], writes=[Wtk])
                if kb > 0:
                    p.op("pool", lambda e: e.tensor_tensor(out=Lacc[:, qs:512], in0=Lacc[:, qs:512], in1=Lb[:, qs:512],
                                                           op=ALU.add), reads=[Ltk, t_lacc], writes=[t_lacc])
                    lab, labtk = Lab_r.next()
                    p.op("pool", lambda e: e.tensor_copy(out=lab[:, :], in_=Lacc[:, :]), reads=[t_lacc], writes=[labtk])
                    s_prev_lab[0] = (lab, labtk)
                s["Wb"], s["Wtk"] = Wb, Wtk

            def sb_c(n):
                s = stt.pop(n)
                hh, kb, qs, first, kg = s["hh"], s["kb"], s["qs"], s["first"], s["kg"]
                Wb, Wtk = s["Wb"], s["Wtk"]
                for qi in range(qs // 128, 4):
                    c0 = hh * 256 + qi * 64
                    p.op("pe", lambda e, qi=qi, c0=c0: e.matmul(
                        SBACC[:, c0:c0 + 64], Wb[:, qi * 128:(qi + 1) * 128], V[:, kb, 65 + 64 * hh:65 + 64 * hh + 64],
                        start=(first and hh == 0 and qi == 3), stop=(kb == 0), skip_group_check=True),
                        reads=[Wtk, kg], writes=[t_sbacc])
                if kb == 0:
                    p.op("act", lambda e, o_t=o_t: e.activation(
                        out=o_t[:, :, 128 + 64 * hh:128 + 64 * hh + 64],
                        in_=SBACC[:, hh * 256:(hh + 1) * 256].rearrange("p (q d) -> p q d", d=64), func=AF.Identity),
                        reads=[t_sbacc], writes=[o_tk])

            s_prev_lab = [None]
            pipeline(len(its), [sb_a, sb_b, sb_c])

            dits = [(kb, m) for kb in range(0, g * 4 + 4) for m in range(2)]
            dst = {}

            def dacc(m, qi):
                if qi < 3:
                    return DACC[2 * m], t_dacc[2 * m], qi * 129
                return DACC[2 * m + 1], t_dacc[2 * m + 1], 0

            def df_a(n):
                kb, m = dits[n]
                qs = max(0, kb - 4 * g) * 128
                kg = t_k[kb // 4]
                bk, tk = scr.next()
                p.op("pe", lambda e: e.matmul(bk[:, qs:512], KC[64 * m:64 * m + 64, kb * 128:(kb + 1) * 128],
                                              QC[64 * m:64 * m + 64, qs:512], start=True, stop=False),
                     reads=[kg, t_q], writes=[tk])
                p.op("pe", lambda e: e.matmul(bk[:, qs:512], ones[0:2, :], qaug[0:2, qs:512], start=False, stop=True),
                     reads=[t_c], writes=[tk])
                Eb, Etk = E_r.next()
                jidx = (4 * g - kb) + 3
                p.op("act", lambda e: e.activation(out=Eb[:, qs:512], in_=bk[:, qs:512], func=AF.Exp, scale=SCALE,
                                                   bias=dbias(jidx)), reads=[tk, t_c], writes=[Etk])
                if kb >= 4 * g:
                    p.op("pool", lambda e: e.tensor_tensor(out=Eb[:, qs:qs + 128], in0=Eb[:, qs:qs + 128], in1=incl,
                                                           op=ALU.mult), reads=[Etk, t_c], writes=[Etk])
                dst[n] = dict(kb=kb, m=m, qs=qs, kg=kg, Eb=Eb, Etk=Etk)

            def df_b(n):
                s = dst.pop(n)
                kb, m, qs, kg, Eb, Etk = s["kb"], s["m"], s["qs"], s["kg"], s["Eb"], s["Etk"]
                for qi in range(qs // 128, 4):
                    ab, atk, c0 = dacc(m, qi)
                    p.op("pe", lambda e, qi=qi, ab=ab, c0=c0: e.matmul(
                        ab[:, c0:c0 + 129], Eb[:, qi * 128:(qi + 1) * 128], V[:, kb, 193:322],
                        start=(kb == 0 and qi in (0, 3)), stop=(kb == 4 * g + qi), skip_group_check=True),
                        reads=[Etk, kg, t_c], writes=[atk])

            pipeline(len(dits), [df_a, df_b])
            for qi in range(4):
                a1, a1tk, c1 = dacc(0, qi)
                a2, a2tk, c2 = dacc(1, qi)
                fb, ftk = fin_r.next()
                p.op("dve", lambda e, fb=fb, a1=a1, c1=c1: e.reciprocal(out=fb[:, 0:1], in_=a1[:, c1 + 128:c1 + 129]),
                     reads=[a1tk], writes=[ftk])
                p.op("dve", lambda e, fb=fb, a2=a2, c2=c2: e.reciprocal(out=fb[:, 1:2], in_=a2[:, c2 + 128:c2 + 129]),
                     reads=[a2tk], writes=[ftk])
                p.op("dve", lambda e, fb=fb: e.tensor_tensor(out=fb[:, 2:3], in0=fb[:, 1:2], in1=misc[:, 4:5], op=ALU.mult),
                     reads=[ftk, t_misc], writes=[ftk])
                da, datk = da_r.next()
                p.op("dve", lambda e, fb=fb, a1=a1, c1=c1, da=da: e.tensor_scalar(
                    out=da[:], in0=a1[:, c1:c1 + 128], scalar1=fb[:, 0:1], scalar2=None, op0=ALU.mult),
                    reads=[a1tk, ftk], writes=[datk])
                dd, ddtk = dd_r.next()
                p.op("dve", lambda e, fb=fb, a2=a2, c2=c2, da=da, dd=dd: e.scalar_tensor_tensor(
                    out=dd[:], in0=a2[:, c2:c2 + 128], scalar=fb[:, 2:3], in1=da[:], op0=ALU.mult, op1=ALU.add),
                    reads=[a2tk, ftk, datk], writes=[ddtk])
                p.op("dve", lambda e, dd=dd, da=da: e.tensor_tensor(out=da[:], in0=dd[:], in1=dd[:], op=ALU.mult),
                     reads=[ddtk], writes=[datk])
                p.op("dve", lambda e, fb=fb, da=da: e.reduce_sum(out=fb[:, 3:4], in_=da[:], axis=AX.X),
                     reads=[datk], writes=[ftk])
                p.op("act", lambda e, fb=fb: e.activation(out=fb[:, 4:5], in_=fb[:, 3:4], func=AF.Ln, bias=RMS_EPS,
                                                          scale=1.0 / 128.0), reads=[ftk], writes=[ftk])
                p.op("act", lambda e, fb=fb: e.activation(out=fb[:, 5:6], in_=fb[:, 4:5], func=AF.Exp, scale=-0.5),
                     reads=[ftk], writes=[ftk])
                p.op("dve", lambda e, fb=fb, dd=dd, qi=qi, o_t=o_t: e.scalar_tensor_tensor(
                    out=o_t[:, qi, 256:384], in0=dd[:], scalar=fb[:, 5:6], in1=gsub[:], op0=ALU.mult, op1=ALU.mult),
                    reads=[ddtk, ftk, t_misc], writes=[o_tk])
            p.dma("sp", o_d[g * 512:(g + 1) * 512, :].rearrange("(q p) c -> p q c", p=128), o_t[:], reads=[o_tk],
                  is_out=True)
        p.finish()
    return nc


def phaseA_inputs(core, S, x, modA, w_in, sinks, lq1, lk1, lq2, lk2, subg, lam_init):
    h0 = 2 * core
    kv = core // 2
    qA = w_in[:, h0 * 64:h0 * 64 + 128]
    kA = w_in[:, 1024 + kv * 64:1024 + kv * 64 + 64]
    vA = w_in[:, 1280 + kv * 64:1280 + kv * 64 + 64]
    qB = w_in[:, 1536 + h0 * 64:1536 + h0 * 64 + 128]
    kB = w_in[:, 2560 + h0 * 64:2560 + h0 * 64 + 128]
    vB = w_in[:, 3584 + h0 * 64:3584 + h0 * 64 + 128]
    qC = w_in[:, 4608 + core * 128:4608 + core * 128 + 128]
    kC = w_in[:, 5632 + core * 128:5632 + core * 128 + 128]
    vC = w_in[:, 6656 + core * 128:6656 + core * 128 + 128]
    w_t = np.ascontiguousarray(np.concatenate([qA, kA, kA, qB, kB, qC, kC], axis=1), np.float32)
    w_v = np.ascontiguousarray(np.concatenate([vA, vB, vC], axis=1), np.float32)
    cf, cb = phaseA_consts2(core, S)
    sm = np.zeros((1, A_SM), np.float32)
    sm[0, 0:2] = sinks[h0:h0 + 2]
    sm[0, 2:66] = lq1
    sm[0, 66:130] = lk1
    sm[0, 130:194] = lq2
    sm[0, 194:258] = lk2
    sm[0, 258:386] = subg
    sm[0, 386] = lam_init
    sm[0, 387] = 1.0 - lam_init
    return {"x": np.ascontiguousarray(x, np.float32), "mod": np.ascontiguousarray(modA, np.float32), "w_t": w_t,
            "w_v": w_v, "cf": cf, "cb": cb, "sm": sm}


def emit_layernorm(p, r, t_r, xo, t_xo, gB, bB, t_c, stats, t_st):
    for j in range(4):
        p.op("dve", lambda e, j=j: e.bn_stats(out=stats[:, j * 6:(j + 1) * 6], in_=r[:, j * 512:(j + 1) * 512]),
             reads=[t_r], writes=[t_st])
    p.op("dve", lambda e: e.bn_aggr(out=stats[:, 24:26], in_=stats[:, 0:24]), reads=[t_st], writes=[t_st])
    p.op("act", lambda e: e.activation(out=stats[:, 26:27], in_=stats[:, 25:26], func=AF.Ln, bias=LN_EPS, scale=1.0),
         reads=[t_st], writes=[t_st])
    p.op("act", lambda e: e.activation(out=stats[:, 27:28], in_=stats[:, 26:27], func=AF.Exp, scale=-0.5),
         reads=[t_st], writes=[t_st])
    p.op("dve", lambda e: e.tensor_scalar(out=r[:], in0=r[:], scalar1=stats[:, 24:25], scalar2=stats[:, 27:28],
                                          op0=ALU.subtract, op1=ALU.mult), reads=[t_st, t_r], writes=[t_r])
    p.op("pool", lambda e: e.tensor_tensor(out=r[:], in0=r[:], in1=gB[:], op=ALU.mult), reads=[t_r, t_c], writes=[t_r])
    p.op("dve", lambda e: e.tensor_tensor(out=xo[:], in0=r[:], in1=bB[:], op=ALU.add), reads=[t_r, t_c], writes=[t_xo])


TB = SEQ // NCORE


def build_phaseB(TBK=TB):
    NH = TBK // 512
    nc = bass.Bass("TRN2", target_bir_lowering=False)
    x = nc.dram_tensor("x", [TBK, D], F32, kind="ExternalInput").ap()
    o = nc.dram_tensor("o", [TBK, 3072], F32, kind="ExternalInput").ap()
    mod = nc.dram_tensor("mod", [6, D], F32, kind="ExternalInput").ap()
    wg_d = nc.dram_tensor("wg", [48, 128, DC * 128], F32, kind="ExternalInput").ap()
    bg_d = nc.dram_tensor("bg", [6144], F32, kind="ExternalInput").ap()
    wb_d = nc.dram_tensor("wb", [48, 128, 8 * 128], F32, kind="ExternalInput").ap()
    wo_d = nc.dram_tensor("wo", [4, DC, 128, 512], F32, kind="ExternalInput").ap()
    ln_d = nc.dram_tensor("ln", [2, D], F32, kind="ExternalInput").ap()
    wr_d = nc.dram_tensor("wr", [D, 72], F32, kind="ExternalInput").ap()
    br_d = nc.dram_tensor("br", [72], F32, kind="ExternalInput").ap()
    id_d = nc.dram_tensor("ident", [128, 128], F32, kind="ExternalInput").ap()
    x1_d = nc.dram_tensor("x1", [TBK, D], F32, kind="ExternalOutput").ap()
    h2_d = nc.dram_tensor("h2T", [D, TBK], BF16, kind="ExternalOutput").ap()
    rw_d = nc.dram_tensor("rw", [TBK, 64], F32, kind="ExternalOutput").ap()
    with ExitStack() as st:
        p = Prog(nc, st)
        sb = lambda name, shape, dt: _sb(nc, st, name, shape, dt)
        hT = sb("hT", [128, DC, 512], BF16)
        oT = sb("oT", [128, 24, 512], BF16)
        zT = sb("zT", [128, DC, 512], BF16)
        stg = sb("stg", [128, 4096], F32)
        wg_r = Rot([sb("wg%d" % i, [128, DC * 128], BF16) for i in range(3)])
        wb_r = Rot([sb("wb%d" % i, [128, 8 * 128], BF16) for i in range(3)])
        wo = sb("wo", [128, DC, 512], BF16)
        g1B = sb("g1B", [128, D], F32)
        lgB = sb("lgB", [128, D], F32)
        lbB = sb("lbB", [128, D], F32)
        r_r = Rot([sb("r%d" % i, [128, D], F32) for i in range(4)])
        x1t = sb("x1t", [128, D], F32)
        xres = sb("xres", [128, 512], F32)
        h2f = sb("h2f", [128, DC, 128], F32)
        h2b = sb("h2b", [128, DC, 128], BF16)
        wr = sb("wr", [128, DC, 72], F32)
        brB = sb("brB", [128, 72], F32)
        ident = sb("ident", [128, 128], F32)
        modT = sb("modT", [128, 6, DC], F32)
        bgT = sb("bgT", [128, 48], F32)
        gs_r = Rot([sb("gs%d" % i, [128, 512], F32) for i in range(2)])
        zt_r = Rot([sb("zt%d" % i, [128, 512], F32) for i in range(2)])
        zacc = sb("zacc", [128, 512], F32)
        stats = sb("stats", [128, 32], F32)
        rt = sb("rt", [128, 512], F32)
        banks = [_ps(nc, st, "bk%d" % i, [128, 512], F32) for i in range(8)]
        scr = Rot(banks)
        t_c, t_stg, t_hT, t_oT, t_zT, t_wo, t_x1, t_xres, t_h2, t_st, t_rt, t_zacc = [Tk() for _ in range(12)]

        p.dma("sp", ident[:], id_d[:, :], writes=[t_c])
        p.dma("sp", modT[:], mod.rearrange("m (k p) -> p m k", p=128), writes=[t_c], slow=True)
        p.dma("sp", bgT[:], bg_d.rearrange("(j p) -> p j", p=128), writes=[t_c], slow=True)
        p.dma("sp", g1B[:], mod[2, :].partition_broadcast(128), writes=[t_c])
        p.dma("sp", lgB[:], ln_d[0, :].partition_broadcast(128), writes=[t_c])
        p.dma("sp", lbB[:], ln_d[1, :].partition_broadcast(128), writes=[t_c])
        p.dma("sp", brB[:], br_d.partition_broadcast(128), writes=[t_c])
        p.dma("sp", wr[:], wr_d.rearrange("(k p) n -> p k n", p=128), writes=[t_c])

        for hf in range(NH):
            for t in range(4):
                r0 = hf * 512 + t * 128
                p.dma("sp", stg[:, 0:D], x[r0:r0 + 128, :], writes=[t_stg])
                for c4 in range(4):
                    bk, tk = scr.next()
                    for j in range(4):
                        c = c4 * 4 + j
                        p.op("pe", lambda e, bk=bk, j=j, c=c: e.transpose(
                            bk[:, j * 128:(j + 1) * 128], stg[:, c * 128:(c + 1) * 128], ident[:]),
                            reads=[t_stg, t_c], writes=[tk])
                    for j in range(4):
                        c = c4 * 4 + j
                        p.op("act", lambda e, bk=bk, j=j, c=c, t=t: e.activation(
                            out=hT[:, c, t * 128:(t + 1) * 128], in_=bk[:, j * 128:(j + 1) * 128], func=AF.Identity,
                            bias=modT[:, 1, c:c + 1], scale=modT[:, 0, c:c + 1]), reads=[tk, t_c], writes=[t_hT])
                p.dma("sp", stg[:, 0:3072], o[r0:r0 + 128, :], writes=[t_stg])
                for c4 in range(6):
                    bk, tk = scr.next()
                    for j in range(4):
                        c = c4 * 4 + j
                        p.op("pe", lambda e, bk=bk, j=j, c=c: e.transpose(
                            bk[:, j * 128:(j + 1) * 128], stg[:, c * 128:(c + 1) * 128], ident[:]),
                            reads=[t_stg, t_c], writes=[tk])
                    p.op("dve", lambda e, bk=bk, c4=c4, t=t: e.tensor_copy(
                        out=oT[:, c4 * 4:c4 * 4 + 4, t * 128:(t + 1) * 128],
                        in_=bk[:, :].rearrange("p (j q) -> p j q", q=128)), reads=[tk], writes=[t_oT])
            for dc in range(DC):
                for n in range(3):
                    blk = n * DC + dc
                    wgb, wgtk = wg_r.next()
                    p.dma("pool", wgb[:], wg_d[blk, :, :], writes=[wgtk])
                    wbb, wbtk = wb_r.next()
                    p.dma("pool", wbb[:], wb_d[blk, :, :], writes=[wbtk])
                    gbk, gtk = scr.next()
                    for k in range(DC):
                        p.op("pe", lambda e, gbk=gbk, wgb=wgb, k=k: e.matmul(
                            gbk[:, :], wgb[:, k * 128:(k + 1) * 128], hT[:, k, :], start=(k == 0), stop=(k == DC - 1)),
                            reads=[wgtk, t_hT], writes=[gtk])
                    gs, gstk = gs_r.next()
                    p.op("act", lambda e, gbk=gbk, gs=gs, blk=blk: e.activation(
                        out=gs[:], in_=gbk[:, :], func=AF.Sigmoid, bias=bgT[:, blk:blk + 1], scale=1.0),
                        reads=[gtk, t_c], writes=[gstk])
                    ybk, ytk = scr.next()
                    for f in range(8):
                        p.op("pe", lambda e, ybk=ybk, wbb=wbb, f=f, n=n: e.matmul(
                            ybk[:, :], wbb[:, f * 128:(f + 1) * 128], oT[:, n * 8 + f, :], start=(f == 0), stop=(f == 7)),
                            reads=[wbtk, t_oT], writes=[ytk])
                    if n == 0:
                        p.op("dve", lambda e, ybk=ybk, gs=gs: e.tensor_tensor(out=zacc[:], in0=ybk[:, :], in1=gs[:], op=ALU.mult),
                             reads=[ytk, gstk], writes=[t_zacc])
                    else:
                        zt, zttk = zt_r.next()
                        p.op("dve", lambda e, ybk=ybk, gs=gs, zt=zt: e.tensor_tensor(out=zt[:], in0=ybk[:, :], in1=gs[:], op=ALU.mult),
                             reads=[ytk, gstk], writes=[zttk])
                        if n == 1:
                            p.op("pool", lambda e, zt=zt: e.tensor_tensor(out=zacc[:], in0=zacc[:], in1=zt[:], op=ALU.add),
                                 reads=[zttk, t_zacc], writes=[t_zacc])
                        else:
                            p.op("pool", lambda e, zt=zt, dc=dc: e.tensor_tensor(out=zT[:, dc, :], in0=zacc[:], in1=zt[:], op=ALU.add),
                                 reads=[zttk, t_zacc], writes=[t_zT])
            rts = [r_r.next() for _ in range(4)]
            for cbk in range(4):
                for k in range(DC):
                    p.dma("pool", wo[:, k, :], wo_d[cbk, k, :, :], writes=[t_wo])
                for t in range(4):
                    r0 = hf * 512 + t * 128
                    rb, rtk = rts[t]
                    bk, tk = scr.next()
                    for k in range(DC):
                        p.op("pe", lambda e, bk=bk, k=k, t=t: e.matmul(
                            bk[:, :], zT[:, k, t * 128:(t + 1) * 128], wo[:, k, :], start=(k == 0), stop=(k == DC - 1)),
                            reads=[t_zT, t_wo], writes=[tk])
                    p.dma("sp", xres[:], x[r0:r0 + 128, cbk * 512:(cbk + 1) * 512], writes=[t_xres])
                    p.op("dve", lambda e, bk=bk, rb=rb, cbk=cbk: e.tensor_tensor(
                        out=rb[:, cbk * 512:(cbk + 1) * 512], in0=bk[:, :], in1=g1B[:, cbk * 512:(cbk + 1) * 512], op=ALU.mult),
                        reads=[tk, t_c], writes=[rtk])
                    p.op("dve", lambda e, rb=rb, cbk=cbk: e.scalar_tensor_tensor(
                        out=rb[:, cbk * 512:(cbk + 1) * 512], in0=xres[:], scalar=ALPHA, in1=rb[:, cbk * 512:(cbk + 1) * 512],
                        op0=ALU.mult, op1=ALU.add), reads=[t_xres, rtk], writes=[rtk])
            for t in range(4):
                r0 = hf * 512 + t * 128
                rb, rtk = rts[t]
                emit_layernorm(p, rb, rtk, x1t, t_x1, lgB, lbB, t_c, stats, t_st)
                p.dma("sp", x1_d[r0:r0 + 128, :], x1t[:], reads=[t_x1], is_out=True)
                for c4 in range(4):
                    bk, tk = scr.next()
                    for j in range(4):
                        c = c4 * 4 + j
                        p.op("pe", lambda e, bk=bk, j=j, c=c: e.transpose(
                            bk[:, j * 128:(j + 1) * 128], x1t[:, c * 128:(c + 1) * 128], ident[:]),
                            reads=[t_x1, t_c], writes=[tk])
                    for j in range(4):
                        c = c4 * 4 + j
                        p.op("act", lambda e, bk=bk, j=j, c=c: e.activation(
                            out=h2f[:, c, :], in_=bk[:, j * 128:(j + 1) * 128], func=AF.Identity,
                            bias=modT[:, 4, c:c + 1], scale=modT[:, 3, c:c + 1]), reads=[tk, t_c], writes=[t_h2])
                p.op("pool", lambda e: e.tensor_copy(out=h2b[:], in_=h2f[:]), reads=[t_h2], writes=[t_h2])
                p.dma("sp", h2_d[:, r0:r0 + 128].rearrange("(k p) t -> p k t", p=128), h2b[:], reads=[t_h2], is_out=True)
                lbk, ltk = scr.next()
                for k in range(DC):
                    p.op("pe", lambda e, lbk=lbk, k=k: e.matmul(lbk[:, 0:72], h2f[:, k, :], wr[:, k, :], start=(k == 0),
                                                                  stop=(k == DC - 1)), reads=[t_h2, t_c], writes=[ltk])
                R = rt
                T = [t_rt]
                p.op("dve", lambda e, lbk=lbk: e.tensor_tensor(out=R[:, 0:72], in0=lbk[:, 0:72], in1=brB[:], op=ALU.add),
                     reads=[ltk, t_c], writes=T)
                p.op("dve", lambda e: e.reduce_max(out=R[:, 72:73], in_=R[:, 0:8], axis=AX.X), reads=T, writes=T)
                p.op("dve", lambda e: e.tensor_scalar(out=R[:, 73:74], in0=R[:, 72:73], scalar1=-1.0, scalar2=None, op0=ALU.mult),
                     reads=T, writes=T)
                p.op("act", lambda e: e.activation(out=R[:, 80:88], in_=R[:, 0:8], func=AF.Exp, bias=R[:, 73:74], scale=1.0),
                     reads=T, writes=T)
                p.op("dve", lambda e: e.reduce_sum(out=R[:, 88:89], in_=R[:, 80:88], axis=AX.X), reads=T, writes=T)
                p.op("dve", lambda e: e.reciprocal(out=R[:, 89:90], in_=R[:, 88:89]), reads=T, writes=T)
                p.op("dve", lambda e: e.tensor_scalar(out=R[:, 96:104], in0=R[:, 0:8], scalar1=R[:, 72:73], scalar2=None,
                                                      op0=ALU.is_ge), reads=T, writes=T)
                p.op("dve", lambda e: e.tensor_scalar(out=R[:, 104:112], in0=R[:, 96:104], scalar1=-1.0, scalar2=30000.0,
                                                      op0=ALU.add, op1=ALU.mult), reads=T, writes=T)
                for gi in range(8):
                    p.op("dve", lambda e, gi=gi: e.tensor_scalar(
                        out=R[:, 128 + gi * 8:136 + gi * 8], in0=R[:, 8 + gi * 8:16 + gi * 8], scalar1=R[:, 104 + gi:105 + gi],
                        scalar2=None, op0=ALU.add), reads=T, writes=T)
                p.op("dve", lambda e: e.max(out=R[:, 192:200], in_=R[:, 128:192]), reads=T, writes=T)
                p.op("dve", lambda e: e.tensor_tensor(out=R[:, 200:201], in0=R[:, 192:193], in1=R[:, 193:194], op=ALU.subtract),
                     reads=T, writes=T)
                p.op("act", lambda e: e.activation(out=R[:, 201:202], in_=R[:, 200:201], func=AF.Sigmoid), reads=T, writes=T)
                p.op("dve", lambda e: e.tensor_tensor(out=R[:, 202:203], in0=R[:, 201:202], in1=R[:, 89:90], op=ALU.mult),
                     reads=T, writes=T)
                p.op("dve", lambda e: e.tensor_tensor(out=R[:, 203:204], in0=R[:, 89:90], in1=R[:, 202:203], op=ALU.subtract),
                     reads=T, writes=T)
                p.op("dve", lambda e: e.tensor_tensor(out=R[:, 204:205], in0=R[:, 202:203], in1=R[:, 203:204], op=ALU.subtract),
                     reads=T, writes=T)
                p.op("dve", lambda e: e.tensor_scalar(out=R[:, 256:320], in0=R[:, 128:192], scalar1=R[:, 193:194], scalar2=R[:, 203:204],
                                                      op0=ALU.is_ge, op1=ALU.mult), reads=T, writes=T)
                p.op("dve", lambda e: e.tensor_scalar(out=R[:, 320:384], in0=R[:, 128:192], scalar1=R[:, 192:193], scalar2=R[:, 204:205],
                                                      op0=ALU.is_ge, op1=ALU.mult), reads=T, writes=T)
                p.op("dve", lambda e: e.tensor_tensor(out=R[:, 384:448], in0=R[:, 256:320], in1=R[:, 320:384], op=ALU.add),
                     reads=T, writes=T)
                p.dma("sp", rw_d[r0:r0 + 128, :], R[:, 384:448], reads=T, is_out=True)
        p.finish()
    return nc


def phaseB_weights(w_gate, b_gate, w_branch, w_out, ln_g, ln_b, w_rg, b_rg, w_re, b_re):
    wg = np.ascontiguousarray(w_gate.reshape(DC, 128, 48, 128).transpose(2, 1, 0, 3).reshape(48, 128, DC * 128))
    wb = np.ascontiguousarray(w_branch.reshape(3, 8, 128, DC, 128).transpose(0, 3, 2, 1, 4).reshape(48, 128, 8 * 128))
    wo = np.ascontiguousarray(w_out.reshape(DC, 128, 4, 512).transpose(2, 0, 1, 3))
    return {"wg": wg, "bg": np.ascontiguousarray(b_gate), "wb": wb, "wo": wo,
            "ln": np.ascontiguousarray(np.stack([ln_g, ln_b])),
            "wr": np.ascontiguousarray(np.concatenate([w_rg, w_re], axis=1)),
            "br": np.ascontiguousarray(np.concatenate([b_rg, b_re])),
            "ident": np.eye(128, dtype=np.float32)}


FE = 384


def build_phaseC(S):
    NT = S // 1024
    nc = bass.Bass("TRN2", target_bir_lowering=False)
    h2_d = nc.dram_tensor("h2T", [D, S], BF16, kind="ExternalInput").ap()
    rw_d = nc.dram_tensor("rw", [S, 8], F32, kind="ExternalInput").ap()
    wgu_d = nc.dram_tensor("wgu", [8, 2, 128, DC * FE], F32, kind="ExternalInput").ap()
    wd_d = nc.dram_tensor("wd", [8, 128, 3 * D], F32, kind="ExternalInput").ap()
    y_d = nc.dram_tensor("y", [S, D], BF16, kind="ExternalOutput").ap()
    with ExitStack() as st:
        p = Prog(nc, st)
        sb = lambda name, shape, dt: _sb(nc, st, name, shape, dt)
        h2 = sb("h2", [128, DC, 1024], BF16)
        rwt = sb("rwt", [128, 8, 8], F32)
        Yacc = sb("Yacc", [128, 8, D], F32)
        wg_r = Rot([sb("wgt%d" % i, [128, DC * FE], BF16) for i in range(2)])
        wu_r = Rot([sb("wut%d" % i, [128, DC * FE], BF16) for i in range(2)])
        wd_r = Rot([sb("wdt%d" % i, [128, 3 * D], BF16) for i in range(2)])
        A = sb("A", [128, 3, 1024], BF16)
        sg_r = Rot([sb("sg%d" % i, [128, 512], F32) for i in range(2)])
        banks = [_ps(nc, st, "bk%d" % i, [128, 512], F32) for i in range(8)]
        scr = Rot(banks)
        t_h2, t_rw, t_A = Tk(), Tk(), Tk()
        t_y = [Tk() for _ in range(8)]
        for tc in range(NT):
            t0 = tc * 1024
            p.dma("sp", h2[:], h2_d[:, t0:t0 + 1024].rearrange("(k p) t -> p k t", p=128), writes=[t_h2])
            p.dma("sp", rwt[:], rw_d[t0:t0 + 1024, :].rearrange("(t p) e -> p t e", p=128), writes=[t_rw])
            for ex in range(8):
                wgb, wgtk = wg_r.next()
                wub, wutk = wu_r.next()
                wdb, wdtk = wd_r.next()
                for j in range(3):
                    p.dma("pool", wgb[:, j * 2048:(j + 1) * 2048], wgu_d[ex, 0, :, j * 2048:(j + 1) * 2048], writes=[wgtk])
                    p.dma("pool", wub[:, j * 2048:(j + 1) * 2048], wgu_d[ex, 1, :, j * 2048:(j + 1) * 2048], writes=[wutk])
                for j in range(3):
                    p.dma("pool", wdb[:, j * 2048:(j + 1) * 2048], wd_d[ex, :, j * 2048:(j + 1) * 2048], writes=[wdtk])
                for f in range(3):
                    for nh in range(2):
                        gbk, gtk = scr.next()
                        for k in range(DC):
                            p.op("pe", lambda e, gbk=gbk, wgb=wgb, k=k, f=f, nh=nh: e.matmul(
                                gbk[:, :], wgb[:, k * FE + f * 128:k * FE + (f + 1) * 128], h2[:, k, nh * 512:(nh + 1) * 512],
                                start=(k == 0), stop=(k == DC - 1)), reads=[wgtk, t_h2], writes=[gtk])
                        ubk, utk = scr.next()
                        for k in range(DC):
                            p.op("pe", lambda e, ubk=ubk, wub=wub, k=k, f=f, nh=nh: e.matmul(
                                ubk[:, :], wub[:, k * FE + f * 128:k * FE + (f + 1) * 128], h2[:, k, nh * 512:(nh + 1) * 512],
                                start=(k == 0), stop=(k == DC - 1)), reads=[wutk, t_h2], writes=[utk])
                        sg, sgtk = sg_r.next()
                        p.op("act", lambda e, gbk=gbk, sg=sg: e.activation(out=sg[:], in_=gbk[:, :], func=AF.Silu),
                             reads=[gtk], writes=[sgtk])
                        p.op("dve", lambda e, ubk=ubk, sg=sg, f=f, nh=nh: e.tensor_tensor(
                            out=A[:, f, nh * 512:(nh + 1) * 512], in0=ubk[:, :], in1=sg[:], op=ALU.mult),
                            reads=[utk, sgtk], writes=[t_A])
                for t in range(8):
                    for db in range(4):
                        ybk, ytk = scr.next()
                        for f in range(3):
                            p.op("pe", lambda e, ybk=ybk, wdb=wdb, f=f, t=t, db=db: e.matmul(
                                ybk[:, :], A[:, f, t * 128:(t + 1) * 128], wdb[:, f * D + db * 512:f * D + (db + 1) * 512],
                                start=(f == 0), stop=(f == 2)), reads=[t_A, wdtk], writes=[ytk])
                        if ex == 0:
                            p.op("dve", lambda e, ybk=ybk, t=t, db=db, ex=ex: e.tensor_scalar(
                                out=Yacc[:, t, db * 512:(db + 1) * 512], in0=ybk[:, :], scalar1=rwt[:, t, ex:ex + 1], scalar2=None,
                                op0=ALU.mult), reads=[ytk, t_rw], writes=[t_y[t]])
                        else:
                            p.op("dve", lambda e, ybk=ybk, t=t, db=db, ex=ex: e.scalar_tensor_tensor(
                                out=Yacc[:, t, db * 512:(db + 1) * 512], in0=ybk[:, :], scalar=rwt[:, t, ex:ex + 1],
                                in1=Yacc[:, t, db * 512:(db + 1) * 512], op0=ALU.mult, op1=ALU.add),
                                reads=[ytk, t_rw, t_y[t]], writes=[t_y[t]])
            for t in range(8):
                p.dma("pool", y_d[t0 + t * 128:t0 + (t + 1) * 128, :], Yacc[:, t, :], reads=[t_y[t]], is_out=True)
        p.finish()
    return nc


def phaseC_weights(w_g, w_u, w_d):
    g = w_g.reshape(8, DC, 128, FE).transpose(0, 2, 1, 3).reshape(8, 128, DC * FE)
    u = w_u.reshape(8, DC, 128, FE).transpose(0, 2, 1, 3).reshape(8, 128, DC * FE)
    wgu = np.ascontiguousarray(np.stack([g, u], axis=1))
    wd = np.ascontiguousarray(w_d.reshape(8, 3, 128, D).transpose(0, 2, 1, 3).reshape(8, 128, 3 * D))
    return {"wgu": wgu, "wd": wd}


def build_phaseD(TBK=TB):
    NTT = TBK // 128
    nc = bass.Bass("TRN2", target_bir_lowering=False)
    x1_d = nc.dram_tensor("x1", [TBK, D], F32, kind="ExternalInput").ap()
    yg_d = nc.dram_tensor("yg", [8, TBK, D], BF16, kind="ExternalInput").ap()
    mod = nc.dram_tensor("mod", [6, D], F32, kind="ExternalInput").ap()
    ln_d = nc.dram_tensor("ln", [2, D], F32, kind="ExternalInput").ap()
    x2_d = nc.dram_tensor("x2", [TBK, D], F32, kind="ExternalOutput").ap()
    with ExitStack() as st:
        p = Prog(nc, st)
        sb = lambda name, shape, dt: _sb(nc, st, name, shape, dt)
        g2B = sb("g2B", [128, D], F32)
        lgB = sb("lgB", [128, D], F32)
        lbB = sb("lbB", [128, D], F32)
        yt_r = Rot([sb("yt%d" % i, [128, 8, D], BF16) for i in range(2)])
        x_r = Rot([sb("xt%d" % i, [128, D], F32) for i in range(2)])
        r_r = Rot([sb("rr%d" % i, [128, D], F32) for i in range(2)])
        o_r = Rot([sb("xo%d" % i, [128, D], F32) for i in range(2)])
        stats = sb("stats", [128, 32], F32)
        t_c, t_st = Tk(), Tk()
        p.dma("sp", g2B[:], mod[5, :].partition_broadcast(128), writes=[t_c])
        p.dma("sp", lgB[:], ln_d[0, :].partition_broadcast(128), writes=[t_c])
        p.dma("sp", lbB[:], ln_d[1, :].partition_broadcast(128), writes=[t_c])
        for t in range(NTT):
            r0 = t * 128
            yt, yttk = yt_r.next()
            p.dma("sp", yt[:], yg_d[:, r0:r0 + 128, :].rearrange("g p d -> p g d"), writes=[yttk])
            xt, xtk = x_r.next()
            p.dma("sp", xt[:], x1_d[r0:r0 + 128, :], writes=[xtk])
            rb, rtk = r_r.next()
            p.op("dve", lambda e, rb=rb, yt=yt: e.tensor_tensor(out=rb[:], in0=yt[:, 0, :], in1=yt[:, 1, :], op=ALU.add),
                 reads=[yttk], writes=[rtk])
            for gi in range(2, 8):
                eng = "dve" if gi % 2 == 0 else "pool"
                p.op(eng, lambda e, rb=rb, yt=yt, gi=gi: e.tensor_tensor(out=rb[:], in0=rb[:], in1=yt[:, gi, :], op=ALU.add),
                     reads=[yttk, rtk], writes=[rtk])
            p.op("pool", lambda e, rb=rb: e.tensor_tensor(out=rb[:], in0=rb[:], in1=g2B[:], op=ALU.mult),
                 reads=[rtk, t_c], writes=[rtk])
            p.op("dve", lambda e, rb=rb, xt=xt: e.scalar_tensor_tensor(out=rb[:], in0=xt[:], scalar=ALPHA, in1=rb[:],
                                                                      op0=ALU.mult, op1=ALU.add),
                 reads=[xtk, rtk], writes=[rtk])
            xo, xotk = o_r.next()
            emit_layernorm(p, rb, rtk, xo, xotk, lgB, lbB, t_c, stats, t_st)
            p.dma("sp", x2_d[r0:r0 + 128, :], xo[:], reads=[xotk], is_out=True)
        p.finish()
    return nc


def _run(nc, maps):
    res = run_bass_kernel_spmd(nc, maps, core_ids=list(range(NCORE)))
    return res.results


def kernel(x, c, w_ada, b_ada, w_in, w_branch_gate, b_branch_gate, attn_sinks, lambda_q1, lambda_k1, lambda_q2,
           lambda_k2, subln_g, w_branch, w_out, ln1_g, ln1_b, w_router_group, b_router_group, w_router_expert,
           b_router_expert, w_exp_gate, w_exp_up, w_exp_down, ln2_g, ln2_b):
    f32 = lambda a: np.asarray(a, np.float32)
    xs = np.ascontiguousarray(f32(x)[0])
    mod_all = run_phase0(f32(c), f32(w_ada), f32(b_ada))
    ncA = build_phaseA2(SEQ)
    ncB = build_phaseB(TB)
    ncC = build_phaseC(SEQ)
    ncD = build_phaseD(TB)
    for l in range(DEPTH):
        lam_init = 0.8 - 0.6 * math.exp(-0.3 * l)
        m = mod_all[l].reshape(6, D)
        mod6 = np.ascontiguousarray(np.stack([m[1], m[0], m[2], m[4], m[3], m[5]]))
        maps = [phaseA_inputs(i, SEQ, xs, mod6[0:2], f32(w_in[l]), f32(attn_sinks[l]), f32(lambda_q1[l]),
                              f32(lambda_k1[l]), f32(lambda_q2[l]), f32(lambda_k2[l]), f32(subln_g[l]), lam_init)
                for i in range(NCORE)]
        ra = _run(ncA, maps)
        o_cat = np.empty((SEQ, 3, 1024), np.float32)
        for i in range(NCORE):
            o_cat[:, :, 128 * i:128 * (i + 1)] = ra[i]["o"].reshape(SEQ, 3, 128)
        o_cat = o_cat.reshape(SEQ, 3072)
        del ra, maps
        wts = phaseB_weights(f32(w_branch_gate[l]), f32(b_branch_gate[l]), f32(w_branch[l]), f32(w_out[l]), f32(ln1_g[l]),
                             f32(ln1_b[l]), f32(w_router_group[l]), f32(b_router_group[l]), f32(w_router_expert[l]),
                             f32(b_router_expert[l]))
        maps = []
        for i in range(NCORE):
            mm = dict(wts)
            mm.update({"x": np.ascontiguousarray(xs[i * TB:(i + 1) * TB]),
                       "o": np.ascontiguousarray(o_cat[i * TB:(i + 1) * TB]), "mod": mod6})
            maps.append(mm)
        rb = _run(ncB, maps)
        x1 = [rb[i]["x1"] for i in range(NCORE)]
        h2T = np.ascontiguousarray(np.concatenate([rb[i]["h2T"] for i in range(NCORE)], axis=1))
        rw = np.concatenate([rb[i]["rw"] for i in range(NCORE)], axis=0)
        del rb, maps, wts, o_cat
        maps = []
        for g in range(NCORE):
            mm = phaseC_weights(f32(w_exp_gate[l, 8 * g:8 * g + 8]), f32(w_exp_up[l, 8 * g:8 * g + 8]),
                                f32(w_exp_down[l, 8 * g:8 * g + 8]))
            mm.update({"h2T": h2T, "rw": np.ascontiguousarray(rw[:, 8 * g:8 * g + 8])})
            maps.append(mm)
        rc = _run(ncC, maps)
        del maps
        ln2 = np.ascontiguousarray(np.stack([f32(ln2_g[l]), f32(ln2_b[l])]))
        maps = []
        for i in range(NCORE):
            yg = np.ascontiguousarray(np.stack([rc[g]["y"][i * TB:(i + 1) * TB] for g in range(NCORE)]))
            maps.append({"x1": x1[i], "yg": yg, "mod": mod6, "ln": ln2})
        del rc
        rd = _run(ncD, maps)
        xs = np.ascontiguousarray(np.concatenate([rd[i]["x2"] for i in range(NCORE)], axis=0))
        del rd, maps
    return xs[None].astype(np.float32)


def phaseA_consts2(core, S):
    NQ = S // 128
    sl_swa = [2.0 ** (-8.0 * (h + 1) / 16.0) for h in (2 * core, 2 * core + 1)]
    sl_d = 2.0 ** (-8.0 * (core + 1) / 8.0)
    p = np.arange(128)[:, None].astype(np.float64)
    t = np.arange(128)[None, :].astype(np.float64)
    swab = np.zeros((128, 4, 128), np.float32)
    for hh in range(2):
        d1 = t - p
        swab[:, 2 * hh, :] = np.where(d1 >= 0, -sl_swa[hh] * d1, NEG)
        d0 = 128 + t - p
        swab[:, 2 * hh + 1, :] = np.where(d0 < 128, -sl_swa[hh] * d0, NEG)
    nj = NQ + 3
    jj = np.arange(nj)[None, :] - 3
    dbias = (sl_d * p - sl_d * 128.0 * jj).astype(np.float32)
    ident = np.eye(128, dtype=np.float32)
    cf = np.concatenate([swab.reshape(128, 512), dbias, ident], axis=1)
    tri = (p >= t).astype(np.float32)
    ones = np.ones((128, 128), np.float32)
    strict = (p < t).astype(np.float32)
    incl = (p <= t).astype(np.float32)
    upper = (p < t).astype(np.float32)
    tl = np.arange(512).astype(np.float64)
    v = (-8.0 * sl_d * tl)
    hi = v.astype(np.float32).astype(ml_dtypes.bfloat16)
    lo = (v - hi.astype(np.float64)).astype(np.float32).astype(ml_dtypes.bfloat16)
    qaug = np.zeros((128, 512), np.float32)
    qaug[0] = hi.astype(np.float32)
    qaug[1] = lo.astype(np.float32)
    qaug[64] = hi.astype(np.float32)
    qaug[65] = lo.astype(np.float32)
    cb = _bf(np.concatenate([tri, ones, strict, incl, qaug, upper], axis=1))
    return np.ascontiguousarray(cf), np.ascontiguousarray(cb)


def build_phaseA2(S, parts=("swa", "sb", "df")):
    NQ = S // 128
    NG = S // 512
    NJ = NQ + 3
    NCF = 512 + NJ + 128
    nc = bass.Bass("TRN2", target_bir_lowering=False)
    x = nc.dram_tensor("x", [S, D], F32, kind="ExternalInput").ap()
    mod = nc.dram_tensor("mod", [2, D], F32, kind="ExternalInput").ap()
    w_t = nc.dram_tensor("w_t", [D, A_WT], F32, kind="ExternalInput").ap()
    w_v = nc.dram_tensor("w_v", [D, A_WV], F32, kind="ExternalInput").ap()
    cf_d = nc.dram_tensor("cf", [128, NCF], F32, kind="ExternalInput").ap()
    cb_d = nc.dram_tensor("cb", [128, 1152], BF16, kind="ExternalInput").ap()
    sm_d = nc.dram_tensor("sm", [1, A_SM], F32, kind="ExternalInput").ap()
    o_d = nc.dram_tensor("o", [S, 384], F32, kind="ExternalOutput").ap()
    with ExitStack() as st:
        p = Prog(nc, st)
        sb = lambda name, shape, dt: _sb(nc, st, name, shape, dt)
        wT = sb("wT", [128, DC, A_WT], BF16)
        wV = sb("wV", [128, DC, A_WV], BF16)
        KA = sb("KA", [128, S], BF16)
        KB = sb("KB", [128, S], BF16)
        KC = sb("KC", [128, S], BF16)
        V = sb("V", [128, NQ, VW], BF16)
        xs = sb("xs", [128, D], F32)
        hT = sb("hT", [128, DC, 512], BF16)
        QA = sb("QA", [128, 512], BF16)
        QB = sb("QB", [128, 512], BF16)
        QC = sb("QC", [128, 512], BF16)
        cf = sb("cfs", [128, NCF], F32)
        cb = sb("cbs", [128, 1152], BF16)
        sm = sb("sms", [128, A_SM], F32)
        sclT = sb("sclT", [128, DC], F32)
        shT = sb("shT", [128, DC], F32)
        misc = sb("misc", [128, 16], F32)
        gsub = sb("gsub", [128, 128], F32)
        junk = sb("junk", [128, 128], F32)
        ost = [sb("ost%d" % i, [128, 4, 384], F32) for i in range(2)]
        t_ost = [Tk(), Tk()]
        banks = [_ps(nc, st, "bk%d" % i, [128, 512], F32) for i in range(8)]
        scr = Rot(banks[0:2])
        CARRY = banks[2:4]
        t_carry = [Tk(), Tk()]
        SBACC, t_sbacc = banks[4], Tk()
        DACC = banks[5:8]
        t_dacc = [Tk() for _ in range(3)]
        e_r = Rot([sb("e%d" % i, [128, 512], F32) for i in range(4)])
        L_r = Rot([sb("L%d" % i, [128, 512], BF16) for i in range(6)])
        W_r = Rot([sb("W%d" % i, [128, 512], BF16) for i in range(4)])
        E_r = Rot([sb("E%d" % i, [128, 512], BF16) for i in range(4)])
        X_r = Rot([sb("X%d" % i, [128, 512], BF16) for i in range(4)])
        sE_r = Rot([sb("sE%d" % i, [128, 512], BF16) for i in range(2)])
        fin_r = Rot([sb("fin%d" % i, [128, 8], F32) for i in range(2)])
        da_r = Rot([sb("da%d" % i, [128, 128], F32) for i in range(2)])
        dd_r = Rot([sb("dd%d" % i, [128, 128], F32) for i in range(2)])

        t_c = Tk()
        t_k = [Tk() for _ in range(NG)]
        t_xs, t_hT, t_q, t_misc = Tk(), Tk(), Tk(), Tk()

        swab = cf[:, 0:512]
        dbias = lambda j: cf[:, 512 + j:512 + j + 1]
        ident = cf[:, 512 + NJ:512 + NJ + 128]
        tri = cb[:, 0:128]
        ones = cb[:, 128:256]
        strict = cb[:, 256:384]
        incl = cb[:, 384:512]
        qaug = cb[:, 512:1024]
        upper = cb[:, 1024:1152]

        p.dma("sp", cf[:], cf_d[:, :], writes=[t_c])
        p.dma("sp", cb[:], cb_d[:, :], writes=[t_c])
        p.dma("sp", sm[:], sm_d[0, :].partition_broadcast(128), writes=[t_c])
        p.dma("sp", sclT[:], mod[0, :].rearrange("(k p) -> p k", p=128), writes=[t_c], slow=True)
        p.dma("sp", shT[:], mod[1, :].rearrange("(k p) -> p k", p=128), writes=[t_c], slow=True)
        for k in range(DC):
            p.dma("pool", wT[:, k, :], w_t[k * 128:(k + 1) * 128, :], writes=[t_c])
            p.dma("pool", wV[:, k, :], w_v[k * 128:(k + 1) * 128, :], writes=[t_c])
        p.op("pool", lambda e: e.memset(V[:, :, 64:65], 1.0), writes=[t_c])
        p.op("pool", lambda e: e.memset(V[:, :, 321:322], 1.0), writes=[t_c])
        p.op("act", lambda e: e.activation(out=misc[:, 0:2], in_=sm[:, 0:2], func=AF.Exp), reads=[t_c], writes=[t_misc])
        p.op("dve", lambda e: e.tensor_tensor(out=junk[:, 0:64], in0=sm[:, 2:66], in1=sm[:, 66:130], op=ALU.mult),
             reads=[t_c], writes=[t_misc])
        p.op("dve", lambda e: e.reduce_sum(out=misc[:, 2:3], in_=junk[:, 0:64], axis=AX.X), reads=[t_misc], writes=[t_misc])
        p.op("dve", lambda e: e.tensor_tensor(out=junk[:, 64:128], in0=sm[:, 130:194], in1=sm[:, 194:258], op=ALU.mult),
             reads=[t_c], writes=[t_misc])
        p.op("dve", lambda e: e.reduce_sum(out=misc[:, 3:4], in_=junk[:, 64:128], axis=AX.X), reads=[t_misc], writes=[t_misc])
        p.op("act", lambda e: e.activation(out=misc[:, 5:7], in_=misc[:, 2:4], func=AF.Exp), reads=[t_misc], writes=[t_misc])
        p.op("dve", lambda e: e.tensor_tensor(out=misc[:, 7:8], in0=misc[:, 6:7], in1=misc[:, 5:6], op=ALU.subtract),
             reads=[t_misc], writes=[t_misc])
        p.op("dve", lambda e: e.tensor_tensor(out=misc[:, 4:5], in0=misc[:, 7:8], in1=sm[:, 386:387], op=ALU.subtract),
             reads=[t_misc, t_c], writes=[t_misc])
        p.op("dve", lambda e: e.tensor_scalar(out=gsub[:], in0=sm[:, 258:386], scalar1=sm[:, 387:388], scalar2=None,
                                              op0=ALU.mult), reads=[t_c], writes=[t_misc])

        def dacc(m, qi):
            j = m * 4 + qi
            return DACC[j // 3], t_dacc[j // 3], (j % 3) * 129, j // 3

        for g in range(NG):
            for tt in range(4):
                r0 = g * 512 + tt * 128
                p.dma("sp", xs[:], x[r0:r0 + 128, :], writes=[t_xs])
                for c4 in range(4):
                    bk, tk = scr.next()
                    for j in range(4):
                        c = c4 * 4 + j
                        p.op("pe", lambda e, bk=bk, j=j, c=c: e.transpose(
                            bk[:, j * 128:(j + 1) * 128], xs[:, c * 128:(c + 1) * 128], ident),
                            reads=[t_xs, t_c], writes=[tk])
                    for j in range(4):
                        c = c4 * 4 + j
                        p.op("dve", lambda e, bk=bk, j=j, c=c, tt=tt: e.tensor_scalar(
                            out=hT[:, c, tt * 128:(tt + 1) * 128], in0=bk[:, j * 128:(j + 1) * 128], scalar1=sclT[:, c:c + 1],
                            scalar2=shT[:, c:c + 1], op0=ALU.mult, op1=ALU.add), reads=[tk, t_c], writes=[t_hT])
            dests = [QA[:, :], KA[:, g * 512:(g + 1) * 512], QB[:, :], KB[:, g * 512:(g + 1) * 512],
                     QC[:, :], KC[:, g * 512:(g + 1) * 512]]
            for m in range(6):
                bk, tk = scr.next()
                for k in range(DC):
                    p.op("pe", lambda e, bk=bk, k=k, m=m: e.matmul(
                        bk[:, :], wT[:, k, m * 128:(m + 1) * 128], hT[:, k, :], start=(k == 0), stop=(k == DC - 1)),
                        reads=[t_c, t_hT], writes=[tk])
                wtk = t_q if m % 2 == 0 else t_k[g]
                p.op("act", lambda e, bk=bk, m=m, dests=dests: e.activation(out=dests[m], in_=bk[:, :], func=AF.Identity),
                     reads=[tk], writes=[wtk])
            for t in range(4):
                bk, tk = scr.next()
                for k in range(DC):
                    p.op("pe", lambda e, bk=bk, k=k, t=t: e.matmul(
                        bk[:, 0:A_WV], hT[:, k, t * 128:(t + 1) * 128], wV[:, k, :], start=(k == 0), stop=(k == DC - 1)),
                        reads=[t_c, t_hT], writes=[tk])
                qb = g * 4 + t
                p.op("dve", lambda e, bk=bk, qb=qb: e.tensor_copy(out=V[:, qb, 0:64], in_=bk[:, 0:64]),
                     reads=[tk], writes=[t_k[g]])
                p.op("dve", lambda e, bk=bk, qb=qb: e.tensor_copy(out=V[:, qb, 65:321], in_=bk[:, 64:320]),
                     reads=[tk], writes=[t_k[g]])

            o_t, o_tk = ost[g % 2], t_ost[g % 2]

            swst = {}

            def sw_a(qi):
                i = g * 4 + qi
                kbs = [i] + ([i - 1] if i > 0 else [])
                wd = 128 * len(kbs)
                tm, ttk = e_r.next()
                sE, setk = sE_r.next()
                segs = []
                for hh in range(2):
                    bk, tk = scr.next()
                    for n, kb in enumerate(kbs):
                        p.op("pe", lambda e, bk=bk, hh=hh, kb=kb, n=n: e.matmul(
                            bk[:, n * 128:(n + 1) * 128], KA[64 * hh:64 * hh + 64, kb * 128:(kb + 1) * 128],
                            QA[64 * hh:64 * hh + 64, qi * 128:(qi + 1) * 128], start=True, stop=True),
                            reads=[t_k[kb // 4], t_q], writes=[tk])
                        segs.append((hh, kb, 2 * hh + n))
                    p.op("dve", lambda e, bk=bk, hh=hh: e.scalar_tensor_tensor(
                        out=tm[:, 256 * hh:256 * hh + wd], in0=bk[:, 0:wd], scalar=SCALE, in1=swab[:, 256 * hh:256 * hh + wd],
                        op0=ALU.mult, op1=ALU.add), reads=[tk, t_c], writes=[ttk])
                    p.op("act", lambda e, hh=hh: e.activation(out=sE[:, 256 * hh:256 * hh + wd], in_=tm[:, 256 * hh:256 * hh + wd],
                                                              func=AF.Exp), reads=[ttk], writes=[setk])
                swst[qi] = (segs, sE, setk)

            def sw_b(qi):
                segs, sE, setk = swst.pop(qi)
                abk, atk = scr.next()
                first = True
                for hh in range(2):
                    mine = [s for s in segs if s[0] == hh]
                    for n, (_, kb, slot) in enumerate(mine):
                        p.op("pe", lambda e, hh=hh, kb=kb, slot=slot, first=first: e.matmul(
                            abk[:, hh * 65:hh * 65 + 65], sE[:, slot * 128:(slot + 1) * 128], V[:, kb, 0:65],
                            start=first, stop=True, skip_group_check=True),
                            reads=[setk, t_k[kb // 4], t_c], writes=[atk])
                        first = False
                fb, ftk = fin_r.next()
                for hh in range(2):
                    p.op("dve", lambda e, hh=hh: e.tensor_tensor(
                        out=fb[:, hh:hh + 1], in0=abk[:, hh * 65 + 64:hh * 65 + 65], in1=misc[:, hh:hh + 1], op=ALU.add),
                        reads=[atk, t_misc], writes=[ftk])
                p.op("dve", lambda e: e.reciprocal(out=fb[:, 2:4], in_=fb[:, 0:2]), reads=[ftk], writes=[ftk])
                for hh in range(2):
                    p.op("dve", lambda e, hh=hh, o_t=o_t: e.tensor_scalar(
                        out=o_t[:, qi, 64 * hh:64 * hh + 64], in0=abk[:, hh * 65:hh * 65 + 64], scalar1=fb[:, 2 + hh:3 + hh],
                        scalar2=None, op0=ALU.mult), reads=[atk, ftk], writes=[o_tk])

            if "swa" in parts:
                pipeline(4, [sw_a, sw_b])

            nst = 4 * g + 4
            sbst = {}
            dfst = {}
            dstarted = set()

            def sb_a(hh, s):
                kb = 4 * g + 3 - s
                qs = max(0, kb - 4 * g) * 128
                kg = t_k[kb // 4]
                bk, tk = scr.next()
                p.op("pe", lambda e: e.matmul(bk[:, qs:512], KB[64 * hh:64 * hh + 64, kb * 128:(kb + 1) * 128],
                                              QB[64 * hh:64 * hh + 64, qs:512], start=True, stop=True),
                     reads=[kg, t_q], writes=[tk])
                eb, etk = e_r.next()
                p.op("act", lambda e: e.activation(out=eb[:, qs:512], in_=bk[:, qs:512], func=AF.Exp, scale=SCALE),
                     reads=[tk], writes=[etk])
                Lb, Ltk = L_r.next()
                p.op("act", lambda e: e.activation(out=Lb[:, qs:512], in_=eb[:, qs:512], func=AF.Ln, bias=1.0, scale=1.0),
                     reads=[etk], writes=[Ltk])
                if kb >= 4 * g:
                    p.op("pool", lambda e: e.tensor_tensor(out=Lb[:, qs:qs + 128], in0=Lb[:, qs:qs + 128], in1=strict,
                                                           op=ALU.mult), reads=[Ltk, t_c], writes=[Ltk])
                sbst[(hh, s)] = dict(kb=kb, qs=qs, kg=kg, Lb=Lb, Ltk=Ltk, eb=eb, etk=etk)

            def sb_b(hh, s):
                d = sbst[(hh, s)]
                kb, qs, kg, Lb, Ltk, eb, etk = d["kb"], d["qs"], d["kg"], d["Lb"], d["Ltk"], d["eb"], d["etk"]
                cbk, ctk = CARRY[hh], t_carry[hh]
                p.op("pe", lambda e: e.matmul(cbk[:, qs:512], tri, Lb[:, qs:512], start=(s == 0), stop=True,
                                              skip_group_check=True), reads=[Ltk, t_c], writes=[ctk])
                Xb, Xtk = X_r.next()
                p.op("act", lambda e: e.activation(out=Xb[:, qs:512], in_=cbk[:, qs:512], func=AF.Exp, scale=-1.0),
                     reads=[ctk], writes=[Xtk])
                Wb, Wtk = W_r.next()
                p.op("dve", lambda e: e.tensor_tensor(out=Wb[:, qs:512], in0=eb[:, qs:512], in1=Xb[:, qs:512], op=ALU.mult),
                     reads=[etk, Xtk], writes=[Wtk])
                if kb >= 4 * g:
                    p.op("pool", lambda e: e.tensor_tensor(out=Wb[:, qs:qs + 128], in0=Wb[:, qs:qs + 128], in1=strict,
                                                           op=ALU.mult), reads=[Wtk, t_c], writes=[Wtk])
                d["Wb"], d["Wtk"] = Wb, Wtk

            def sb_c(hh, s):
                d = sbst.pop((hh, s))
                kb, qs, kg, Lb, Ltk, Wb, Wtk = d["kb"], d["qs"], d["kg"], d["Lb"], d["Ltk"], d["Wb"], d["Wtk"]
                cbk, ctk = CARRY[hh], t_carry[hh]
                if kb > 0:
                    p.op("pe", lambda e: e.matmul(cbk[:, qs:512], upper, Lb[:, qs:512], start=False, stop=True,
                                                  skip_group_check=True), reads=[Ltk, t_c], writes=[ctk])
                for qi in range(qs // 128, 4):
                    c0 = hh * 256 + qi * 64
                    p.op("pe", lambda e, qi=qi, c0=c0: e.matmul(
                        SBACC[:, c0:c0 + 64], Wb[:, qi * 128:(qi + 1) * 128], V[:, kb, 65 + 64 * hh:65 + 64 * hh + 64],
                        start=(s == 0 and hh == 0 and qi == 3), stop=(kb == 0), skip_group_check=True),
                        reads=[Wtk, kg], writes=[t_sbacc])
                if kb == 0:
                    p.op("dve", lambda e, o_t=o_t: e.tensor_copy(
                        out=o_t[:, :, 128 + 64 * hh:128 + 64 * hh + 64],
                        in_=SBACC[:, hh * 256:(hh + 1) * 256].rearrange("p (q d) -> p q d", d=64)),
                        reads=[t_sbacc], writes=[o_tk])

            def df_a(m, s):
                kb = s
                qs = max(0, kb - 4 * g) * 128
                kg = t_k[kb // 4]
                bk, tk = scr.next()
                p.op("pe", lambda e: e.matmul(bk[:, qs:512], KC[64 * m:64 * m + 64, kb * 128:(kb + 1) * 128],
                                              QC[64 * m:64 * m + 64, qs:512], start=True, stop=False),
                     reads=[kg, t_q], writes=[tk])
                p.op("pe", lambda e: e.matmul(bk[:, qs:512], ones[64 * m:64 * m + 2, :], qaug[64 * m:64 * m + 2, qs:512],
                                              start=False, stop=True), reads=[t_c], writes=[tk])
                Eb, Etk = E_r.next()
                jidx = (4 * g - kb) + 3
                p.op("act", lambda e: e.activation(out=Eb[:, qs:512], in_=bk[:, qs:512], func=AF.Exp, scale=SCALE,
                                                   bias=dbias(jidx)), reads=[tk, t_c], writes=[Etk])
                if kb >= 4 * g:
                    p.op("pool", lambda e: e.tensor_tensor(out=Eb[:, qs:qs + 128], in0=Eb[:, qs:qs + 128], in1=incl,
                                                           op=ALU.mult), reads=[Etk, t_c], writes=[Etk])
                dfst[(m, s)] = dict(kb=kb, qs=qs, kg=kg, Eb=Eb, Etk=Etk)

            def df_b(m, s):
                d = dfst.pop((m, s))
                kb, qs, kg, Eb, Etk = d["kb"], d["qs"], d["kg"], d["Eb"], d["Etk"]
                for qi in range(qs // 128, 4):
                    ab, atk, c0, bi = dacc(m, qi)
                    stt = bi not in dstarted
                    dstarted.add(bi)
                    p.op("pe", lambda e, qi=qi, ab=ab, c0=c0, stt=stt: e.matmul(
                        ab[:, c0:c0 + 129], Eb[:, qi * 128:(qi + 1) * 128], V[:, kb, 193:322],
                        start=stt, stop=(kb == 4 * g + qi), skip_group_check=True),
                        reads=[Etk, kg, t_c], writes=[atk])

            do_sb = "sb" in parts
            do_df = "df" in parts
            for step in range(nst + 2):
                if step < nst:
                    if do_sb:
                        sb_a(0, step)
                        sb_a(1, step)
                    if do_df:
                        df_a(0, step)
                        df_a(1, step)
                if 0 <= step - 2 < nst and do_sb:
                    sb_c(0, step - 2)
                    sb_c(1, step - 2)
                if 0 <= step - 1 < nst:
                    if do_sb:
                        sb_b(0, step - 1)
                        sb_b(1, step - 1)
                    if do_df:
                        df_b(0, step - 1)
                        df_b(1, step - 1)

            for qi in (range(4) if do_df else ()):
                a1, a1tk, c1, _ = dacc(0, qi)
                a2, a2tk, c2, _ = dacc(1, qi)
                fb, ftk = fin_r.next()
                p.op("dve", lambda e, fb=fb, a1=a1, c1=c1: e.reciprocal(out=fb[:, 0:1], in_=a1[:, c1 + 128:c1 + 129]),
                     reads=[a1tk], writes=[ftk])
                p.op("dve", lambda e, fb=fb, a2=a2, c2=c2: e.reciprocal(out=fb[:, 1:2], in_=a2[:, c2 + 128:c2 + 129]),
                     reads=[a2tk], writes=[ftk])
                p.op("dve", lambda e, fb=fb: e.tensor_tensor(out=fb[:, 2:3], in0=fb[:, 1:2], in1=misc[:, 4:5], op=ALU.mult),
                     reads=[ftk, t_misc], writes=[ftk])
                da, datk = da_r.next()
                p.op("dve", lambda e, fb=fb, a1=a1, c1=c1, da=da: e.tensor_scalar(
                    out=da[:], in0=a1[:, c1:c1 + 128], scalar1=fb[:, 0:1], scalar2=None, op0=ALU.mult),
                    reads=[a1tk, ftk], writes=[datk])
                dd, ddtk = dd_r.next()
                p.op("dve", lambda e, fb=fb, a2=a2, c2=c2, da=da, dd=dd: e.scalar_tensor_tensor(
                    out=dd[:], in0=a2[:, c2:c2 + 128], scalar=fb[:, 2:3], in1=da[:], op0=ALU.mult, op1=ALU.add),
                    reads=[a2tk, ftk, datk], writes=[ddtk])
                p.op("dve", lambda e, dd=dd, da=da: e.tensor_tensor(out=da[:], in0=dd[:], in1=dd[:], op=ALU.mult),
                     reads=[ddtk], writes=[datk])
                p.op("dve", lambda e, fb=fb, da=da: e.reduce_sum(out=fb[:, 3:4], in_=da[:], axis=AX.X),
                     reads=[datk], writes=[ftk])
                p.op("act", lambda e, fb=fb: e.activation(out=fb[:, 4:5], in_=fb[:, 3:4], func=AF.Ln, bias=RMS_EPS,
                                                          scale=1.0 / 128.0), reads=[ftk], writes=[ftk])
                p.op("act", lambda e, fb=fb: e.activation(out=fb[:, 5:6], in_=fb[:, 4:5], func=AF.Exp, scale=-0.5),
                     reads=[ftk], writes=[ftk])
                p.op("dve", lambda e, fb=fb, dd=dd, qi=qi, o_t=o_t: e.scalar_tensor_tensor(
                    out=o_t[:, qi, 256:384], in0=dd[:], scalar=fb[:, 5:6], in1=gsub[:], op0=ALU.mult, op1=ALU.mult),
                    reads=[ddtk, ftk, t_misc], writes=[o_tk])
            p.dma("sp", o_d[g * 512:(g + 1) * 512, :].rearrange("(q p) c -> p q c", p=128), o_t[:], reads=[o_tk],
                  is_out=True)
        p.finish()
    return nc
```

```python
import math
from contextlib import ExitStack
import numpy as np
import ml_dtypes
import concourse.bass as bass
import concourse.mybir as mybir
from concourse.bass_utils import run_bass_kernel_spmd

F32 = mybir.dt.float32
BF16 = mybir.dt.bfloat16
AF = mybir.ActivationFunctionType
ALU = mybir.AluOpType
AX = mybir.AxisListType

D = 2048
SEQ = 8192
DEPTH = 4
NCORE = 8
DC = D // 128
ALPHA = (2.0 * DEPTH) ** 0.25
LN_EPS = 1e-5
RMS_EPS = 1e-5
SCALE = 1.0 / 8.0
NEG = -30000.0


class Tk:
    __slots__ = ("w", "r")

    def __init__(self):
        self.w = None
        self.r = []


class Prog:
    ENG = ("pe", "act", "dve", "pool", "sp")
    NDMA = 40

    def __init__(self, nc, st):
        self.nc = nc
        self.st = st
        self.ops = {e: [] for e in self.ENG}
        self.sem = {e: st.enter_context(nc.semaphore("s_" + e)) for e in self.ENG}
        self.cnt = {e: 0 for e in self.ENG}
        self.seen = {e: {} for e in self.ENG}
        self.dsem = [st.enter_context(nc.semaphore("d%d" % i)) for i in range(self.NDMA)]
        self.dcnt = [0] * self.NDMA
        self.dnext = 0
        self.outs = []
        self.meta = {e: [] for e in self.ENG}

    def _need(self, e, ev, waits, raw, strict=False):
        if ev is None:
            return
        key, val, sem = ev
        if key == e and not strict and e == "pe":
            return
        if self.seen[e].get(key, 0) >= val:
            return
        self.seen[e][key] = val
        waits.append((sem, val))

    def _deps(self, e, reads, writes, strict=False):
        waits = []
        for t in reads:
            self._need(e, t.w, waits, True, strict)
        for t in writes:
            self._need(e, t.w, waits, False, strict)
            for r in t.r:
                self._need(e, r, waits, False, strict)
        return waits

    def op(self, e, fn, reads=(), writes=()):
        waits = self._deps(e, reads, writes)
        self.cnt[e] += 1
        sem = self.sem[e]
        ev = (e, self.cnt[e], sem)

        def run(eng, waits=waits, fn=fn, sem=sem):
            for s, v in waits:
                eng.wait_ge(s, v)
            fn(eng).then_inc(sem, 1)

        self.ops[e].append(run)
        self.meta[e].append(([(id(s_), v) for s_, v in waits], (id(sem), 1)))
        for t in reads:
            t.r.append(ev)
        for t in writes:
            t.w = ev
            t.r = []

    FRESH_POOL = False

    def dma(self, q, out, in_, reads=(), writes=(), is_out=False, slow=False):
        waits = self._deps(q, reads, writes, strict=True)
        if q == "pool" and Prog.FRESH_POOL:
            n = getattr(self, "n_fp", 0)
            self.n_fp = n + 1
            sem = self.st.enter_context(self.nc.semaphore("fp%d" % n))
            ev = ("fp%d" % n, 16, sem)
        else:
            i = self.dnext
            self.dnext = (self.dnext + 1) % self.NDMA
            sem = self.dsem[i]
            key = "d%d" % i
            prev = self.dcnt[i]
            if prev and self.seen[q].get(key, 0) < prev:
                self.seen[q][key] = prev
                waits.append((sem, prev))
            self.dcnt[i] += 16
            ev = (key, self.dcnt[i], sem)

        def run(eng, waits=waits, sem=sem, out=out, in_=in_, slow=slow):
            for s, v in waits:
                eng.wait_ge(s, v)
            if slow:
                eng.dma_start(out=out, in_=in_, allow_slow_non_contiguous=True).then_inc(sem, 16)
            else:
                eng.dma_start(out=out, in_=in_).then_inc(sem, 16)

        self.ops[q].append(run)
        self.meta[q].append(([(id(s_), v) for s_, v in waits], (id(sem), 16)))
        for t in reads:
            t.r.append(ev)
        for t in writes:
            t.w = ev
            t.r = []
        if is_out:
            self.outs.append(ev)

    def check_deadlock(self):
        cnt = {}
        pos = {e: 0 for e in self.ENG}
        progress = True
        while progress:
            progress = False
            for e in self.ENG:
                while pos[e] < len(self.meta[e]):
                    waits, (sk, inc) = self.meta[e][pos[e]]
                    if all(cnt.get(k, 0) >= v for k, v in waits):
                        cnt[sk] = cnt.get(sk, 0) + inc
                        pos[e] += 1
                        progress = True
                    else:
                        break
        stuck = {e: (pos[e], len(self.meta[e])) for e in self.ENG if pos[e] < len(self.meta[e])}
        return stuck

    def finish(self):
        waits = []
        for ev in self.outs:
            self._need("sp", ev, waits, True)

        def run(eng, waits=waits):
            for s, v in waits:
                eng.wait_ge(s, v)

        self.ops["sp"].append(run)
        nc = self.nc
        with nc.Block() as blk:
            @blk.tensor
            def _(e):
                for f in self.ops["pe"]:
                    f(e)

            @blk.scalar
            def _(e):
                for f in self.ops["act"]:
                    f(e)

            @blk.vector
            def _(e):
                for f in self.ops["dve"]:
                    f(e)

            @blk.gpsimd
            def _(e):
                for f in self.ops["pool"]:
                    f(e)

            @blk.sync
            def _(e):
                for f in self.ops["sp"]:
                    f(e)


def _sb(nc, st, name, shape, dt):
    return st.enter_context(nc.sbuf_tensor("sb_" + name, shape, dt))


def _ps(nc, st, name, shape, dt):
    return st.enter_context(nc.psum_tensor("ps_" + name, shape, dt))


P0_COLS = 6 * D // NCORE
P0_HALF = 768


def build_phase0():
    nc = bass.Bass("TRN2", target_bir_lowering=False)
    c = nc.dram_tensor("c", [1, D], F32, kind="ExternalInput").ap()
    w = nc.dram_tensor("w", [DEPTH, D, P0_COLS], F32, kind="ExternalInput").ap()
    b = nc.dram_tensor("b", [DEPTH, P0_COLS], F32, kind="ExternalInput").ap()
    addc = nc.dram_tensor("addc", [1, P0_COLS], F32, kind="ExternalInput").ap()
    out = nc.dram_tensor("mod", [DEPTH, P0_COLS], F32, kind="ExternalOutput").ap()
    with ExitStack() as st:
        p = Prog(nc, st)
        cT = _sb(nc, st, "cT", [128, DC], F32)
        bt = _sb(nc, st, "bt", [1, DEPTH * P0_COLS], F32)
        at = _sb(nc, st, "at", [1, P0_COLS], F32)
        res = _sb(nc, st, "res", [1, DEPTH * P0_COLS], F32)
        wb = [_sb(nc, st, "wb%d" % i, [128, DC, P0_HALF], F32) for i in range(2)]
        ps = [_ps(nc, st, "ps%d" % i, [128, 512], F32) for i in range(4)]
        t_c, t_b, t_a, t_res = Tk(), Tk(), Tk(), Tk()
        t_w = [Tk(), Tk()]
        t_ps = [Tk() for _ in range(4)]
        p.dma("sp", cT[:], c[0, :].rearrange("(k p) -> p k", p=128), writes=[t_c], slow=True)
        p.dma("sp", bt[:], b.rearrange("l n -> (l n)")[None, :], writes=[t_b])
        p.dma("sp", at[:], addc[:, :], writes=[t_a])
        u = 0
        pi = 0
        for l in range(DEPTH):
            for hf in range(2):
                buf = u % 2
                q = "sp" if u % 2 == 0 else "pool"
                p.dma(q, wb[buf][:], w[l, :, hf * P0_HALF:(hf + 1) * P0_HALF].rearrange("(k p) n -> p k n", p=128),
                      writes=[t_w[buf]])
                for n0 in range(0, P0_HALF, 384):
                    pt = ps[pi % 4]
                    tk = t_ps[pi % 4]
                    pi += 1
                    for k in range(DC):
                        p.op("pe", lambda e, pt=pt, k=k, buf=buf, n0=n0: e.matmul(
                            pt[0:1, 0:384], cT[:, k:k + 1], wb[buf][:, k, n0:n0 + 384], start=(k == 0), stop=(k == DC - 1)),
                            reads=[t_c, t_w[buf]], writes=[tk])
                    o0 = l * P0_COLS + hf * P0_HALF + n0
                    a0 = hf * P0_HALF + n0
                    p.op("dve", lambda e, pt=pt, o0=o0: e.tensor_tensor(
                        out=res[0:1, o0:o0 + 384], in0=pt[0:1, 0:384], in1=bt[0:1, o0:o0 + 384], op=ALU.add),
                        reads=[tk, t_b], writes=[t_res])
                    p.op("dve", lambda e, o0=o0, a0=a0: e.tensor_tensor(
                        out=res[0:1, o0:o0 + 384], in0=res[0:1, o0:o0 + 384], in1=at[0:1, a0:a0 + 384], op=ALU.add),
                        reads=[t_res, t_a], writes=[t_res])
                u += 1
        p.dma("sp", out.rearrange("l n -> (l n)")[None, :], res[:], reads=[t_res], is_out=True)
        p.finish()
    return nc


def run_phase0(c, w_ada, b_ada):
    nc = build_phase0()
    addfull = np.zeros((1, 6 * D), np.float32)
    addfull[0, D:2 * D] = 1.0
    addfull[0, 4 * D:5 * D] = 1.0
    maps = []
    for i in range(NCORE):
        sl = slice(i * P0_COLS, (i + 1) * P0_COLS)
        maps.append({"c": np.ascontiguousarray(c, np.float32),
                     "w": np.ascontiguousarray(w_ada[:, :, sl]),
                     "b": np.ascontiguousarray(b_ada[:, sl]),
                     "addc": np.ascontiguousarray(addfull[:, sl])})
    res = run_bass_kernel_spmd(nc, maps, core_ids=list(range(NCORE)))
    return np.concatenate([r["mod"] for r in res.results], axis=1)


def pipeline(n_iter, stages, skew=1):
    for step in range(n_iter + (len(stages) - 1) * skew):
        for k, f in enumerate(stages):
            i = step - k * skew
            if 0 <= i < n_iter:
                f(i)


class Rot:
    def __init__(self, bufs):
        self.bufs = bufs
        self.tks = [Tk() for _ in bufs]
        self.i = 0

    def next(self):
        j = self.i % len(self.bufs)
        self.i += 1
        return self.bufs[j], self.tks[j]


def _bf(a):
    return np.asarray(a, np.float32).astype(ml_dtypes.bfloat16)


A_WT = 768
A_WV = 320
VW = 322
A_SM = 2 + 4 * 64 + 128 + 2


def phaseA_consts(core, S):
    NQ = S // 128
    sl_swa = [2.0 ** (-8.0 * (h + 1) / 16.0) for h in (2 * core, 2 * core + 1)]
    sl_d = 2.0 ** (-8.0 * (core + 1) / 8.0)
    p = np.arange(128)[:, None].astype(np.float64)
    t = np.arange(128)[None, :].astype(np.float64)
    swab = np.zeros((128, 2, 2, 128), np.float32)
    for hh in range(2):
        d0 = 128 + t - p
        swab[:, hh, 0, :] = np.where(d0 < 128, -sl_swa[hh] * d0, NEG)
        d1 = t - p
        swab[:, hh, 1, :] = np.where(d1 >= 0, -sl_swa[hh] * d1, NEG)
    nj = NQ + 3
    jj = np.arange(nj)[None, :] - 3
    dbias = (sl_d * p - sl_d * 128.0 * jj).astype(np.float32)
    ident = np.eye(128, dtype=np.float32)
    cf = np.concatenate([swab.reshape(128, 512), dbias, ident], axis=1)
    tri = (p >= t).astype(np.float32)
    ones = np.ones((128, 128), np.float32)
    strict = (p < t).astype(np.float32)
    incl = (p <= t).astype(np.float32)
    tl = np.arange(512).astype(np.float64)
    v = (-8.0 * sl_d * tl)
    hi = v.astype(np.float32).astype(ml_dtypes.bfloat16)
    lo = (v - hi.astype(np.float64)).astype(np.float32).astype(ml_dtypes.bfloat16)
    qaug = np.zeros((128, 512), np.float32)
    qaug[0] = hi.astype(np.float32)
    qaug[1] = lo.astype(np.float32)
    cb = _bf(np.concatenate([tri, ones, strict, incl, qaug], axis=1))
    return np.ascontiguousarray(cf), np.ascontiguousarray(cb)


def build_phaseA(S):
    NQ = S // 128
    NG = S // 512
    NJ = NQ + 3
    NCF = 512 + NJ + 128
    nc = bass.Bass("TRN2", target_bir_lowering=False)
    x = nc.dram_tensor("x", [S, D], F32, kind="ExternalInput").ap()
    mod = nc.dram_tensor("mod", [2, D], F32, kind="ExternalInput").ap()
    w_t = nc.dram_tensor("w_t", [D, A_WT], F32, kind="ExternalInput").ap()
    w_v = nc.dram_tensor("w_v", [D, A_WV], F32, kind="ExternalInput").ap()
    cf_d = nc.dram_tensor("cf", [128, NCF], F32, kind="ExternalInput").ap()
    cb_d = nc.dram_tensor("cb", [128, 1024], BF16, kind="ExternalInput").ap()
    sm_d = nc.dram_tensor("sm", [1, A_SM], F32, kind="ExternalInput").ap()
    o_d = nc.dram_tensor("o", [S, 384], F32, kind="ExternalOutput").ap()
    with ExitStack() as st:
        p = Prog(nc, st)
        sb = lambda name, shape, dt: _sb(nc, st, name, shape, dt)
        wT = sb("wT", [128, DC, A_WT], BF16)
        wV = sb("wV", [128, DC, A_WV], BF16)
        KA = sb("KA", [128, S], BF16)
        KB = sb("KB", [128, S], BF16)
        KC = sb("KC", [128, S], BF16)
        V = sb("V", [128, NQ, VW], BF16)
        xs = sb("xs", [128, 2, D], F32)
        hT = sb("hT", [128, DC, 512], BF16)
        QA = sb("QA", [128, 512], BF16)
        QB = sb("QB", [128, 512], BF16)
        QC = sb("QC", [128, 512], BF16)
        cf = sb("cfs", [128, NCF], F32)
        cb = sb("cbs", [128, 1024], BF16)
        sm = sb("sms", [128, A_SM], F32)
        sclT = sb("sclT", [128, DC], F32)
        shT = sb("shT", [128, DC], F32)
        misc = sb("misc", [128, 16], F32)
        gsub = sb("gsub", [128, 128], F32)
        junk = sb("junk", [128, 128], F32)
        ost = [sb("ost%d" % i, [128, 4, 384], F32) for i in range(2)]
        t_ost = [Tk(), Tk()]
        banks = [_ps(nc, st, "bk%d" % i, [128, 512], F32) for i in range(8)]
        scr = Rot(banks[0:3])
        SBACC, t_sbacc = banks[3], Tk()
        DACC = banks[4:8]
        t_dacc = [Tk() for _ in range(4)]
        e_r = Rot([sb("e%d" % i, [128, 512], F32) for i in range(3)])
        L_r = Rot([sb("L%d" % i, [128, 512], BF16) for i in range(3)])
        X_r = Rot([sb("X%d" % i, [128, 512], F32) for i in range(2)])
        W_r = Rot([sb("W%d" % i, [128, 512], BF16) for i in range(3)])
        E_r = Rot([sb("E%d" % i, [128, 512], BF16) for i in range(3)])
        Lacc = sb("Lacc", [128, 512], F32)
        Lab_r = Rot([sb("Lab%d" % i, [128, 512], BF16) for i in range(2)])
        st_r = Rot([sb("stmp%d" % i, [128, 128], F32) for i in range(2)])
        sE_r = Rot([sb("sE%d" % i, [128, 128], BF16) for i in range(2)])
        fin_r = Rot([sb("fin%d" % i, [128, 8], F32) for i in range(2)])
        da_r = Rot([sb("da%d" % i, [128, 128], F32) for i in range(2)])
        dd_r = Rot([sb("dd%d" % i, [128, 128], F32) for i in range(2)])

        t_c = Tk()
        t_k = [Tk() for _ in range(NG)]
        t_xs, t_hT, t_q = Tk(), Tk(), Tk()
        t_lacc = Tk()
        t_misc = Tk()

        swab = lambda hh, kind: cf[:, (hh * 2 + kind) * 128:(hh * 2 + kind + 1) * 128]
        dbias = lambda j: cf[:, 512 + j:512 + j + 1]
        ident = cf[:, 512 + NJ:512 + NJ + 128]
        tri = cb[:, 0:128]
        ones = cb[:, 128:256]
        strict = cb[:, 256:384]
        incl = cb[:, 384:512]
        qaug = cb[:, 512:1024]

        p.dma("sp", cf[:], cf_d[:, :], writes=[t_c])
        p.dma("sp", cb[:], cb_d[:, :], writes=[t_c])
        p.dma("sp", sm[:], sm_d[0, :].partition_broadcast(128), writes=[t_c])
        p.dma("sp", sclT[:], mod[0, :].rearrange("(k p) -> p k", p=128), writes=[t_c], slow=True)
        p.dma("sp", shT[:], mod[1, :].rearrange("(k p) -> p k", p=128), writes=[t_c], slow=True)
        for k in range(DC):
            p.dma("pool", wT[:, k, :], w_t[k * 128:(k + 1) * 128, :], writes=[t_c])
            p.dma("pool", wV[:, k, :], w_v[k * 128:(k + 1) * 128, :], writes=[t_c])
        p.op("pool", lambda e: e.memset(V[:, :, 64:65], 1.0), writes=[t_c])
        p.op("pool", lambda e: e.memset(V[:, :, 321:322], 1.0), writes=[t_c])
        p.op("act", lambda e: e.activation(out=misc[:, 0:2], in_=sm[:, 0:2], func=AF.Exp), reads=[t_c], writes=[t_misc])
        p.op("dve", lambda e: e.tensor_tensor(out=junk[:, 0:64], in0=sm[:, 2:66], in1=sm[:, 66:130], op=ALU.mult),
             reads=[t_c], writes=[t_misc])
        p.op("dve", lambda e: e.reduce_sum(out=misc[:, 2:3], in_=junk[:, 0:64], axis=AX.X), reads=[t_misc], writes=[t_misc])
        p.op("dve", lambda e: e.tensor_tensor(out=junk[:, 64:128], in0=sm[:, 130:194], in1=sm[:, 194:258], op=ALU.mult),
             reads=[t_c], writes=[t_misc])
        p.op("dve", lambda e: e.reduce_sum(out=misc[:, 3:4], in_=junk[:, 64:128], axis=AX.X), reads=[t_misc], writes=[t_misc])
        p.op("act", lambda e: e.activation(out=misc[:, 5:7], in_=misc[:, 2:4], func=AF.Exp), reads=[t_misc], writes=[t_misc])
        p.op("dve", lambda e: e.tensor_tensor(out=misc[:, 7:8], in0=misc[:, 6:7], in1=misc[:, 5:6], op=ALU.subtract),
             reads=[t_misc], writes=[t_misc])
        p.op("dve", lambda e: e.tensor_tensor(out=misc[:, 4:5], in0=misc[:, 7:8], in1=sm[:, 386:387], op=ALU.subtract),
             reads=[t_misc, t_c], writes=[t_misc])
        p.op("dve", lambda e: e.tensor_scalar(out=gsub[:], in0=sm[:, 258:386], scalar1=sm[:, 387:388], scalar2=None,
                                              op0=ALU.mult), reads=[t_c], writes=[t_misc])

        for g in range(NG):
            for half in range(2):
                r0 = g * 512 + half * 256
                p.dma("sp", xs[:], x[r0:r0 + 256, :].rearrange("(t q) d -> q t d", q=128), writes=[t_xs])
                for c in range(DC):
                    bk, tk = scr.next()
                    for t in range(2):
                        p.op("pe", lambda e, bk=bk, t=t, c=c: e.transpose(
                            bk[:, t * 128:(t + 1) * 128], xs[:, t, c * 128:(c + 1) * 128], ident),
                            reads=[t_xs, t_c], writes=[tk])
                    p.op("act", lambda e, bk=bk, c=c, half=half: e.activation(
                        out=hT[:, c, half * 256:(half + 1) * 256], in_=bk[:, 0:256], func=AF.Identity,
                        bias=shT[:, c:c + 1], scale=sclT[:, c:c + 1]), reads=[tk, t_c], writes=[t_hT])
            dests = [QA[:, :], KA[:, g * 512:(g + 1) * 512], QB[:, :], KB[:, g * 512:(g + 1) * 512],
                     QC[:, :], KC[:, g * 512:(g + 1) * 512]]
            for m in range(6):
                bk, tk = scr.next()
                for k in range(DC):
                    p.op("pe", lambda e, bk=bk, k=k, m=m: e.matmul(
                        bk[:, :], wT[:, k, m * 128:(m + 1) * 128], hT[:, k, :], start=(k == 0), stop=(k == DC - 1)),
                        reads=[t_c, t_hT], writes=[tk])
                wtk = t_q if m % 2 == 0 else t_k[g]
                p.op("dve", lambda e, bk=bk, m=m, dests=dests: e.tensor_copy(out=dests[m], in_=bk[:, :]),
                     reads=[tk], writes=[wtk])
            for t in range(4):
                bk, tk = scr.next()
                for k in range(DC):
                    p.op("pe", lambda e, bk=bk, k=k, t=t: e.matmul(
                        bk[:, 0:A_WV], hT[:, k, t * 128:(t + 1) * 128], wV[:, k, :], start=(k == 0), stop=(k == DC - 1)),
                        reads=[t_c, t_hT], writes=[tk])
                qb = g * 4 + t
                p.op("dve", lambda e, bk=bk, qb=qb: e.tensor_copy(out=V[:, qb, 0:64], in_=bk[:, 0:64]),
                     reads=[tk], writes=[t_k[g]])
                p.op("dve", lambda e, bk=bk, qb=qb: e.tensor_copy(out=V[:, qb, 65:321], in_=bk[:, 64:320]),
                     reads=[tk], writes=[t_k[g]])

            o_t, o_tk = ost[g % 2], t_ost[g % 2]

            for hh in range(2):
                for qi in range(4):
                    i = g * 4 + qi
                    kbs = [kb for kb in (i - 1, i) if kb >= 0]
                    abk, atk = scr.next()
                    ps_l = []
                    for kb in kbs:
                        kind = 1 if kb == i else 0
                        bk, tk = scr.next()
                        kg = t_k[kb // 4]
                        p.op("pe", lambda e, bk=bk, kb=kb, hh=hh, qi=qi: e.matmul(
                            bk[:, 0:128], KA[64 * hh:64 * hh + 64, kb * 128:(kb + 1) * 128],
                            QA[64 * hh:64 * hh + 64, qi * 128:(qi + 1) * 128], start=True, stop=True),
                            reads=[kg, t_q], writes=[tk])
                        tm, ttk = st_r.next()
                        p.op("dve", lambda e, bk=bk, tm=tm, hh=hh, kind=kind: e.scalar_tensor_tensor(
                            out=tm[:], in0=bk[:, 0:128], scalar=SCALE, in1=swab(hh, kind), op0=ALU.mult, op1=ALU.add),
                            reads=[tk, t_c], writes=[ttk])
                        sE, setk = sE_r.next()
                        p.op("act", lambda e, tm=tm, sE=sE: e.activation(out=sE[:], in_=tm[:], func=AF.Exp),
                             reads=[ttk], writes=[setk])
                        ps_l.append((sE, setk, kb, kg))
                    for n, (sE, setk, kb, kg) in enumerate(ps_l):
                        p.op("pe", lambda e, abk=abk, sE=sE, kb=kb, n=n, ln=len(ps_l): e.matmul(
                            abk[:, 0:65], sE[:], V[:, kb, 0:65], start=(n == 0), stop=(n == ln - 1)),
                            reads=[setk, kg, t_c], writes=[atk])
                    fb, ftk = fin_r.next()
                    p.op("dve", lambda e, fb=fb, abk=abk, hh=hh: e.tensor_tensor(
                        out=fb[:, 0:1], in0=abk[:, 64:65], in1=misc[:, hh:hh + 1], op=ALU.add),
                        reads=[atk, t_misc], writes=[ftk])
                    p.op("dve", lambda e, fb=fb: e.reciprocal(out=fb[:, 1:2], in_=fb[:, 0:1]), reads=[ftk], writes=[ftk])
                    p.op("dve", lambda e, fb=fb, abk=abk, hh=hh, qi=qi, o_t=o_t: e.tensor_scalar(
                        out=o_t[:, qi, 64 * hh:64 * hh + 64], in0=abk[:, 0:64], scalar1=fb[:, 1:2], scalar2=None,
                        op0=ALU.mult), reads=[atk, ftk], writes=[o_tk])

            its = [(hh, kb) for hh in range(2) for kb in range(g * 4 + 3, -1, -1)]
            stt = {}

            def sb_a(n):
                hh, kb = its[n]
                qs = max(0, kb - 4 * g) * 128
                first = (kb == g * 4 + 3)
                kg = t_k[kb // 4]
                bk, tk = scr.next()
                p.op("pe", lambda e: e.matmul(bk[:, qs:512], KB[64 * hh:64 * hh + 64, kb * 128:(kb + 1) * 128],
                                              QB[64 * hh:64 * hh + 64, qs:512], start=True, stop=True),
                     reads=[kg, t_q], writes=[tk])
                eb, etk = e_r.next()
                p.op("act", lambda e: e.activation(out=eb[:, qs:512], in_=bk[:, qs:512], func=AF.Exp, scale=SCALE),
                     reads=[tk], writes=[etk])
                Lb, Ltk = L_r.next()
                p.op("act", lambda e: e.activation(out=Lb[:, qs:512], in_=eb[:, qs:512], func=AF.Ln, bias=1.0, scale=1.0),
                     reads=[etk], writes=[Ltk])
                if kb >= 4 * g:
                    p.op("pool", lambda e: e.tensor_tensor(out=Lb[:, qs:qs + 128], in0=Lb[:, qs:qs + 128], in1=strict,
                                                           op=ALU.mult), reads=[Ltk, t_c], writes=[Ltk])
                if first:
                    p.op("pool", lambda e: e.memset(Lacc[:], 0.0), writes=[t_lacc])
                stt[n] = dict(hh=hh, kb=kb, qs=qs, first=first, kg=kg, eb=eb, etk=etk, Lb=Lb, Ltk=Ltk)

            def sb_b(n):
                s = stt[n]
                hh, kb, qs, first = s["hh"], s["kb"], s["qs"], s["first"]
                eb, etk, Lb, Ltk = s["eb"], s["etk"], s["Lb"], s["Ltk"]
                cbk, ctk = scr.next()
                p.op("pe", lambda e: e.matmul(cbk[:, qs:512], tri, Lb[:, qs:512], start=True, stop=first),
                     reads=[Ltk, t_c], writes=[ctk])
                if not first:
                    plab, plabtk = s_prev_lab[0]
                    p.op("pe", lambda e: e.matmul(cbk[:, qs:512], ones, plab[:, qs:512], start=False, stop=True),
                         reads=[plabtk, t_c], writes=[ctk])
                Xb, Xtk = X_r.next()
                p.op("act", lambda e: e.activation(out=Xb[:, qs:512], in_=cbk[:, qs:512], func=AF.Exp, scale=-1.0),
                     reads=[ctk], writes=[Xtk])
                Wb, Wtk = W_r.next()
                p.op("dve", lambda e: e.tensor_tensor(out=Wb[:, qs:512], in0=eb[:, qs:512], in1=Xb[:, qs:512], op=ALU.mult),
                     reads=[etk, Xtk], writes=[Wtk])
                if kb >= 4 * g:
                    p.op("pool", lambda e: e.tensor_tensor(out=Wb[:, qs:qs + 128], in0=Wb[:, qs:qs + 128], in1=strict,
                                                           op=ALU.mult), reads=[Wtk, t_c], writes=[Wtk])
                if kb > 0:
                    p.op("pool", lambda e: e.tensor_tensor(out=Lacc[:, qs:512], in0=Lacc[:, qs:512], in1=Lb[:, qs:512],
                                                           op=ALU.add), reads=[Ltk, t_lacc], writes=[t_lacc])
                    lab, labtk = Lab_r.next()
                    p.op("pool", lambda e: e.tensor_copy(out=lab[:, :], in_=Lacc[:, :]), reads=[t_lacc], writes=[labtk])
                    s_prev_lab[0] = (lab, labtk)
                s["Wb"], s["Wtk"] = Wb, Wtk

            def sb_c(n):
                s = stt.pop(n)
                hh, kb, qs, first, kg = s["hh"], s["kb"], s["qs"], s["first"], s["kg"]
                Wb, Wtk = s["Wb"], s["Wtk"]
                for qi in range(qs // 128, 4):
                    c0 = hh * 256 + qi * 64
                    p.op("pe", lambda e, qi=qi, c0=c0: e.matmul(
                        SBACC[:, c0:c0 + 64], Wb[:, qi * 128:(qi + 1) * 128], V[:, kb, 65 + 64 * hh:65 + 64 * hh + 64],
                        start=(first and hh == 0 and qi == 3), stop=(kb == 0), skip_group_check=True),
                        reads=[Wtk, kg], writes=[t_sbacc])
                if kb == 0:
                    p.op("act", lambda e, o_t=o_t: e.activation(
                        out=o_t[:, :, 128 + 64 * hh:128 + 64 * hh + 64],
                        in_=SBACC[:, hh * 256:(hh + 1) * 256].rearrange("p (q d) -> p q d", d=64), func=AF.Identity),
                        reads=[t_sbacc], writes=[o_tk])

            s_prev_lab = [None]
            pipeline(len(its), [sb_a, sb_b, sb_c])

            dits = [(kb, m) for kb in range(0, g * 4 + 4) for m in range(2)]
            dst = {}

            def dacc(m, qi):
                if qi < 3:
                    return DACC[2 * m], t_dacc[2 * m], qi * 129
                return DACC[2 * m + 1], t_dacc[2 * m + 1], 0

            def df_a(n):
                kb, m = dits[n]
                qs = max(0, kb - 4 * g) * 128
                kg = t_k[kb // 4]
                bk, tk = scr.next()
                p.op("pe", lambda e: e.matmul(bk[:, qs:512], KC[64 * m:64 * m + 64, kb * 128:(kb + 1) * 128],
                                              QC[64 * m:64 * m + 64, qs:512], start=True, stop=False),
                     reads=[kg, t_q], writes=[tk])
                p.op("pe", lambda e: e.matmul(bk[:, qs:512], ones[0:2, :], qaug[0:2, qs:512], start=False, stop=True),
                     reads=[t_c], writes=[tk])
                Eb, Etk = E_r.next()
                jidx = (4 * g - kb) + 3
                p.op("act", lambda e: e.activation(out=Eb[:, qs:512], in_=bk[:, qs:512], func=AF.Exp, scale=SCALE,
                                                   bias=dbias(jidx)), reads=[tk, t_c], writes=[Etk])
                if kb >= 4 * g:
                    p.op("pool", lambda e: e.tensor_tensor(out=Eb[:, qs:qs + 128], in0=Eb[:, qs:qs + 128], in1=incl,
                                                           op=ALU.mult), reads=[Etk, t_c], writes=[Etk])
                dst[n] = dict(kb=kb, m=m, qs=qs, kg=kg, Eb=Eb, Etk=Etk)

            def df_b(n):
                s = dst.pop(n)
                kb, m, qs, kg, Eb, Etk = s["kb"], s["m"], s["qs"], s["kg"], s["Eb"], s["Etk"]
                for qi in range(qs // 128, 4):
                    ab, atk, c0 = dacc(m, qi)
                    p.op("pe", lambda e, qi=qi, ab=ab, c0=c0: e.matmul(
                        ab[:, c0:c0 + 129], Eb[:, qi * 128:(qi + 1) * 128], V[:, kb, 193:322],
                        start=(kb == 0 and qi in (0, 3)), stop=(kb == 4 * g + qi), skip_group_check=True),
                        reads=[Etk, kg, t_c], writes=[atk])

            pipeline(len(dits), [df_a, df_b])
            for qi in range(4):
                a1, a1tk, c1 = dacc(0, qi)
                a2, a2tk, c2 = dacc(1, qi)
                fb, ftk = fin_r.next()
                p.op("dve", lambda e, fb=fb, a1=a1, c1=c1: e.reciprocal(out=fb[:, 0:1], in_=a1[:, c1 + 128:c1 + 129]),
                     reads=[a1tk], writes=[ftk])
                p.op("dve", lambda e, fb=fb, a2=a2, c2=c2: e.reciprocal(out=fb[:, 1:2], in_=a2[:, c2 + 128:c2 + 129]),
                     reads=[a2tk], writes=[ftk])
                p.op("dve", lambda e, fb=fb: e.tensor_tensor(out=fb[:, 2:3], in0=fb[:, 1:2], in1=misc[:, 4:5], op=ALU.mult),
                     reads=[ftk, t_misc], writes=[ftk])
                da, datk = da_r.next()
                p.op("dve", lambda e, fb=fb, a1=a1, c1=c1, da=da: e.tensor_scalar(
                    out=da[:], in0=a1[:, c1:c1 + 128], scalar1=fb[:, 0:1], scalar2=None, op0=ALU.mult),
                    reads=[a1tk, ftk], writes=[datk])
                dd, ddtk = dd_r.next()
                p.op("dve", lambda e, fb=fb, a2=a2, c2=c2, da=da, dd=dd: e.scalar_tensor_tensor(
                    out=dd[:], in0=a2[:, c2:c2 + 128], scalar=fb[:, 2:3], in1=da[:], op0=ALU.mult, op1=ALU.add),
                    reads=[a2tk, ftk, datk], writes=[ddtk])
                p.op("dve", lambda e, dd=dd, da=da: e.tensor_tensor(out=da[:], in0=dd[:], in1=dd[:], op=ALU.mult),
                     reads=[ddtk], writes=[datk])
                p.op("dve", lambda e, fb=fb, da=da: e.reduce_sum(out=fb[:, 3:4], in_=da[:], axis=AX.X),
                     reads=[datk], writes=[ftk])
                p.op("act", lambda e, fb=fb: e.activation(out=fb[:, 4:5], in_=fb[:, 3:4], func=AF.Ln, bias=RMS_EPS,
                                                          scale=1.0 / 128.0), reads=[ftk], writes=[ftk])
                p.op("act", lambda e, fb=fb: e.activation(out=fb[:, 5:6], in_=fb[:, 4:5], func=AF.Exp, scale=-0.5),
                     reads=[ftk], writes=[ftk])
                p.op("dve", lambda e, fb=fb, dd=dd, qi=qi, o_t=o_t: e.scalar_tensor_tensor(
                    out=o_t[:, qi, 256:384], in0=dd[:], scalar=fb[:, 5:6], in1=gsub[:], op0=ALU.mult, op1=ALU.mult),
                    reads=[ddtk, ftk, t_misc], writes=[o_tk])
            p.dma("sp", o_d[g * 512:(g + 1) * 512, :].rearrange("(q p) c -> p q c", p=128), o_t[:], reads=[o_tk],
                  is_out=True)
        p.finish()
    return nc


def phaseA_inputs(core, S, x, modA, w_in, sinks, lq1, lk1, lq2, lk2, subg, lam_init):
    h0 = 2 * core
    kv = core // 2
    qA = w_in[:, h0 * 64:h0 * 64 + 128]
    kA = w_in[:, 1024 + kv * 64:1024 + kv * 64 + 64]
    vA = w_in[:, 1280 + kv * 64:1280 + kv * 64 + 64]
    qB = w_in[:, 1536 + h0 * 64:1536 + h0 * 64 + 128]
    kB = w_in[:, 2560 + h0 * 64:2560 + h0 * 64 + 128]
    vB = w_in[:, 3584 + h0 * 64:3584 + h0 * 64 + 128]
    qC = w_in[:, 4608 + core * 128:4608 + core * 128 + 128]
    kC = w_in[:, 5632 + core * 128:5632 + core * 128 + 128]
    vC = w_in[:, 6656 + core * 128:6656 + core * 128 + 128]
    w_t = np.ascontiguousarray(np.concatenate([qA, kA, kA, qB, kB, qC, kC], axis=1), np.float32)
    w_v = np.ascontiguousarray(np.concatenate([vA, vB, vC], axis=1), np.float32)
    cf, cb = phaseA_consts2(core, S)
    sm = np.zeros((1, A_SM), np.float32)
    sm[0, 0:2] = sinks[h0:h0 + 2]
    sm[0, 2:66] = lq1
    sm[0, 66:130] = lk1
    sm[0, 130:194] = lq2
    sm[0, 194:258] = lk2
    sm[0, 258:386] = subg
    sm[0, 386] = lam_init
    sm[0, 387] = 1.0 - lam_init
    return {"x": np.ascontiguousarray(x, np.float32), "mod": np.ascontiguousarray(modA, np.float32), "w_t": w_t,
            "w_v": w_v, "cf": cf, "cb": cb, "sm": sm}


def emit_layernorm(p, r, t_r, xo, t_xo, gB, bB, t_c, stats, t_st):
    for j in range(4):
        p.op("dve", lambda e, j=j: e.bn_stats(out=stats[:, j * 6:(j + 1) * 6], in_=r[:, j * 512:(j + 1) * 512]),
             reads=[t_r], writes=[t_st])
    p.op("dve", lambda e: e.bn_aggr(out=stats[:, 24:26], in_=stats[:, 0:24]), reads=[t_st], writes=[t_st])
    p.op("act", lambda e: e.activation(out=stats[:, 26:27], in_=stats[:, 25:26], func=AF.Ln, bias=LN_EPS, scale=1.0),
         reads=[t_st], writes=[t_st])
    p.op("act", lambda e: e.activation(out=stats[:, 27:28], in_=stats[:, 26:27], func=AF.Exp, scale=-0.5),
         reads=[t_st], writes=[t_st])
    p.op("dve", lambda e: e.tensor_scalar(out=r[:], in0=r[:], scalar1=stats[:, 24:25], scalar2=stats[:, 27:28],
                                          op0=ALU.subtract, op1=ALU.mult), reads=[t_st, t_r], writes=[t_r])
    p.op("pool", lambda e: e.tensor_tensor(out=r[:], in0=r[:], in1=gB[:], op=ALU.mult), reads=[t_r, t_c], writes=[t_r])
    p.op("dve", lambda e: e.tensor_tensor(out=xo[:], in0=r[:], in1=bB[:], op=ALU.add), reads=[t_r, t_c], writes=[t_xo])


TB = SEQ // NCORE


def build_phaseB(TBK=TB):
    NH = TBK // 512
    nc = bass.Bass("TRN2", target_bir_lowering=False)
    x = nc.dram_tensor("x", [TBK, D], F32, kind="ExternalInput").ap()
    o = nc.dram_tensor("o", [TBK, 3072], F32, kind="ExternalInput").ap()
    mod = nc.dram_tensor("mod", [6, D], F32, kind="ExternalInput").ap()
    wg_d = nc.dram_tensor("wg", [48, 128, DC * 128], F32, kind="ExternalInput").ap()
    bg_d = nc.dram_tensor("bg", [6144], F32, kind="ExternalInput").ap()
    wb_d = nc.dram_tensor("wb", [48, 128, 8 * 128], F32, kind="ExternalInput").ap()
    wo_d = nc.dram_tensor("wo", [4, DC, 128, 512], F32, kind="ExternalInput").ap()
    ln_d = nc.dram_tensor("ln", [2, D], F32, kind="ExternalInput").ap()
    wr_d = nc.dram_tensor("wr", [D, 72], F32, kind="ExternalInput").ap()
    br_d = nc.dram_tensor("br", [72], F32, kind="ExternalInput").ap()
    id_d = nc.dram_tensor("ident", [128, 128], F32, kind="ExternalInput").ap()
    x1_d = nc.dram_tensor("x1", [TBK, D], F32, kind="ExternalOutput").ap()
    h2_d = nc.dram_tensor("h2T", [D, TBK], BF16, kind="ExternalOutput").ap()
    rw_d = nc.dram_tensor("rw", [TBK, 64], F32, kind="ExternalOutput").ap()
    with ExitStack() as st:
        p = Prog(nc, st)
        sb = lambda name, shape, dt: _sb(nc, st, name, shape, dt)
        hT = sb("hT", [128, DC, 512], BF16)
        oT = sb("oT", [128, 24, 512], BF16)
        zT = sb("zT", [128, DC, 512], BF16)
        stg = sb("stg", [128, 4096], F32)
        wg_r = Rot([sb("wg%d" % i, [128, DC * 128], BF16) for i in range(3)])
        wb_r = Rot([sb("wb%d" % i, [128, 8 * 128], BF16) for i in range(3)])
        wo = sb("wo", [128, DC, 512], BF16)
        g1B = sb("g1B", [128, D], F32)
        lgB = sb("lgB", [128, D], F32)
        lbB = sb("lbB", [128, D], F32)
        r_r = Rot([sb("r%d" % i, [128, D], F32) for i in range(4)])
        x1t = sb("x1t", [128, D], F32)
        xres = sb("xres", [128, 512], F32)
        h2f = sb("h2f", [128, DC, 128], F32)
        h2b = sb("h2b", [128, DC, 128], BF16)
        wr = sb("wr", [128, DC, 72], F32)
        brB = sb("brB", [128, 72], F32)
        ident = sb("ident", [128, 128], F32)
        modT = sb("modT", [128, 6, DC], F32)
        bgT = sb("bgT", [128, 48], F32)
        gs_r = Rot([sb("gs%d" % i, [128, 512], F32) for i in range(2)])
        zt_r = Rot([sb("zt%d" % i, [128, 512], F32) for i in range(2)])
        zacc = sb("zacc", [128, 512], F32)
        stats = sb("stats", [128, 32], F32)
        rt = sb("rt", [128, 512], F32)
        banks = [_ps(nc, st, "bk%d" % i, [128, 512], F32) for i in range(8)]
        scr = Rot(banks)
        t_c, t_stg, t_hT, t_oT, t_zT, t_wo, t_x1, t_xres, t_h2, t_st, t_rt, t_zacc = [Tk() for _ in range(12)]

        p.dma("sp", ident[:], id_d[:, :], writes=[t_c])
        p.dma("sp", modT[:], mod.rearrange("m (k p) -> p m k", p=128), writes=[t_c], slow=True)
        p.dma("sp", bgT[:], bg_d.rearrange("(j p) -> p j", p=128), writes=[t_c], slow=True)
        p.dma("sp", g1B[:], mod[2, :].partition_broadcast(128), writes=[t_c])
        p.dma("sp", lgB[:], ln_d[0, :].partition_broadcast(128), writes=[t_c])
        p.dma("sp", lbB[:], ln_d[1, :].partition_broadcast(128), writes=[t_c])
        p.dma("sp", brB[:], br_d.partition_broadcast(128), writes=[t_c])
        p.dma("sp", wr[:], wr_d.rearrange("(k p) n -> p k n", p=128), writes=[t_c])

        for hf in range(NH):
            for t in range(4):
                r0 = hf * 512 + t * 128
                p.dma("sp", stg[:, 0:D], x[r0:r0 + 128, :], writes=[t_stg])
                for c4 in range(4):
                    bk, tk = scr.next()
                    for j in range(4):
                        c = c4 * 4 + j
                        p.op("pe", lambda e, bk=bk, j=j, c=c: e.transpose(
                            bk[:, j * 128:(j + 1) * 128], stg[:, c * 128:(c + 1) * 128], ident[:]),
                            reads=[t_stg, t_c], writes=[tk])
                    for j in range(4):
                        c = c4 * 4 + j
                        p.op("act", lambda e, bk=bk, j=j, c=c, t=t: e.activation(
                            out=hT[:, c, t * 128:(t + 1) * 128], in_=bk[:, j * 128:(j + 1) * 128], func=AF.Identity,
                            bias=modT[:, 1, c:c + 1], scale=modT[:, 0, c:c + 1]), reads=[tk, t_c], writes=[t_hT])
                p.dma("sp", stg[:, 0:3072], o[r0:r0 + 128, :], writes=[t_stg])
                for c4 in range(6):
                    bk, tk = scr.next()
                    for j in range(4):
                        c = c4 * 4 + j
                        p.op("pe", lambda e, bk=bk, j=j, c=c: e.transpose(
                            bk[:, j * 128:(j + 1) * 128], stg[:, c * 128:(c + 1) * 128], ident[:]),
                            reads=[t_stg, t_c], writes=[tk])
                    p.op("dve", lambda e, bk=bk, c4=c4, t=t: e.tensor_copy(
                        out=oT[:, c4 * 4:c4 * 4 + 4, t * 128:(t + 1) * 128],
                        in_=bk[:, :].rearrange("p (j q) -> p j q", q=128)), reads=[tk], writes=[t_oT])
            for dc in range(DC):
                for n in range(3):
                    blk = n * DC + dc
                    wgb, wgtk = wg_r.next()
                    p.dma("pool", wgb[:], wg_d[blk, :, :], writes=[wgtk])
                    wbb, wbtk = wb_r.next()
                    p.dma("pool", wbb[:], wb_d[blk, :, :], writes=[wbtk])
                    gbk, gtk = scr.next()
                    for k in range(DC):
                        p.op("pe", lambda e, gbk=gbk, wgb=wgb, k=k: e.matmul(
                            gbk[:, :], wgb[:, k * 128:(k + 1) * 128], hT[:, k, :], start=(k == 0), stop=(k == DC - 1)),
                            reads=[wgtk, t_hT], writes=[gtk])
                    gs, gstk = gs_r.next()
                    p.op("act", lambda e, gbk=gbk, gs=gs, blk=blk: e.activation(
                        out=gs[:], in_=gbk[:, :], func=AF.Sigmoid, bias=bgT[:, blk:blk + 1], scale=1.0),
                        reads=[gtk, t_c], writes=[gstk])
                    ybk, ytk = scr.next()
                    for f in range(8):
                        p.op("pe", lambda e, ybk=ybk, wbb=wbb, f=f, n=n: e.matmul(
                            ybk[:, :], wbb[:, f * 128:(f + 1) * 128], oT[:, n * 8 + f, :], start=(f == 0), stop=(f == 7)),
                            reads=[wbtk, t_oT], writes=[ytk])
                    if n == 0:
                        p.op("dve", lambda e, ybk=ybk, gs=gs: e.tensor_tensor(out=zacc[:], in0=ybk[:, :], in1=gs[:], op=ALU.mult),
                             reads=[ytk, gstk], writes=[t_zacc])
                    else:
                        zt, zttk = zt_r.next()
                        p.op("dve", lambda e, ybk=ybk, gs=gs, zt=zt: e.tensor_tensor(out=zt[:], in0=ybk[:, :], in1=gs[:], op=ALU.mult),
                             reads=[ytk, gstk], writes=[zttk])
                        if n == 1:
                            p.op("pool", lambda e, zt=zt: e.tensor_tensor(out=zacc[:], in0=zacc[:], in1=zt[:], op=ALU.add),
                                 reads=[zttk, t_zacc], writes=[t_zacc])
                        else:
                            p.op("pool", lambda e, zt=zt, dc=dc: e.tensor_tensor(out=zT[:, dc, :], in0=zacc[:], in1=zt[:], op=ALU.add),
                                 reads=[zttk, t_zacc], writes=[t_zT])
            rts = [r_r.next() for _ in range(4)]
            for cbk in range(4):
                for k in range(DC):
                    p.dma("pool", wo[:, k, :], wo_d[cbk, k, :, :], writes=[t_wo])
                for t in range(4):
                    r0 = hf * 512 + t * 128
                    rb, rtk = rts[t]
                    bk, tk = scr.next()
                    for k in range(DC):
                        p.op("pe", lambda e, bk=bk, k=k, t=t: e.matmul(
                            bk[:, :], zT[:, k, t * 128:(t + 1) * 128], wo[:, k, :], start=(k == 0), stop=(k == DC - 1)),
                            reads=[t_zT, t_wo], writes=[tk])
                    p.dma("sp", xres[:], x[r0:r0 + 128, cbk * 512:(cbk + 1) * 512], writes=[t_xres])
                    p.op("dve", lambda e, bk=bk, rb=rb, cbk=cbk: e.tensor_tensor(
                        out=rb[:, cbk * 512:(cbk + 1) * 512], in0=bk[:, :], in1=g1B[:, cbk * 512:(cbk + 1) * 512], op=ALU.mult),
                        reads=[tk, t_c], writes=[rtk])
                    p.op("dve", lambda e, rb=rb, cbk=cbk: e.scalar_tensor_tensor(
                        out=rb[:, cbk * 512:(cbk + 1) * 512], in0=xres[:], scalar=ALPHA, in1=rb[:, cbk * 512:(cbk + 1) * 512],
                        op0=ALU.mult, op1=ALU.add), reads=[t_xres, rtk], writes=[rtk])
            for t in range(4):
                r0 = hf * 512 + t * 128
                rb, rtk = rts[t]
                emit_layernorm(p, rb, rtk, x1t, t_x1, lgB, lbB, t_c, stats, t_st)
                p.dma("sp", x1_d[r0:r0 + 128, :], x1t[:], reads=[t_x1], is_out=True)
                for c4 in range(4):
                    bk, tk = scr.next()
                    for j in range(4):
                        c = c4 * 4 + j
                        p.op("pe", lambda e, bk=bk, j=j, c=c: e.transpose(
                            bk[:, j * 128:(j + 1) * 128], x1t[:, c * 128:(c + 1) * 128], ident[:]),
                            reads=[t_x1, t_c], writes=[tk])
                    for j in range(4):
                        c = c4 * 4 + j
                        p.op("act", lambda e, bk=bk, j=j, c=c: e.activation(
                            out=h2f[:, c, :], in_=bk[:, j * 128:(j + 1) * 128], func=AF.Identity,
                            bias=modT[:, 4, c:c + 1], scale=modT[:, 3, c:c + 1]), reads=[tk, t_c], writes=[t_h2])
                p.op("pool", lambda e: e.tensor_copy(out=h2b[:], in_=h2f[:]), reads=[t_h2], writes=[t_h2])
                p.dma("sp", h2_d[:, r0:r0 + 128].rearrange("(k p) t -> p k t", p=128), h2b[:], reads=[t_h2], is_out=True)
                lbk, ltk = scr.next()
                for k in range(DC):
                    p.op("pe", lambda e, lbk=lbk, k=k: e.matmul(lbk[:, 0:72], h2f[:, k, :], wr[:, k, :], start=(k == 0),
                                                                  stop=(k == DC - 1)), reads=[t_h2, t_c], writes=[ltk])
                R = rt
                T = [t_rt]
                p.op("dve", lambda e, lbk=lbk: e.tensor_tensor(out=R[:, 0:72], in0=lbk[:, 0:72], in1=brB[:], op=ALU.add),
                     reads=[ltk, t_c], writes=T)
                p.op("dve", lambda e: e.reduce_max(out=R[:, 72:73], in_=R[:, 0:8], axis=AX.X), reads=T, writes=T)
                p.op("dve", lambda e: e.tensor_scalar(out=R[:, 73:74], in0=R[:, 72:73], scalar1=-1.0, scalar2=None, op0=ALU.mult),
                     reads=T, writes=T)
                p.op("act", lambda e: e.activation(out=R[:, 80:88], in_=R[:, 0:8], func=AF.Exp, bias=R[:, 73:74], scale=1.0),
                     reads=T, writes=T)
                p.op("dve", lambda e: e.reduce_sum(out=R[:, 88:89], in_=R[:, 80:88], axis=AX.X), reads=T, writes=T)
                p.op("dve", lambda e: e.reciprocal(out=R[:, 89:90], in_=R[:, 88:89]), reads=T, writes=T)
                p.op("dve", lambda e: e.tensor_scalar(out=R[:, 96:104], in0=R[:, 0:8], scalar1=R[:, 72:73], scalar2=None,
                                                      op0=ALU.is_ge), reads=T, writes=T)
                p.op("dve", lambda e: e.tensor_scalar(out=R[:, 104:112], in0=R[:, 96:104], scalar1=-1.0, scalar2=30000.0,
                                                      op0=ALU.add, op1=ALU.mult), reads=T, writes=T)
                for gi in range(8):
                    p.op("dve", lambda e, gi=gi: e.tensor_scalar(
                        out=R[:, 128 + gi * 8:136 + gi * 8], in0=R[:, 8 + gi * 8:16 + gi * 8], scalar1=R[:, 104 + gi:105 + gi],
                        scalar2=None, op0=ALU.add), reads=T, writes=T)
                p.op("dve", lambda e: e.max(out=R[:, 192:200], in_=R[:, 128:192]), reads=T, writes=T)
                p.op("dve", lambda e: e.tensor_tensor(out=R[:, 200:201], in0=R[:, 192:193], in1=R[:, 193:194], op=ALU.subtract),
                     reads=T, writes=T)
                p.op("act", lambda e: e.activation(out=R[:, 201:202], in_=R[:, 200:201], func=AF.Sigmoid), reads=T, writes=T)
                p.op("dve", lambda e: e.tensor_tensor(out=R[:, 202:203], in0=R[:, 201:202], in1=R[:, 89:90], op=ALU.mult),
                     reads=T, writes=T)
                p.op("dve", lambda e: e.tensor_tensor(out=R[:, 203:204], in0=R[:, 89:90], in1=R[:, 202:203], op=ALU.subtract),
                     reads=T, writes=T)
                p.op("dve", lambda e: e.tensor_tensor(out=R[:, 204:205], in0=R[:, 202:203], in1=R[:, 203:204], op=ALU.subtract),
                     reads=T, writes=T)
                p.op("dve", lambda e: e.tensor_scalar(out=R[:, 256:320], in0=R[:, 128:192], scalar1=R[:, 193:194], scalar2=R[:, 203:204],
                                                      op0=ALU.is_ge, op1=ALU.mult), reads=T, writes=T)
                p.op("dve", lambda e: e.tensor_scalar(out=R[:, 320:384], in0=R[:, 128:192], scalar1=R[:, 192:193], scalar2=R[:, 204:205],
                                                      op0=ALU.is_ge, op1=ALU.mult), reads=T, writes=T)
                p.op("dve", lambda e: e.tensor_tensor(out=R[:, 384:448], in0=R[:, 256:320], in1=R[:, 320:384], op=ALU.add),
                     reads=T, writes=T)
                p.dma("sp", rw_d[r0:r0 + 128, :], R[:, 384:448], reads=T, is_out=True)
        p.finish()
    return nc


def phaseB_weights(w_gate, b_gate, w_branch, w_out, ln_g, ln_b, w_rg, b_rg, w_re, b_re):
    wg = np.ascontiguousarray(w_gate.reshape(DC, 128, 48, 128).transpose(2, 1, 0, 3).reshape(48, 128, DC * 128))
    wb = np.ascontiguousarray(w_branch.reshape(3, 8, 128, DC, 128).transpose(0, 3, 2, 1, 4).reshape(48, 128, 8 * 128))
    wo = np.ascontiguousarray(w_out.reshape(DC, 128, 4, 512).transpose(2, 0, 1, 3))
    return {"wg": wg, "bg": np.ascontiguousarray(b_gate), "wb": wb, "wo": wo,
            "ln": np.ascontiguousarray(np.stack([ln_g, ln_b])),
            "wr": np.ascontiguousarray(np.concatenate([w_rg, w_re], axis=1)),
            "br": np.ascontiguousarray(np.concatenate([b_rg, b_re])),
            "ident": np.eye(128, dtype=np.float32)}


FE = 384


def build_phaseC(S):
    NT = S // 1024
    nc = bass.Bass("TRN2", target_bir_lowering=False)
    h2_d = nc.dram_tensor("h2T", [D, S], BF16, kind="ExternalInput").ap()
    rw_d = nc.dram_tensor("rw", [S, 8], F32, kind="ExternalInput").ap()
    wgu_d = nc.dram_tensor("wgu", [8, 2, 128, DC * FE], F32, kind="ExternalInput").ap()
    wd_d = nc.dram_tensor("wd", [8, 128, 3 * D], F32, kind="ExternalInput").ap()
    y_d = nc.dram_tensor("y", [S, D], BF16, kind="ExternalOutput").ap()
    with ExitStack() as st:
        p = Prog(nc, st)
        sb = lambda name, shape, dt: _sb(nc, st, name, shape, dt)
        h2 = sb("h2", [128, DC, 1024], BF16)
        rwt = sb("rwt", [128, 8, 8], F32)
        Yacc = sb("Yacc", [128, 8, D], F32)
        wg_r = Rot([sb("wgt%d" % i, [128, DC * FE], BF16) for i in range(2)])
        wu_r = Rot([sb("wut%d" % i, [128, DC * FE], BF16) for i in range(2)])
        wd_r = Rot([sb("wdt%d" % i, [128, 3 * D], BF16) for i in range(2)])
        A = sb("A", [128, 3, 1024], BF16)
        sg_r = Rot([sb("sg%d" % i, [128, 512], F32) for i in range(2)])
        banks = [_ps(nc, st, "bk%d" % i, [128, 512], F32) for i in range(8)]
        scr = Rot(banks)
        t_h2, t_rw, t_A = Tk(), Tk(), Tk()
        t_y = [Tk() for _ in range(8)]
        for tc in range(NT):
            t0 = tc * 1024
            p.dma("sp", h2[:], h2_d[:, t0:t0 + 1024].rearrange("(k p) t -> p k t", p=128), writes=[t_h2])
            p.dma("sp", rwt[:], rw_d[t0:t0 + 1024, :].rearrange("(t p) e -> p t e", p=128), writes=[t_rw])
            for ex in range(8):
                wgb, wgtk = wg_r.next()
                wub, wutk = wu_r.next()
                wdb, wdtk = wd_r.next()
                for j in range(3):
                    p.dma("pool", wgb[:, j * 2048:(j + 1) * 2048], wgu_d[ex, 0, :, j * 2048:(j + 1) * 2048], writes=[wgtk])
                    p.dma("pool", wub[:, j * 2048:(j + 1) * 2048], wgu_d[ex, 1, :, j * 2048:(j + 1) * 2048], writes=[wutk])
                for j in range(3):
                    p.dma("pool", wdb[:, j * 2048:(j + 1) * 2048], wd_d[ex, :, j * 2048:(j + 1) * 2048], writes=[wdtk])
                for f in range(3):
                    for nh in range(2):
                        gbk, gtk = scr.next()
                        for k in range(DC):
                            p.op("pe", lambda e, gbk=gbk, wgb=wgb, k=k, f=f, nh=nh: e.matmul(
                                gbk[:, :], wgb[:, k * FE + f * 128:k * FE + (f + 1) * 128], h2[:, k, nh * 512:(nh + 1) * 512],
                                start=(k == 0), stop=(k == DC - 1)), reads=[wgtk, t_h2], writes=[gtk])
                        ubk, utk = scr.next()
                        for k in range(DC):
                            p.op("pe", lambda e, ubk=ubk, wub=wub, k=k, f=f, nh=nh: e.matmul(
                                ubk[:, :], wub[:, k * FE + f * 128:k * FE + (f + 1) * 128], h2[:, k, nh * 512:(nh + 1) * 512],
                                start=(k == 0), stop=(k == DC - 1)), reads=[wutk, t_h2], writes=[utk])
                        sg, sgtk = sg_r.next()
                        p.op("act", lambda e, gbk=gbk, sg=sg: e.activation(out=sg[:], in_=gbk[:, :], func=AF.Silu),
                             reads=[gtk], writes=[sgtk])
                        p.op("dve", lambda e, ubk=ubk, sg=sg, f=f, nh=nh: e.tensor_tensor(
                            out=A[:, f, nh * 512:(nh + 1) * 512], in0=ubk[:, :], in1=sg[:], op=ALU.mult),
                            reads=[utk, sgtk], writes=[t_A])
                for t in range(8):
                    for db in range(4):
                        ybk, ytk = scr.next()
                        for f in range(3):
                            p.op("pe", lambda e, ybk=ybk, wdb=wdb, f=f, t=t, db=db: e.matmul(
                                ybk[:, :], A[:, f, t * 128:(t + 1) * 128], wdb[:, f * D + db * 512:f * D + (db + 1) * 512],
                                start=(f == 0), stop=(f == 2)), reads=[t_A, wdtk], writes=[ytk])
                        if ex == 0:
                            p.op("dve", lambda e, ybk=ybk, t=t, db=db, ex=ex: e.tensor_scalar(
                                out=Yacc[:, t, db * 512:(db + 1) * 512], in0=ybk[:, :], scalar1=rwt[:, t, ex:ex + 1], scalar2=None,
                                op0=ALU.mult), reads=[ytk, t_rw], writes=[t_y[t]])
                        else:
                            p.op("dve", lambda e, ybk=ybk, t=t, db=db, ex=ex: e.scalar_tensor_tensor(
                                out=Yacc[:, t, db * 512:(db + 1) * 512], in0=ybk[:, :], scalar=rwt[:, t, ex:ex + 1],
                                in1=Yacc[:, t, db * 512:(db + 1) * 512], op0=ALU.mult, op1=ALU.add),
                                reads=[ytk, t_rw, t_y[t]], writes=[t_y[t]])
            for t in range(8):
                p.dma("pool", y_d[t0 + t * 128:t0 + (t + 1) * 128, :], Yacc[:, t, :], reads=[t_y[t]], is_out=True)
        p.finish()
    return nc


def phaseC_weights(w_g, w_u, w_d):
    g = w_g.reshape(8, DC, 128, FE).transpose(0, 2, 1, 3).reshape(8, 128, DC * FE)
    u = w_u.reshape(8, DC, 128, FE).transpose(0, 2, 1, 3).reshape(8, 128, DC * FE)
    wgu = np.ascontiguousarray(np.stack([g, u], axis=1))
    wd = np.ascontiguousarray(w_d.reshape(8, 3, 128, D).transpose(0, 2, 1, 3).reshape(8, 128, 3 * D))
    return {"wgu": wgu, "wd": wd}


def build_phaseD(TBK=TB):
    NTT = TBK // 128
    nc = bass.Bass("TRN2", target_bir_lowering=False)
    x1_d = nc.dram_tensor("x1", [TBK, D], F32, kind="ExternalInput").ap()
    yg_d = nc.dram_tensor("yg", [8, TBK, D], BF16, kind="ExternalInput").ap()
    mod = nc.dram_tensor("mod", [6, D], F32, kind="ExternalInput").ap()
    ln_d = nc.dram_tensor("ln", [2, D], F32, kind="ExternalInput").ap()
    x2_d = nc.dram_tensor("x2", [TBK, D], F32, kind="ExternalOutput").ap()
    with ExitStack() as st:
        p = Prog(nc, st)
        sb = lambda name, shape, dt: _sb(nc, st, name, shape, dt)
        g2B = sb("g2B", [128, D], F32)
        lgB = sb("lgB", [128, D], F32)
        lbB = sb("lbB", [128, D], F32)
        yt_r = Rot([sb("yt%d" % i, [128, 8, D], BF16) for i in range(2)])
        x_r = Rot([sb("xt%d" % i, [128, D], F32) for i in range(2)])
        r_r = Rot([sb("rr%d" % i, [128, D], F32) for i in range(2)])
        o_r = Rot([sb("xo%d" % i, [128, D], F32) for i in range(2)])
        stats = sb("stats", [128, 32], F32)
        t_c, t_st = Tk(), Tk()
        p.dma("sp", g2B[:], mod[5, :].partition_broadcast(128), writes=[t_c])
        p.dma("sp", lgB[:], ln_d[0, :].partition_broadcast(128), writes=[t_c])
        p.dma("sp", lbB[:], ln_d[1, :].partition_broadcast(128), writes=[t_c])
        for t in range(NTT):
            r0 = t * 128
            yt, yttk = yt_r.next()
            p.dma("sp", yt[:], yg_d[:, r0:r0 + 128, :].rearrange("g p d -> p g d"), writes=[yttk])
            xt, xtk = x_r.next()
            p.dma("sp", xt[:], x1_d[r0:r0 + 128, :], writes=[xtk])
            rb, rtk = r_r.next()
            p.op("dve", lambda e, rb=rb, yt=yt: e.tensor_tensor(out=rb[:], in0=yt[:, 0, :], in1=yt[:, 1, :], op=ALU.add),
                 reads=[yttk], writes=[rtk])
            for gi in range(2, 8):
                eng = "dve" if gi % 2 == 0 else "pool"
                p.op(eng, lambda e, rb=rb, yt=yt, gi=gi: e.tensor_tensor(out=rb[:], in0=rb[:], in1=yt[:, gi, :], op=ALU.add),
                     reads=[yttk, rtk], writes=[rtk])
            p.op("pool", lambda e, rb=rb: e.tensor_tensor(out=rb[:], in0=rb[:], in1=g2B[:], op=ALU.mult),
                 reads=[rtk, t_c], writes=[rtk])
            p.op("dve", lambda e, rb=rb, xt=xt: e.scalar_tensor_tensor(out=rb[:], in0=xt[:], scalar=ALPHA, in1=rb[:],
                                                                      op0=ALU.mult, op1=ALU.add),
                 reads=[xtk, rtk], writes=[rtk])
            xo, xotk = o_r.next()
            emit_layernorm(p, rb, rtk, xo, xotk, lgB, lbB, t_c, stats, t_st)
            p.dma("sp", x2_d[r0:r0 + 128, :], xo[:], reads=[xotk], is_out=True)
        p.finish()
    return nc


def _run(nc, maps):
    res = run_bass_kernel_spmd(nc, maps, core_ids=list(range(NCORE)))
    return res.results


def kernel(x, c, w_ada, b_ada, w_in, w_branch_gate, b_branch_gate, attn_sinks, lambda_q1, lambda_k1, lambda_q2,
           lambda_k2, subln_g, w_branch, w_out, ln1_g, ln1_b, w_router_group, b_router_group, w_router_expert,
           b_router_expert, w_exp_gate, w_exp_up, w_exp_down, ln2_g, ln2_b):
    f32 = lambda a: np.asarray(a, np.float32)
    xs = np.ascontiguousarray(f32(x)[0])
    mod_all = run_phase0(f32(c), f32(w_ada), f32(b_ada))
    ncA = build_phaseA2(SEQ)
    ncB = build_phaseB(TB)
    ncC = [None]
    ncC2 = {}
    ncD = build_phaseD(TB)
    for l in range(DEPTH):
        lam_init = 0.8 - 0.6 * math.exp(-0.3 * l)
        m = mod_all[l].reshape(6, D)
        mod6 = np.ascontiguousarray(np.stack([m[1], m[0], m[2], m[4], m[3], m[5]]))
        maps = [phaseA_inputs(i, SEQ, xs, mod6[0:2], f32(w_in[l]), f32(attn_sinks[l]), f32(lambda_q1[l]),
                              f32(lambda_k1[l]), f32(lambda_q2[l]), f32(lambda_k2[l]), f32(subln_g[l]), lam_init)
                for i in range(NCORE)]
        ra = _run(ncA, maps)
        o_cat = np.empty((SEQ, 3, 1024), np.float32)
        for i in range(NCORE):
            o_cat[:, :, 128 * i:128 * (i + 1)] = ra[i]["o"].reshape(SEQ, 3, 128)
        o_cat = o_cat.reshape(SEQ, 3072)
        del ra, maps
        wts = phaseB_weights(f32(w_branch_gate[l]), f32(b_branch_gate[l]), f32(w_branch[l]), f32(w_out[l]), f32(ln1_g[l]),
                             f32(ln1_b[l]), f32(w_router_group[l]), f32(b_router_group[l]), f32(w_router_expert[l]),
                             f32(b_router_expert[l]))
        maps = []
        for i in range(NCORE):
            mm = dict(wts)
            mm.update({"x": np.ascontiguousarray(xs[i * TB:(i + 1) * TB]),
                       "o": np.ascontiguousarray(o_cat[i * TB:(i + 1) * TB]), "mod": mod6})
            maps.append(mm)
        rb = _run(ncB, maps)
        x1 = [rb[i]["x1"] for i in range(NCORE)]
        h2T = np.ascontiguousarray(np.concatenate([rb[i]["h2T"] for i in range(NCORE)], axis=1))
        rw = np.concatenate([rb[i]["rw"] for i in range(NCORE)], axis=0)
        del rb, maps, wts, o_cat
        sizes = [int(np.count_nonzero(rw[:, 8 * g:8 * g + 8].max(axis=1) > 0)) for g in range(NCORE)]
        cap = max(1024, -(-max(sizes) // 1024) * 1024)
        print("[kernel] layer", l, "group sizes", sizes, "cap", cap, flush=True)
        maps = []
        if cap <= 3072:
            if cap not in ncC2:
                ncC2[cap] = build_phaseC2(SEQ, cap)
            x1_full = np.ascontiguousarray(np.concatenate(x1, axis=0))
            c2c = phaseC2_consts(SEQ)
            modC = np.ascontiguousarray(mod6[3:5])
            for g in range(NCORE):
                mm = phaseC_weights(f32(w_exp_gate[l, 8 * g:8 * g + 8]), f32(w_exp_up[l, 8 * g:8 * g + 8]),
                                    f32(w_exp_down[l, 8 * g:8 * g + 8]))
                mm.update(c2c)
                mm.update({"x1": x1_full, "rw": np.ascontiguousarray(rw[:, 8 * g:8 * g + 8]), "mod": modC})
                maps.append(mm)
            rc = _run(ncC2[cap], maps)
            del x1_full
        else:
            if ncC[0] is None:
                ncC[0] = build_phaseC(SEQ)
            for g in range(NCORE):
                mm = phaseC_weights(f32(w_exp_gate[l, 8 * g:8 * g + 8]), f32(w_exp_up[l, 8 * g:8 * g + 8]),
                                    f32(w_exp_down[l, 8 * g:8 * g + 8]))
                mm.update({"h2T": h2T, "rw": np.ascontiguousarray(rw[:, 8 * g:8 * g + 8])})
                maps.append(mm)
            rc = _run(ncC[0], maps)
        del maps
        ln2 = np.ascontiguousarray(np.stack([f32(ln2_g[l]), f32(ln2_b[l])]))
        maps = []
        for i in range(NCORE):
            yg = np.ascontiguousarray(np.stack([rc[g]["y"][i * TB:(i + 1) * TB] for g in range(NCORE)]))
            maps.append({"x1": x1[i], "yg": yg, "mod": mod6, "ln": ln2})
        del rc
        rd = _run(ncD, maps)
        xs = np.ascontiguousarray(np.concatenate([rd[i]["x2"] for i in range(NCORE)], axis=0))
        del rd, maps
    return xs[None].astype(np.float32)


def phaseA_consts2(core, S):
    NQ = S // 128
    sl_swa = [2.0 ** (-8.0 * (h + 1) / 16.0) for h in (2 * core, 2 * core + 1)]
    sl_d = 2.0 ** (-8.0 * (core + 1) / 8.0)
    p = np.arange(128)[:, None].astype(np.float64)
    t = np.arange(128)[None, :].astype(np.float64)
    swab = np.zeros((128, 4, 128), np.float32)
    for hh in range(2):
        d1 = t - p
        swab[:, 2 * hh, :] = np.where(d1 >= 0, -sl_swa[hh] * d1, NEG)
        d0 = 128 + t - p
        swab[:, 2 * hh + 1, :] = np.where(d0 < 128, -sl_swa[hh] * d0, NEG)
    nj = NQ + 3
    jj = np.arange(nj)[None, :] - 3
    dbias = (sl_d * p - sl_d * 128.0 * jj).astype(np.float32)
    ident = np.eye(128, dtype=np.float32)
    cf = np.concatenate([swab.reshape(128, 512), dbias, ident], axis=1)
    tri = (p >= t).astype(np.float32)
    ones = np.ones((128, 128), np.float32)
    strict = (p < t).astype(np.float32)
    incl = (p <= t).astype(np.float32)
    upper = (p < t).astype(np.float32)
    tl = np.arange(512).astype(np.float64)
    v = (-8.0 * sl_d * tl)
    hi = v.astype(np.float32).astype(ml_dtypes.bfloat16)
    lo = (v - hi.astype(np.float64)).astype(np.float32).astype(ml_dtypes.bfloat16)
    qaug = np.zeros((128, 512), np.float32)
    qaug[0] = hi.astype(np.float32)
    qaug[1] = lo.astype(np.float32)
    qaug[64] = hi.astype(np.float32)
    qaug[65] = lo.astype(np.float32)
    cb = _bf(np.concatenate([tri, ones, strict, incl, qaug, upper], axis=1))
    return np.ascontiguousarray(cf), np.ascontiguousarray(cb)


def build_phaseA2(S, parts=("swa", "sb", "df")):
    NQ = S // 128
    NG = S // 512
    NJ = NQ + 3
    NCF = 512 + NJ + 128
    nc = bass.Bass("TRN2", target_bir_lowering=False)
    x = nc.dram_tensor("x", [S, D], F32, kind="ExternalInput").ap()
    mod = nc.dram_tensor("mod", [2, D], F32, kind="ExternalInput").ap()
    w_t = nc.dram_tensor("w_t", [D, A_WT], F32, kind="ExternalInput").ap()
    w_v = nc.dram_tensor("w_v", [D, A_WV], F32, kind="ExternalInput").ap()
    cf_d = nc.dram_tensor("cf", [128, NCF], F32, kind="ExternalInput").ap()
    cb_d = nc.dram_tensor("cb", [128, 1152], BF16, kind="ExternalInput").ap()
    sm_d = nc.dram_tensor("sm", [1, A_SM], F32, kind="ExternalInput").ap()
    o_d = nc.dram_tensor("o", [S, 384], F32, kind="ExternalOutput").ap()
    with ExitStack() as st:
        p = Prog(nc, st)
        sb = lambda name, shape, dt: _sb(nc, st, name, shape, dt)
        wT = sb("wT", [128, DC, A_WT], BF16)
        wV = sb("wV", [128, DC, A_WV], BF16)
        KA = sb("KA", [128, S], BF16)
        KB = sb("KB", [128, S], BF16)
        KC = sb("KC", [128, S], BF16)
        V = sb("V", [128, NQ, VW], BF16)
        xs = sb("xs", [128, D], F32)
        hT = sb("hT", [128, DC, 512], BF16)
        QA = sb("QA", [128, 512], BF16)
        QB = sb("QB", [128, 512], BF16)
        QC = sb("QC", [128, 512], BF16)
        cf = sb("cfs", [128, NCF], F32)
        cb = sb("cbs", [128, 1152], BF16)
        sm = sb("sms", [128, A_SM], F32)
        sclT = sb("sclT", [128, DC], F32)
        shT = sb("shT", [128, DC], F32)
        misc = sb("misc", [128, 16], F32)
        gsub = sb("gsub", [128, 128], F32)
        junk = sb("junk", [128, 128], F32)
        ost = [sb("ost%d" % i, [128, 4, 384], F32) for i in range(2)]
        t_ost = [Tk(), Tk()]
        banks = [_ps(nc, st, "bk%d" % i, [128, 512], F32) for i in range(8)]
        scr = Rot(banks[0:2])
        CARRY = banks[2:4]
        t_carry = [Tk(), Tk()]
        SBACC, t_sbacc = banks[4], Tk()
        DACC = banks[5:8]
        t_dacc = [Tk() for _ in range(3)]
        e_r = Rot([sb("e%d" % i, [128, 512], F32) for i in range(4)])
        L_r = Rot([sb("L%d" % i, [128, 512], BF16) for i in range(6)])
        W_r = Rot([sb("W%d" % i, [128, 512], BF16) for i in range(4)])
        E_r = Rot([sb("E%d" % i, [128, 512], BF16) for i in range(4)])
        X_r = Rot([sb("X%d" % i, [128, 512], BF16) for i in range(4)])
        sE_r = Rot([sb("sE%d" % i, [128, 512], BF16) for i in range(2)])
        fin_r = Rot([sb("fin%d" % i, [128, 8], F32) for i in range(2)])
        da_r = Rot([sb("da%d" % i, [128, 128], F32) for i in range(2)])
        dd_r = Rot([sb("dd%d" % i, [128, 128], F32) for i in range(2)])

        t_c = Tk()
        t_k = [Tk() for _ in range(NG)]
        t_xs, t_hT, t_q, t_misc = Tk(), Tk(), Tk(), Tk()

        swab = cf[:, 0:512]
        dbias = lambda j: cf[:, 512 + j:512 + j + 1]
        ident = cf[:, 512 + NJ:512 + NJ + 128]
        tri = cb[:, 0:128]
        ones = cb[:, 128:256]
        strict = cb[:, 256:384]
        incl = cb[:, 384:512]
        qaug = cb[:, 512:1024]
        upper = cb[:, 1024:1152]

        p.dma("sp", cf[:], cf_d[:, :], writes=[t_c])
        p.dma("sp", cb[:], cb_d[:, :], writes=[t_c])
        p.dma("sp", sm[:], sm_d[0, :].partition_broadcast(128), writes=[t_c])
        p.dma("sp", sclT[:], mod[0, :].rearrange("(k p) -> p k", p=128), writes=[t_c], slow=True)
        p.dma("sp", shT[:], mod[1, :].rearrange("(k p) -> p k", p=128), writes=[t_c], slow=True)
        for k in range(DC):
            p.dma("pool", wT[:, k, :], w_t[k * 128:(k + 1) * 128, :], writes=[t_c])
            p.dma("pool", wV[:, k, :], w_v[k * 128:(k + 1) * 128, :], writes=[t_c])
        p.op("pool", lambda e: e.memset(V[:, :, 64:65], 1.0), writes=[t_c])
        p.op("pool", lambda e: e.memset(V[:, :, 321:322], 1.0), writes=[t_c])
        p.op("act", lambda e: e.activation(out=misc[:, 0:2], in_=sm[:, 0:2], func=AF.Exp), reads=[t_c], writes=[t_misc])
        p.op("dve", lambda e: e.tensor_tensor(out=junk[:, 0:64], in0=sm[:, 2:66], in1=sm[:, 66:130], op=ALU.mult),
             reads=[t_c], writes=[t_misc])
        p.op("dve", lambda e: e.reduce_sum(out=misc[:, 2:3], in_=junk[:, 0:64], axis=AX.X), reads=[t_misc], writes=[t_misc])
        p.op("dve", lambda e: e.tensor_tensor(out=junk[:, 64:128], in0=sm[:, 130:194], in1=sm[:, 194:258], op=ALU.mult),
             reads=[t_c], writes=[t_misc])
        p.op("dve", lambda e: e.reduce_sum(out=misc[:, 3:4], in_=junk[:, 64:128], axis=AX.X), reads=[t_misc], writes=[t_misc])
        p.op("act", lambda e: e.activation(out=misc[:, 5:7], in_=misc[:, 2:4], func=AF.Exp), reads=[t_misc], writes=[t_misc])
        p.op("dve", lambda e: e.tensor_tensor(out=misc[:, 7:8], in0=misc[:, 6:7], in1=misc[:, 5:6], op=ALU.subtract),
             reads=[t_misc], writes=[t_misc])
        p.op("dve", lambda e: e.tensor_tensor(out=misc[:, 4:5], in0=misc[:, 7:8], in1=sm[:, 386:387], op=ALU.subtract),
             reads=[t_misc, t_c], writes=[t_misc])
        p.op("dve", lambda e: e.tensor_scalar(out=gsub[:], in0=sm[:, 258:386], scalar1=sm[:, 387:388], scalar2=None,
                                              op0=ALU.mult), reads=[t_c], writes=[t_misc])

        def dacc(m, qi):
            j = m * 4 + qi
            return DACC[j // 3], t_dacc[j // 3], (j % 3) * 129, j // 3

        for g in range(NG):
            for tt in range(4):
                r0 = g * 512 + tt * 128
                p.dma("sp", xs[:], x[r0:r0 + 128, :], writes=[t_xs])
                for c4 in range(4):
                    bk, tk = scr.next()
                    for j in range(4):
                        c = c4 * 4 + j
                        p.op("pe", lambda e, bk=bk, j=j, c=c: e.transpose(
                            bk[:, j * 128:(j + 1) * 128], xs[:, c * 128:(c + 1) * 128], ident),
                            reads=[t_xs, t_c], writes=[tk])
                    for j in range(4):
                        c = c4 * 4 + j
                        p.op("dve", lambda e, bk=bk, j=j, c=c, tt=tt: e.tensor_scalar(
                            out=hT[:, c, tt * 128:(tt + 1) * 128], in0=bk[:, j * 128:(j + 1) * 128], scalar1=sclT[:, c:c + 1],
                            scalar2=shT[:, c:c + 1], op0=ALU.mult, op1=ALU.add), reads=[tk, t_c], writes=[t_hT])
            dests = [QA[:, :], KA[:, g * 512:(g + 1) * 512], QB[:, :], KB[:, g * 512:(g + 1) * 512],
                     QC[:, :], KC[:, g * 512:(g + 1) * 512]]
            for m in range(6):
                bk, tk = scr.next()
                for k in range(DC):
                    p.op("pe", lambda e, bk=bk, k=k, m=m: e.matmul(
                        bk[:, :], wT[:, k, m * 128:(m + 1) * 128], hT[:, k, :], start=(k == 0), stop=(k == DC - 1)),
                        reads=[t_c, t_hT], writes=[tk])
                wtk = t_q if m % 2 == 0 else t_k[g]
                p.op("act", lambda e, bk=bk, m=m, dests=dests: e.activation(out=dests[m], in_=bk[:, :], func=AF.Identity),
                     reads=[tk], writes=[wtk])
            for t in range(4):
                bk, tk = scr.next()
                for k in range(DC):
                    p.op("pe", lambda e, bk=bk, k=k, t=t: e.matmul(
                        bk[:, 0:A_WV], hT[:, k, t * 128:(t + 1) * 128], wV[:, k, :], start=(k == 0), stop=(k == DC - 1)),
                        reads=[t_c, t_hT], writes=[tk])
                qb = g * 4 + t
                p.op("dve", lambda e, bk=bk, qb=qb: e.tensor_copy(out=V[:, qb, 0:64], in_=bk[:, 0:64]),
                     reads=[tk], writes=[t_k[g]])
                p.op("dve", lambda e, bk=bk, qb=qb: e.tensor_copy(out=V[:, qb, 65:321], in_=bk[:, 64:320]),
                     reads=[tk], writes=[t_k[g]])

            o_t, o_tk = ost[g % 2], t_ost[g % 2]

            swst = {}

            def sw_a(qi):
                i = g * 4 + qi
                kbs = [i] + ([i - 1] if i > 0 else [])
                wd = 128 * len(kbs)
                tm, ttk = e_r.next()
                sE, setk = sE_r.next()
                segs = []
                for hh in range(2):
                    bk, tk = scr.next()
                    for n, kb in enumerate(kbs):
                        p.op("pe", lambda e, bk=bk, hh=hh, kb=kb, n=n: e.matmul(
                            bk[:, n * 128:(n + 1) * 128], KA[64 * hh:64 * hh + 64, kb * 128:(kb + 1) * 128],
                            QA[64 * hh:64 * hh + 64, qi * 128:(qi + 1) * 128], start=True, stop=True),
                            reads=[t_k[kb // 4], t_q], writes=[tk])
                        segs.append((hh, kb, 2 * hh + n))
                    p.op("dve", lambda e, bk=bk, hh=hh: e.scalar_tensor_tensor(
                        out=tm[:, 256 * hh:256 * hh + wd], in0=bk[:, 0:wd], scalar=SCALE, in1=swab[:, 256 * hh:256 * hh + wd],
                        op0=ALU.mult, op1=ALU.add), reads=[tk, t_c], writes=[ttk])
                    p.op("act", lambda e, hh=hh: e.activation(out=sE[:, 256 * hh:256 * hh + wd], in_=tm[:, 256 * hh:256 * hh + wd],
                                                              func=AF.Exp), reads=[ttk], writes=[setk])
                swst[qi] = (segs, sE, setk)

            def sw_b(qi):
                segs, sE, setk = swst.pop(qi)
                abk, atk = scr.next()
                first = True
                for hh in range(2):
                    mine = [s for s in segs if s[0] == hh]
                    for n, (_, kb, slot) in enumerate(mine):
                        p.op("pe", lambda e, hh=hh, kb=kb, slot=slot, first=first: e.matmul(
                            abk[:, hh * 65:hh * 65 + 65], sE[:, slot * 128:(slot + 1) * 128], V[:, kb, 0:65],
                            start=first, stop=True, skip_group_check=True),
                            reads=[setk, t_k[kb // 4], t_c], writes=[atk])
                        first = False
                fb, ftk = fin_r.next()
                for hh in range(2):
                    p.op("dve", lambda e, hh=hh: e.tensor_tensor(
                        out=fb[:, hh:hh + 1], in0=abk[:, hh * 65 + 64:hh * 65 + 65], in1=misc[:, hh:hh + 1], op=ALU.add),
                        reads=[atk, t_misc], writes=[ftk])
                p.op("dve", lambda e: e.reciprocal(out=fb[:, 2:4], in_=fb[:, 0:2]), reads=[ftk], writes=[ftk])
                for hh in range(2):
                    p.op("dve", lambda e, hh=hh, o_t=o_t: e.tensor_scalar(
                        out=o_t[:, qi, 64 * hh:64 * hh + 64], in0=abk[:, hh * 65:hh * 65 + 64], scalar1=fb[:, 2 + hh:3 + hh],
                        scalar2=None, op0=ALU.mult), reads=[atk, ftk], writes=[o_tk])

            if "swa" in parts:
                pipeline(4, [sw_a, sw_b])

            nst = 4 * g + 4
            sbst = {}
            dfst = {}
            dstarted = set()

            def sb_a(hh, s):
                kb = 4 * g + 3 - s
                qs = max(0, kb - 4 * g) * 128
                kg = t_k[kb // 4]
                bk, tk = scr.next()
                p.op("pe", lambda e: e.matmul(bk[:, qs:512], KB[64 * hh:64 * hh + 64, kb * 128:(kb + 1) * 128],
                                              QB[64 * hh:64 * hh + 64, qs:512], start=True, stop=True),
                     reads=[kg, t_q], writes=[tk])
                eb, etk = e_r.next()
                p.op("act", lambda e: e.activation(out=eb[:, qs:512], in_=bk[:, qs:512], func=AF.Exp, scale=SCALE),
                     reads=[tk], writes=[etk])
                Lb, Ltk = L_r.next()
                p.op("act", lambda e: e.activation(out=Lb[:, qs:512], in_=eb[:, qs:512], func=AF.Ln, bias=1.0, scale=1.0),
                     reads=[etk], writes=[Ltk])
                if kb >= 4 * g:
                    p.op("pool", lambda e: e.tensor_tensor(out=Lb[:, qs:qs + 128], in0=Lb[:, qs:qs + 128], in1=strict,
                                                           op=ALU.mult), reads=[Ltk, t_c], writes=[Ltk])
                sbst[(hh, s)] = dict(kb=kb, qs=qs, kg=kg, Lb=Lb, Ltk=Ltk, eb=eb, etk=etk)

            def sb_b(hh, s):
                d = sbst[(hh, s)]
                kb, qs, kg, Lb, Ltk, eb, etk = d["kb"], d["qs"], d["kg"], d["Lb"], d["Ltk"], d["eb"], d["etk"]
                cbk, ctk = CARRY[hh], t_carry[hh]
                p.op("pe", lambda e: e.matmul(cbk[:, qs:512], tri, Lb[:, qs:512], start=(s == 0), stop=True,
                                              skip_group_check=True), reads=[Ltk, t_c], writes=[ctk])
                Xb, Xtk = X_r.next()
                p.op("act", lambda e: e.activation(out=Xb[:, qs:512], in_=cbk[:, qs:512], func=AF.Exp, scale=-1.0),
                     reads=[ctk], writes=[Xtk])
                Wb, Wtk = W_r.next()
                p.op("dve", lambda e: e.tensor_tensor(out=Wb[:, qs:512], in0=eb[:, qs:512], in1=Xb[:, qs:512], op=ALU.mult),
                     reads=[etk, Xtk], writes=[Wtk])
                if kb >= 4 * g:
                    p.op("pool", lambda e: e.tensor_tensor(out=Wb[:, qs:qs + 128], in0=Wb[:, qs:qs + 128], in1=strict,
                                                           op=ALU.mult), reads=[Wtk, t_c], writes=[Wtk])
                d["Wb"], d["Wtk"] = Wb, Wtk

            def sb_c(hh, s):
                d = sbst.pop((hh, s))
                kb, qs, kg, Lb, Ltk, Wb, Wtk = d["kb"], d["qs"], d["kg"], d["Lb"], d["Ltk"], d["Wb"], d["Wtk"]
                cbk, ctk = CARRY[hh], t_carry[hh]
                if kb > 0:
                    p.op("pe", lambda e: e.matmul(cbk[:, qs:512], upper, Lb[:, qs:512], start=False, stop=True,
                                                  skip_group_check=True), reads=[Ltk, t_c], writes=[ctk])
                for qi in range(qs // 128, 4):
                    c0 = hh * 256 + qi * 64
                    p.op("pe", lambda e, qi=qi, c0=c0: e.matmul(
                        SBACC[:, c0:c0 + 64], Wb[:, qi * 128:(qi + 1) * 128], V[:, kb, 65 + 64 * hh:65 + 64 * hh + 64],
                        start=(s == 0 and hh == 0 and qi == 3), stop=(kb == 0), skip_group_check=True),
                        reads=[Wtk, kg], writes=[t_sbacc])
                if kb == 0:
                    p.op("dve", lambda e, o_t=o_t: e.tensor_copy(
                        out=o_t[:, :, 128 + 64 * hh:128 + 64 * hh + 64],
                        in_=SBACC[:, hh * 256:(hh + 1) * 256].rearrange("p (q d) -> p q d", d=64)),
                        reads=[t_sbacc], writes=[o_tk])

            def df_a(m, s):
                kb = s
                qs = max(0, kb - 4 * g) * 128
                kg = t_k[kb // 4]
                bk, tk = scr.next()
                p.op("pe", lambda e: e.matmul(bk[:, qs:512], KC[64 * m:64 * m + 64, kb * 128:(kb + 1) * 128],
                                              QC[64 * m:64 * m + 64, qs:512], start=True, stop=False),
                     reads=[kg, t_q], writes=[tk])
                p.op("pe", lambda e: e.matmul(bk[:, qs:512], ones[64 * m:64 * m + 2, :], qaug[64 * m:64 * m + 2, qs:512],
                                              start=False, stop=True), reads=[t_c], writes=[tk])
                Eb, Etk = E_r.next()
                jidx = (4 * g - kb) + 3
                p.op("act", lambda e: e.activation(out=Eb[:, qs:512], in_=bk[:, qs:512], func=AF.Exp, scale=SCALE,
                                                   bias=dbias(jidx)), reads=[tk, t_c], writes=[Etk])
                if kb >= 4 * g:
                    p.op("pool", lambda e: e.tensor_tensor(out=Eb[:, qs:qs + 128], in0=Eb[:, qs:qs + 128], in1=incl,
                                                           op=ALU.mult), reads=[Etk, t_c], writes=[Etk])
                dfst[(m, s)] = dict(kb=kb, qs=qs, kg=kg, Eb=Eb, Etk=Etk)

            def df_b(m, s):
                d = dfst.pop((m, s))
                kb, qs, kg, Eb, Etk = d["kb"], d["qs"], d["kg"], d["Eb"], d["Etk"]
                for qi in range(qs // 128, 4):
                    ab, atk, c0, bi = dacc(m, qi)
                    stt = bi not in dstarted
                    dstarted.add(bi)
                    p.op("pe", lambda e, qi=qi, ab=ab, c0=c0, stt=stt: e.matmul(
                        ab[:, c0:c0 + 129], Eb[:, qi * 128:(qi + 1) * 128], V[:, kb, 193:322],
                        start=stt, stop=(kb == 4 * g + qi), skip_group_check=True),
                        reads=[Etk, kg, t_c], writes=[atk])

            do_sb = "sb" in parts
            do_df = "df" in parts
            for step in range(nst + 2):
                if step < nst:
                    if do_sb:
                        sb_a(0, step)
                        sb_a(1, step)
                    if do_df:
                        df_a(0, step)
                        df_a(1, step)
                if 0 <= step - 2 < nst and do_sb:
                    sb_c(0, step - 2)
                    sb_c(1, step - 2)
                if 0 <= step - 1 < nst:
                    if do_sb:
                        sb_b(0, step - 1)
                        sb_b(1, step - 1)
                    if do_df:
                        df_b(0, step - 1)
                        df_b(1, step - 1)

            for qi in (range(4) if do_df else ()):
                a1, a1tk, c1, _ = dacc(0, qi)
                a2, a2tk, c2, _ = dacc(1, qi)
                fb, ftk = fin_r.next()
                p.op("dve", lambda e, fb=fb, a1=a1, c1=c1: e.reciprocal(out=fb[:, 0:1], in_=a1[:, c1 + 128:c1 + 129]),
                     reads=[a1tk], writes=[ftk])
                p.op("dve", lambda e, fb=fb, a2=a2, c2=c2: e.reciprocal(out=fb[:, 1:2], in_=a2[:, c2 + 128:c2 + 129]),
                     reads=[a2tk], writes=[ftk])
                p.op("dve", lambda e, fb=fb: e.tensor_tensor(out=fb[:, 2:3], in0=fb[:, 1:2], in1=misc[:, 4:5], op=ALU.mult),
                     reads=[ftk, t_misc], writes=[ftk])
                da, datk = da_r.next()
                p.op("dve", lambda e, fb=fb, a1=a1, c1=c1, da=da: e.tensor_scalar(
                    out=da[:], in0=a1[:, c1:c1 + 128], scalar1=fb[:, 0:1], scalar2=None, op0=ALU.mult),
                    reads=[a1tk, ftk], writes=[datk])
                dd, ddtk = dd_r.next()
                p.op("dve", lambda e, fb=fb, a2=a2, c2=c2, da=da, dd=dd: e.scalar_tensor_tensor(
                    out=dd[:], in0=a2[:, c2:c2 + 128], scalar=fb[:, 2:3], in1=da[:], op0=ALU.mult, op1=ALU.add),
                    reads=[a2tk, ftk, datk], writes=[ddtk])
                p.op("dve", lambda e, dd=dd, da=da: e.tensor_tensor(out=da[:], in0=dd[:], in1=dd[:], op=ALU.mult),
                     reads=[ddtk], writes=[datk])
                p.op("dve", lambda e, fb=fb, da=da: e.reduce_sum(out=fb[:, 3:4], in_=da[:], axis=AX.X),
                     reads=[datk], writes=[ftk])
                p.op("act", lambda e, fb=fb: e.activation(out=fb[:, 4:5], in_=fb[:, 3:4], func=AF.Ln, bias=RMS_EPS,
                                                          scale=1.0 / 128.0), reads=[ftk], writes=[ftk])
                p.op("act", lambda e, fb=fb: e.activation(out=fb[:, 5:6], in_=fb[:, 4:5], func=AF.Exp, scale=-0.5),
                     reads=[ftk], writes=[ftk])
                p.op("dve", lambda e, fb=fb, dd=dd, qi=qi, o_t=o_t: e.scalar_tensor_tensor(
                    out=o_t[:, qi, 256:384], in0=dd[:], scalar=fb[:, 5:6], in1=gsub[:], op0=ALU.mult, op1=ALU.mult),
                    reads=[ddtk, ftk, t_misc], writes=[o_tk])
            p.dma("sp", o_d[g * 512:(g + 1) * 512, :].rearrange("(q p) c -> p q c", p=128), o_t[:], reads=[o_tk],
                  is_out=True)
        p.finish()
    return nc


I32 = mybir.dt.int32
BIGPOS = 1.0e6


def _prog_dma_custom(p, q, fn, reads=(), writes=(), is_out=False):
    waits = p._deps(q, reads, writes, strict=True)
    n = getattr(p, "n_ind", 0)
    p.n_ind = n + 1
    sem = p.st.enter_context(p.nc.semaphore("ind%d" % n))
    ev = ("ind%d" % n, 16, sem)

    def run(eng, waits=waits, sem=sem, fn=fn):
        for s, v in waits:
            eng.wait_ge(s, v)
        fn(eng).then_inc(sem, 16)

    p.ops[q].append(run)
    p.meta[q].append(([(id(s_), v) for s_, v in waits], (id(sem), 16)))
    for t in reads:
        t.r.append(ev)
    for t in writes:
        t.w = ev
        t.r = []
    if is_out:
        p.outs.append(ev)


def build_phaseC2(S, CAP, NEX=8):
    J = S // 128
    NCH = CAP // 1024
    NK = CAP // 128
    nc = bass.Bass("TRN2", target_bir_lowering=False)
    x1_d = nc.dram_tensor("x1", [S, D], F32, kind="ExternalInput").ap()
    rw_d = nc.dram_tensor("rw", [S, 8], F32, kind="ExternalInput").ap()
    mod_d = nc.dram_tensor("mod", [2, D], F32, kind="ExternalInput").ap()
    wgu_d = nc.dram_tensor("wgu", [8, 2, 128, DC * FE], F32, kind="ExternalInput").ap()
    wd_d = nc.dram_tensor("wd", [8, 128, 3 * D], F32, kind="ExternalInput").ap()
    tid_d = nc.dram_tensor("tid", [128, J], F32, kind="ExternalInput").ap()
    cst_d = nc.dram_tensor("cst", [128, 384], F32, kind="ExternalInput").ap()
    y_d = nc.dram_tensor("y", [S, D], BF16, kind="ExternalOutput").ap()
    with ExitStack() as st:
        p = Prog(nc, st)
        sb = lambda name, shape, dt: _sb(nc, st, name, shape, dt)
        h2 = sb("h2", [128, DC, 1024], BF16)
        rwc = sb("rwc", [128, NK, 8], F32)
        idxc = sb("idxc", [128, NK], I32)
        RH = sb("RH", [128, J, 10], F32)
        tidf = sb("tidf", [128, J], F32)
        pk_r = Rot([sb("pk%d" % i, [128, J], F32) for i in range(2)])
        oh_r = Rot([sb("oh%d" % i, [128, 128], F32) for i in range(3)])
        Yacc = sb("Yacc", [128, 8, D], F32)
        wg_r = Rot([sb("wgt%d" % i, [128, DC * FE], BF16) for i in range(2)])
        wu_r = Rot([sb("wut%d" % i, [128, DC * FE], BF16) for i in range(2)])
        wd_r = Rot([sb("wdt%d" % i, [128, 3 * D], BF16) for i in range(2)])
        A = sb("A", [128, 3, 1024], BF16)
        sg_r = Rot([sb("sg%d" % i, [128, 512], F32) for i in range(2)])
        xg = sb("xg", [128, D], F32)
        R = sb("R", [128, J, 8], F32)
        sc = [sb("sc%d" % i, [128, J], F32) for i in range(2)]
        msk = sb("msk", [128, J], F32)
        posf = sb("posf", [128, J], F32)
        cst = sb("cst", [128, 384], F32)
        modT = sb("modT", [128, 2, DC], F32)
        offs = sb("offs", [128, 2], F32)
        zt = sb("zt", [128, D], BF16)
        banks = [_ps(nc, st, "bk%d" % i, [128, 512], F32) for i in range(8)]
        scr = Rot(banks)
        t_c, t_ix, t_h2, t_rw, t_A, t_xg, t_idl, t_ic, t_yd = [Tk() for _ in range(9)]
        t_y = [Tk() for _ in range(8)]
        ident = cst[:, 0:128]
        U2 = cst[:, 128:256]
        _bregs = {}

        def breg(e):
            if "r" not in _bregs:
                _bregs["r"] = e.to_reg(S - 1)
            return _bregs["r"]

        iota_s = cst[:, 256:384]

        p.dma("sp", cst[:], cst_d[:, :], writes=[t_c])
        p.dma("sp", tidf[:], tid_d[:, :], writes=[t_c])
        p.dma("sp", modT[:], mod_d.rearrange("m (k p) -> p m k", p=128), writes=[t_c], slow=True)
        p.dma("sp", R[:], rw_d.rearrange("(p j) e -> p j e", j=J), writes=[t_ix])
        p.op("dve", lambda e: e.memset(zt[:], 0.0), writes=[t_c])
        for k in range(J):
            p.dma("sp", y_d[k * 128:(k + 1) * 128, :], zt[:], reads=[t_c], writes=[t_yd], is_out=True)
        p.op("dve", lambda e: e.reduce_sum(out=sc[0][:], in_=R[:], axis=AX.X), reads=[t_ix], writes=[t_ix])
        p.op("dve", lambda e: e.tensor_scalar(out=msk[:], in0=sc[0][:], scalar1=0.0, scalar2=None, op0=ALU.is_gt),
             reads=[t_ix], writes=[t_ix])
        p.op("dve", lambda e: e.tensor_copy(out=sc[0][:], in_=msk[:]), reads=[t_ix], writes=[t_ix])
        cur = 0
        sh = 1
        while sh < J:
            a, b = sc[cur], sc[1 - cur]
            p.op("dve", lambda e, a=a, b=b, sh=sh: e.tensor_copy(out=b[:, 0:sh], in_=a[:, 0:sh]), reads=[t_ix], writes=[t_ix])
            p.op("dve", lambda e, a=a, b=b, sh=sh: e.tensor_tensor(out=b[:, sh:J], in0=a[:, sh:J], in1=a[:, 0:J - sh], op=ALU.add),
                 reads=[t_ix], writes=[t_ix])
            cur = 1 - cur
            sh *= 2
        incl = sc[cur]
        obk, otk = scr.next()
        p.op("pe", lambda e: e.matmul(obk[:, 0:1], U2, incl[:, J - 1:J], start=True, stop=True), reads=[t_ix, t_c], writes=[otk])
        p.op("dve", lambda e: e.tensor_scalar(out=offs[:, 0:1], in0=obk[:, 0:1], scalar1=-BIGPOS, scalar2=None, op0=ALU.add),
             reads=[otk], writes=[t_ix])
        p.op("dve", lambda e: e.tensor_tensor(out=posf[:], in0=incl[:], in1=msk[:], op=ALU.subtract), reads=[t_ix], writes=[t_ix])
        p.op("dve", lambda e: e.tensor_scalar(out=posf[:], in0=posf[:], scalar1=offs[:, 0:1], scalar2=None, op0=ALU.add),
             reads=[t_ix], writes=[t_ix])
        p.op("dve", lambda e: e.tensor_tensor(out=posf[:], in0=posf[:], in1=msk[:], op=ALU.mult), reads=[t_ix], writes=[t_ix])
        p.op("dve", lambda e: e.tensor_scalar(out=posf[:], in0=posf[:], scalar1=BIGPOS, scalar2=None, op0=ALU.add),
             reads=[t_ix], writes=[t_ix])
        p.op("dve", lambda e: e.memset(RH[:], 1.0), writes=[t_ix])
        p.op("dve", lambda e: e.tensor_copy(out=RH[:, :, 0], in_=tidf[:]), reads=[t_c], writes=[t_ix])
        p.op("dve", lambda e: e.tensor_copy(out=RH[:, :, 2:10], in_=R[:]), reads=[t_ix], writes=[t_ix])
        for k in range(NK):
            pk, pktk = pk_r.next()
            p.op("dve", lambda e, pk=pk, k=k: e.tensor_scalar(out=pk[:], in0=posf[:], scalar1=float(-128 * k), scalar2=None,
                                                              op0=ALU.add), reads=[t_ix], writes=[pktk])
            cbk, ctk = scr.next()
            for j in range(J):
                oh, ohtk = oh_r.next()
                p.op("dve", lambda e, oh=oh, pk=pk, j=j: e.tensor_scalar(out=oh[:], in0=iota_s, scalar1=pk[:, j:j + 1], scalar2=None,
                                                                         op0=ALU.is_equal), reads=[pktk, t_c], writes=[ohtk])
                p.op("pe", lambda e, oh=oh, cbk=cbk, j=j: e.matmul(cbk[:, 0:10], oh[:], RH[:, j, :], start=(j == 0), stop=(j == J - 1)),
                     reads=[ohtk, t_ix], writes=[ctk])
            p.op("dve", lambda e, cbk=cbk: e.tensor_scalar(out=offs[:, 1:2], in0=cbk[:, 1:2], scalar1=float(-S), scalar2=float(S),
                                                           op0=ALU.mult, op1=ALU.add), reads=[ctk], writes=[t_ic])
            p.op("dve", lambda e, cbk=cbk: e.tensor_tensor(out=offs[:, 1:2], in0=offs[:, 1:2], in1=cbk[:, 0:1], op=ALU.add),
                 reads=[ctk, t_ic], writes=[t_ic])
            p.op("dve", lambda e, k=k: e.tensor_copy(out=idxc[:, k:k + 1], in_=offs[:, 1:2]), reads=[t_ic], writes=[t_ic])
            p.op("dve", lambda e, cbk=cbk, k=k: e.tensor_copy(out=rwc[:, k, :], in_=cbk[:, 2:10]), reads=[ctk], writes=[t_rw])

        for ch in range(NCH):
            for t in range(8):
                k = ch * 8 + t
                p.op("pool", lambda e: e.memset(xg[:], 0.0), writes=[t_xg])
                _prog_dma_custom(p, "pool", lambda e, k=k: e.indirect_dma_start(
                    out=xg[:, :], out_offset=None, in_=x1_d[:, :],
                    in_offset=bass.IndirectOffsetOnAxis(ap=idxc[:, k:k + 1], axis=0), bounds_check=breg(e), oob_is_err=False),
                    reads=[t_ic], writes=[t_xg])
                for c4 in range(4):
                    bk, tk = scr.next()
                    for jj in range(4):
                        c = c4 * 4 + jj
                        p.op("pe", lambda e, bk=bk, jj=jj, c=c: e.transpose(
                            bk[:, jj * 128:(jj + 1) * 128], xg[:, c * 128:(c + 1) * 128], ident), reads=[t_xg, t_c], writes=[tk])
                    for jj in range(4):
                        c = c4 * 4 + jj
                        p.op("dve", lambda e, bk=bk, jj=jj, c=c, t=t: e.tensor_scalar(
                            out=h2[:, c, t * 128:(t + 1) * 128], in0=bk[:, jj * 128:(jj + 1) * 128], scalar1=modT[:, 0, c:c + 1],
                            scalar2=modT[:, 1, c:c + 1], op0=ALU.mult, op1=ALU.add), reads=[tk, t_c], writes=[t_h2])
            for ex in range(NEX):
                wgb, wgtk = wg_r.next()
                wub, wutk = wu_r.next()
                wdb, wdtk = wd_r.next()
                for j in range(3):
                    p.dma("pool", wgb[:, j * 2048:(j + 1) * 2048], wgu_d[ex, 0, :, j * 2048:(j + 1) * 2048], writes=[wgtk])
                    p.dma("pool", wub[:, j * 2048:(j + 1) * 2048], wgu_d[ex, 1, :, j * 2048:(j + 1) * 2048], writes=[wutk])
                for j in range(3):
                    p.dma("pool", wdb[:, j * 2048:(j + 1) * 2048], wd_d[ex, :, j * 2048:(j + 1) * 2048], writes=[wdtk])
                for f in range(3):
                    for nh in range(2):
                        gbk, gtk = scr.next()
                        for k in range(DC):
                            p.op("pe", lambda e, gbk=gbk, wgb=wgb, k=k, f=f, nh=nh: e.matmul(
                                gbk[:, :], wgb[:, k * FE + f * 128:k * FE + (f + 1) * 128], h2[:, k, nh * 512:(nh + 1) * 512],
                                start=(k == 0), stop=(k == DC - 1)), reads=[wgtk, t_h2], writes=[gtk])
                        ubk, utk = scr.next()
                        for k in range(DC):
                            p.op("pe", lambda e, ubk=ubk, wub=wub, k=k, f=f, nh=nh: e.matmul(
                                ubk[:, :], wub[:, k * FE + f * 128:k * FE + (f + 1) * 128], h2[:, k, nh * 512:(nh + 1) * 512],
                                start=(k == 0), stop=(k == DC - 1)), reads=[wutk, t_h2], writes=[utk])
                        sg, sgtk = sg_r.next()
                        p.op("act", lambda e, gbk=gbk, sg=sg: e.activation(out=sg[:], in_=gbk[:, :], func=AF.Silu),
                             reads=[gtk], writes=[sgtk])
                        p.op("dve", lambda e, ubk=ubk, sg=sg, f=f, nh=nh: e.tensor_tensor(
                            out=A[:, f, nh * 512:(nh + 1) * 512], in0=ubk[:, :], in1=sg[:], op=ALU.mult),
                            reads=[utk, sgtk], writes=[t_A])
                for t in range(8):
                    for db in range(4):
                        ybk, ytk = scr.next()
                        for f in range(3):
                            p.op("pe", lambda e, ybk=ybk, wdb=wdb, f=f, t=t, db=db: e.matmul(
                                ybk[:, :], A[:, f, t * 128:(t + 1) * 128], wdb[:, f * D + db * 512:f * D + (db + 1) * 512],
                                start=(f == 0), stop=(f == 2)), reads=[t_A, wdtk], writes=[ytk])
                        if ex == 0:
                            p.op("dve", lambda e, ybk=ybk, t=t, db=db, ex=ex, ch=ch: e.tensor_scalar(
                                out=Yacc[:, t, db * 512:(db + 1) * 512], in0=ybk[:, :], scalar1=rwc[:, ch * 8 + t, ex:ex + 1], scalar2=None,
                                op0=ALU.mult), reads=[ytk, t_rw], writes=[t_y[t]])
                        else:
                            p.op("dve", lambda e, ybk=ybk, t=t, db=db, ex=ex, ch=ch: e.scalar_tensor_tensor(
                                out=Yacc[:, t, db * 512:(db + 1) * 512], in0=ybk[:, :], scalar=rwc[:, ch * 8 + t, ex:ex + 1],
                                in1=Yacc[:, t, db * 512:(db + 1) * 512], op0=ALU.mult, op1=ALU.add),
                                reads=[ytk, t_rw, t_y[t]], writes=[t_y[t]])
            for t in range(8):
                _prog_dma_custom(p, "pool", lambda e, t=t, ch=ch: e.indirect_dma_start(
                    out=y_d[:, :], out_offset=bass.IndirectOffsetOnAxis(ap=idxc[:, ch * 8 + t:ch * 8 + t + 1], axis=0),
                    in_=Yacc[:, t, :], in_offset=None, bounds_check=breg(e), oob_is_err=False),
                    reads=[t_y[t], t_ic], writes=[t_yd], is_out=True)
        p.finish()
    return nc


def phaseC2_consts(S):
    J = S // 128
    tid = (np.arange(128)[:, None] * J + np.arange(J)[None, :]).astype(np.float32)
    pp = np.arange(128)
    U2 = (pp[:, None] < pp[None, :]).astype(np.float32)
    iota = np.broadcast_to(np.arange(128, dtype=np.float32)[None, :], (128, 128))
    cst = np.ascontiguousarray(np.concatenate([np.eye(128, dtype=np.float32), U2, iota], axis=1))
    return {"tid": np.ascontiguousarray(tid), "cst": cst}
```

```python
import math
from contextlib import ExitStack
import numpy as np
import ml_dtypes
import concourse.bass as bass
import concourse.mybir as mybir
from concourse.bass_utils import run_bass_kernel_spmd

F32 = mybir.dt.float32
BF16 = mybir.dt.bfloat16
AF = mybir.ActivationFunctionType
ALU = mybir.AluOpType
AX = mybir.AxisListType

D = 2048
SEQ = 8192
DEPTH = 4
NCORE = 8
DC = D // 128
ALPHA = (2.0 * DEPTH) ** 0.25
LN_EPS = 1e-5
RMS_EPS = 1e-5
SCALE = 1.0 / 8.0
NEG = -30000.0


class Tk:
    __slots__ = ("w", "r")

    def __init__(self):
        self.w = None
        self.r = []


class Prog:
    ENG = ("pe", "act", "dve", "pool", "sp")
    NDMA = 40

    def __init__(self, nc, st):
        self.nc = nc
        self.st = st
        self.ops = {e: [] for e in self.ENG}
        self.sem = {e: st.enter_context(nc.semaphore("s_" + e)) for e in self.ENG}
        self.cnt = {e: 0 for e in self.ENG}
        self.seen = {e: {} for e in self.ENG}
        self.dsem = [st.enter_context(nc.semaphore("d%d" % i)) for i in range(self.NDMA)]
        self.dcnt = [0] * self.NDMA
        self.dnext = 0
        self.outs = []
        self.meta = {e: [] for e in self.ENG}

    def _need(self, e, ev, waits, raw, strict=False):
        if ev is None:
            return
        key, val, sem = ev
        if key == e and not strict and e == "pe":
            return
        if self.seen[e].get(key, 0) >= val:
            return
        self.seen[e][key] = val
        waits.append((sem, val))

    def _deps(self, e, reads, writes, strict=False):
        waits = []
        for t in reads:
            self._need(e, t.w, waits, True, strict)
        for t in writes:
            self._need(e, t.w, waits, False, strict)
            for r in t.r:
                self._need(e, r, waits, False, strict)
        return waits

    def op(self, e, fn, reads=(), writes=()):
        waits = self._deps(e, reads, writes)
        self.cnt[e] += 1
        sem = self.sem[e]
        ev = (e, self.cnt[e], sem)

        def run(eng, waits=waits, fn=fn, sem=sem):
            for s, v in waits:
                eng.wait_ge(s, v)
            fn(eng).then_inc(sem, 1)

        self.ops[e].append(run)
        self.meta[e].append(([(id(s_), v) for s_, v in waits], (id(sem), 1)))
        for t in reads:
            t.r.append(ev)
        for t in writes:
            t.w = ev
            t.r = []

    FRESH_POOL = False

    def dma(self, q, out, in_, reads=(), writes=(), is_out=False, slow=False):
        waits = self._deps(q, reads, writes, strict=True)
        if q == "pool" and Prog.FRESH_POOL:
            n = getattr(self, "n_fp", 0)
            self.n_fp = n + 1
            sem = self.st.enter_context(self.nc.semaphore("fp%d" % n))
            ev = ("fp%d" % n, 16, sem)
        else:
            i = self.dnext
            self.dnext = (self.dnext + 1) % self.NDMA
            sem = self.dsem[i]
            key = "d%d" % i
            prev = self.dcnt[i]
            if prev and self.seen[q].get(key, 0) < prev:
                self.seen[q][key] = prev
                waits.append((sem, prev))
            self.dcnt[i] += 16
            ev = (key, self.dcnt[i], sem)

        def run(eng, waits=waits, sem=sem, out=out, in_=in_, slow=slow):
            for s, v in waits:
                eng.wait_ge(s, v)
            if slow:
                eng.dma_start(out=out, in_=in_, allow_slow_non_contiguous=True).then_inc(sem, 16)
            else:
                eng.dma_start(out=out, in_=in_).then_inc(sem, 16)

        self.ops[q].append(run)
        self.meta[q].append(([(id(s_), v) for s_, v in waits], (id(sem), 16)))
        for t in reads:
            t.r.append(ev)
        for t in writes:
            t.w = ev
            t.r = []
        if is_out:
            self.outs.append(ev)

    def check_deadlock(self):
        cnt = {}
        pos = {e: 0 for e in self.ENG}
        progress = True
        while progress:
            progress = False
            for e in self.ENG:
                while pos[e] < len(self.meta[e]):
                    waits, (sk, inc) = self.meta[e][pos[e]]
                    if all(cnt.get(k, 0) >= v for k, v in waits):
                        cnt[sk] = cnt.get(sk, 0) + inc
                        pos[e] += 1
                        progress = True
                    else:
                        break
        stuck = {e: (pos[e], len(self.meta[e])) for e in self.ENG if pos[e] < len(self.meta[e])}
        return stuck

    def finish(self):
        waits = []
        for ev in self.outs:
            self._need("sp", ev, waits, True)

        def run(eng, waits=waits):
            for s, v in waits:
                eng.wait_ge(s, v)

        self.ops["sp"].append(run)
        nc = self.nc
        with nc.Block() as blk:
            @blk.tensor
            def _(e):
                for f in self.ops["pe"]:
                    f(e)

            @blk.scalar
            def _(e):
                for f in self.ops["act"]:
                    f(e)

            @blk.vector
            def _(e):
                for f in self.ops["dve"]:
                    f(e)

            @blk.gpsimd
            def _(e):
                for f in self.ops["pool"]:
                    f(e)

            @blk.sync
            def _(e):
                for f in self.ops["sp"]:
                    f(e)


def _sb(nc, st, name, shape, dt):
    return st.enter_context(nc.sbuf_tensor("sb_" + name, shape, dt))


def _ps(nc, st, name, shape, dt):
    return st.enter_context(nc.psum_tensor("ps_" + name, shape, dt))


P0_COLS = 6 * D // NCORE
P0_HALF = 768


def build_phase0():
    nc = bass.Bass("TRN2", target_bir_lowering=False)
    c = nc.dram_tensor("c", [1, D], F32, kind="ExternalInput").ap()
    w = nc.dram_tensor("w", [DEPTH, D, P0_COLS], F32, kind="ExternalInput").ap()
    b = nc.dram_tensor("b", [DEPTH, P0_COLS], F32, kind="ExternalInput").ap()
    addc = nc.dram_tensor("addc", [1, P0_COLS], F32, kind="ExternalInput").ap()
    out = nc.dram_tensor("mod", [DEPTH, P0_COLS], F32, kind="ExternalOutput").ap()
    with ExitStack() as st:
        p = Prog(nc, st)
        cT = _sb(nc, st, "cT", [128, DC], F32)
        bt = _sb(nc, st, "bt", [1, DEPTH * P0_COLS], F32)
        at = _sb(nc, st, "at", [1, P0_COLS], F32)
        res = _sb(nc, st, "res", [1, DEPTH * P0_COLS], F32)
        wb = [_sb(nc, st, "wb%d" % i, [128, DC, P0_HALF], F32) for i in range(2)]
        ps = [_ps(nc, st, "ps%d" % i, [128, 512], F32) for i in range(4)]
        t_c, t_b, t_a, t_res = Tk(), Tk(), Tk(), Tk()
        t_w = [Tk(), Tk()]
        t_ps = [Tk() for _ in range(4)]
        p.dma("sp", cT[:], c[0, :].rearrange("(k p) -> p k", p=128), writes=[t_c], slow=True)
        p.dma("sp", bt[:], b.rearrange("l n -> (l n)")[None, :], writes=[t_b])
        p.dma("sp", at[:], addc[:, :], writes=[t_a])
        u = 0
        pi = 0
        for l in range(DEPTH):
            for hf in range(2):
                buf = u % 2
                q = "sp" if u % 2 == 0 else "pool"
                p.dma(q, wb[buf][:], w[l, :, hf * P0_HALF:(hf + 1) * P0_HALF].rearrange("(k p) n -> p k n", p=128),
                      writes=[t_w[buf]])
                for n0 in range(0, P0_HALF, 384):
                    pt = ps[pi % 4]
                    tk = t_ps[pi % 4]
                    pi += 1
                    for k in range(DC):
                        p.op("pe", lambda e, pt=pt, k=k, buf=buf, n0=n0: e.matmul(
                            pt[0:1, 0:384], cT[:, k:k + 1], wb[buf][:, k, n0:n0 + 384], start=(k == 0), stop=(k == DC - 1)),
                            reads=[t_c, t_w[buf]], writes=[tk])
                    o0 = l * P0_COLS + hf * P0_HALF + n0
                    a0 = hf * P0_HALF + n0
                    p.op("dve", lambda e, pt=pt, o0=o0: e.tensor_tensor(
                        out=res[0:1, o0:o0 + 384], in0=pt[0:1, 0:384], in1=bt[0:1, o0:o0 + 384], op=ALU.add),
                        reads=[tk, t_b], writes=[t_res])
                    p.op("dve", lambda e, o0=o0, a0=a0: e.tensor_tensor(
                        out=res[0:1, o0:o0 + 384], in0=res[0:1, o0:o0 + 384], in1=at[0:1, a0:a0 + 384], op=ALU.add),
                        reads=[t_res, t_a], writes=[t_res])
                u += 1
        p.dma("sp", out.rearrange("l n -> (l n)")[None, :], res[:], reads=[t_res], is_out=True)
        p.finish()
    return nc


def run_phase0(c, w_ada, b_ada):
    nc = build_phase0()
    addfull = np.zeros((1, 6 * D), np.float32)
    addfull[0, D:2 * D] = 1.0
    addfull[0, 4 * D:5 * D] = 1.0
    maps = []
    for i in range(NCORE):
        sl = slice(i * P0_COLS, (i + 1) * P0_COLS)
        maps.append({"c": np.ascontiguousarray(c, np.float32),
                     "w": np.ascontiguousarray(w_ada[:, :, sl]),
                     "b": np.ascontiguousarray(b_ada[:, sl]),
                     "addc": np.ascontiguousarray(addfull[:, sl])})
    res = run_bass_kernel_spmd(nc, maps, core_ids=list(range(NCORE)))
    return np.concatenate([r["mod"] for r in res.results], axis=1)


def pipeline(n_iter, stages, skew=1):
    for step in range(n_iter + (len(stages) - 1) * skew):
        for k, f in enumerate(stages):
            i = step - k * skew
            if 0 <= i < n_iter:
                f(i)


class Rot:
    def __init__(self, bufs):
        self.bufs = bufs
        self.tks = [Tk() for _ in bufs]
        self.i = 0

    def next(self):
        j = self.i % len(self.bufs)
        self.i += 1
        return self.bufs[j], self.tks[j]


def _bf(a):
    return np.asarray(a, np.float32).astype(ml_dtypes.bfloat16)


A_WT = 768
A_WV = 320
VW = 322
A_SM = 2 + 4 * 64 + 128 + 2


def phaseA_consts(core, S):
    NQ = S // 128
    sl_swa = [2.0 ** (-8.0 * (h + 1) / 16.0) for h in (2 * core, 2 * core + 1)]
    sl_d = 2.0 ** (-8.0 * (core + 1) / 8.0)
    p = np.arange(128)[:, None].astype(np.float64)
    t = np.arange(128)[None, :].astype(np.float64)
    swab = np.zeros((128, 2, 2, 128), np.float32)
    for hh in range(2):
        d0 = 128 + t - p
        swab[:, hh, 0, :] = np.where(d0 < 128, -sl_swa[hh] * d0, NEG)
        d1 = t - p
        swab[:, hh, 1, :] = np.where(d1 >= 0, -sl_swa[hh] * d1, NEG)
    nj = NQ + 3
    jj = np.arange(nj)[None, :] - 3
    dbias = (sl_d * p - sl_d * 128.0 * jj).astype(np.float32)
    ident = np.eye(128, dtype=np.float32)
    cf = np.concatenate([swab.reshape(128, 512), dbias, ident], axis=1)
    tri = (p >= t).astype(np.float32)
    ones = np.ones((128, 128), np.float32)
    strict = (p < t).astype(np.float32)
    incl = (p <= t).astype(np.float32)
    tl = np.arange(512).astype(np.float64)
    v = (-8.0 * sl_d * tl)
    hi = v.astype(np.float32).astype(ml_dtypes.bfloat16)
    lo = (v - hi.astype(np.float64)).astype(np.float32).astype(ml_dtypes.bfloat16)
    qaug = np.zeros((128, 512), np.float32)
    qaug[0] = hi.astype(np.float32)
    qaug[1] = lo.astype(np.float32)
    cb = _bf(np.concatenate([tri, ones, strict, incl, qaug], axis=1))
    return np.ascontiguousarray(cf), np.ascontiguousarray(cb)


def build_phaseA(S):
    NQ = S // 128
    NG = S // 512
    NJ = NQ + 3
    NCF = 512 + NJ + 128
    nc = bass.Bass("TRN2", target_bir_lowering=False)
    x = nc.dram_tensor("x", [S, D], F32, kind="ExternalInput").ap()
    mod = nc.dram_tensor("mod", [2, D], F32, kind="ExternalInput").ap()
    w_t = nc.dram_tensor("w_t", [D, A_WT], F32, kind="ExternalInput").ap()
    w_v = nc.dram_tensor("w_v", [D, A_WV], F32, kind="ExternalInput").ap()
    cf_d = nc.dram_tensor("cf", [128, NCF], F32, kind="ExternalInput").ap()
    cb_d = nc.dram_tensor("cb", [128, 1024], BF16, kind="ExternalInput").ap()
    sm_d = nc.dram_tensor("sm", [1, A_SM], F32, kind="ExternalInput").ap()
    o_d = nc.dram_tensor("o", [S, 384], F32, kind="ExternalOutput").ap()
    with ExitStack() as st:
        p = Prog(nc, st)
        sb = lambda name, shape, dt: _sb(nc, st, name, shape, dt)
        wT = sb("wT", [128, DC, A_WT], BF16)
        wV = sb("wV", [128, DC, A_WV], BF16)
        KA = sb("KA", [128, S], BF16)
        KB = sb("KB", [128, S], BF16)
        KC = sb("KC", [128, S], BF16)
        V = sb("V", [128, NQ, VW], BF16)
        xs = sb("xs", [128, 2, D], F32)
        hT = sb("hT", [128, DC, 512], BF16)
        QA = sb("QA", [128, 512], BF16)
        QB = sb("QB", [128, 512], BF16)
        QC = sb("QC", [128, 512], BF16)
        cf = sb("cfs", [128, NCF], F32)
        cb = sb("cbs", [128, 1024], BF16)
        sm = sb("sms", [128, A_SM], F32)
        sclT = sb("sclT", [128, DC], F32)
        shT = sb("shT", [128, DC], F32)
        misc = sb("misc", [128, 16], F32)
        gsub = sb("gsub", [128, 128], F32)
        junk = sb("junk", [128, 128], F32)
        ost = [sb("ost%d" % i, [128, 4, 384], F32) for i in range(2)]
        t_ost = [Tk(), Tk()]
        banks = [_ps(nc, st, "bk%d" % i, [128, 512], F32) for i in range(8)]
        scr = Rot(banks[0:3])
        SBACC, t_sbacc = banks[3], Tk()
        DACC = banks[4:8]
        t_dacc = [Tk() for _ in range(4)]
        e_r = Rot([sb("e%d" % i, [128, 512], F32) for i in range(3)])
        L_r = Rot([sb("L%d" % i, [128, 512], BF16) for i in range(3)])
        X_r = Rot([sb("X%d" % i, [128, 512], F32) for i in range(2)])
        W_r = Rot([sb("W%d" % i, [128, 512], BF16) for i in range(3)])
        E_r = Rot([sb("E%d" % i, [128, 512], BF16) for i in range(3)])
        Lacc = sb("Lacc", [128, 512], F32)
        Lab_r = Rot([sb("Lab%d" % i, [128, 512], BF16) for i in range(2)])
        st_r = Rot([sb("stmp%d" % i, [128, 128], F32) for i in range(2)])
        sE_r = Rot([sb("sE%d" % i, [128, 128], BF16) for i in range(2)])
        fin_r = Rot([sb("fin%d" % i, [128, 8], F32) for i in range(2)])
        da_r = Rot([sb("da%d" % i, [128, 128], F32) for i in range(2)])
        dd_r = Rot([sb("dd%d" % i, [128, 128], F32) for i in range(2)])

        t_c = Tk()
        t_k = [Tk() for _ in range(NG)]
        t_xs, t_hT, t_q = Tk(), Tk(), Tk()
        t_lacc = Tk()
        t_misc = Tk()

        swab = lambda hh, kind: cf[:, (hh * 2 + kind) * 128:(hh * 2 + kind + 1) * 128]
        dbias = lambda j: cf[:, 512 + j:512 + j + 1]
        ident = cf[:, 512 + NJ:512 + NJ + 128]
        tri = cb[:, 0:128]
        ones = cb[:, 128:256]
        strict = cb[:, 256:384]
        incl = cb[:, 384:512]
        qaug = cb[:, 512:1024]

        p.dma("sp", cf[:], cf_d[:, :], writes=[t_c])
        p.dma("sp", cb[:], cb_d[:, :], writes=[t_c])
        p.dma("sp", sm[:], sm_d[0, :].partition_broadcast(128), writes=[t_c])
        p.dma("sp", sclT[:], mod[0, :].rearrange("(k p) -> p k", p=128), writes=[t_c], slow=True)
        p.dma("sp", shT[:], mod[1, :].rearrange("(k p) -> p k", p=128), writes=[t_c], slow=True)
        for k in range(DC):
            p.dma("pool", wT[:, k, :], w_t[k * 128:(k + 1) * 128, :], writes=[t_c])
            p.dma("pool", wV[:, k, :], w_v[k * 128:(k + 1) * 128, :], writes=[t_c])
        p.op("pool", lambda e: e.memset(V[:, :, 64:65], 1.0), writes=[t_c])
        p.op("pool", lambda e: e.memset(V[:, :, 321:322], 1.0), writes=[t_c])
        p.op("act", lambda e: e.activation(out=misc[:, 0:2], in_=sm[:, 0:2], func=AF.Exp), reads=[t_c], writes=[t_misc])
        p.op("dve", lambda e: e.tensor_tensor(out=junk[:, 0:64], in0=sm[:, 2:66], in1=sm[:, 66:130], op=ALU.mult),
             reads=[t_c], writes=[t_misc])
        p.op("dve", lambda e: e.reduce_sum(out=misc[:, 2:3], in_=junk[:, 0:64], axis=AX.X), reads=[t_misc], writes=[t_misc])
        p.op("dve", lambda e: e.tensor_tensor(out=junk[:, 64:128], in0=sm[:, 130:194], in1=sm[:, 194:258], op=ALU.mult),
             reads=[t_c], writes=[t_misc])
        p.op("dve", lambda e: e.reduce_sum(out=misc[:, 3:4], in_=junk[:, 64:128], axis=AX.X), reads=[t_misc], writes=[t_misc])
        p.op("act", lambda e: e.activation(out=misc[:, 5:7], in_=misc[:, 2:4], func=AF.Exp), reads=[t_misc], writes=[t_misc])
        p.op("dve", lambda e: e.tensor_tensor(out=misc[:, 7:8], in0=misc[:, 6:7], in1=misc[:, 5:6], op=ALU.subtract),
             reads=[t_misc], writes=[t_misc])
        p.op("dve", lambda e: e.tensor_tensor(out=misc[:, 4:5], in0=misc[:, 7:8], in1=sm[:, 386:387], op=ALU.subtract),
             reads=[t_misc, t_c], writes=[t_misc])
        p.op("dve", lambda e: e.tensor_scalar(out=gsub[:], in0=sm[:, 258:386], scalar1=sm[:, 387:388], scalar2=None,
                                              op0=ALU.mult), reads=[t_c], writes=[t_misc])

        for g in range(NG):
            for half in range(2):
                r0 = g * 512 + half * 256
                p.dma("sp", xs[:], x[r0:r0 + 256, :].rearrange("(t q) d -> q t d", q=128), writes=[t_xs])
                for c in range(DC):
                    bk, tk = scr.next()
                    for t in range(2):
                        p.op("pe", lambda e, bk=bk, t=t, c=c: e.transpose(
                            bk[:, t * 128:(t + 1) * 128], xs[:, t, c * 128:(c + 1) * 128], ident),
                            reads=[t_xs, t_c], writes=[tk])
                    p.op("act", lambda e, bk=bk, c=c, half=half: e.activation(
                        out=hT[:, c, half * 256:(half + 1) * 256], in_=bk[:, 0:256], func=AF.Identity,
                        bias=shT[:, c:c + 1], scale=sclT[:, c:c + 1]), reads=[tk, t_c], writes=[t_hT])
            dests = [QA[:, :], KA[:, g * 512:(g + 1) * 512], QB[:, :], KB[:, g * 512:(g + 1) * 512],
                     QC[:, :], KC[:, g * 512:(g + 1) * 512]]
            for m in range(6):
                bk, tk = scr.next()
                for k in range(DC):
                    p.op("pe", lambda e, bk=bk, k=k, m=m: e.matmul(
                        bk[:, :], wT[:, k, m * 128:(m + 1) * 128], hT[:, k, :], start=(k == 0), stop=(k == DC - 1)),
                        reads=[t_c, t_hT], writes=[tk])
                wtk = t_q if m % 2 == 0 else t_k[g]
                p.op("dve", lambda e, bk=bk, m=m, dests=dests: e.tensor_copy(out=dests[m], in_=bk[:, :]),
                     reads=[tk], writes=[wtk])
            for t in range(4):
                bk, tk = scr.next()
                for k in range(DC):
                    p.op("pe", lambda e, bk=bk, k=k, t=t: e.matmul(
                        bk[:, 0:A_WV], hT[:, k, t * 128:(t + 1) * 128], wV[:, k, :], start=(k == 0), stop=(k == DC - 1)),
                        reads=[t_c, t_hT], writes=[tk])
                qb = g * 4 + t
                p.op("dve", lambda e, bk=bk, qb=qb: e.tensor_copy(out=V[:, qb, 0:64], in_=bk[:, 0:64]),
                     reads=[tk], writes=[t_k[g]])
                p.op("dve", lambda e, bk=bk, qb=qb: e.tensor_copy(out=V[:, qb, 65:321], in_=bk[:, 64:320]),
                     reads=[tk], writes=[t_k[g]])

            o_t, o_tk = ost[g % 2], t_ost[g % 2]

            for hh in range(2):
                for qi in range(4):
                    i = g * 4 + qi
                    kbs = [kb for kb in (i - 1, i) if kb >= 0]
                    abk, atk = scr.next()
                    ps_l = []
                    for kb in kbs:
                        kind = 1 if kb == i else 0
                        bk, tk = scr.next()
                        kg = t_k[kb // 4]
                        p.op("pe", lambda e, bk=bk, kb=kb, hh=hh, qi=qi: e.matmul(
                            bk[:, 0:128], KA[64 * hh:64 * hh + 64, kb * 128:(kb + 1) * 128],
                            QA[64 * hh:64 * hh + 64, qi * 128:(qi + 1) * 128], start=True, stop=True),
                            reads=[kg, t_q], writes=[tk])
                        tm, ttk = st_r.next()
                        p.op("dve", lambda e, bk=bk, tm=tm, hh=hh, kind=kind: e.scalar_tensor_tensor(
                            out=tm[:], in0=bk[:, 0:128], scalar=SCALE, in1=swab(hh, kind), op0=ALU.mult, op1=ALU.add),
                            reads=[tk, t_c], writes=[ttk])
                        sE, setk = sE_r.next()
                        p.op("act", lambda e, tm=tm, sE=sE: e.activation(out=sE[:], in_=tm[:], func=AF.Exp),
                             reads=[ttk], writes=[setk])
                        ps_l.append((sE, setk, kb, kg))
                    for n, (sE, setk, kb, kg) in enumerate(ps_l):
                        p.op("pe", lambda e, abk=abk, sE=sE, kb=kb, n=n, ln=len(ps_l): e.matmul(
                            abk[:, 0:65], sE[:], V[:, kb, 0:65], start=(n == 0), stop=(n == ln - 1)),
                            reads=[setk, kg, t_c], writes=[atk])
                    fb, ftk = fin_r.next()
                    p.op("dve", lambda e, fb=fb, abk=abk, hh=hh: e.tensor_tensor(
                        out=fb[:, 0:1], in0=abk[:, 64:65], in1=misc[:, hh:hh + 1], op=ALU.add),
                        reads=[atk, t_misc], writes=[ftk])
                    p.op("dve", lambda e, fb=fb: e.reciprocal(out=fb[:, 1:2], in_=fb[:, 0:1]), reads=[ftk], writes=[ftk])
                    p.op("dve", lambda e, fb=fb, abk=abk, hh=hh, qi=qi, o_t=o_t: e.tensor_scalar(
                        out=o_t[:, qi, 64 * hh:64 * hh + 64], in0=abk[:, 0:64], scalar1=fb[:, 1:2], scalar2=None,
                        op0=ALU.mult), reads=[atk, ftk], writes=[o_tk])

            its = [(hh, kb) for hh in range(2) for kb in range(g * 4 + 3, -1, -1)]
            stt = {}

            def sb_a(n):
                hh, kb = its[n]
                qs = max(0, kb - 4 * g) * 128
                first = (kb == g * 4 + 3)
                kg = t_k[kb // 4]
                bk, tk = scr.next()
                p.op("pe", lambda e: e.matmul(bk[:, qs:512], KB[64 * hh:64 * hh + 64, kb * 128:(kb + 1) * 128],
                                              QB[64 * hh:64 * hh + 64, qs:512], start=True, stop=True),
                     reads=[kg, t_q], writes=[tk])
                eb, etk = e_r.next()
                p.op("act", lambda e: e.activation(out=eb[:, qs:512], in_=bk[:, qs:512], func=AF.Exp, scale=SCALE),
                     reads=[tk], writes=[etk])
                Lb, Ltk = L_r.next()
                p.op("act", lambda e: e.activation(out=Lb[:, qs:512], in_=eb[:, qs:512], func=AF.Ln, bias=1.0, scale=1.0),
                     reads=[etk], writes=[Ltk])
                if kb >= 4 * g:
                    p.op("pool", lambda e: e.tensor_tensor(out=Lb[:, qs:qs + 128], in0=Lb[:, qs:qs + 128], in1=strict,
                                                           op=ALU.mult), reads=[Ltk, t_c], writes=[Ltk])
                if first:
                    p.op("pool", lambda e: e.memset(Lacc[:], 0.0), writes=[t_lacc])
                stt[n] = dict(hh=hh, kb=kb, qs=qs, first=first, kg=kg, eb=eb, etk=etk, Lb=Lb, Ltk=Ltk)

            def sb_b(n):
                s = stt[n]
                hh, kb, qs, first = s["hh"], s["kb"], s["qs"], s["first"]
                eb, etk, Lb, Ltk = s["eb"], s["etk"], s["Lb"], s["Ltk"]
                cbk, ctk = scr.next()
                p.op("pe", lambda e: e.matmul(cbk[:, qs:512], tri, Lb[:, qs:512], start=True, stop=first),
                     reads=[Ltk, t_c], writes=[ctk])
                if not first:
                    plab, plabtk = s_prev_lab[0]
                    p.op("pe", lambda e: e.matmul(cbk[:, qs:512], ones, plab[:, qs:512], start=False, stop=True),
                         reads=[plabtk, t_c], writes=[ctk])
                Xb, Xtk = X_r.next()
                p.op("act", lambda e: e.activation(out=Xb[:, qs:512], in_=cbk[:, qs:512], func=AF.Exp, scale=-1.0),
                     reads=[ctk], writes=[Xtk])
                Wb, Wtk = W_r.next()
                p.op("dve", lambda e: e.tensor_tensor(out=Wb[:, qs:512], in0=eb[:, qs:512], in1=Xb[:, qs:512], op=ALU.mult),
                     reads=[etk, Xtk], writes=[Wtk])
                if kb >= 4 * g:
                    p.op("pool", lambda e: e.tensor_tensor(out=Wb[:, qs:qs + 128], in0=Wb[:, qs:qs + 128], in1=strict,
                                                           op=ALU.mult), reads=[Wtk, t_c], writes=[Wtk])
                if kb > 0:
                    p.op("pool", lambda e: e.tensor_tensor(out=Lacc[:, qs:512], in0=Lacc[:, qs:512], in1=Lb[:, qs:512],
                                                           op=ALU.add), reads=[Ltk, t_lacc], writes=[t_lacc])
                    lab, labtk = Lab_r.next()
                    p.op("pool", lambda e: e.tensor_copy(out=lab[:, :], in_=Lacc[:, :]), reads=[t_lacc], writes=[labtk])
                    s_prev_lab[0] = (lab, labtk)
                s["Wb"], s["Wtk"] = Wb, Wtk

            def sb_c(n):
                s = stt.pop(n)
                hh, kb, qs, first, kg = s["hh"], s["kb"], s["qs"], s["first"], s["kg"]
                Wb, Wtk = s["Wb"], s["Wtk"]
                for qi in range(qs // 128, 4):
                    c0 = hh * 256 + qi * 64
                    p.op("pe", lambda e, qi=qi, c0=c0: e.matmul(
                        SBACC[:, c0:c0 + 64], Wb[:, qi * 128:(qi + 1) * 128], V[:, kb, 65 + 64 * hh:65 + 64 * hh + 64],
                        start=(first and hh == 0 and qi == 3), stop=(kb == 0), skip_group_check=True),
                        reads=[Wtk, kg], writes=[t_sbacc])
                if kb == 0:
                    p.op("act", lambda e, o_t=o_t: e.activation(
                        out=o_t[:, :, 128 + 64 * hh:128 + 64 * hh + 64],
                        in_=SBACC[:, hh * 256:(hh + 1) * 256].rearrange("p (q d) -> p q d", d=64), func=AF.Identity),
                        reads=[t_sbacc], writes=[o_tk])

            s_prev_lab = [None]
            pipeline(len(its), [sb_a, sb_b, sb_c])

            dits = [(kb, m) for kb in range(0, g * 4 + 4) for m in range(2)]
            dst = {}

            def dacc(m, qi):
                if qi < 3:
                    return DACC[2 * m], t_dacc[2 * m], qi * 129
                return DACC[2 * m + 1], t_dacc[2 * m + 1], 0

            def df_a(n):
                kb, m = dits[n]
                qs = max(0, kb - 4 * g) * 128
                kg = t_k[kb // 4]
                bk, tk = scr.next()
                p.op("pe", lambda e: e.matmul(bk[:, qs:512], KC[64 * m:64 * m + 64, kb * 128:(kb + 1) * 128],
                                              QC[64 * m:64 * m + 64, qs:512], start=True, stop=False),
                     reads=[kg, t_q], writes=[tk])
                p.op("pe", lambda e: e.matmul(bk[:, qs:512], ones[0:2, :], qaug[0:2, qs:512], start=False, stop=True),
                     reads=[t_c], writes=[tk])
                Eb, Etk = E_r.next()
                jidx = (4 * g - kb) + 3
                p.op("act", lambda e: e.activation(out=Eb[:, qs:512], in_=bk[:, qs:512], func=AF.Exp, scale=SCALE,
                                                   bias=dbias(jidx)), reads=[tk, t_c], writes=[Etk])
                if kb >= 4 * g:
                    p.op("pool", lambda e: e.tensor_tensor(out=Eb[:, qs:qs + 128], in0=Eb[:, qs:qs + 128], in1=incl,
                                                           op=ALU.mult), reads=[Etk, t_c], writes=[Etk])
                dst[n] = dict(kb=kb, m=m, qs=qs, kg=kg, Eb=Eb, Etk=Etk)

            def df_b(n):
                s = dst.pop(n)
                kb, m, qs, kg, Eb, Etk = s["kb"], s["m"], s["qs"], s["kg"], s["Eb"], s["Etk"]
                for qi in range(qs // 128, 4):
                    ab, atk, c0 = dacc(m, qi)
                    p.op("pe", lambda e, qi=qi, ab=ab, c0=c0: e.matmul(
                        ab[:, c0:c0 + 129], Eb[:, qi * 128:(qi + 1) * 128], V[:, kb, 193:322],
                        start=(kb == 0 and qi in (0, 3)), stop=(kb == 4 * g + qi), skip_group_check=True),
                        reads=[Etk, kg, t_c], writes=[atk])

            pipeline(len(dits), [df_a, df_b])
            for qi in range(4):
                a1, a1tk, c1 = dacc(0, qi)
                a2, a2tk, c2 = dacc(1, qi)
                fb, ftk = fin_r.next()
                p.op("dve", lambda e, fb=fb, a1=a1, c1=c1: e.reciprocal(out=fb[:, 0:1], in_=a1[:, c1 + 128:c1 + 129]),
                     reads=[a1tk], writes=[ftk])
                p.op("dve", lambda e, fb=fb, a2=a2, c2=c2: e.reciprocal(out=fb[:, 1:2], in_=a2[:, c2 + 128:c2 + 129]),
                     reads=[a2tk], writes=[ftk])
                p.op("dve", lambda e, fb=fb: e.tensor_tensor(out=fb[:, 2:3], in0=fb[:, 1:2], in1=misc[:, 4:5], op=ALU.mult),
                     reads=[ftk, t_misc], writes=[ftk])
                da, datk = da_r.next()
                p.op("dve", lambda e, fb=fb, a1=a1, c1=c1, da=da: e.tensor_scalar(
                    out=da[:], in0=a1[:, c1:c1 + 128], scalar1=fb[:, 0:1], scalar2=None, op0=ALU.mult),
                    reads=[a1tk, ftk], writes=[datk])
                dd, ddtk = dd_r.next()
                p.op("dve", lambda e, fb=fb, a2=a2, c2=c2, da=da, dd=dd: e.scalar_tensor_tensor(
                    out=dd[:], in0=a2[:, c2:c2 + 128], scalar=fb[:, 2:3], in1=da[:], op0=ALU.mult, op1=ALU.add),
                    reads=[a2tk, ftk, datk], writes=[ddtk])
                p.op("dve", lambda e, dd=dd, da=da: e.tensor_tensor(out=da[:], in0=dd[:], in1=dd[:], op=ALU.mult),
                     reads=[ddtk], writes=[datk])
                p.op("dve", lambda e, fb=fb, da=da: e.reduce_sum(out=fb[:, 3:4], in_=da[:], axis=AX.X),
                     reads=[datk], writes=[ftk])
                p.op("act", lambda e, fb=fb: e.activation(out=fb[:, 4:5], in_=fb[:, 3:4], func=AF.Ln, bias=RMS_EPS,
                                                          scale=1.0 / 128.0), reads=[ftk], writes=[ftk])
                p.op("act", lambda e, fb=fb: e.activation(out=fb[:, 5:6], in_=fb[:, 4:5], func=AF.Exp, scale=-0.5),
                     reads=[ftk], writes=[ftk])
                p.op("dve", lambda e, fb=fb, dd=dd, qi=qi, o_t=o_t: e.scalar_tensor_tensor(
                    out=o_t[:, qi, 256:384], in0=dd[:], scalar=fb[:, 5:6], in1=gsub[:], op0=ALU.mult, op1=ALU.mult),
                    reads=[ddtk, ftk, t_misc], writes=[o_tk])
            p.dma("sp", o_d[g * 512:(g + 1) * 512, :].rearrange("(q p) c -> p q c", p=128), o_t[:], reads=[o_tk],
                  is_out=True)
        p.finish()
    return nc


def phaseA_inputs(core, S, x, modA, w_in, sinks, lq1, lk1, lq2, lk2, subg, lam_init):
    h0 = 2 * core
    kv = core // 2
    qA = w_in[:, h0 * 64:h0 * 64 + 128]
    kA = w_in[:, 1024 + kv * 64:1024 + kv * 64 + 64]
    vA = w_in[:, 1280 + kv * 64:1280 + kv * 64 + 64]
    qB = w_in[:, 1536 + h0 * 64:1536 + h0 * 64 + 128]
    kB = w_in[:, 2560 + h0 * 64:2560 + h0 * 64 + 128]
    vB = w_in[:, 3584 + h0 * 64:3584 + h0 * 64 + 128]
    qC = w_in[:, 4608 + core * 128:4608 + core * 128 + 128]
    kC = w_in[:, 5632 + core * 128:5632 + core * 128 + 128]
    vC = w_in[:, 6656 + core * 128:6656 + core * 128 + 128]
    w_t = np.ascontiguousarray(np.concatenate([qA, kA, kA, qB, kB, qC, kC], axis=1), np.float32)
    w_v = np.ascontiguousarray(np.concatenate([vA, vB, vC], axis=1), np.float32)
    cf, cb = phaseA_consts2(core, S)
    sm = np.zeros((1, A_SM), np.float32)
    sm[0, 0:2] = sinks[h0:h0 + 2]
    sm[0, 2:66] = lq1
    sm[0, 66:130] = lk1
    sm[0, 130:194] = lq2
    sm[0, 194:258] = lk2
    sm[0, 258:386] = subg
    sm[0, 386] = lam_init
    sm[0, 387] = 1.0 - lam_init
    return {"x": np.ascontiguousarray(x, np.float32), "mod": np.ascontiguousarray(modA, np.float32), "w_t": w_t,
            "w_v": w_v, "cf": cf, "cb": cb, "sm": sm}


def emit_layernorm(p, r, t_r, xo, t_xo, gB, bB, t_c, stats, t_st):
    for j in range(4):
        p.op("dve", lambda e, j=j: e.bn_stats(out=stats[:, j * 6:(j + 1) * 6], in_=r[:, j * 512:(j + 1) * 512]),
             reads=[t_r], writes=[t_st])
    p.op("dve", lambda e: e.bn_aggr(out=stats[:, 24:26], in_=stats[:, 0:24]), reads=[t_st], writes=[t_st])
    p.op("act", lambda e: e.activation(out=stats[:, 26:27], in_=stats[:, 25:26], func=AF.Ln, bias=LN_EPS, scale=1.0),
         reads=[t_st], writes=[t_st])
    p.op("act", lambda e: e.activation(out=stats[:, 27:28], in_=stats[:, 26:27], func=AF.Exp, scale=-0.5),
         reads=[t_st], writes=[t_st])
    p.op("dve", lambda e: e.tensor_scalar(out=r[:], in0=r[:], scalar1=stats[:, 24:25], scalar2=stats[:, 27:28],
                                          op0=ALU.subtract, op1=ALU.mult), reads=[t_st, t_r], writes=[t_r])
    p.op("pool", lambda e: e.tensor_tensor(out=r[:], in0=r[:], in1=gB[:], op=ALU.mult), reads=[t_r, t_c], writes=[t_r])
    p.op("dve", lambda e: e.tensor_tensor(out=xo[:], in0=r[:], in1=bB[:], op=ALU.add), reads=[t_r, t_c], writes=[t_xo])


TB = SEQ // NCORE


def build_phaseB(TBK=TB):
    NH = TBK // 512
    nc = bass.Bass("TRN2", target_bir_lowering=False)
    x = nc.dram_tensor("x", [TBK, D], F32, kind="ExternalInput").ap()
    o = nc.dram_tensor("o", [TBK, 3072], F32, kind="ExternalInput").ap()
    mod = nc.dram_tensor("mod", [6, D], F32, kind="ExternalInput").ap()
    wg_d = nc.dram_tensor("wg", [48, 128, DC * 128], F32, kind="ExternalInput").ap()
    bg_d = nc.dram_tensor("bg", [6144], F32, kind="ExternalInput").ap()
    wb_d = nc.dram_tensor("wb", [48, 128, 8 * 128], F32, kind="ExternalInput").ap()
    wo_d = nc.dram_tensor("wo", [4, DC, 128, 512], F32, kind="ExternalInput").ap()
    ln_d = nc.dram_tensor("ln", [2, D], F32, kind="ExternalInput").ap()
    wr_d = nc.dram_tensor("wr", [D, 72], F32, kind="ExternalInput").ap()
    br_d = nc.dram_tensor("br", [72], F32, kind="ExternalInput").ap()
    id_d = nc.dram_tensor("ident", [128, 128], F32, kind="ExternalInput").ap()
    x1_d = nc.dram_tensor("x1", [TBK, D], F32, kind="ExternalOutput").ap()
    h2_d = nc.dram_tensor("h2T", [D, TBK], BF16, kind="ExternalOutput").ap()
    rw_d = nc.dram_tensor("rw", [TBK, 64], F32, kind="ExternalOutput").ap()
    with ExitStack() as st:
        p = Prog(nc, st)
        sb = lambda name, shape, dt: _sb(nc, st, name, shape, dt)
        hT = sb("hT", [128, DC, 512], BF16)
        oT = sb("oT", [128, 24, 512], BF16)
        zT = sb("zT", [128, DC, 512], BF16)
        stg = sb("stg", [128, 4096], F32)
        wg_r = Rot([sb("wg%d" % i, [128, DC * 128], BF16) for i in range(3)])
        wb_r = Rot([sb("wb%d" % i, [128, 8 * 128], BF16) for i in range(3)])
        wo = sb("wo", [128, DC, 512], BF16)
        g1B = sb("g1B", [128, D], F32)
        lgB = sb("lgB", [128, D], F32)
        lbB = sb("lbB", [128, D], F32)
        r_r = Rot([sb("r%d" % i, [128, D], F32) for i in range(4)])
        x1t = sb("x1t", [128, D], F32)
        xres = sb("xres", [128, 512], F32)
        h2f = sb("h2f", [128, DC, 128], F32)
        h2b = sb("h2b", [128, DC, 128], BF16)
        wr = sb("wr", [128, DC, 72], F32)
        brB = sb("brB", [128, 72], F32)
        ident = sb("ident", [128, 128], F32)
        modT = sb("modT", [128, 6, DC], F32)
        bgT = sb("bgT", [128, 48], F32)
        gs_r = Rot([sb("gs%d" % i, [128, 512], F32) for i in range(2)])
        zt_r = Rot([sb("zt%d" % i, [128, 512], F32) for i in range(2)])
        zacc = sb("zacc", [128, 512], F32)
        stats = sb("stats", [128, 32], F32)
        rt = sb("rt", [128, 512], F32)
        banks = [_ps(nc, st, "bk%d" % i, [128, 512], F32) for i in range(8)]
        scr = Rot(banks)
        t_c, t_stg, t_hT, t_oT, t_zT, t_wo, t_x1, t_xres, t_h2, t_st, t_rt, t_zacc = [Tk() for _ in range(12)]

        p.dma("sp", ident[:], id_d[:, :], writes=[t_c])
        p.dma("sp", modT[:], mod.rearrange("m (k p) -> p m k", p=128), writes=[t_c], slow=True)
        p.dma("sp", bgT[:], bg_d.rearrange("(j p) -> p j", p=128), writes=[t_c], slow=True)
        p.dma("sp", g1B[:], mod[2, :].partition_broadcast(128), writes=[t_c])
        p.dma("sp", lgB[:], ln_d[0, :].partition_broadcast(128), writes=[t_c])
        p.dma("sp", lbB[:], ln_d[1, :].partition_broadcast(128), writes=[t_c])
        p.dma("sp", brB[:], br_d.partition_broadcast(128), writes=[t_c])
        p.dma("sp", wr[:], wr_d.rearrange("(k p) n -> p k n", p=128), writes=[t_c])

        for hf in range(NH):
            for t in range(4):
                r0 = hf * 512 + t * 128
                p.dma("sp", stg[:, 0:D], x[r0:r0 + 128, :], writes=[t_stg])
                for c4 in range(4):
                    bk, tk = scr.next()
                    for j in range(4):
                        c = c4 * 4 + j
                        p.op("pe", lambda e, bk=bk, j=j, c=c: e.transpose(
                            bk[:, j * 128:(j + 1) * 128], stg[:, c * 128:(c + 1) * 128], ident[:]),
                            reads=[t_stg, t_c], writes=[tk])
                    for j in range(4):
                        c = c4 * 4 + j
                        p.op("act", lambda e, bk=bk, j=j, c=c, t=t: e.activation(
                            out=hT[:, c, t * 128:(t + 1) * 128], in_=bk[:, j * 128:(j + 1) * 128], func=AF.Identity,
                            bias=modT[:, 1, c:c + 1], scale=modT[:, 0, c:c + 1]), reads=[tk, t_c], writes=[t_hT])
                p.dma("sp", stg[:, 0:3072], o[r0:r0 + 128, :], writes=[t_stg])
                for c4 in range(6):
                    bk, tk = scr.next()
                    for j in range(4):
                        c = c4 * 4 + j
                        p.op("pe", lambda e, bk=bk, j=j, c=c: e.transpose(
                            bk[:, j * 128:(j + 1) * 128], stg[:, c * 128:(c + 1) * 128], ident[:]),
                            reads=[t_stg, t_c], writes=[tk])
                    p.op("dve", lambda e, bk=bk, c4=c4, t=t: e.tensor_copy(
                        out=oT[:, c4 * 4:c4 * 4 + 4, t * 128:(t + 1) * 128],
                        in_=bk[:, :].rearrange("p (j q) -> p j q", q=128)), reads=[tk], writes=[t_oT])
            blocks = [(dc, n) for dc in range(DC) for n in range(3)]
            wbufs = {}

            def issue(bi):
                dc, n = blocks[bi]
                blk = n * DC + dc
                wgb, wgtk = wg_r.next()
                p.dma("pool", wgb[:], wg_d[blk, :, :], writes=[wgtk])
                wbb, wbtk = wb_r.next()
                p.dma("pool", wbb[:], wb_d[blk, :, :], writes=[wbtk])
                wbufs[bi] = (wgb, wgtk, wbb, wbtk)

            issue(0)
            issue(1)
            for bi, (dc, n) in enumerate(blocks):
                if bi + 2 < len(blocks):
                    issue(bi + 2)
                blk = n * DC + dc
                wgb, wgtk, wbb, wbtk = wbufs.pop(bi)
                gbk, gtk = scr.next()
                for k in range(DC):
                    p.op("pe", lambda e, gbk=gbk, wgb=wgb, k=k: e.matmul(
                        gbk[:, :], wgb[:, k * 128:(k + 1) * 128], hT[:, k, :], start=(k == 0), stop=(k == DC - 1)),
                        reads=[wgtk, t_hT], writes=[gtk])
                gs, gstk = gs_r.next()
                p.op("act", lambda e, gbk=gbk, gs=gs, blk=blk: e.activation(
                    out=gs[:], in_=gbk[:, :], func=AF.Sigmoid, bias=bgT[:, blk:blk + 1], scale=1.0),
                    reads=[gtk, t_c], writes=[gstk])
                ybk, ytk = scr.next()
                for f in range(8):
                    p.op("pe", lambda e, ybk=ybk, wbb=wbb, f=f, n=n: e.matmul(
                        ybk[:, :], wbb[:, f * 128:(f + 1) * 128], oT[:, n * 8 + f, :], start=(f == 0), stop=(f == 7)),
                        reads=[wbtk, t_oT], writes=[ytk])
                if n == 0:
                    p.op("dve", lambda e, ybk=ybk, gs=gs: e.tensor_tensor(out=zacc[:], in0=ybk[:, :], in1=gs[:], op=ALU.mult),
                         reads=[ytk, gstk], writes=[t_zacc])
                else:
                    zt, zttk = zt_r.next()
                    p.op("dve", lambda e, ybk=ybk, gs=gs, zt=zt: e.tensor_tensor(out=zt[:], in0=ybk[:, :], in1=gs[:], op=ALU.mult),
                         reads=[ytk, gstk], writes=[zttk])
                    if n == 1:
                        p.op("dve", lambda e, zt=zt: e.tensor_tensor(out=zacc[:], in0=zacc[:], in1=zt[:], op=ALU.add),
                             reads=[zttk, t_zacc], writes=[t_zacc])
                    else:
                        p.op("dve", lambda e, zt=zt, dc=dc: e.tensor_tensor(out=zT[:, dc, :], in0=zacc[:], in1=zt[:], op=ALU.add),
                             reads=[zttk, t_zacc], writes=[t_zT])
            rts = [r_r.next() for _ in range(4)]
            for cbk in range(4):
                for k in range(DC):
                    p.dma("pool", wo[:, k, :], wo_d[cbk, k, :, :], writes=[t_wo])
                for t in range(4):
                    r0 = hf * 512 + t * 128
                    rb, rtk = rts[t]
                    bk, tk = scr.next()
                    for k in range(DC):
                        p.op("pe", lambda e, bk=bk, k=k, t=t: e.matmul(
                            bk[:, :], zT[:, k, t * 128:(t + 1) * 128], wo[:, k, :], start=(k == 0), stop=(k == DC - 1)),
                            reads=[t_zT, t_wo], writes=[tk])
                    p.dma("sp", xres[:], x[r0:r0 + 128, cbk * 512:(cbk + 1) * 512], writes=[t_xres])
                    p.op("dve", lambda e, bk=bk, rb=rb, cbk=cbk: e.tensor_tensor(
                        out=rb[:, cbk * 512:(cbk + 1) * 512], in0=bk[:, :], in1=g1B[:, cbk * 512:(cbk + 1) * 512], op=ALU.mult),
                        reads=[tk, t_c], writes=[rtk])
                    p.op("dve", lambda e, rb=rb, cbk=cbk: e.scalar_tensor_tensor(
                        out=rb[:, cbk * 512:(cbk + 1) * 512], in0=xres[:], scalar=ALPHA, in1=rb[:, cbk * 512:(cbk + 1) * 512],
                        op0=ALU.mult, op1=ALU.add), reads=[t_xres, rtk], writes=[rtk])
            for t in range(4):
                r0 = hf * 512 + t * 128
                rb, rtk = rts[t]
                emit_layernorm(p, rb, rtk, x1t, t_x1, lgB, lbB, t_c, stats, t_st)
                p.dma("sp", x1_d[r0:r0 + 128, :], x1t[:], reads=[t_x1], is_out=True)
                for c4 in range(4):
                    bk, tk = scr.next()
                    for j in range(4):
                        c = c4 * 4 + j
                        p.op("pe", lambda e, bk=bk, j=j, c=c: e.transpose(
                            bk[:, j * 128:(j + 1) * 128], x1t[:, c * 128:(c + 1) * 128], ident[:]),
                            reads=[t_x1, t_c], writes=[tk])
                    for j in range(4):
                        c = c4 * 4 + j
                        p.op("act", lambda e, bk=bk, j=j, c=c: e.activation(
                            out=h2f[:, c, :], in_=bk[:, j * 128:(j + 1) * 128], func=AF.Identity,
                            bias=modT[:, 4, c:c + 1], scale=modT[:, 3, c:c + 1]), reads=[tk, t_c], writes=[t_h2])
                p.op("pool", lambda e: e.tensor_copy(out=h2b[:], in_=h2f[:]), reads=[t_h2], writes=[t_h2])
                p.dma("sp", h2_d[:, r0:r0 + 128].rearrange("(k p) t -> p k t", p=128), h2b[:], reads=[t_h2], is_out=True)
                lbk, ltk = scr.next()
                for k in range(DC):
                    p.op("pe", lambda e, lbk=lbk, k=k: e.matmul(lbk[:, 0:72], h2f[:, k, :], wr[:, k, :], start=(k == 0),
                                                                  stop=(k == DC - 1)), reads=[t_h2, t_c], writes=[ltk])
                R = rt
                T = [t_rt]
                p.op("dve", lambda e, lbk=lbk: e.tensor_tensor(out=R[:, 0:72], in0=lbk[:, 0:72], in1=brB[:], op=ALU.add),
                     reads=[ltk, t_c], writes=T)
                p.op("dve", lambda e: e.reduce_max(out=R[:, 72:73], in_=R[:, 0:8], axis=AX.X), reads=T, writes=T)
                p.op("dve", lambda e: e.tensor_scalar(out=R[:, 73:74], in0=R[:, 72:73], scalar1=-1.0, scalar2=None, op0=ALU.mult),
                     reads=T, writes=T)
                p.op("act", lambda e: e.activation(out=R[:, 80:88], in_=R[:, 0:8], func=AF.Exp, bias=R[:, 73:74], scale=1.0),
                     reads=T, writes=T)
                p.op("dve", lambda e: e.reduce_sum(out=R[:, 88:89], in_=R[:, 80:88], axis=AX.X), reads=T, writes=T)
                p.op("dve", lambda e: e.reciprocal(out=R[:, 89:90], in_=R[:, 88:89]), reads=T, writes=T)
                p.op("dve", lambda e: e.tensor_scalar(out=R[:, 96:104], in0=R[:, 0:8], scalar1=R[:, 72:73], scalar2=None,
                                                      op0=ALU.is_ge), reads=T, writes=T)
                p.op("dve", lambda e: e.tensor_scalar(out=R[:, 104:112], in0=R[:, 96:104], scalar1=-1.0, scalar2=30000.0,
                                                      op0=ALU.add, op1=ALU.mult), reads=T, writes=T)
                for gi in range(8):
                    p.op("dve", lambda e, gi=gi: e.tensor_scalar(
                        out=R[:, 128 + gi * 8:136 + gi * 8], in0=R[:, 8 + gi * 8:16 + gi * 8], scalar1=R[:, 104 + gi:105 + gi],
                        scalar2=None, op0=ALU.add), reads=T, writes=T)
                p.op("dve", lambda e: e.max(out=R[:, 192:200], in_=R[:, 128:192]), reads=T, writes=T)
                p.op("dve", lambda e: e.tensor_tensor(out=R[:, 200:201], in0=R[:, 192:193], in1=R[:, 193:194], op=ALU.subtract),
                     reads=T, writes=T)
                p.op("act", lambda e: e.activation(out=R[:, 201:202], in_=R[:, 200:201], func=AF.Sigmoid), reads=T, writes=T)
                p.op("dve", lambda e: e.tensor_tensor(out=R[:, 202:203], in0=R[:, 201:202], in1=R[:, 89:90], op=ALU.mult),
                     reads=T, writes=T)
                p.op("dve", lambda e: e.tensor_tensor(out=R[:, 203:204], in0=R[:, 89:90], in1=R[:, 202:203], op=ALU.subtract),
                     reads=T, writes=T)
                p.op("dve", lambda e: e.tensor_tensor(out=R[:, 204:205], in0=R[:, 202:203], in1=R[:, 203:204], op=ALU.subtract),
                     reads=T, writes=T)
                p.op("dve", lambda e: e.tensor_scalar(out=R[:, 256:320], in0=R[:, 128:192], scalar1=R[:, 193:194], scalar2=R[:, 203:204],
                                                      op0=ALU.is_ge, op1=ALU.mult), reads=T, writes=T)
                p.op("dve", lambda e: e.tensor_scalar(out=R[:, 320:384], in0=R[:, 128:192], scalar1=R[:, 192:193], scalar2=R[:, 204:205],
                                                      op0=ALU.is_ge, op1=ALU.mult), reads=T, writes=T)
                p.op("dve", lambda e: e.tensor_tensor(out=R[:, 384:448], in0=R[:, 256:320], in1=R[:, 320:384], op=ALU.add),
                     reads=T, writes=T)
                p.dma("sp", rw_d[r0:r0 + 128, :], R[:, 384:448], reads=T, is_out=True)
        p.finish()
    return nc


def phaseB_weights(w_gate, b_gate, w_branch, w_out, ln_g, ln_b, w_rg, b_rg, w_re, b_re):
    wg = np.ascontiguousarray(w_gate.reshape(DC, 128, 48, 128).transpose(2, 1, 0, 3).reshape(48, 128, DC * 128))
    wb = np.ascontiguousarray(w_branch.reshape(3, 8, 128, DC, 128).transpose(0, 3, 2, 1, 4).reshape(48, 128, 8 * 128))
    wo = np.ascontiguousarray(w_out.reshape(DC, 128, 4, 512).transpose(2, 0, 1, 3))
    return {"wg": wg, "bg": np.ascontiguousarray(b_gate), "wb": wb, "wo": wo,
            "ln": np.ascontiguousarray(np.stack([ln_g, ln_b])),
            "wr": np.ascontiguousarray(np.concatenate([w_rg, w_re], axis=1)),
            "br": np.ascontiguousarray(np.concatenate([b_rg, b_re])),
            "ident": np.eye(128, dtype=np.float32)}


FE = 384


def build_phaseC(S):
    NT = S // 1024
    nc = bass.Bass("TRN2", target_bir_lowering=False)
    h2_d = nc.dram_tensor("h2T", [D, S], BF16, kind="ExternalInput").ap()
    rw_d = nc.dram_tensor("rw", [S, 8], F32, kind="ExternalInput").ap()
    wgu_d = nc.dram_tensor("wgu", [8, 2, 128, DC * FE], F32, kind="ExternalInput").ap()
    wd_d = nc.dram_tensor("wd", [8, 128, 3 * D], F32, kind="ExternalInput").ap()
    y_d = nc.dram_tensor("y", [S, D], BF16, kind="ExternalOutput").ap()
    with ExitStack() as st:
        p = Prog(nc, st)
        sb = lambda name, shape, dt: _sb(nc, st, name, shape, dt)
        h2 = sb("h2", [128, DC, 1024], BF16)
        rwt = sb("rwt", [128, 8, 8], F32)
        Yacc = sb("Yacc", [128, 8, D], F32)
        wg_r = Rot([sb("wgt%d" % i, [128, DC * FE], BF16) for i in range(2)])
        wu_r = Rot([sb("wut%d" % i, [128, DC * FE], BF16) for i in range(2)])
        wd_r = Rot([sb("wdt%d" % i, [128, 3 * D], BF16) for i in range(2)])
        A = sb("A", [128, 3, 1024], BF16)
        sg_r = Rot([sb("sg%d" % i, [128, 512], F32) for i in range(2)])
        banks = [_ps(nc, st, "bk%d" % i, [128, 512], F32) for i in range(8)]
        scr = Rot(banks)
        t_h2, t_rw, t_A = Tk(), Tk(), Tk()
        t_y = [Tk() for _ in range(8)]
        for tc in range(NT):
            t0 = tc * 1024
            p.dma("sp", h2[:], h2_d[:, t0:t0 + 1024].rearrange("(k p) t -> p k t", p=128), writes=[t_h2])
            p.dma("sp", rwt[:], rw_d[t0:t0 + 1024, :].rearrange("(t p) e -> p t e", p=128), writes=[t_rw])
            for ex in range(8):
                wgb, wgtk = wg_r.next()
                wub, wutk = wu_r.next()
                wdb, wdtk = wd_r.next()
                for j in range(3):
                    p.dma("pool", wgb[:, j * 2048:(j + 1) * 2048], wgu_d[ex, 0, :, j * 2048:(j + 1) * 2048], writes=[wgtk])
                    p.dma("pool", wub[:, j * 2048:(j + 1) * 2048], wgu_d[ex, 1, :, j * 2048:(j + 1) * 2048], writes=[wutk])
                for j in range(3):
                    p.dma("pool", wdb[:, j * 2048:(j + 1) * 2048], wd_d[ex, :, j * 2048:(j + 1) * 2048], writes=[wdtk])
                for f in range(3):
                    for nh in range(2):
                        gbk, gtk = scr.next()
                        for k in range(DC):
                            p.op("pe", lambda e, gbk=gbk, wgb=wgb, k=k, f=f, nh=nh: e.matmul(
                                gbk[:, :], wgb[:, k * FE + f * 128:k * FE + (f + 1) * 128], h2[:, k, nh * 512:(nh + 1) * 512],
                                start=(k == 0), stop=(k == DC - 1)), reads=[wgtk, t_h2], writes=[gtk])
                        ubk, utk = scr.next()
                        for k in range(DC):
                            p.op("pe", lambda e, ubk=ubk, wub=wub, k=k, f=f, nh=nh: e.matmul(
                                ubk[:, :], wub[:, k * FE + f * 128:k * FE + (f + 1) * 128], h2[:, k, nh * 512:(nh + 1) * 512],
                                start=(k == 0), stop=(k == DC - 1)), reads=[wutk, t_h2], writes=[utk])
                        sg, sgtk = sg_r.next()
                        p.op("act", lambda e, gbk=gbk, sg=sg: e.activation(out=sg[:], in_=gbk[:, :], func=AF.Silu),
                             reads=[gtk], writes=[sgtk])
                        p.op("dve", lambda e, ubk=ubk, sg=sg, f=f, nh=nh: e.tensor_tensor(
                            out=A[:, f, nh * 512:(nh + 1) * 512], in0=ubk[:, :], in1=sg[:], op=ALU.mult),
                            reads=[utk, sgtk], writes=[t_A])
                for t in range(8):
                    for db in range(4):
                        ybk, ytk = scr.next()
                        for f in range(3):
                            p.op("pe", lambda e, ybk=ybk, wdb=wdb, f=f, t=t, db=db: e.matmul(
                                ybk[:, :], A[:, f, t * 128:(t + 1) * 128], wdb[:, f * D + db * 512:f * D + (db + 1) * 512],
                                start=(f == 0), stop=(f == 2)), reads=[t_A, wdtk], writes=[ytk])
                        if ex == 0:
                            p.op("dve", lambda e, ybk=ybk, t=t, db=db, ex=ex: e.tensor_scalar(
                                out=Yacc[:, t, db * 512:(db + 1) * 512], in0=ybk[:, :], scalar1=rwt[:, t, ex:ex + 1], scalar2=None,
                                op0=ALU.mult), reads=[ytk, t_rw], writes=[t_y[t]])
                        else:
                            p.op("dve", lambda e, ybk=ybk, t=t, db=db, ex=ex: e.scalar_tensor_tensor(
                                out=Yacc[:, t, db * 512:(db + 1) * 512], in0=ybk[:, :], scalar=rwt[:, t, ex:ex + 1],
                                in1=Yacc[:, t, db * 512:(db + 1) * 512], op0=ALU.mult, op1=ALU.add),
                                reads=[ytk, t_rw, t_y[t]], writes=[t_y[t]])
            for t in range(8):
                p.dma("pool", y_d[t0 + t * 128:t0 + (t + 1) * 128, :], Yacc[:, t, :], reads=[t_y[t]], is_out=True)
        p.finish()
    return nc


def phaseC_weights(w_g, w_u, w_d):
    g = w_g.reshape(8, DC, 128, FE).transpose(0, 2, 1, 3).reshape(8, 128, DC * FE)
    u = w_u.reshape(8, DC, 128, FE).transpose(0, 2, 1, 3).reshape(8, 128, DC * FE)
    wgu = np.ascontiguousarray(np.stack([g, u], axis=1))
    wd = np.ascontiguousarray(w_d.reshape(8, 3, 128, D).transpose(0, 2, 1, 3).reshape(8, 128, 3 * D))
    return {"wgu": wgu, "wd": wd}


def build_phaseD(TBK=TB):
    NTT = TBK // 128
    nc = bass.Bass("TRN2", target_bir_lowering=False)
    x1_d = nc.dram_tensor("x1", [TBK, D], F32, kind="ExternalInput").ap()
    yg_d = nc.dram_tensor("yg", [8, TBK, D], BF16, kind="ExternalInput").ap()
    mod = nc.dram_tensor("mod", [6, D], F32, kind="ExternalInput").ap()
    ln_d = nc.dram_tensor("ln", [2, D], F32, kind="ExternalInput").ap()
    x2_d = nc.dram_tensor("x2", [TBK, D], F32, kind="ExternalOutput").ap()
    with ExitStack() as st:
        p = Prog(nc, st)
        sb = lambda name, shape, dt: _sb(nc, st, name, shape, dt)
        g2B = sb("g2B", [128, D], F32)
        lgB = sb("lgB", [128, D], F32)
        lbB = sb("lbB", [128, D], F32)
        yt_r = Rot([sb("yt%d" % i, [128, 8, D], BF16) for i in range(2)])
        x_r = Rot([sb("xt%d" % i, [128, D], F32) for i in range(2)])
        r_r = Rot([sb("rr%d" % i, [128, D], F32) for i in range(2)])
        o_r = Rot([sb("xo%d" % i, [128, D], F32) for i in range(2)])
        stats = sb("stats", [128, 32], F32)
        t_c, t_st = Tk(), Tk()
        p.dma("sp", g2B[:], mod[5, :].partition_broadcast(128), writes=[t_c])
        p.dma("sp", lgB[:], ln_d[0, :].partition_broadcast(128), writes=[t_c])
        p.dma("sp", lbB[:], ln_d[1, :].partition_broadcast(128), writes=[t_c])
        for t in range(NTT):
            r0 = t * 128
            yt, yttk = yt_r.next()
            p.dma("sp", yt[:], yg_d[:, r0:r0 + 128, :].rearrange("g p d -> p g d"), writes=[yttk])
            xt, xtk = x_r.next()
            p.dma("sp", xt[:], x1_d[r0:r0 + 128, :], writes=[xtk])
            rb, rtk = r_r.next()
            p.op("dve", lambda e, rb=rb, yt=yt: e.tensor_tensor(out=rb[:], in0=yt[:, 0, :], in1=yt[:, 1, :], op=ALU.add),
                 reads=[yttk], writes=[rtk])
            for gi in range(2, 8):
                eng = "dve" if gi % 2 == 0 else "pool"
                p.op(eng, lambda e, rb=rb, yt=yt, gi=gi: e.tensor_tensor(out=rb[:], in0=rb[:], in1=yt[:, gi, :], op=ALU.add),
                     reads=[yttk, rtk], writes=[rtk])
            p.op("pool", lambda e, rb=rb: e.tensor_tensor(out=rb[:], in0=rb[:], in1=g2B[:], op=ALU.mult),
                 reads=[rtk, t_c], writes=[rtk])
            p.op("dve", lambda e, rb=rb, xt=xt: e.scalar_tensor_tensor(out=rb[:], in0=xt[:], scalar=ALPHA, in1=rb[:],
                                                                      op0=ALU.mult, op1=ALU.add),
                 reads=[xtk, rtk], writes=[rtk])
            xo, xotk = o_r.next()
            emit_layernorm(p, rb, rtk, xo, xotk, lgB, lbB, t_c, stats, t_st)
            p.dma("sp", x2_d[r0:r0 + 128, :], xo[:], reads=[xotk], is_out=True)
        p.finish()
    return nc


def _run(nc, maps):
    res = run_bass_kernel_spmd(nc, maps, core_ids=list(range(NCORE)))
    return res.results


def kernel(x, c, w_ada, b_ada, w_in, w_branch_gate, b_branch_gate, attn_sinks, lambda_q1, lambda_k1, lambda_q2,
           lambda_k2, subln_g, w_branch, w_out, ln1_g, ln1_b, w_router_group, b_router_group, w_router_expert,
           b_router_expert, w_exp_gate, w_exp_up, w_exp_down, ln2_g, ln2_b):
    f32 = lambda a: np.asarray(a, np.float32)
    xs = np.ascontiguousarray(f32(x)[0])
    mod_all = run_phase0(f32(c), f32(w_ada), f32(b_ada))
    ncA = build_phaseA2(SEQ)
    ncB = build_phaseB(TB)
    ncC = [None]
    ncC2 = {}
    ncD = build_phaseD(TB)
    for l in range(DEPTH):
        lam_init = 0.8 - 0.6 * math.exp(-0.3 * l)
        m = mod_all[l].reshape(6, D)
        mod6 = np.ascontiguousarray(np.stack([m[1], m[0], m[2], m[4], m[3], m[5]]))
        maps = [phaseA_inputs(i, SEQ, xs, mod6[0:2], f32(w_in[l]), f32(attn_sinks[l]), f32(lambda_q1[l]),
                              f32(lambda_k1[l]), f32(lambda_q2[l]), f32(lambda_k2[l]), f32(subln_g[l]), lam_init)
                for i in range(NCORE)]
        ra = _run(ncA, maps)
        o_cat = np.empty((SEQ, 3, 1024), np.float32)
        for i in range(NCORE):
            o_cat[:, :, 128 * i:128 * (i + 1)] = ra[i]["o"].reshape(SEQ, 3, 128)
        o_cat = o_cat.reshape(SEQ, 3072)
        del ra, maps
        wts = phaseB_weights(f32(w_branch_gate[l]), f32(b_branch_gate[l]), f32(w_branch[l]), f32(w_out[l]), f32(ln1_g[l]),
                             f32(ln1_b[l]), f32(w_router_group[l]), f32(b_router_group[l]), f32(w_router_expert[l]),
                             f32(b_router_expert[l]))
        maps = []
        for i in range(NCORE):
            mm = dict(wts)
            mm.update({"x": np.ascontiguousarray(xs[i * TB:(i + 1) * TB]),
                       "o": np.ascontiguousarray(o_cat[i * TB:(i + 1) * TB]), "mod": mod6})
            maps.append(mm)
        rb = _run(ncB, maps)
        x1 = [rb[i]["x1"] for i in range(NCORE)]
        h2T = np.ascontiguousarray(np.concatenate([rb[i]["h2T"] for i in range(NCORE)], axis=1))
        rw = np.concatenate([rb[i]["rw"] for i in range(NCORE)], axis=0)
        del rb, maps, wts, o_cat
        sizes = [int(np.count_nonzero(rw[:, 8 * g:8 * g + 8].max(axis=1) > 0)) for g in range(NCORE)]
        cap = max(1024, -(-max(sizes) // 1024) * 1024)
        print("[kernel] layer", l, "group sizes", sizes, "cap", cap, flush=True)
        maps = []
        if cap <= 3072:
            if cap not in ncC2:
                ncC2[cap] = build_phaseC2(SEQ, cap)
            x1_full = np.ascontiguousarray(np.concatenate(x1, axis=0))
            c2c = phaseC2_consts(SEQ)
            modC = np.ascontiguousarray(mod6[3:5])
            for g in range(NCORE):
                mm = phaseC_weights(f32(w_exp_gate[l, 8 * g:8 * g + 8]), f32(w_exp_up[l, 8 * g:8 * g + 8]),
                                    f32(w_exp_down[l, 8 * g:8 * g + 8]))
                mm.update(c2c)
                mm.update({"x1": x1_full, "rw": np.ascontiguousarray(rw[:, 8 * g:8 * g + 8]), "mod": modC})
                maps.append(mm)
            rc = _run(ncC2[cap], maps)
            del x1_full
        else:
            if ncC[0] is None:
                ncC[0] = build_phaseC(SEQ)
            for g in range(NCORE):
                mm = phaseC_weights(f32(w_exp_gate[l, 8 * g:8 * g + 8]), f32(w_exp_up[l, 8 * g:8 * g + 8]),
                                    f32(w_exp_down[l, 8 * g:8 * g + 8]))
                mm.update({"h2T": h2T, "rw": np.ascontiguousarray(rw[:, 8 * g:8 * g + 8])})
                maps.append(mm)
            rc = _run(ncC[0], maps)
        del maps
        ln2 = np.ascontiguousarray(np.stack([f32(ln2_g[l]), f32(ln2_b[l])]))
        maps = []
        for i in range(NCORE):
            yg = np.ascontiguousarray(np.stack([rc[g]["y"][i * TB:(i + 1) * TB] for g in range(NCORE)]))
            maps.append({"x1": x1[i], "yg": yg, "mod": mod6, "ln": ln2})
        del rc
        rd = _run(ncD, maps)
        xs = np.ascontiguousarray(np.concatenate([rd[i]["x2"] for i in range(NCORE)], axis=0))
        del rd, maps
    return xs[None].astype(np.float32)


def phaseA_consts2(core, S):
    NQ = S // 128
    sl_swa = [2.0 ** (-8.0 * (h + 1) / 16.0) for h in (2 * core, 2 * core + 1)]
    sl_d = 2.0 ** (-8.0 * (core + 1) / 8.0)
    p = np.arange(128)[:, None].astype(np.float64)
    t = np.arange(128)[None, :].astype(np.float64)
    swab = np.zeros((128, 4, 128), np.float32)
    for hh in range(2):
        d1 = t - p
        swab[:, 2 * hh, :] = np.where(d1 >= 0, -sl_swa[hh] * d1, NEG)
        d0 = 128 + t - p
        swab[:, 2 * hh + 1, :] = np.where(d0 < 128, -sl_swa[hh] * d0, NEG)
    nj = NQ + 3
    jj = np.arange(nj)[None, :] - 3
    dbias = (sl_d * p - sl_d * 128.0 * jj).astype(np.float32)
    ident = np.eye(128, dtype=np.float32)
    cf = np.concatenate([swab.reshape(128, 512), dbias, ident], axis=1)
    tri = (p >= t).astype(np.float32)
    ones = np.ones((128, 128), np.float32)
    strict = (p < t).astype(np.float32)
    incl = (p <= t).astype(np.float32)
    upper = (p < t).astype(np.float32)
    tl = np.arange(512).astype(np.float64)
    v = (-8.0 * sl_d * tl)
    hi = v.astype(np.float32).astype(ml_dtypes.bfloat16)
    lo = (v - hi.astype(np.float64)).astype(np.float32).astype(ml_dtypes.bfloat16)
    qaug = np.zeros((128, 512), np.float32)
    qaug[0] = hi.astype(np.float32)
    qaug[1] = lo.astype(np.float32)
    qaug[64] = hi.astype(np.float32)
    qaug[65] = lo.astype(np.float32)
    cb = _bf(np.concatenate([tri, ones, strict, incl, qaug, upper], axis=1))
    return np.ascontiguousarray(cf), np.ascontiguousarray(cb)


def build_phaseA2(S, parts=("swa", "sb", "df"), ndummy=0):
    NQ = S // 128
    NG = S // 512
    NJ = NQ + 3
    NCF = 512 + NJ + 128
    nc = bass.Bass("TRN2", target_bir_lowering=False)
    x = nc.dram_tensor("x", [S, D], F32, kind="ExternalInput").ap()
    mod = nc.dram_tensor("mod", [2, D], F32, kind="ExternalInput").ap()
    w_t = nc.dram_tensor("w_t", [D, A_WT], F32, kind="ExternalInput").ap()
    w_v = nc.dram_tensor("w_v", [D, A_WV], F32, kind="ExternalInput").ap()
    cf_d = nc.dram_tensor("cf", [128, NCF], F32, kind="ExternalInput").ap()
    cb_d = nc.dram_tensor("cb", [128, 1152], BF16, kind="ExternalInput").ap()
    sm_d = nc.dram_tensor("sm", [1, A_SM], F32, kind="ExternalInput").ap()
    o_d = nc.dram_tensor("o", [S, 384], F32, kind="ExternalOutput").ap()
    with ExitStack() as st:
        p = Prog(nc, st)
        sb = lambda name, shape, dt: _sb(nc, st, name, shape, dt)
        wT = sb("wT", [128, DC, A_WT], BF16)
        wV = sb("wV", [128, DC, A_WV], BF16)
        KA = sb("KA", [128, S], BF16)
        KB = sb("KB", [128, S], BF16)
        KC = sb("KC", [128, S], BF16)
        V = sb("V", [128, NQ, VW], BF16)
        xs = sb("xs", [128, D], F32)
        hT = sb("hT", [128, DC, 512], BF16)
        QA = sb("QA", [128, 512], BF16)
        QB = sb("QB", [128, 512], BF16)
        QC = sb("QC", [128, 512], BF16)
        cf = sb("cfs", [128, NCF], F32)
        cb = sb("cbs", [128, 1152], BF16)
        sm = sb("sms", [128, A_SM], F32)
        sclT = sb("sclT", [128, DC], F32)
        shT = sb("shT", [128, DC], F32)
        misc = sb("misc", [128, 16], F32)
        gsub = sb("gsub", [128, 128], F32)
        junk = sb("junk", [128, 128], F32)
        ost = [sb("ost%d" % i, [128, 4, 384], F32) for i in range(2)]
        t_ost = [Tk(), Tk()]
        banks = [_ps(nc, st, "bk%d" % i, [128, 512], F32) for i in range(8)]
        scr = Rot(banks[0:2])
        CARRY = banks[2:4]
        t_carry = [Tk(), Tk()]
        SBACC, t_sbacc = banks[4], Tk()
        DACC = banks[5:8]
        t_dacc = [Tk() for _ in range(3)]
        e_r = Rot([sb("e%d" % i, [128, 512], F32) for i in range(4)])
        L_r = Rot([sb("L%d" % i, [128, 512], BF16) for i in range(6)])
        W_r = Rot([sb("W%d" % i, [128, 512], BF16) for i in range(4)])
        E_r = Rot([sb("E%d" % i, [128, 512], BF16) for i in range(4)])
        X_r = Rot([sb("X%d" % i, [128, 512], BF16) for i in range(4)])
        sE_r = Rot([sb("sE%d" % i, [128, 512], BF16) for i in range(2)])
        fin_r = Rot([sb("fin%d" % i, [128, 8], F32) for i in range(2)])
        da_r = Rot([sb("da%d" % i, [128, 128], F32) for i in range(2)])
        dd_r = Rot([sb("dd%d" % i, [128, 128], F32) for i in range(2)])

        t_c = Tk()
        t_k = [Tk() for _ in range(NG)]
        t_xs, t_hT, t_q, t_misc = Tk(), Tk(), Tk(), Tk()
        t_junk = Tk()

        swab = cf[:, 0:512]
        dbias = lambda j: cf[:, 512 + j:512 + j + 1]
        ident = cf[:, 512 + NJ:512 + NJ + 128]
        tri = cb[:, 0:128]
        ones = cb[:, 128:256]
        strict = cb[:, 256:384]
        incl = cb[:, 384:512]
        qaug = cb[:, 512:1024]
        upper = cb[:, 1024:1152]

        p.dma("sp", cf[:], cf_d[:, :], writes=[t_c])
        p.dma("sp", cb[:], cb_d[:, :], writes=[t_c])
        p.dma("sp", sm[:], sm_d[0, :].partition_broadcast(128), writes=[t_c])
        p.dma("sp", sclT[:], mod[0, :].rearrange("(k p) -> p k", p=128), writes=[t_c], slow=True)
        p.dma("sp", shT[:], mod[1, :].rearrange("(k p) -> p k", p=128), writes=[t_c], slow=True)
        for k in range(DC):
            p.dma("pool", wT[:, k, :], w_t[k * 128:(k + 1) * 128, :], writes=[t_c])
            p.dma("pool", wV[:, k, :], w_v[k * 128:(k + 1) * 128, :], writes=[t_c])
        p.op("pool", lambda e: e.memset(V[:, :, 64:65], 1.0), writes=[t_c])
        p.op("pool", lambda e: e.memset(V[:, :, 321:322], 1.0), writes=[t_c])
        p.op("act", lambda e: e.activation(out=misc[:, 0:2], in_=sm[:, 0:2], func=AF.Exp), reads=[t_c], writes=[t_misc])
        p.op("dve", lambda e: e.tensor_tensor(out=junk[:, 0:64], in0=sm[:, 2:66], in1=sm[:, 66:130], op=ALU.mult),
             reads=[t_c], writes=[t_misc])
        p.op("dve", lambda e: e.reduce_sum(out=misc[:, 2:3], in_=junk[:, 0:64], axis=AX.X), reads=[t_misc], writes=[t_misc])
        p.op("dve", lambda e: e.tensor_tensor(out=junk[:, 64:128], in0=sm[:, 130:194], in1=sm[:, 194:258], op=ALU.mult),
             reads=[t_c], writes=[t_misc])
        p.op("dve", lambda e: e.reduce_sum(out=misc[:, 3:4], in_=junk[:, 64:128], axis=AX.X), reads=[t_misc], writes=[t_misc])
        p.op("act", lambda e: e.activation(out=misc[:, 5:7], in_=misc[:, 2:4], func=AF.Exp), reads=[t_misc], writes=[t_misc])
        p.op("dve", lambda e: e.tensor_tensor(out=misc[:, 7:8], in0=misc[:, 6:7], in1=misc[:, 5:6], op=ALU.subtract),
             reads=[t_misc], writes=[t_misc])
        p.op("dve", lambda e: e.tensor_tensor(out=misc[:, 4:5], in0=misc[:, 7:8], in1=sm[:, 386:387], op=ALU.subtract),
             reads=[t_misc, t_c], writes=[t_misc])
        p.op("dve", lambda e: e.tensor_scalar(out=gsub[:], in0=sm[:, 258:386], scalar1=sm[:, 387:388], scalar2=None,
                                              op0=ALU.mult), reads=[t_c], writes=[t_misc])

        def dacc(m, qi):
            j = m * 4 + qi
            return DACC[j // 3], t_dacc[j // 3], (j % 3) * 129, j // 3

        for g in range(NG):
            for tt in range(4):
                r0 = g * 512 + tt * 128
                p.dma("sp", xs[:], x[r0:r0 + 128, :], writes=[t_xs])
                for c4 in range(4):
                    bk, tk = scr.next()
                    for j in range(4):
                        c = c4 * 4 + j
                        p.op("pe", lambda e, bk=bk, j=j, c=c: e.transpose(
                            bk[:, j * 128:(j + 1) * 128], xs[:, c * 128:(c + 1) * 128], ident),
                            reads=[t_xs, t_c], writes=[tk])
                    for j in range(4):
                        c = c4 * 4 + j
                        p.op("dve", lambda e, bk=bk, j=j, c=c, tt=tt: e.tensor_scalar(
                            out=hT[:, c, tt * 128:(tt + 1) * 128], in0=bk[:, j * 128:(j + 1) * 128], scalar1=sclT[:, c:c + 1],
                            scalar2=shT[:, c:c + 1], op0=ALU.mult, op1=ALU.add), reads=[tk, t_c], writes=[t_hT])
            dests = [QA[:, :], KA[:, g * 512:(g + 1) * 512], QB[:, :], KB[:, g * 512:(g + 1) * 512],
                     QC[:, :], KC[:, g * 512:(g + 1) * 512]]
            for m in range(6):
                bk, tk = scr.next()
                for k in range(DC):
                    p.op("pe", lambda e, bk=bk, k=k, m=m: e.matmul(
                        bk[:, :], wT[:, k, m * 128:(m + 1) * 128], hT[:, k, :], start=(k == 0), stop=(k == DC - 1)),
                        reads=[t_c, t_hT], writes=[tk])
                wtk = t_q if m % 2 == 0 else t_k[g]
                p.op("act", lambda e, bk=bk, m=m, dests=dests: e.activation(out=dests[m], in_=bk[:, :], func=AF.Identity),
                     reads=[tk], writes=[wtk])
            for t in range(4):
                bk, tk = scr.next()
                for k in range(DC):
                    p.op("pe", lambda e, bk=bk, k=k, t=t: e.matmul(
                        bk[:, 0:A_WV], hT[:, k, t * 128:(t + 1) * 128], wV[:, k, :], start=(k == 0), stop=(k == DC - 1)),
                        reads=[t_c, t_hT], writes=[tk])
                qb = g * 4 + t
                p.op("dve", lambda e, bk=bk, qb=qb: e.tensor_copy(out=V[:, qb, 0:64], in_=bk[:, 0:64]),
                     reads=[tk], writes=[t_k[g]])
                p.op("dve", lambda e, bk=bk, qb=qb: e.tensor_copy(out=V[:, qb, 65:321], in_=bk[:, 64:320]),
                     reads=[tk], writes=[t_k[g]])

            o_t, o_tk = ost[g % 2], t_ost[g % 2]

            swst = {}

            def sw_a(qi):
                i = g * 4 + qi
                kbs = [i] + ([i - 1] if i > 0 else [])
                wd = 128 * len(kbs)
                tm, ttk = e_r.next()
                sE, setk = sE_r.next()
                segs = []
                for hh in range(2):
                    bk, tk = scr.next()
                    for n, kb in enumerate(kbs):
                        p.op("pe", lambda e, bk=bk, hh=hh, kb=kb, n=n: e.matmul(
                            bk[:, n * 128:(n + 1) * 128], KA[64 * hh:64 * hh + 64, kb * 128:(kb + 1) * 128],
                            QA[64 * hh:64 * hh + 64, qi * 128:(qi + 1) * 128], start=True, stop=True),
                            reads=[t_k[kb // 4], t_q], writes=[tk])
                        segs.append((hh, kb, 2 * hh + n))
                    p.op("dve", lambda e, bk=bk, hh=hh: e.scalar_tensor_tensor(
                        out=tm[:, 256 * hh:256 * hh + wd], in0=bk[:, 0:wd], scalar=SCALE, in1=swab[:, 256 * hh:256 * hh + wd],
                        op0=ALU.mult, op1=ALU.add), reads=[tk, t_c], writes=[ttk])
                    p.op("act", lambda e, hh=hh: e.activation(out=sE[:, 256 * hh:256 * hh + wd], in_=tm[:, 256 * hh:256 * hh + wd],
                                                              func=AF.Exp), reads=[ttk], writes=[setk])
                swst[qi] = (segs, sE, setk)

            def sw_b(qi):
                segs, sE, setk = swst.pop(qi)
                abk, atk = scr.next()
                first = True
                for hh in range(2):
                    mine = [s for s in segs if s[0] == hh]
                    for n, (_, kb, slot) in enumerate(mine):
                        p.op("pe", lambda e, hh=hh, kb=kb, slot=slot, first=first: e.matmul(
                            abk[:, hh * 65:hh * 65 + 65], sE[:, slot * 128:(slot + 1) * 128], V[:, kb, 0:65],
                            start=first, stop=True, skip_group_check=True),
                            reads=[setk, t_k[kb // 4], t_c], writes=[atk])
                        first = False
                fb, ftk = fin_r.next()
                for hh in range(2):
                    p.op("dve", lambda e, hh=hh: e.tensor_tensor(
                        out=fb[:, hh:hh + 1], in0=abk[:, hh * 65 + 64:hh * 65 + 65], in1=misc[:, hh:hh + 1], op=ALU.add),
                        reads=[atk, t_misc], writes=[ftk])
                p.op("dve", lambda e: e.reciprocal(out=fb[:, 2:4], in_=fb[:, 0:2]), reads=[ftk], writes=[ftk])
                for hh in range(2):
                    p.op("dve", lambda e, hh=hh, o_t=o_t: e.tensor_scalar(
                        out=o_t[:, qi, 64 * hh:64 * hh + 64], in0=abk[:, hh * 65:hh * 65 + 64], scalar1=fb[:, 2 + hh:3 + hh],
                        scalar2=None, op0=ALU.mult), reads=[atk, ftk], writes=[o_tk])

            if "swa" in parts:
                pipeline(4, [sw_a, sw_b])

            nst = 4 * g + 4
            sbst = {}
            dfst = {}
            dstarted = set()

            def sb_a(hh, s):
                kb = 4 * g + 3 - s
                qs = max(0, kb - 4 * g) * 128
                kg = t_k[kb // 4]
                bk, tk = scr.next()
                p.op("pe", lambda e: e.matmul(bk[:, qs:512], KB[64 * hh:64 * hh + 64, kb * 128:(kb + 1) * 128],
                                              QB[64 * hh:64 * hh + 64, qs:512], start=True, stop=True),
                     reads=[kg, t_q], writes=[tk])
                eb, etk = e_r.next()
                p.op("act", lambda e: e.activation(out=eb[:, qs:512], in_=bk[:, qs:512], func=AF.Exp, scale=SCALE),
                     reads=[tk], writes=[etk])
                Lb, Ltk = L_r.next()
                p.op("act", lambda e: e.activation(out=Lb[:, qs:512], in_=eb[:, qs:512], func=AF.Ln, bias=1.0, scale=1.0),
                     reads=[etk], writes=[Ltk])
                if kb >= 4 * g:
                    p.op("pool", lambda e: e.tensor_tensor(out=Lb[:, qs:qs + 128], in0=Lb[:, qs:qs + 128], in1=strict,
                                                           op=ALU.mult), reads=[Ltk, t_c], writes=[Ltk])
                sbst[(hh, s)] = dict(kb=kb, qs=qs, kg=kg, Lb=Lb, Ltk=Ltk, eb=eb, etk=etk)

            def sb_b(hh, s):
                d = sbst[(hh, s)]
                kb, qs, kg, Lb, Ltk, eb, etk = d["kb"], d["qs"], d["kg"], d["Lb"], d["Ltk"], d["eb"], d["etk"]
                cbk, ctk = CARRY[hh], t_carry[hh]
                p.op("pe", lambda e: e.matmul(cbk[:, qs:512], tri, Lb[:, qs:512], start=(s == 0), stop=True,
                                              skip_group_check=True), reads=[Ltk, t_c], writes=[ctk])
                Xb, Xtk = X_r.next()
                p.op("act", lambda e: e.activation(out=Xb[:, qs:512], in_=cbk[:, qs:512], func=AF.Exp, scale=-1.0),
                     reads=[ctk], writes=[Xtk])
                Wb, Wtk = W_r.next()
                p.op("dve", lambda e: e.tensor_tensor(out=Wb[:, qs:512], in0=eb[:, qs:512], in1=Xb[:, qs:512], op=ALU.mult),
                     reads=[etk, Xtk], writes=[Wtk])
                if kb >= 4 * g:
                    p.op("pool", lambda e: e.tensor_tensor(out=Wb[:, qs:qs + 128], in0=Wb[:, qs:qs + 128], in1=strict,
                                                           op=ALU.mult), reads=[Wtk, t_c], writes=[Wtk])
                d["Wb"], d["Wtk"] = Wb, Wtk

            def sb_c(hh, s):
                d = sbst.pop((hh, s))
                kb, qs, kg, Lb, Ltk, Wb, Wtk = d["kb"], d["qs"], d["kg"], d["Lb"], d["Ltk"], d["Wb"], d["Wtk"]
                cbk, ctk = CARRY[hh], t_carry[hh]
                if kb > 0:
                    p.op("pe", lambda e: e.matmul(cbk[:, qs:512], upper, Lb[:, qs:512], start=False, stop=True,
                                                  skip_group_check=True), reads=[Ltk, t_c], writes=[ctk])
                for qi in range(qs // 128, 4):
                    c0 = hh * 256 + qi * 64
                    p.op("pe", lambda e, qi=qi, c0=c0: e.matmul(
                        SBACC[:, c0:c0 + 64], Wb[:, qi * 128:(qi + 1) * 128], V[:, kb, 65 + 64 * hh:65 + 64 * hh + 64],
                        start=(s == 0 and hh == 0 and qi == 3), stop=(kb == 0), skip_group_check=True),
                        reads=[Wtk, kg], writes=[t_sbacc])
                if kb == 0:
                    p.op("dve", lambda e, o_t=o_t: e.tensor_copy(
                        out=o_t[:, :, 128 + 64 * hh:128 + 64 * hh + 64],
                        in_=SBACC[:, hh * 256:(hh + 1) * 256].rearrange("p (q d) -> p q d", d=64)),
                        reads=[t_sbacc], writes=[o_tk])

            def df_a(m, s):
                kb = s
                qs = max(0, kb - 4 * g) * 128
                kg = t_k[kb // 4]
                bk, tk = scr.next()
                p.op("pe", lambda e: e.matmul(bk[:, qs:512], KC[64 * m:64 * m + 64, kb * 128:(kb + 1) * 128],
                                              QC[64 * m:64 * m + 64, qs:512], start=True, stop=False),
                     reads=[kg, t_q], writes=[tk])
                p.op("pe", lambda e: e.matmul(bk[:, qs:512], ones[64 * m:64 * m + 2, :], qaug[64 * m:64 * m + 2, qs:512],
                                              start=False, stop=True), reads=[t_c], writes=[tk])
                Eb, Etk = E_r.next()
                jidx = (4 * g - kb) + 3
                p.op("act", lambda e: e.activation(out=Eb[:, qs:512], in_=bk[:, qs:512], func=AF.Exp, scale=SCALE,
                                                   bias=dbias(jidx)), reads=[tk, t_c], writes=[Etk])
                if kb >= 4 * g:
                    p.op("pool", lambda e: e.tensor_tensor(out=Eb[:, qs:qs + 128], in0=Eb[:, qs:qs + 128], in1=incl,
                                                           op=ALU.mult), reads=[Etk, t_c], writes=[Etk])
                dfst[(m, s)] = dict(kb=kb, qs=qs, kg=kg, Eb=Eb, Etk=Etk)

            def df_b(m, s):
                d = dfst.pop((m, s))
                kb, qs, kg, Eb, Etk = d["kb"], d["qs"], d["kg"], d["Eb"], d["Etk"]
                for qi in range(qs // 128, 4):
                    ab, atk, c0, bi = dacc(m, qi)
                    stt = bi not in dstarted
                    dstarted.add(bi)
                    p.op("pe", lambda e, qi=qi, ab=ab, c0=c0, stt=stt: e.matmul(
                        ab[:, c0:c0 + 129], Eb[:, qi * 128:(qi + 1) * 128], V[:, kb, 193:322],
                        start=stt, stop=(kb == 4 * g + qi), skip_group_check=True),
                        reads=[Etk, kg, t_c], writes=[atk])

            do_sb = "sb" in parts
            do_df = "df" in parts
            for step in range(nst + 2):
                if step < nst:
                    if do_sb:
                        sb_a(0, step)
                        sb_a(1, step)
                    if do_df:
                        df_a(0, step)
                        df_a(1, step)
                if 0 <= step - 2 < nst and do_sb:
                    sb_c(0, step - 2)
                    sb_c(1, step - 2)
                if 0 <= step - 1 < nst:
                    if do_sb:
                        sb_b(0, step - 1)
                        sb_b(1, step - 1)
                    if do_df:
                        df_b(0, step - 1)
                        df_b(1, step - 1)
                for _ in range(ndummy):
                    p.op("pe", lambda e: e.matmul(DACC[2][:, 260:508], tri, cb[:, 0:248], start=False, stop=True,
                                                  skip_group_check=True), reads=[t_c], writes=[t_junk])

            for qi in (range(4) if do_df else ()):
                a1, a1tk, c1, _ = dacc(0, qi)
                a2, a2tk, c2, _ = dacc(1, qi)
                fb, ftk = fin_r.next()
                p.op("dve", lambda e, fb=fb, a1=a1, c1=c1: e.reciprocal(out=fb[:, 0:1], in_=a1[:, c1 + 128:c1 + 129]),
                     reads=[a1tk], writes=[ftk])
                p.op("dve", lambda e, fb=fb, a2=a2, c2=c2: e.reciprocal(out=fb[:, 1:2], in_=a2[:, c2 + 128:c2 + 129]),
                     reads=[a2tk], writes=[ftk])
                p.op("dve", lambda e, fb=fb: e.tensor_tensor(out=fb[:, 2:3], in0=fb[:, 1:2], in1=misc[:, 4:5], op=ALU.mult),
                     reads=[ftk, t_misc], writes=[ftk])
                da, datk = da_r.next()
                p.op("dve", lambda e, fb=fb, a1=a1, c1=c1, da=da: e.tensor_scalar(
                    out=da[:], in0=a1[:, c1:c1 + 128], scalar1=fb[:, 0:1], scalar2=None, op0=ALU.mult),
                    reads=[a1tk, ftk], writes=[datk])
                dd, ddtk = dd_r.next()
                p.op("dve", lambda e, fb=fb, a2=a2, c2=c2, da=da, dd=dd: e.scalar_tensor_tensor(
                    out=dd[:], in0=a2[:, c2:c2 + 128], scalar=fb[:, 2:3], in1=da[:], op0=ALU.mult, op1=ALU.add),
                    reads=[a2tk, ftk, datk], writes=[ddtk])
                p.op("dve", lambda e, dd=dd, da=da: e.tensor_tensor(out=da[:], in0=dd[:], in1=dd[:], op=ALU.mult),
                     reads=[ddtk], writes=[datk])
                p.op("dve", lambda e, fb=fb, da=da: e.reduce_sum(out=fb[:, 3:4], in_=da[:], axis=AX.X),
                     reads=[datk], writes=[ftk])
                p.op("act", lambda e, fb=fb: e.activation(out=fb[:, 4:5], in_=fb[:, 3:4], func=AF.Ln, bias=RMS_EPS,
                                                          scale=1.0 / 128.0), reads=[ftk], writes=[ftk])
                p.op("act", lambda e, fb=fb: e.activation(out=fb[:, 5:6], in_=fb[:, 4:5], func=AF.Exp, scale=-0.5),
                     reads=[ftk], writes=[ftk])
                p.op("dve", lambda e, fb=fb, dd=dd, qi=qi, o_t=o_t: e.scalar_tensor_tensor(
                    out=o_t[:, qi, 256:384], in0=dd[:], scalar=fb[:, 5:6], in1=gsub[:], op0=ALU.mult, op1=ALU.mult),
                    reads=[ddtk, ftk, t_misc], writes=[o_tk])
            p.dma("sp", o_d[g * 512:(g + 1) * 512, :].rearrange("(q p) c -> p q c", p=128), o_t[:], reads=[o_tk],
                  is_out=True)
        p.finish()
    return nc


I32 = mybir.dt.int32
BIGPOS = 1.0e6


def _prog_dma_custom(p, q, fn, reads=(), writes=(), is_out=False):
    waits = p._deps(q, reads, writes, strict=True)
    n = getattr(p, "n_ind", 0)
    p.n_ind = n + 1
    sem = p.st.enter_context(p.nc.semaphore("ind%d" % n))
    ev = ("ind%d" % n, 16, sem)

    def run(eng, waits=waits, sem=sem, fn=fn):
        for s, v in waits:
            eng.wait_ge(s, v)
        fn(eng).then_inc(sem, 16)

    p.ops[q].append(run)
    p.meta[q].append(([(id(s_), v) for s_, v in waits], (id(sem), 16)))
    for t in reads:
        t.r.append(ev)
    for t in writes:
        t.w = ev
        t.r = []
    if is_out:
        p.outs.append(ev)


def build_phaseC2(S, CAP, NEX=8):
    J = S // 128
    NCH = CAP // 1024
    NK = CAP // 128
    nc = bass.Bass("TRN2", target_bir_lowering=False)
    x1_d = nc.dram_tensor("x1", [S, D], F32, kind="ExternalInput").ap()
    rw_d = nc.dram_tensor("rw", [S, 8], F32, kind="ExternalInput").ap()
    mod_d = nc.dram_tensor("mod", [2, D], F32, kind="ExternalInput").ap()
    wgu_d = nc.dram_tensor("wgu", [8, 2, 128, DC * FE], F32, kind="ExternalInput").ap()
    wd_d = nc.dram_tensor("wd", [8, 128, 3 * D], F32, kind="ExternalInput").ap()
    tid_d = nc.dram_tensor("tid", [128, J], F32, kind="ExternalInput").ap()
    cst_d = nc.dram_tensor("cst", [128, 384], F32, kind="ExternalInput").ap()
    y_d = nc.dram_tensor("y", [S, D], BF16, kind="ExternalOutput").ap()
    with ExitStack() as st:
        p = Prog(nc, st)
        sb = lambda name, shape, dt: _sb(nc, st, name, shape, dt)
        h2 = sb("h2", [128, DC, 1024], BF16)
        rwc = sb("rwc", [128, NK, 8], F32)
        idxc = sb("idxc", [128, NK], I32)
        RH = sb("RH", [128, J, 10], F32)
        tidf = sb("tidf", [128, J], F32)
        pk_r = Rot([sb("pk%d" % i, [128, J], F32) for i in range(2)])
        oh_r = Rot([sb("oh%d" % i, [128, 128], F32) for i in range(3)])
        Yacc = sb("Yacc", [128, 8, D], F32)
        wg_r = Rot([sb("wgt%d" % i, [128, DC * FE], BF16) for i in range(2)])
        wu_r = Rot([sb("wut%d" % i, [128, DC * FE], BF16) for i in range(2)])
        wd_r = Rot([sb("wdt%d" % i, [128, 3 * D], BF16) for i in range(2)])
        A = sb("A", [128, 3, 1024], BF16)
        sg_r = Rot([sb("sg%d" % i, [128, 512], F32) for i in range(2)])
        xg = sb("xg", [128, D], F32)
        R = sb("R", [128, J, 8], F32)
        sc = [sb("sc%d" % i, [128, J], F32) for i in range(2)]
        msk = sb("msk", [128, J], F32)
        posf = sb("posf", [128, J], F32)
        cst = sb("cst", [128, 384], F32)
        modT = sb("modT", [128, 2, DC], F32)
        offs = sb("offs", [128, 2], F32)
        zt = sb("zt", [128, D], BF16)
        banks = [_ps(nc, st, "bk%d" % i, [128, 512], F32) for i in range(8)]
        scr = Rot(banks)
        t_c, t_ix, t_h2, t_rw, t_A, t_xg, t_idl, t_ic, t_yd = [Tk() for _ in range(9)]
        t_y = [Tk() for _ in range(8)]
        ident = cst[:, 0:128]
        U2 = cst[:, 128:256]
        _bregs = {}

        def breg(e):
            if "r" not in _bregs:
                _bregs["r"] = e.to_reg(S - 1)
            return _bregs["r"]

        iota_s = cst[:, 256:384]

        p.dma("sp", cst[:], cst_d[:, :], writes=[t_c])
        p.dma("sp", tidf[:], tid_d[:, :], writes=[t_c])
        p.dma("sp", modT[:], mod_d.rearrange("m (k p) -> p m k", p=128), writes=[t_c], slow=True)
        p.dma("sp", R[:], rw_d.rearrange("(p j) e -> p j e", j=J), writes=[t_ix])
        p.op("dve", lambda e: e.memset(zt[:], 0.0), writes=[t_c])
        for k in range(J):
            p.dma("sp", y_d[k * 128:(k + 1) * 128, :], zt[:], reads=[t_c], writes=[t_yd], is_out=True)
        p.op("dve", lambda e: e.reduce_sum(out=sc[0][:], in_=R[:], axis=AX.X), reads=[t_ix], writes=[t_ix])
        p.op("dve", lambda e: e.tensor_scalar(out=msk[:], in0=sc[0][:], scalar1=0.0, scalar2=None, op0=ALU.is_gt),
             reads=[t_ix], writes=[t_ix])
        p.op("dve", lambda e: e.tensor_copy(out=sc[0][:], in_=msk[:]), reads=[t_ix], writes=[t_ix])
        cur = 0
        sh = 1
        while sh < J:
            a, b = sc[cur], sc[1 - cur]
            p.op("dve", lambda e, a=a, b=b, sh=sh: e.tensor_copy(out=b[:, 0:sh], in_=a[:, 0:sh]), reads=[t_ix], writes=[t_ix])
            p.op("dve", lambda e, a=a, b=b, sh=sh: e.tensor_tensor(out=b[:, sh:J], in0=a[:, sh:J], in1=a[:, 0:J - sh], op=ALU.add),
                 reads=[t_ix], writes=[t_ix])
            cur = 1 - cur
            sh *= 2
        incl = sc[cur]
        obk, otk = scr.next()
        p.op("pe", lambda e: e.matmul(obk[:, 0:1], U2, incl[:, J - 1:J], start=True, stop=True), reads=[t_ix, t_c], writes=[otk])
        p.op("dve", lambda e: e.tensor_scalar(out=offs[:, 0:1], in0=obk[:, 0:1], scalar1=-BIGPOS, scalar2=None, op0=ALU.add),
             reads=[otk], writes=[t_ix])
        p.op("dve", lambda e: e.tensor_tensor(out=posf[:], in0=incl[:], in1=msk[:], op=ALU.subtract), reads=[t_ix], writes=[t_ix])
        p.op("dve", lambda e: e.tensor_scalar(out=posf[:], in0=posf[:], scalar1=offs[:, 0:1], scalar2=None, op0=ALU.add),
             reads=[t_ix], writes=[t_ix])
        p.op("dve", lambda e: e.tensor_tensor(out=posf[:], in0=posf[:], in1=msk[:], op=ALU.mult), reads=[t_ix], writes=[t_ix])
        p.op("dve", lambda e: e.tensor_scalar(out=posf[:], in0=posf[:], scalar1=BIGPOS, scalar2=None, op0=ALU.add),
             reads=[t_ix], writes=[t_ix])
        p.op("dve", lambda e: e.memset(RH[:], 1.0), writes=[t_ix])
        p.op("dve", lambda e: e.tensor_copy(out=RH[:, :, 0], in_=tidf[:]), reads=[t_c], writes=[t_ix])
        p.op("dve", lambda e: e.tensor_copy(out=RH[:, :, 2:10], in_=R[:]), reads=[t_ix], writes=[t_ix])
        for k in range(NK):
            pk, pktk = pk_r.next()
            p.op("dve", lambda e, pk=pk, k=k: e.tensor_scalar(out=pk[:], in0=posf[:], scalar1=float(-128 * k), scalar2=None,
                                                              op0=ALU.add), reads=[t_ix], writes=[pktk])
            cbk, ctk = scr.next()
            for j in range(J):
                oh, ohtk = oh_r.next()
                p.op("dve", lambda e, oh=oh, pk=pk, j=j: e.tensor_scalar(out=oh[:], in0=iota_s, scalar1=pk[:, j:j + 1], scalar2=None,
                                                                         op0=ALU.is_equal), reads=[pktk, t_c], writes=[ohtk])
                p.op("pe", lambda e, oh=oh, cbk=cbk, j=j: e.matmul(cbk[:, 0:10], oh[:], RH[:, j, :], start=(j == 0), stop=(j == J - 1)),
                     reads=[ohtk, t_ix], writes=[ctk])
            p.op("dve", lambda e, cbk=cbk: e.tensor_scalar(out=offs[:, 1:2], in0=cbk[:, 1:2], scalar1=float(-S), scalar2=float(S),
                                                           op0=ALU.mult, op1=ALU.add), reads=[ctk], writes=[t_ic])
            p.op("dve", lambda e, cbk=cbk: e.tensor_tensor(out=offs[:, 1:2], in0=offs[:, 1:2], in1=cbk[:, 0:1], op=ALU.add),
                 reads=[ctk, t_ic], writes=[t_ic])
            p.op("dve", lambda e, k=k: e.tensor_copy(out=idxc[:, k:k + 1], in_=offs[:, 1:2]), reads=[t_ic], writes=[t_ic])
            p.op("dve", lambda e, cbk=cbk, k=k: e.tensor_copy(out=rwc[:, k, :], in_=cbk[:, 2:10]), reads=[ctk], writes=[t_rw])

        for ch in range(NCH):
            for t in range(8):
                k = ch * 8 + t
                p.op("pool", lambda e: e.memset(xg[:], 0.0), writes=[t_xg])
                _prog_dma_custom(p, "pool", lambda e, k=k: e.indirect_dma_start(
                    out=xg[:, :], out_offset=None, in_=x1_d[:, :],
                    in_offset=bass.IndirectOffsetOnAxis(ap=idxc[:, k:k + 1], axis=0), bounds_check=breg(e), oob_is_err=False),
                    reads=[t_ic], writes=[t_xg])
                for c4 in range(4):
                    bk, tk = scr.next()
                    for jj in range(4):
                        c = c4 * 4 + jj
                        p.op("pe", lambda e, bk=bk, jj=jj, c=c: e.transpose(
                            bk[:, jj * 128:(jj + 1) * 128], xg[:, c * 128:(c + 1) * 128], ident), reads=[t_xg, t_c], writes=[tk])
                    for jj in range(4):
                        c = c4 * 4 + jj
                        p.op("dve", lambda e, bk=bk, jj=jj, c=c, t=t: e.tensor_scalar(
                            out=h2[:, c, t * 128:(t + 1) * 128], in0=bk[:, jj * 128:(jj + 1) * 128], scalar1=modT[:, 0, c:c + 1],
                            scalar2=modT[:, 1, c:c + 1], op0=ALU.mult, op1=ALU.add), reads=[tk, t_c], writes=[t_h2])
            for ex in range(NEX):
                wgb, wgtk = wg_r.next()
                wub, wutk = wu_r.next()
                wdb, wdtk = wd_r.next()
                for j in range(3):
                    p.dma("pool", wgb[:, j * 2048:(j + 1) * 2048], wgu_d[ex, 0, :, j * 2048:(j + 1) * 2048], writes=[wgtk])
                    p.dma("pool", wub[:, j * 2048:(j + 1) * 2048], wgu_d[ex, 1, :, j * 2048:(j + 1) * 2048], writes=[wutk])
                for j in range(3):
                    p.dma("pool", wdb[:, j * 2048:(j + 1) * 2048], wd_d[ex, :, j * 2048:(j + 1) * 2048], writes=[wdtk])
                for f in range(3):
                    for nh in range(2):
                        gbk, gtk = scr.next()
                        for k in range(DC):
                            p.op("pe", lambda e, gbk=gbk, wgb=wgb, k=k, f=f, nh=nh: e.matmul(
                                gbk[:, :], wgb[:, k * FE + f * 128:k * FE + (f + 1) * 128], h2[:, k, nh * 512:(nh + 1) * 512],
                                start=(k == 0), stop=(k == DC - 1)), reads=[wgtk, t_h2], writes=[gtk])
                        ubk, utk = scr.next()
                        for k in range(DC):
                            p.op("pe", lambda e, ubk=ubk, wub=wub, k=k, f=f, nh=nh: e.matmul(
                                ubk[:, :], wub[:, k * FE + f * 128:k * FE + (f + 1) * 128], h2[:, k, nh * 512:(nh + 1) * 512],
                                start=(k == 0), stop=(k == DC - 1)), reads=[wutk, t_h2], writes=[utk])
                        sg, sgtk = sg_r.next()
                        p.op("act", lambda e, gbk=gbk, sg=sg: e.activation(out=sg[:], in_=gbk[:, :], func=AF.Silu),
                             reads=[gtk], writes=[sgtk])
                        p.op("dve", lambda e, ubk=ubk, sg=sg, f=f, nh=nh: e.tensor_tensor(
                            out=A[:, f, nh * 512:(nh + 1) * 512], in0=ubk[:, :], in1=sg[:], op=ALU.mult),
                            reads=[utk, sgtk], writes=[t_A])
                for t in range(8):
                    for db in range(4):
                        ybk, ytk = scr.next()
                        for f in range(3):
                            p.op("pe", lambda e, ybk=ybk, wdb=wdb, f=f, t=t, db=db: e.matmul(
                                ybk[:, :], A[:, f, t * 128:(t + 1) * 128], wdb[:, f * D + db * 512:f * D + (db + 1) * 512],
                                start=(f == 0), stop=(f == 2)), reads=[t_A, wdtk], writes=[ytk])
                        if ex == 0:
                            p.op("dve", lambda e, ybk=ybk, t=t, db=db, ex=ex, ch=ch: e.tensor_scalar(
                                out=Yacc[:, t, db * 512:(db + 1) * 512], in0=ybk[:, :], scalar1=rwc[:, ch * 8 + t, ex:ex + 1], scalar2=None,
                                op0=ALU.mult), reads=[ytk, t_rw], writes=[t_y[t]])
                        else:
                            p.op("dve", lambda e, ybk=ybk, t=t, db=db, ex=ex, ch=ch: e.scalar_tensor_tensor(
                                out=Yacc[:, t, db * 512:(db + 1) * 512], in0=ybk[:, :], scalar=rwc[:, ch * 8 + t, ex:ex + 1],
                                in1=Yacc[:, t, db * 512:(db + 1) * 512], op0=ALU.mult, op1=ALU.add),
                                reads=[ytk, t_rw, t_y[t]], writes=[t_y[t]])
            for t in range(8):
                _prog_dma_custom(p, "pool", lambda e, t=t, ch=ch: e.indirect_dma_start(
                    out=y_d[:, :], out_offset=bass.IndirectOffsetOnAxis(ap=idxc[:, ch * 8 + t:ch * 8 + t + 1], axis=0),
                    in_=Yacc[:, t, :], in_offset=None, bounds_check=breg(e), oob_is_err=False),
                    reads=[t_y[t], t_ic], writes=[t_yd], is_out=True)
        p.finish()
    return nc


def phaseC2_consts(S):
    J = S // 128
    tid = (np.arange(128)[:, None] * J + np.arange(J)[None, :]).astype(np.float32)
    pp = np.arange(128)
    U2 = (pp[:, None] < pp[None, :]).astype(np.float32)
    iota = np.broadcast_to(np.arange(128, dtype=np.float32)[None, :], (128, 128))
    cst = np.ascontiguousarray(np.concatenate([np.eye(128, dtype=np.float32), U2, iota], axis=1))
    return {"tid": np.ascontiguousarray(tid), "cst": cst}
```

```python
import math
from contextlib import ExitStack
import numpy as np
import ml_dtypes
import concourse.bass as bass
import concourse.mybir as mybir
from concourse.bass_utils import run_bass_kernel_spmd

F32 = mybir.dt.float32
BF16 = mybir.dt.bfloat16
AF = mybir.ActivationFunctionType
ALU = mybir.AluOpType
AX = mybir.AxisListType

D = 2048
SEQ = 8192
DEPTH = 4
NCORE = 8
DC = D // 128
ALPHA = (2.0 * DEPTH) ** 0.25
LN_EPS = 1e-5
RMS_EPS = 1e-5
SCALE = 1.0 / 8.0
NEG = -30000.0


class Tk:
    __slots__ = ("w", "r")

    def __init__(self):
        self.w = None
        self.r = []


class Prog:
    ENG = ("pe", "act", "dve", "pool", "sp")
    NDMA = 40

    def __init__(self, nc, st):
        self.nc = nc
        self.st = st
        self.ops = {e: [] for e in self.ENG}
        self.sem = {e: st.enter_context(nc.semaphore("s_" + e)) for e in self.ENG}
        self.cnt = {e: 0 for e in self.ENG}
        self.seen = {e: {} for e in self.ENG}
        self.dsem = [st.enter_context(nc.semaphore("d%d" % i)) for i in range(self.NDMA)]
        self.dcnt = [0] * self.NDMA
        self.dnext = 0
        self.outs = []
        self.meta = {e: [] for e in self.ENG}

    def _need(self, e, ev, waits, raw, strict=False):
        if ev is None:
            return
        key, val, sem = ev
        if key == e and not strict and e == "pe":
            return
        if self.seen[e].get(key, 0) >= val:
            return
        self.seen[e][key] = val
        waits.append((sem, val))

    def _deps(self, e, reads, writes, strict=False):
        waits = []
        for t in reads:
            self._need(e, t.w, waits, True, strict)
        for t in writes:
            self._need(e, t.w, waits, False, strict)
            for r in t.r:
                self._need(e, r, waits, False, strict)
        return waits

    def op(self, e, fn, reads=(), writes=()):
        waits = self._deps(e, reads, writes)
        self.cnt[e] += 1
        sem = self.sem[e]
        ev = (e, self.cnt[e], sem)

        def run(eng, waits=waits, fn=fn, sem=sem):
            for s, v in waits:
                eng.wait_ge(s, v)
            fn(eng).then_inc(sem, 1)

        self.ops[e].append(run)
        self.meta[e].append(([(id(s_), v) for s_, v in waits], (id(sem), 1)))
        for t in reads:
            t.r.append(ev)
        for t in writes:
            t.w = ev
            t.r = []

    FRESH_POOL = False

    def dma(self, q, out, in_, reads=(), writes=(), is_out=False, slow=False):
        waits = self._deps(q, reads, writes, strict=True)
        if q == "pool" and Prog.FRESH_POOL:
            n = getattr(self, "n_fp", 0)
            self.n_fp = n + 1
            sem = self.st.enter_context(self.nc.semaphore("fp%d" % n))
            ev = ("fp%d" % n, 16, sem)
        else:
            i = self.dnext
            self.dnext = (self.dnext + 1) % self.NDMA
            sem = self.dsem[i]
            key = "d%d" % i
            prev = self.dcnt[i]
            if prev and self.seen[q].get(key, 0) < prev:
                self.seen[q][key] = prev
                waits.append((sem, prev))
            self.dcnt[i] += 16
            ev = (key, self.dcnt[i], sem)

        def run(eng, waits=waits, sem=sem, out=out, in_=in_, slow=slow):
            for s, v in waits:
                eng.wait_ge(s, v)
            if slow:
                eng.dma_start(out=out, in_=in_, allow_slow_non_contiguous=True).then_inc(sem, 16)
            else:
                eng.dma_start(out=out, in_=in_).then_inc(sem, 16)

        self.ops[q].append(run)
        self.meta[q].append(([(id(s_), v) for s_, v in waits], (id(sem), 16)))
        for t in reads:
            t.r.append(ev)
        for t in writes:
            t.w = ev
            t.r = []
        if is_out:
            self.outs.append(ev)

    def check_deadlock(self):
        cnt = {}
        pos = {e: 0 for e in self.ENG}
        progress = True
        while progress:
            progress = False
            for e in self.ENG:
                while pos[e] < len(self.meta[e]):
                    waits, (sk, inc) = self.meta[e][pos[e]]
                    if all(cnt.get(k, 0) >= v for k, v in waits):
                        cnt[sk] = cnt.get(sk, 0) + inc
                        pos[e] += 1
                        progress = True
                    else:
                        break
        stuck = {e: (pos[e], len(self.meta[e])) for e in self.ENG if pos[e] < len(self.meta[e])}
        return stuck

    def finish(self):
        waits = []
        for ev in self.outs:
            self._need("sp", ev, waits, True)

        def run(eng, waits=waits):
            for s, v in waits:
                eng.wait_ge(s, v)

        self.ops["sp"].append(run)
        nc = self.nc
        with nc.Block() as blk:
            @blk.tensor
            def _(e):
                for f in self.ops["pe"]:
                    f(e)

            @blk.scalar
            def _(e):
                for f in self.ops["act"]:
                    f(e)

            @blk.vector
            def _(e):
                for f in self.ops["dve"]:
                    f(e)

            @blk.gpsimd
            def _(e):
                for f in self.ops["pool"]:
                    f(e)

            @blk.sync
            def _(e):
                for f in self.ops["sp"]:
                    f(e)


def _sb(nc, st, name, shape, dt):
    return st.enter_context(nc.sbuf_tensor("sb_" + name, shape, dt))


def _ps(nc, st, name, shape, dt):
    return st.enter_context(nc.psum_tensor("ps_" + name, shape, dt))


P0_COLS = 6 * D // NCORE
P0_HALF = 768


def build_phase0():
    nc = bass.Bass("TRN2", target_bir_lowering=False)
    c = nc.dram_tensor("c", [1, D], F32, kind="ExternalInput").ap()
    w = nc.dram_tensor("w", [DEPTH, D, P0_COLS], F32, kind="ExternalInput").ap()
    b = nc.dram_tensor("b", [DEPTH, P0_COLS], F32, kind="ExternalInput").ap()
    addc = nc.dram_tensor("addc", [1, P0_COLS], F32, kind="ExternalInput").ap()
    out = nc.dram_tensor("mod", [DEPTH, P0_COLS], F32, kind="ExternalOutput").ap()
    with ExitStack() as st:
        p = Prog(nc, st)
        cT = _sb(nc, st, "cT", [128, DC], F32)
        bt = _sb(nc, st, "bt", [1, DEPTH * P0_COLS], F32)
        at = _sb(nc, st, "at", [1, P0_COLS], F32)
        res = _sb(nc, st, "res", [1, DEPTH * P0_COLS], F32)
        wb = [_sb(nc, st, "wb%d" % i, [128, DC, P0_HALF], F32) for i in range(2)]
        ps = [_ps(nc, st, "ps%d" % i, [128, 512], F32) for i in range(4)]
        t_c, t_b, t_a, t_res = Tk(), Tk(), Tk(), Tk()
        t_w = [Tk(), Tk()]
        t_ps = [Tk() for _ in range(4)]
        p.dma("sp", cT[:], c[0, :].rearrange("(k p) -> p k", p=128), writes=[t_c], slow=True)
        p.dma("sp", bt[:], b.rearrange("l n -> (l n)")[None, :], writes=[t_b])
        p.dma("sp", at[:], addc[:, :], writes=[t_a])
        u = 0
        pi = 0
        for l in range(DEPTH):
            for hf in range(2):
                buf = u % 2
                q = "sp" if u % 2 == 0 else "pool"
                p.dma(q, wb[buf][:], w[l, :, hf * P0_HALF:(hf + 1) * P0_HALF].rearrange("(k p) n -> p k n", p=128),
                      writes=[t_w[buf]])
                for n0 in range(0, P0_HALF, 384):
                    pt = ps[pi % 4]
                    tk = t_ps[pi % 4]
                    pi += 1
                    for k in range(DC):
                        p.op("pe", lambda e, pt=pt, k=k, buf=buf, n0=n0: e.matmul(
                            pt[0:1, 0:384], cT[:, k:k + 1], wb[buf][:, k, n0:n0 + 384], start=(k == 0), stop=(k == DC - 1)),
                            reads=[t_c, t_w[buf]], writes=[tk])
                    o0 = l * P0_COLS + hf * P0_HALF + n0
                    a0 = hf * P0_HALF + n0
                    p.op("dve", lambda e, pt=pt, o0=o0: e.tensor_tensor(
                        out=res[0:1, o0:o0 + 384], in0=pt[0:1, 0:384], in1=bt[0:1, o0:o0 + 384], op=ALU.add),
                        reads=[tk, t_b], writes=[t_res])
                    p.op("dve", lambda e, o0=o0, a0=a0: e.tensor_tensor(
                        out=res[0:1, o0:o0 + 384], in0=res[0:1, o0:o0 + 384], in1=at[0:1, a0:a0 + 384], op=ALU.add),
                        reads=[t_res, t_a], writes=[t_res])
                u += 1
        p.dma("sp", out.rearrange("l n -> (l n)")[None, :], res[:], reads=[t_res], is_out=True)
        p.finish()
    return nc


def run_phase0(c, w_ada, b_ada):
    nc = build_phase0()
    addfull = np.zeros((1, 6 * D), np.float32)
    addfull[0, D:2 * D] = 1.0
    addfull[0, 4 * D:5 * D] = 1.0
    maps = []
    for i in range(NCORE):
        sl = slice(i * P0_COLS, (i + 1) * P0_COLS)
        maps.append({"c": np.ascontiguousarray(c, np.float32),
                     "w": np.ascontiguousarray(w_ada[:, :, sl]),
                     "b": np.ascontiguousarray(b_ada[:, sl]),
                     "addc": np.ascontiguousarray(addfull[:, sl])})
    res = run_bass_kernel_spmd(nc, maps, core_ids=list(range(NCORE)))
    return np.concatenate([r["mod"] for r in res.results], axis=1)


def pipeline(n_iter, stages, skew=1):
    for step in range(n_iter + (len(stages) - 1) * skew):
        for k, f in enumerate(stages):
            i = step - k * skew
            if 0 <= i < n_iter:
                f(i)


class Rot:
    def __init__(self, bufs):
        self.bufs = bufs
        self.tks = [Tk() for _ in bufs]
        self.i = 0

    def next(self):
        j = self.i % len(self.bufs)
        self.i += 1
        return self.bufs[j], self.tks[j]


def _bf(a):
    return np.asarray(a, np.float32).astype(ml_dtypes.bfloat16)


A_WT = 768
A_WV = 320
VW = 322
A_SM = 2 + 4 * 64 + 128 + 2


def phaseA_consts(core, S):
    NQ = S // 128
    sl_swa = [2.0 ** (-8.0 * (h + 1) / 16.0) for h in (2 * core, 2 * core + 1)]
    sl_d = 2.0 ** (-8.0 * (core + 1) / 8.0)
    p = np.arange(128)[:, None].astype(np.float64)
    t = np.arange(128)[None, :].astype(np.float64)
    swab = np.zeros((128, 2, 2, 128), np.float32)
    for hh in range(2):
        d0 = 128 + t - p
        swab[:, hh, 0, :] = np.where(d0 < 128, -sl_swa[hh] * d0, NEG)
        d1 = t - p
        swab[:, hh, 1, :] = np.where(d1 >= 0, -sl_swa[hh] * d1, NEG)
    nj = NQ + 3
    jj = np.arange(nj)[None, :] - 3
    dbias = (sl_d * p - sl_d * 128.0 * jj).astype(np.float32)
    ident = np.eye(128, dtype=np.float32)
    cf = np.concatenate([swab.reshape(128, 512), dbias, ident], axis=1)
    tri = (p >= t).astype(np.float32)
    ones = np.ones((128, 128), np.float32)
    strict = (p < t).astype(np.float32)
    incl = (p <= t).astype(np.float32)
    tl = np.arange(512).astype(np.float64)
    v = (-8.0 * sl_d * tl)
    hi = v.astype(np.float32).astype(ml_dtypes.bfloat16)
    lo = (v - hi.astype(np.float64)).astype(np.float32).astype(ml_dtypes.bfloat16)
    qaug = np.zeros((128, 512), np.float32)
    qaug[0] = hi.astype(np.float32)
    qaug[1] = lo.astype(np.float32)
    cb = _bf(np.concatenate([tri, ones, strict, incl, qaug], axis=1))
    return np.ascontiguousarray(cf), np.ascontiguousarray(cb)


def build_phaseA(S):
    NQ = S // 128
    NG = S // 512
    NJ = NQ + 3
    NCF = 512 + NJ + 128
    nc = bass.Bass("TRN2", target_bir_lowering=False)
    x = nc.dram_tensor("x", [S, D], F32, kind="ExternalInput").ap()
    mod = nc.dram_tensor("mod", [2, D], F32, kind="ExternalInput").ap()
    w_t = nc.dram_tensor("w_t", [D, A_WT], F32, kind="ExternalInput").ap()
    w_v = nc.dram_tensor("w_v", [D, A_WV], F32, kind="ExternalInput").ap()
    cf_d = nc.dram_tensor("cf", [128, NCF], F32, kind="ExternalInput").ap()
    cb_d = nc.dram_tensor("cb", [128, 1024], BF16, kind="ExternalInput").ap()
    sm_d = nc.dram_tensor("sm", [1, A_SM], F32, kind="ExternalInput").ap()
    o_d = nc.dram_tensor("o", [S, 384], F32, kind="ExternalOutput").ap()
    with ExitStack() as st:
        p = Prog(nc, st)
        sb = lambda name, shape, dt: _sb(nc, st, name, shape, dt)
        wT = sb("wT", [128, DC, A_WT], BF16)
        wV = sb("wV", [128, DC, A_WV], BF16)
        KA = sb("KA", [128, S], BF16)
        KB = sb("KB", [128, S], BF16)
        KC = sb("KC", [128, S], BF16)
        V = sb("V", [128, NQ, VW], BF16)
        xs = sb("xs", [128, 2, D], F32)
        hT = sb("hT", [128, DC, 512], BF16)
        QA = sb("QA", [128, 512], BF16)
        QB = sb("QB", [128, 512], BF16)
        QC = sb("QC", [128, 512], BF16)
        cf = sb("cfs", [128, NCF], F32)
        cb = sb("cbs", [128, 1024], BF16)
        sm = sb("sms", [128, A_SM], F32)
        sclT = sb("sclT", [128, DC], F32)
        shT = sb("shT", [128, DC], F32)
        misc = sb("misc", [128, 16], F32)
        gsub = sb("gsub", [128, 128], F32)
        junk = sb("junk", [128, 128], F32)
        ost = [sb("ost%d" % i, [128, 4, 384], F32) for i in range(2)]
        t_ost = [Tk(), Tk()]
        banks = [_ps(nc, st, "bk%d" % i, [128, 512], F32) for i in range(8)]
        scr = Rot(banks[0:3])
        SBACC, t_sbacc = banks[3], Tk()
        DACC = banks[4:8]
        t_dacc = [Tk() for _ in range(4)]
        e_r = Rot([sb("e%d" % i, [128, 512], F32) for i in range(3)])
        L_r = Rot([sb("L%d" % i, [128, 512], BF16) for i in range(3)])
        X_r = Rot([sb("X%d" % i, [128, 512], F32) for i in range(2)])
        W_r = Rot([sb("W%d" % i, [128, 512], BF16) for i in range(3)])
        E_r = Rot([sb("E%d" % i, [128, 512], BF16) for i in range(3)])
        Lacc = sb("Lacc", [128, 512], F32)
        Lab_r = Rot([sb("Lab%d" % i, [128, 512], BF16) for i in range(2)])
        st_r = Rot([sb("stmp%d" % i, [128, 128], F32) for i in range(2)])
        sE_r = Rot([sb("sE%d" % i, [128, 128], BF16) for i in range(2)])
        fin_r = Rot([sb("fin%d" % i, [128, 8], F32) for i in range(2)])
        da_r = Rot([sb("da%d" % i, [128, 128], F32) for i in range(2)])
        dd_r = Rot([sb("dd%d" % i, [128, 128], F32) for i in range(2)])

        t_c = Tk()
        t_k = [Tk() for _ in range(NG)]
        t_xs, t_hT, t_q = Tk(), Tk(), Tk()
        t_lacc = Tk()
        t_misc = Tk()

        swab = lambda hh, kind: cf[:, (hh * 2 + kind) * 128:(hh * 2 + kind + 1) * 128]
        dbias = lambda j: cf[:, 512 + j:512 + j + 1]
        ident = cf[:, 512 + NJ:512 + NJ + 128]
        tri = cb[:, 0:128]
        ones = cb[:, 128:256]
        strict = cb[:, 256:384]
        incl = cb[:, 384:512]
        qaug = cb[:, 512:1024]

        p.dma("sp", cf[:], cf_d[:, :], writes=[t_c])
        p.dma("sp", cb[:], cb_d[:, :], writes=[t_c])
        p.dma("sp", sm[:], sm_d[0, :].partition_broadcast(128), writes=[t_c])
        p.dma("sp", sclT[:], mod[0, :].rearrange("(k p) -> p k", p=128), writes=[t_c], slow=True)
        p.dma("sp", shT[:], mod[1, :].rearrange("(k p) -> p k", p=128), writes=[t_c], slow=True)
        for k in range(DC):
            p.dma("pool", wT[:, k, :], w_t[k * 128:(k + 1) * 128, :], writes=[t_c])
            p.dma("pool", wV[:, k, :], w_v[k * 128:(k + 1) * 128, :], writes=[t_c])
        p.op("pool", lambda e: e.memset(V[:, :, 64:65], 1.0), writes=[t_c])
        p.op("pool", lambda e: e.memset(V[:, :, 321:322], 1.0), writes=[t_c])
        p.op("act", lambda e: e.activation(out=misc[:, 0:2], in_=sm[:, 0:2], func=AF.Exp), reads=[t_c], writes=[t_misc])
        p.op("dve", lambda e: e.tensor_tensor(out=junk[:, 0:64], in0=sm[:, 2:66], in1=sm[:, 66:130], op=ALU.mult),
             reads=[t_c], writes=[t_misc])
        p.op("dve", lambda e: e.reduce_sum(out=misc[:, 2:3], in_=junk[:, 0:64], axis=AX.X), reads=[t_misc], writes=[t_misc])
        p.op("dve", lambda e: e.tensor_tensor(out=junk[:, 64:128], in0=sm[:, 130:194], in1=sm[:, 194:258], op=ALU.mult),
             reads=[t_c], writes=[t_misc])
        p.op("dve", lambda e: e.reduce_sum(out=misc[:, 3:4], in_=junk[:, 64:128], axis=AX.X), reads=[t_misc], writes=[t_misc])
        p.op("act", lambda e: e.activation(out=misc[:, 5:7], in_=misc[:, 2:4], func=AF.Exp), reads=[t_misc], writes=[t_misc])
        p.op("dve", lambda e: e.tensor_tensor(out=misc[:, 7:8], in0=misc[:, 6:7], in1=misc[:, 5:6], op=ALU.subtract),
             reads=[t_misc], writes=[t_misc])
        p.op("dve", lambda e: e.tensor_tensor(out=misc[:, 4:5], in0=misc[:, 7:8], in1=sm[:, 386:387], op=ALU.subtract),
             reads=[t_misc, t_c], writes=[t_misc])
        p.op("dve", lambda e: e.tensor_scalar(out=gsub[:], in0=sm[:, 258:386], scalar1=sm[:, 387:388], scalar2=None,
                                              op0=ALU.mult), reads=[t_c], writes=[t_misc])

        for g in range(NG):
            for half in range(2):
                r0 = g * 512 + half * 256
                p.dma("sp", xs[:], x[r0:r0 + 256, :].rearrange("(t q) d -> q t d", q=128), writes=[t_xs])
                for c in range(DC):
                    bk, tk = scr.next()
                    for t in range(2):
                        p.op("pe", lambda e, bk=bk, t=t, c=c: e.transpose(
                            bk[:, t * 128:(t + 1) * 128], xs[:, t, c * 128:(c + 1) * 128], ident),
                            reads=[t_xs, t_c], writes=[tk])
                    p.op("act", lambda e, bk=bk, c=c, half=half: e.activation(
                        out=hT[:, c, half * 256:(half + 1) * 256], in_=bk[:, 0:256], func=AF.Identity,
                        bias=shT[:, c:c + 1], scale=sclT[:, c:c + 1]), reads=[tk, t_c], writes=[t_hT])
            dests = [QA[:, :], KA[:, g * 512:(g + 1) * 512], QB[:, :], KB[:, g * 512:(g + 1) * 512],
                     QC[:, :], KC[:, g * 512:(g + 1) * 512]]
            for m in range(6):
                bk, tk = scr.next()
                for k in range(DC):
                    p.op("pe", lambda e, bk=bk, k=k, m=m: e.matmul(
                        bk[:, :], wT[:, k, m * 128:(m + 1) * 128], hT[:, k, :], start=(k == 0), stop=(k == DC - 1)),
                        reads=[t_c, t_hT], writes=[tk])
                wtk = t_q if m % 2 == 0 else t_k[g]
                p.op("dve", lambda e, bk=bk, m=m, dests=dests: e.tensor_copy(out=dests[m], in_=bk[:, :]),
                     reads=[tk], writes=[wtk])
            for t in range(4):
                bk, tk = scr.next()
                for k in range(DC):
                    p.op("pe", lambda e, bk=bk, k=k, t=t: e.matmul(
                        bk[:, 0:A_WV], hT[:, k, t * 128:(t + 1) * 128], wV[:, k, :], start=(k == 0), stop=(k == DC - 1)),
                        reads=[t_c, t_hT], writes=[tk])
                qb = g * 4 + t
                p.op("dve", lambda e, bk=bk, qb=qb: e.tensor_copy(out=V[:, qb, 0:64], in_=bk[:, 0:64]),
                     reads=[tk], writes=[t_k[g]])
                p.op("dve", lambda e, bk=bk, qb=qb: e.tensor_copy(out=V[:, qb, 65:321], in_=bk[:, 64:320]),
                     reads=[tk], writes=[t_k[g]])

            o_t, o_tk = ost[g % 2], t_ost[g % 2]

            for hh in range(2):
                for qi in range(4):
                    i = g * 4 + qi
                    kbs = [kb for kb in (i - 1, i) if kb >= 0]
                    abk, atk = scr.next()
                    ps_l = []
                    for kb in kbs:
                        kind = 1 if kb == i else 0
                        bk, tk = scr.next()
                        kg = t_k[kb // 4]
                        p.op("pe", lambda e, bk=bk, kb=kb, hh=hh, qi=qi: e.matmul(
                            bk[:, 0:128], KA[64 * hh:64 * hh + 64, kb * 128:(kb + 1) * 128],
                            QA[64 * hh:64 * hh + 64, qi * 128:(qi + 1) * 128], start=True, stop=True),
                            reads=[kg, t_q], writes=[tk])
                        tm, ttk = st_r.next()
                        p.op("dve", lambda e, bk=bk, tm=tm, hh=hh, kind=kind: e.scalar_tensor_tensor(
                            out=tm[:], in0=bk[:, 0:128], scalar=SCALE, in1=swab(hh, kind), op0=ALU.mult, op1=ALU.add),
                            reads=[tk, t_c], writes=[ttk])
                        sE, setk = sE_r.next()
                        p.op("act", lambda e, tm=tm, sE=sE: e.activation(out=sE[:], in_=tm[:], func=AF.Exp),
                             reads=[ttk], writes=[setk])
                        ps_l.append((sE, setk, kb, kg))
                    for n, (sE, setk, kb, kg) in enumerate(ps_l):
                        p.op("pe", lambda e, abk=abk, sE=sE, kb=kb, n=n, ln=len(ps_l): e.matmul(
                            abk[:, 0:65], sE[:], V[:, kb, 0:65], start=(n == 0), stop=(n == ln - 1)),
                            reads=[setk, kg, t_c], writes=[atk])
                    fb, ftk = fin_r.next()
                    p.op("dve", lambda e, fb=fb, abk=abk, hh=hh: e.tensor_tensor(
                        out=fb[:, 0:1], in0=abk[:, 64:65], in1=misc[:, hh:hh + 1], op=ALU.add),
                        reads=[atk, t_misc], writes=[ftk])
                    p.op("dve", lambda e, fb=fb: e.reciprocal(out=fb[:, 1:2], in_=fb[:, 0:1]), reads=[ftk], writes=[ftk])
                    p.op("dve", lambda e, fb=fb, abk=abk, hh=hh, qi=qi, o_t=o_t: e.tensor_scalar(
                        out=o_t[:, qi, 64 * hh:64 * hh + 64], in0=abk[:, 0:64], scalar1=fb[:, 1:2], scalar2=None,
                        op0=ALU.mult), reads=[atk, ftk], writes=[o_tk])

            its = [(hh, kb) for hh in range(2) for kb in range(g * 4 + 3, -1, -1)]
            stt = {}

            def sb_a(n):
                hh, kb = its[n]
                qs = max(0, kb - 4 * g) * 128
                first = (kb == g * 4 + 3)
                kg = t_k[kb // 4]
                bk, tk = scr.next()
                p.op("pe", lambda e: e.matmul(bk[:, qs:512], KB[64 * hh:64 * hh + 64, kb * 128:(kb + 1) * 128],
                                              QB[64 * hh:64 * hh + 64, qs:512], start=True, stop=True),
                     reads=[kg, t_q], writes=[tk])
                eb, etk = e_r.next()
                p.op("act", lambda e: e.activation(out=eb[:, qs:512], in_=bk[:, qs:512], func=AF.Exp, scale=SCALE),
                     reads=[tk], writes=[etk])
                Lb, Ltk = L_r.next()
                p.op("act", lambda e: e.activation(out=Lb[:, qs:512], in_=eb[:, qs:512], func=AF.Ln, bias=1.0, scale=1.0),
                     reads=[etk], writes=[Ltk])
                if kb >= 4 * g:
                    p.op("pool", lambda e: e.tensor_tensor(out=Lb[:, qs:qs + 128], in0=Lb[:, qs:qs + 128], in1=strict,
                                                           op=ALU.mult), reads=[Ltk, t_c], writes=[Ltk])
                if first:
                    p.op("pool", lambda e: e.memset(Lacc[:], 0.0), writes=[t_lacc])
                stt[n] = dict(hh=hh, kb=kb, qs=qs, first=first, kg=kg, eb=eb, etk=etk, Lb=Lb, Ltk=Ltk)

            def sb_b(n):
                s = stt[n]
                hh, kb, qs, first = s["hh"], s["kb"], s["qs"], s["first"]
                eb, etk, Lb, Ltk = s["eb"], s["etk"], s["Lb"], s["Ltk"]
                cbk, ctk = scr.next()
                p.op("pe", lambda e: e.matmul(cbk[:, qs:512], tri, Lb[:, qs:512], start=True, stop=first),
                     reads=[Ltk, t_c], writes=[ctk])
                if not first:
                    plab, plabtk = s_prev_lab[0]
                    p.op("pe", lambda e: e.matmul(cbk[:, qs:512], ones, plab[:, qs:512], start=False, stop=True),
                         reads=[plabtk, t_c], writes=[ctk])
                Xb, Xtk = X_r.next()
                p.op("act", lambda e: e.activation(out=Xb[:, qs:512], in_=cbk[:, qs:512], func=AF.Exp, scale=-1.0),
                     reads=[ctk], writes=[Xtk])
                Wb, Wtk = W_r.next()
                p.op("dve", lambda e: e.tensor_tensor(out=Wb[:, qs:512], in0=eb[:, qs:512], in1=Xb[:, qs:512], op=ALU.mult),
                     reads=[etk, Xtk], writes=[Wtk])
                if kb >= 4 * g:
                    p.op("pool", lambda e: e.tensor_tensor(out=Wb[:, qs:qs + 128], in0=Wb[:, qs:qs + 128], in1=strict,
                                                           op=ALU.mult), reads=[Wtk, t_c], writes=[Wtk])
                if kb > 0:
                    p.op("pool", lambda e: e.tensor_tensor(out=Lacc[:, qs:512], in0=Lacc[:, qs:512], in1=Lb[:, qs:512],
                                                           op=ALU.add), reads=[Ltk, t_lacc], writes=[t_lacc])
                    lab, labtk = Lab_r.next()
                    p.op("pool", lambda e: e.tensor_copy(out=lab[:, :], in_=Lacc[:, :]), reads=[t_lacc], writes=[labtk])
                    s_prev_lab[0] = (lab, labtk)
                s["Wb"], s["Wtk"] = Wb, Wtk

            def sb_c(n):
                s = stt.pop(n)
                hh, kb, qs, first, kg = s["hh"], s["kb"], s["qs"], s["first"], s["kg"]
                Wb, Wtk = s["Wb"], s["Wtk"]
                for qi in range(qs // 128, 4):
                    c0 = hh * 256 + qi * 64
                    p.op("pe", lambda e, qi=qi, c0=c0: e.matmul(
                        SBACC[:, c0:c0 + 64], Wb[:, qi * 128:(qi + 1) * 128], V[:, kb, 65 + 64 * hh:65 + 64 * hh + 64],
                        start=(first and hh == 0 and qi == 3), stop=(kb == 0), skip_group_check=True),
                        reads=[Wtk, kg], writes=[t_sbacc])
                if kb == 0:
                    p.op("act", lambda e, o_t=o_t: e.activation(
                        out=o_t[:, :, 128 + 64 * hh:128 + 64 * hh + 64],
                        in_=SBACC[:, hh * 256:(hh + 1) * 256].rearrange("p (q d) -> p q d", d=64), func=AF.Identity),
                        reads=[t_sbacc], writes=[o_tk])

            s_prev_lab = [None]
            pipeline(len(its), [sb_a, sb_b, sb_c])

            dits = [(kb, m) for kb in range(0, g * 4 + 4) for m in range(2)]
            dst = {}

            def dacc(m, qi):
                if qi < 3:
                    return DACC[2 * m], t_dacc[2 * m], qi * 129
                return DACC[2 * m + 1], t_dacc[2 * m + 1], 0

            def df_a(n):
                kb, m = dits[n]
                qs = max(0, kb - 4 * g) * 128
                kg = t_k[kb // 4]
                bk, tk = scr.next()
                p.op("pe", lambda e: e.matmul(bk[:, qs:512], KC[64 * m:64 * m + 64, kb * 128:(kb + 1) * 128],
                                              QC[64 * m:64 * m + 64, qs:512], start=True, stop=False),
                     reads=[kg, t_q], writes=[tk])
                p.op("pe", lambda e: e.matmul(bk[:, qs:512], ones[0:2, :], qaug[0:2, qs:512], start=False, stop=True),
                     reads=[t_c], writes=[tk])
                Eb, Etk = E_r.next()
                jidx = (4 * g - kb) + 3
                p.op("act", lambda e: e.activation(out=Eb[:, qs:512], in_=bk[:, qs:512], func=AF.Exp, scale=SCALE,
                                                   bias=dbias(jidx)), reads=[tk, t_c], writes=[Etk])
                if kb >= 4 * g:
                    p.op("pool", lambda e: e.tensor_tensor(out=Eb[:, qs:qs + 128], in0=Eb[:, qs:qs + 128], in1=incl,
                                                           op=ALU.mult), reads=[Etk, t_c], writes=[Etk])
                dst[n] = dict(kb=kb, m=m, qs=qs, kg=kg, Eb=Eb, Etk=Etk)

            def df_b(n):
                s = dst.pop(n)
                kb, m, qs, kg, Eb, Etk = s["kb"], s["m"], s["qs"], s["kg"], s["Eb"], s["Etk"]
                for qi in range(qs // 128, 4):
                    ab, atk, c0 = dacc(m, qi)
                    p.op("pe", lambda e, qi=qi, ab=ab, c0=c0: e.matmul(
                        ab[:, c0:c0 + 129], Eb[:, qi * 128:(qi + 1) * 128], V[:, kb, 193:322],
                        start=(kb == 0 and qi in (0, 3)), stop=(kb == 4 * g + qi), skip_group_check=True),
                        reads=[Etk, kg, t_c], writes=[atk])

            pipeline(len(dits), [df_a, df_b])
            for qi in range(4):
                a1, a1tk, c1 = dacc(0, qi)
                a2, a2tk, c2 = dacc(1, qi)
                fb, ftk = fin_r.next()
                p.op("dve", lambda e, fb=fb, a1=a1, c1=c1: e.reciprocal(out=fb[:, 0:1], in_=a1[:, c1 + 128:c1 + 129]),
                     reads=[a1tk], writes=[ftk])
                p.op("dve", lambda e, fb=fb, a2=a2, c2=c2: e.reciprocal(out=fb[:, 1:2], in_=a2[:, c2 + 128:c2 + 129]),
                     reads=[a2tk], writes=[ftk])
                p.op("dve", lambda e, fb=fb: e.tensor_tensor(out=fb[:, 2:3], in0=fb[:, 1:2], in1=misc[:, 4:5], op=ALU.mult),
                     reads=[ftk, t_misc], writes=[ftk])
                da, datk = da_r.next()
                p.op("dve", lambda e, fb=fb, a1=a1, c1=c1, da=da: e.tensor_scalar(
                    out=da[:], in0=a1[:, c1:c1 + 128], scalar1=fb[:, 0:1], scalar2=None, op0=ALU.mult),
                    reads=[a1tk, ftk], writes=[datk])
                dd, ddtk = dd_r.next()
                p.op("dve", lambda e, fb=fb, a2=a2, c2=c2, da=da, dd=dd: e.scalar_tensor_tensor(
                    out=dd[:], in0=a2[:, c2:c2 + 128], scalar=fb[:, 2:3], in1=da[:], op0=ALU.mult, op1=ALU.add),
                    reads=[a2tk, ftk, datk], writes=[ddtk])
                p.op("dve", lambda e, dd=dd, da=da: e.tensor_tensor(out=da[:], in0=dd[:], in1=dd[:], op=ALU.mult),
                     reads=[ddtk], writes=[datk])
                p.op("dve", lambda e, fb=fb, da=da: e.reduce_sum(out=fb[:, 3:4], in_=da[:], axis=AX.X),
                     reads=[datk], writes=[ftk])
                p.op("act", lambda e, fb=fb: e.activation(out=fb[:, 4:5], in_=fb[:, 3:4], func=AF.Ln, bias=RMS_EPS,
                                                          scale=1.0 / 128.0), reads=[ftk], writes=[ftk])
                p.op("act", lambda e, fb=fb: e.activation(out=fb[:, 5:6], in_=fb[:, 4:5], func=AF.Exp, scale=-0.5),
                     reads=[ftk], writes=[ftk])
                p.op("dve", lambda e, fb=fb, dd=dd, qi=qi, o_t=o_t: e.scalar_tensor_tensor(
                    out=o_t[:, qi, 256:384], in0=dd[:], scalar=fb[:, 5:6], in1=gsub[:], op0=ALU.mult, op1=ALU.mult),
                    reads=[ddtk, ftk, t_misc], writes=[o_tk])
            p.dma("sp", o_d[g * 512:(g + 1) * 512, :].rearrange("(q p) c -> p q c", p=128), o_t[:], reads=[o_tk],
                  is_out=True)
        p.finish()
    return nc


def phaseA_inputs(core, S, x, modA, w_in, sinks, lq1, lk1, lq2, lk2, subg, lam_init):
    h0 = 2 * core
    kv = core // 2
    qA = w_in[:, h0 * 64:h0 * 64 + 128]
    kA = w_in[:, 1024 + kv * 64:1024 + kv * 64 + 64]
    vA = w_in[:, 1280 + kv * 64:1280 + kv * 64 + 64]
    qB = w_in[:, 1536 + h0 * 64:1536 + h0 * 64 + 128]
    kB = w_in[:, 2560 + h0 * 64:2560 + h0 * 64 + 128]
    vB = w_in[:, 3584 + h0 * 64:3584 + h0 * 64 + 128]
    qC = w_in[:, 4608 + core * 128:4608 + core * 128 + 128]
    kC = w_in[:, 5632 + core * 128:5632 + core * 128 + 128]
    vC = w_in[:, 6656 + core * 128:6656 + core * 128 + 128]
    w_t = np.ascontiguousarray(np.concatenate([qA, kA, kA, qB, kB, qC, kC], axis=1), np.float32)
    w_v = np.ascontiguousarray(np.concatenate([vA, vB, vC], axis=1), np.float32)
    cf, cb = phaseA_consts2(core, S)
    sm = np.zeros((1, A_SM), np.float32)
    sm[0, 0:2] = sinks[h0:h0 + 2]
    sm[0, 2:66] = lq1
    sm[0, 66:130] = lk1
    sm[0, 130:194] = lq2
    sm[0, 194:258] = lk2
    sm[0, 258:386] = subg
    sm[0, 386] = lam_init
    sm[0, 387] = 1.0 - lam_init
    return {"x": np.ascontiguousarray(x, np.float32), "mod": np.ascontiguousarray(modA, np.float32), "w_t": w_t,
            "w_v": w_v, "cf": cf, "cb": cb, "sm": sm}


def emit_layernorm(p, r, t_r, xo, t_xo, gB, bB, t_c, stats, t_st):
    for j in range(4):
        p.op("dve", lambda e, j=j: e.bn_stats(out=stats[:, j * 6:(j + 1) * 6], in_=r[:, j * 512:(j + 1) * 512]),
             reads=[t_r], writes=[t_st])
    p.op("dve", lambda e: e.bn_aggr(out=stats[:, 24:26], in_=stats[:, 0:24]), reads=[t_st], writes=[t_st])
    p.op("act", lambda e: e.activation(out=stats[:, 26:27], in_=stats[:, 25:26], func=AF.Ln, bias=LN_EPS, scale=1.0),
         reads=[t_st], writes=[t_st])
    p.op("act", lambda e: e.activation(out=stats[:, 27:28], in_=stats[:, 26:27], func=AF.Exp, scale=-0.5),
         reads=[t_st], writes=[t_st])
    p.op("dve", lambda e: e.tensor_scalar(out=r[:], in0=r[:], scalar1=stats[:, 24:25], scalar2=stats[:, 27:28],
                                          op0=ALU.subtract, op1=ALU.mult), reads=[t_st, t_r], writes=[t_r])
    p.op("pool", lambda e: e.tensor_tensor(out=r[:], in0=r[:], in1=gB[:], op=ALU.mult), reads=[t_r, t_c], writes=[t_r])
    p.op("dve", lambda e: e.tensor_tensor(out=xo[:], in0=r[:], in1=bB[:], op=ALU.add), reads=[t_r, t_c], writes=[t_xo])


TB = SEQ // NCORE


def build_phaseB(TBK=TB):
    NH = TBK // 512
    nc = bass.Bass("TRN2", target_bir_lowering=False)
    x = nc.dram_tensor("x", [TBK, D], F32, kind="ExternalInput").ap()
    o = nc.dram_tensor("o", [TBK, 3072], F32, kind="ExternalInput").ap()
    mod = nc.dram_tensor("mod", [6, D], F32, kind="ExternalInput").ap()
    wg_d = nc.dram_tensor("wg", [48, 128, DC * 128], F32, kind="ExternalInput").ap()
    bg_d = nc.dram_tensor("bg", [6144], F32, kind="ExternalInput").ap()
    wb_d = nc.dram_tensor("wb", [48, 128, 8 * 128], F32, kind="ExternalInput").ap()
    wo_d = nc.dram_tensor("wo", [4, DC, 128, 512], F32, kind="ExternalInput").ap()
    ln_d = nc.dram_tensor("ln", [2, D], F32, kind="ExternalInput").ap()
    wr_d = nc.dram_tensor("wr", [D, 72], F32, kind="ExternalInput").ap()
    br_d = nc.dram_tensor("br", [72], F32, kind="ExternalInput").ap()
    id_d = nc.dram_tensor("ident", [128, 128], F32, kind="ExternalInput").ap()
    x1_d = nc.dram_tensor("x1", [TBK, D], F32, kind="ExternalOutput").ap()
    h2_d = nc.dram_tensor("h2T", [D, TBK], BF16, kind="ExternalOutput").ap()
    rw_d = nc.dram_tensor("rw", [TBK, 64], F32, kind="ExternalOutput").ap()
    with ExitStack() as st:
        p = Prog(nc, st)
        sb = lambda name, shape, dt: _sb(nc, st, name, shape, dt)
        hT = sb("hT", [128, DC, 512], BF16)
        oT = sb("oT", [128, 24, 512], BF16)
        zT = sb("zT", [128, DC, 512], BF16)
        stg = sb("stg", [128, 4096], F32)
        wg_r = Rot([sb("wg%d" % i, [128, DC * 128], BF16) for i in range(3)])
        wb_r = Rot([sb("wb%d" % i, [128, 8 * 128], BF16) for i in range(3)])
        wo = sb("wo", [128, DC, 512], BF16)
        g1B = sb("g1B", [128, D], F32)
        lgB = sb("lgB", [128, D], F32)
        lbB = sb("lbB", [128, D], F32)
        r_r = Rot([sb("r%d" % i, [128, D], F32) for i in range(4)])
        x1t = sb("x1t", [128, D], F32)
        xres = sb("xres", [128, 512], F32)
        h2f = sb("h2f", [128, DC, 128], F32)
        h2b = sb("h2b", [128, DC, 128], BF16)
        wr = sb("wr", [128, DC, 72], F32)
        brB = sb("brB", [128, 72], F32)
        ident = sb("ident", [128, 128], F32)
        modT = sb("modT", [128, 6, DC], F32)
        bgT = sb("bgT", [128, 48], F32)
        gs_r = Rot([sb("gs%d" % i, [128, 512], F32) for i in range(2)])
        zt_r = Rot([sb("zt%d" % i, [128, 512], F32) for i in range(2)])
        zacc = sb("zacc", [128, 512], F32)
        stats = sb("stats", [128, 32], F32)
        rt = sb("rt", [128, 512], F32)
        banks = [_ps(nc, st, "bk%d" % i, [128, 512], F32) for i in range(8)]
        scr = Rot(banks)
        t_c, t_stg, t_hT, t_oT, t_zT, t_wo, t_x1, t_xres, t_h2, t_st, t_rt, t_zacc = [Tk() for _ in range(12)]

        p.dma("sp", ident[:], id_d[:, :], writes=[t_c])
        p.dma("sp", modT[:], mod.rearrange("m (k p) -> p m k", p=128), writes=[t_c], slow=True)
        p.dma("sp", bgT[:], bg_d.rearrange("(j p) -> p j", p=128), writes=[t_c], slow=True)
        p.dma("sp", g1B[:], mod[2, :].partition_broadcast(128), writes=[t_c])
        p.dma("sp", lgB[:], ln_d[0, :].partition_broadcast(128), writes=[t_c])
        p.dma("sp", lbB[:], ln_d[1, :].partition_broadcast(128), writes=[t_c])
        p.dma("sp", brB[:], br_d.partition_broadcast(128), writes=[t_c])
        p.dma("sp", wr[:], wr_d.rearrange("(k p) n -> p k n", p=128), writes=[t_c])

        for hf in range(NH):
            for t in range(4):
                r0 = hf * 512 + t * 128
                p.dma("sp", stg[:, 0:D], x[r0:r0 + 128, :], writes=[t_stg])
                for c4 in range(4):
                    bk, tk = scr.next()
                    for j in range(4):
                        c = c4 * 4 + j
                        p.op("pe", lambda e, bk=bk, j=j, c=c: e.transpose(
                            bk[:, j * 128:(j + 1) * 128], stg[:, c * 128:(c + 1) * 128], ident[:]),
                            reads=[t_stg, t_c], writes=[tk])
                    for j in range(4):
                        c = c4 * 4 + j
                        p.op("act", lambda e, bk=bk, j=j, c=c, t=t: e.activation(
                            out=hT[:, c, t * 128:(t + 1) * 128], in_=bk[:, j * 128:(j + 1) * 128], func=AF.Identity,
                            bias=modT[:, 1, c:c + 1], scale=modT[:, 0, c:c + 1]), reads=[tk, t_c], writes=[t_hT])
                p.dma("sp", stg[:, 0:3072], o[r0:r0 + 128, :], writes=[t_stg])
                for c4 in range(6):
                    bk, tk = scr.next()
                    for j in range(4):
                        c = c4 * 4 + j
                        p.op("pe", lambda e, bk=bk, j=j, c=c: e.transpose(
                            bk[:, j * 128:(j + 1) * 128], stg[:, c * 128:(c + 1) * 128], ident[:]),
                            reads=[t_stg, t_c], writes=[tk])
                    p.op("dve", lambda e, bk=bk, c4=c4, t=t: e.tensor_copy(
                        out=oT[:, c4 * 4:c4 * 4 + 4, t * 128:(t + 1) * 128],
                        in_=bk[:, :].rearrange("p (j q) -> p j q", q=128)), reads=[tk], writes=[t_oT])
            blocks = [(dc, n) for dc in range(DC) for n in range(3)]
            wbufs = {}

            def issue(bi):
                dc, n = blocks[bi]
                blk = n * DC + dc
                wgb, wgtk = wg_r.next()
                p.dma("pool", wgb[:], wg_d[blk, :, :], writes=[wgtk])
                wbb, wbtk = wb_r.next()
                p.dma("pool", wbb[:], wb_d[blk, :, :], writes=[wbtk])
                wbufs[bi] = (wgb, wgtk, wbb, wbtk)

            issue(0)
            issue(1)
            for bi, (dc, n) in enumerate(blocks):
                if bi + 2 < len(blocks):
                    issue(bi + 2)
                blk = n * DC + dc
                wgb, wgtk, wbb, wbtk = wbufs.pop(bi)
                gbk, gtk = scr.next()
                for k in range(DC):
                    p.op("pe", lambda e, gbk=gbk, wgb=wgb, k=k: e.matmul(
                        gbk[:, :], wgb[:, k * 128:(k + 1) * 128], hT[:, k, :], start=(k == 0), stop=(k == DC - 1)),
                        reads=[wgtk, t_hT], writes=[gtk])
                gs, gstk = gs_r.next()
                p.op("act", lambda e, gbk=gbk, gs=gs, blk=blk: e.activation(
                    out=gs[:], in_=gbk[:, :], func=AF.Sigmoid, bias=bgT[:, blk:blk + 1], scale=1.0),
                    reads=[gtk, t_c], writes=[gstk])
                ybk, ytk = scr.next()
                for f in range(8):
                    p.op("pe", lambda e, ybk=ybk, wbb=wbb, f=f, n=n: e.matmul(
                        ybk[:, :], wbb[:, f * 128:(f + 1) * 128], oT[:, n * 8 + f, :], start=(f == 0), stop=(f == 7)),
                        reads=[wbtk, t_oT], writes=[ytk])
                if n == 0:
                    p.op("dve", lambda e, ybk=ybk, gs=gs: e.tensor_tensor(out=zacc[:], in0=ybk[:, :], in1=gs[:], op=ALU.mult),
                         reads=[ytk, gstk], writes=[t_zacc])
                else:
                    zt, zttk = zt_r.next()
                    p.op("dve", lambda e, ybk=ybk, gs=gs, zt=zt: e.tensor_tensor(out=zt[:], in0=ybk[:, :], in1=gs[:], op=ALU.mult),
                         reads=[ytk, gstk], writes=[zttk])
                    if n == 1:
                        p.op("dve", lambda e, zt=zt: e.tensor_tensor(out=zacc[:], in0=zacc[:], in1=zt[:], op=ALU.add),
                             reads=[zttk, t_zacc], writes=[t_zacc])
                    else:
                        p.op("dve", lambda e, zt=zt, dc=dc: e.tensor_tensor(out=zT[:, dc, :], in0=zacc[:], in1=zt[:], op=ALU.add),
                             reads=[zttk, t_zacc], writes=[t_zT])
            rts = [r_r.next() for _ in range(4)]
            for cbk in range(4):
                for k in range(DC):
                    p.dma("pool", wo[:, k, :], wo_d[cbk, k, :, :], writes=[t_wo])
                for t in range(4):
                    r0 = hf * 512 + t * 128
                    rb, rtk = rts[t]
                    bk, tk = scr.next()
                    for k in range(DC):
                        p.op("pe", lambda e, bk=bk, k=k, t=t: e.matmul(
                            bk[:, :], zT[:, k, t * 128:(t + 1) * 128], wo[:, k, :], start=(k == 0), stop=(k == DC - 1)),
                            reads=[t_zT, t_wo], writes=[tk])
                    p.dma("sp", xres[:], x[r0:r0 + 128, cbk * 512:(cbk + 1) * 512], writes=[t_xres])
                    p.op("dve", lambda e, bk=bk, rb=rb, cbk=cbk: e.tensor_tensor(
                        out=rb[:, cbk * 512:(cbk + 1) * 512], in0=bk[:, :], in1=g1B[:, cbk * 512:(cbk + 1) * 512], op=ALU.mult),
                        reads=[tk, t_c], writes=[rtk])
                    p.op("dve", lambda e, rb=rb, cbk=cbk: e.scalar_tensor_tensor(
                        out=rb[:, cbk * 512:(cbk + 1) * 512], in0=xres[:], scalar=ALPHA, in1=rb[:, cbk * 512:(cbk + 1) * 512],
                        op0=ALU.mult, op1=ALU.add), reads=[t_xres, rtk], writes=[rtk])
            for t in range(4):
                r0 = hf * 512 + t * 128
                rb, rtk = rts[t]
                emit_layernorm(p, rb, rtk, x1t, t_x1, lgB, lbB, t_c, stats, t_st)
                p.dma("sp", x1_d[r0:r0 + 128, :], x1t[:], reads=[t_x1], is_out=True)
                for c4 in range(4):
                    bk, tk = scr.next()
                    for j in range(4):
                        c = c4 * 4 + j
                        p.op("pe", lambda e, bk=bk, j=j, c=c: e.transpose(
                            bk[:, j * 128:(j + 1) * 128], x1t[:, c * 128:(c + 1) * 128], ident[:]),
                            reads=[t_x1, t_c], writes=[tk])
                    for j in range(4):
                        c = c4 * 4 + j
                        p.op("act", lambda e, bk=bk, j=j, c=c: e.activation(
                            out=h2f[:, c, :], in_=bk[:, j * 128:(j + 1) * 128], func=AF.Identity,
                            bias=modT[:, 4, c:c + 1], scale=modT[:, 3, c:c + 1]), reads=[tk, t_c], writes=[t_h2])
                p.op("pool", lambda e: e.tensor_copy(out=h2b[:], in_=h2f[:]), reads=[t_h2], writes=[t_h2])
                p.dma("sp", h2_d[:, r0:r0 + 128].rearrange("(k p) t -> p k t", p=128), h2b[:], reads=[t_h2], is_out=True)
                lbk, ltk = scr.next()
                for k in range(DC):
                    p.op("pe", lambda e, lbk=lbk, k=k: e.matmul(lbk[:, 0:72], h2f[:, k, :], wr[:, k, :], start=(k == 0),
                                                                  stop=(k == DC - 1)), reads=[t_h2, t_c], writes=[ltk])
                R = rt
                T = [t_rt]
                p.op("dve", lambda e, lbk=lbk: e.tensor_tensor(out=R[:, 0:72], in0=lbk[:, 0:72], in1=brB[:], op=ALU.add),
                     reads=[ltk, t_c], writes=T)
                p.op("dve", lambda e: e.reduce_max(out=R[:, 72:73], in_=R[:, 0:8], axis=AX.X), reads=T, writes=T)
                p.op("dve", lambda e: e.tensor_scalar(out=R[:, 73:74], in0=R[:, 72:73], scalar1=-1.0, scalar2=None, op0=ALU.mult),
                     reads=T, writes=T)
                p.op("act", lambda e: e.activation(out=R[:, 80:88], in_=R[:, 0:8], func=AF.Exp, bias=R[:, 73:74], scale=1.0),
                     reads=T, writes=T)
                p.op("dve", lambda e: e.reduce_sum(out=R[:, 88:89], in_=R[:, 80:88], axis=AX.X), reads=T, writes=T)
                p.op("dve", lambda e: e.reciprocal(out=R[:, 89:90], in_=R[:, 88:89]), reads=T, writes=T)
                p.op("dve", lambda e: e.tensor_scalar(out=R[:, 96:104], in0=R[:, 0:8], scalar1=R[:, 72:73], scalar2=None,
                                                      op0=ALU.is_ge), reads=T, writes=T)
                p.op("dve", lambda e: e.tensor_scalar(out=R[:, 104:112], in0=R[:, 96:104], scalar1=-1.0, scalar2=30000.0,
                                                      op0=ALU.add, op1=ALU.mult), reads=T, writes=T)
                for gi in range(8):
                    p.op("dve", lambda e, gi=gi: e.tensor_scalar(
                        out=R[:, 128 + gi * 8:136 + gi * 8], in0=R[:, 8 + gi * 8:16 + gi * 8], scalar1=R[:, 104 + gi:105 + gi],
                        scalar2=None, op0=ALU.add), reads=T, writes=T)
                p.op("dve", lambda e: e.max(out=R[:, 192:200], in_=R[:, 128:192]), reads=T, writes=T)
                p.op("dve", lambda e: e.tensor_tensor(out=R[:, 200:201], in0=R[:, 192:193], in1=R[:, 193:194], op=ALU.subtract),
                     reads=T, writes=T)
                p.op("act", lambda e: e.activation(out=R[:, 201:202], in_=R[:, 200:201], func=AF.Sigmoid), reads=T, writes=T)
                p.op("dve", lambda e: e.tensor_tensor(out=R[:, 202:203], in0=R[:, 201:202], in1=R[:, 89:90], op=ALU.mult),
                     reads=T, writes=T)
                p.op("dve", lambda e: e.tensor_tensor(out=R[:, 203:204], in0=R[:, 89:90], in1=R[:, 202:203], op=ALU.subtract),
                     reads=T, writes=T)
                p.op("dve", lambda e: e.tensor_tensor(out=R[:, 204:205], in0=R[:, 202:203], in1=R[:, 203:204], op=ALU.subtract),
                     reads=T, writes=T)
                p.op("dve", lambda e: e.tensor_scalar(out=R[:, 256:320], in0=R[:, 128:192], scalar1=R[:, 193:194], scalar2=R[:, 203:204],
                                                      op0=ALU.is_ge, op1=ALU.mult), reads=T, writes=T)
                p.op("dve", lambda e: e.tensor_scalar(out=R[:, 320:384], in0=R[:, 128:192], scalar1=R[:, 192:193], scalar2=R[:, 204:205],
                                                      op0=ALU.is_ge, op1=ALU.mult), reads=T, writes=T)
                p.op("dve", lambda e: e.tensor_tensor(out=R[:, 384:448], in0=R[:, 256:320], in1=R[:, 320:384], op=ALU.add),
                     reads=T, writes=T)
                p.dma("sp", rw_d[r0:r0 + 128, :], R[:, 384:448], reads=T, is_out=True)
        p.finish()
    return nc


def phaseB_weights(w_gate, b_gate, w_branch, w_out, ln_g, ln_b, w_rg, b_rg, w_re, b_re):
    wg = np.ascontiguousarray(w_gate.reshape(DC, 128, 48, 128).transpose(2, 1, 0, 3).reshape(48, 128, DC * 128))
    wb = np.ascontiguousarray(w_branch.reshape(3, 8, 128, DC, 128).transpose(0, 3, 2, 1, 4).reshape(48, 128, 8 * 128))
    wo = np.ascontiguousarray(w_out.reshape(DC, 128, 4, 512).transpose(2, 0, 1, 3))
    return {"wg": wg, "bg": np.ascontiguousarray(b_gate), "wb": wb, "wo": wo,
            "ln": np.ascontiguousarray(np.stack([ln_g, ln_b])),
            "wr": np.ascontiguousarray(np.concatenate([w_rg, w_re], axis=1)),
            "br": np.ascontiguousarray(np.concatenate([b_rg, b_re])),
            "ident": np.eye(128, dtype=np.float32)}


FE = 384


def build_phaseC(S):
    NT = S // 1024
    nc = bass.Bass("TRN2", target_bir_lowering=False)
    h2_d = nc.dram_tensor("h2T", [D, S], BF16, kind="ExternalInput").ap()
    rw_d = nc.dram_tensor("rw", [S, 8], F32, kind="ExternalInput").ap()
    wgu_d = nc.dram_tensor("wgu", [8, 2, 128, DC * FE], F32, kind="ExternalInput").ap()
    wd_d = nc.dram_tensor("wd", [8, 128, 3 * D], F32, kind="ExternalInput").ap()
    y_d = nc.dram_tensor("y", [S, D], BF16, kind="ExternalOutput").ap()
    with ExitStack() as st:
        p = Prog(nc, st)
        sb = lambda name, shape, dt: _sb(nc, st, name, shape, dt)
        h2 = sb("h2", [128, DC, 1024], BF16)
        rwt = sb("rwt", [128, 8, 8], F32)
        Yacc = sb("Yacc", [128, 8, D], F32)
        wg_r = Rot([sb("wgt%d" % i, [128, DC * FE], BF16) for i in range(2)])
        wu_r = Rot([sb("wut%d" % i, [128, DC * FE], BF16) for i in range(2)])
        wd_r = Rot([sb("wdt%d" % i, [128, 3 * D], BF16) for i in range(2)])
        A = sb("A", [128, 3, 1024], BF16)
        sg_r = Rot([sb("sg%d" % i, [128, 512], F32) for i in range(2)])
        banks = [_ps(nc, st, "bk%d" % i, [128, 512], F32) for i in range(8)]
        scr = Rot(banks)
        t_h2, t_rw, t_A = Tk(), Tk(), Tk()
        t_y = [Tk() for _ in range(8)]
        for tc in range(NT):
            t0 = tc * 1024
            p.dma("sp", h2[:], h2_d[:, t0:t0 + 1024].rearrange("(k p) t -> p k t", p=128), writes=[t_h2])
            p.dma("sp", rwt[:], rw_d[t0:t0 + 1024, :].rearrange("(t p) e -> p t e", p=128), writes=[t_rw])
            for ex in range(8):
                wgb, wgtk = wg_r.next()
                wub, wutk = wu_r.next()
                wdb, wdtk = wd_r.next()
                for j in range(3):
                    p.dma("pool", wgb[:, j * 2048:(j + 1) * 2048], wgu_d[ex, 0, :, j * 2048:(j + 1) * 2048], writes=[wgtk])
                    p.dma("pool", wub[:, j * 2048:(j + 1) * 2048], wgu_d[ex, 1, :, j * 2048:(j + 1) * 2048], writes=[wutk])
                for j in range(3):
                    p.dma("pool", wdb[:, j * 2048:(j + 1) * 2048], wd_d[ex, :, j * 2048:(j + 1) * 2048], writes=[wdtk])
                for f in range(3):
                    for nh in range(2):
                        gbk, gtk = scr.next()
                        for k in range(DC):
                            p.op("pe", lambda e, gbk=gbk, wgb=wgb, k=k, f=f, nh=nh: e.matmul(
                                gbk[:, :], wgb[:, k * FE + f * 128:k * FE + (f + 1) * 128], h2[:, k, nh * 512:(nh + 1) * 512],
                                start=(k == 0), stop=(k == DC - 1)), reads=[wgtk, t_h2], writes=[gtk])
                        ubk, utk = scr.next()
                        for k in range(DC):
                            p.op("pe", lambda e, ubk=ubk, wub=wub, k=k, f=f, nh=nh: e.matmul(
                                ubk[:, :], wub[:, k * FE + f * 128:k * FE + (f + 1) * 128], h2[:, k, nh * 512:(nh + 1) * 512],
                                start=(k == 0), stop=(k == DC - 1)), reads=[wutk, t_h2], writes=[utk])
                        sg, sgtk = sg_r.next()
                        p.op("act", lambda e, gbk=gbk, sg=sg: e.activation(out=sg[:], in_=gbk[:, :], func=AF.Silu),
                             reads=[gtk], writes=[sgtk])
                        p.op("dve", lambda e, ubk=ubk, sg=sg, f=f, nh=nh: e.tensor_tensor(
                            out=A[:, f, nh * 512:(nh + 1) * 512], in0=ubk[:, :], in1=sg[:], op=ALU.mult),
                            reads=[utk, sgtk], writes=[t_A])
                for t in range(8):
                    for db in range(4):
                        ybk, ytk = scr.next()
                        for f in range(3):
                            p.op("pe", lambda e, ybk=ybk, wdb=wdb, f=f, t=t, db=db: e.matmul(
                                ybk[:, :], A[:, f, t * 128:(t + 1) * 128], wdb[:, f * D + db * 512:f * D + (db + 1) * 512],
                                start=(f == 0), stop=(f == 2)), reads=[t_A, wdtk], writes=[ytk])
                        if ex == 0:
                            p.op("dve", lambda e, ybk=ybk, t=t, db=db, ex=ex: e.tensor_scalar(
                                out=Yacc[:, t, db * 512:(db + 1) * 512], in0=ybk[:, :], scalar1=rwt[:, t, ex:ex + 1], scalar2=None,
                                op0=ALU.mult), reads=[ytk, t_rw], writes=[t_y[t]])
                        else:
                            p.op("dve", lambda e, ybk=ybk, t=t, db=db, ex=ex: e.scalar_tensor_tensor(
                                out=Yacc[:, t, db * 512:(db + 1) * 512], in0=ybk[:, :], scalar=rwt[:, t, ex:ex + 1],
                                in1=Yacc[:, t, db * 512:(db + 1) * 512], op0=ALU.mult, op1=ALU.add),
                                reads=[ytk, t_rw, t_y[t]], writes=[t_y[t]])
            for t in range(8):
                p.dma("pool", y_d[t0 + t * 128:t0 + (t + 1) * 128, :], Yacc[:, t, :], reads=[t_y[t]], is_out=True)
        p.finish()
    return nc


def phaseC_weights(w_g, w_u, w_d):
    g = w_g.reshape(8, DC, 128, FE).transpose(0, 2, 1, 3).reshape(8, 128, DC * FE)
    u = w_u.reshape(8, DC, 128, FE).transpose(0, 2, 1, 3).reshape(8, 128, DC * FE)
    wgu = np.ascontiguousarray(np.stack([g, u], axis=1))
    wd = np.ascontiguousarray(w_d.reshape(8, 3, 128, D).transpose(0, 2, 1, 3).reshape(8, 128, 3 * D))
    return {"wgu": wgu, "wd": wd}


def build_phaseD(TBK=TB):
    NTT = TBK // 128
    nc = bass.Bass("TRN2", target_bir_lowering=False)
    x1_d = nc.dram_tensor("x1", [TBK, D], F32, kind="ExternalInput").ap()
    yg_d = nc.dram_tensor("yg", [8, TBK, D], BF16, kind="ExternalInput").ap()
    mod = nc.dram_tensor("mod", [6, D], F32, kind="ExternalInput").ap()
    ln_d = nc.dram_tensor("ln", [2, D], F32, kind="ExternalInput").ap()
    x2_d = nc.dram_tensor("x2", [TBK, D], F32, kind="ExternalOutput").ap()
    with ExitStack() as st:
        p = Prog(nc, st)
        sb = lambda name, shape, dt: _sb(nc, st, name, shape, dt)
        g2B = sb("g2B", [128, D], F32)
        lgB = sb("lgB", [128, D], F32)
        lbB = sb("lbB", [128, D], F32)
        yt_r = Rot([sb("yt%d" % i, [128, 8, D], BF16) for i in range(2)])
        x_r = Rot([sb("xt%d" % i, [128, D], F32) for i in range(2)])
        r_r = Rot([sb("rr%d" % i, [128, D], F32) for i in range(2)])
        o_r = Rot([sb("xo%d" % i, [128, D], F32) for i in range(2)])
        stats = sb("stats", [128, 32], F32)
        t_c, t_st = Tk(), Tk()
        p.dma("sp", g2B[:], mod[5, :].partition_broadcast(128), writes=[t_c])
        p.dma("sp", lgB[:], ln_d[0, :].partition_broadcast(128), writes=[t_c])
        p.dma("sp", lbB[:], ln_d[1, :].partition_broadcast(128), writes=[t_c])
        for t in range(NTT):
            r0 = t * 128
            yt, yttk = yt_r.next()
            p.dma("sp", yt[:], yg_d[:, r0:r0 + 128, :].rearrange("g p d -> p g d"), writes=[yttk])
            xt, xtk = x_r.next()
            p.dma("sp", xt[:], x1_d[r0:r0 + 128, :], writes=[xtk])
            rb, rtk = r_r.next()
            p.op("dve", lambda e, rb=rb, yt=yt: e.tensor_tensor(out=rb[:], in0=yt[:, 0, :], in1=yt[:, 1, :], op=ALU.add),
                 reads=[yttk], writes=[rtk])
            for gi in range(2, 8):
                eng = "dve" if gi % 2 == 0 else "pool"
                p.op(eng, lambda e, rb=rb, yt=yt, gi=gi: e.tensor_tensor(out=rb[:], in0=rb[:], in1=yt[:, gi, :], op=ALU.add),
                     reads=[yttk, rtk], writes=[rtk])
            p.op("pool", lambda e, rb=rb: e.tensor_tensor(out=rb[:], in0=rb[:], in1=g2B[:], op=ALU.mult),
                 reads=[rtk, t_c], writes=[rtk])
            p.op("dve", lambda e, rb=rb, xt=xt: e.scalar_tensor_tensor(out=rb[:], in0=xt[:], scalar=ALPHA, in1=rb[:],
                                                                      op0=ALU.mult, op1=ALU.add),
                 reads=[xtk, rtk], writes=[rtk])
            xo, xotk = o_r.next()
            emit_layernorm(p, rb, rtk, xo, xotk, lgB, lbB, t_c, stats, t_st)
            p.dma("sp", x2_d[r0:r0 + 128, :], xo[:], reads=[xotk], is_out=True)
        p.finish()
    return nc


def _run(nc, maps):
    res = run_bass_kernel_spmd(nc, maps, core_ids=list(range(NCORE)))
    return res.results


def kernel(x, c, w_ada, b_ada, w_in, w_branch_gate, b_branch_gate, attn_sinks, lambda_q1, lambda_k1, lambda_q2,
           lambda_k2, subln_g, w_branch, w_out, ln1_g, ln1_b, w_router_group, b_router_group, w_router_expert,
           b_router_expert, w_exp_gate, w_exp_up, w_exp_down, ln2_g, ln2_b):
    f32 = lambda a: np.asarray(a, np.float32)
    xs = np.ascontiguousarray(f32(x)[0])
    mod_all = run_phase0(f32(c), f32(w_ada), f32(b_ada))
    ncA = build_phaseA2(SEQ)
    ncB = build_phaseB(TB)
    ncC = [None]
    ncC2 = {}
    ncD = build_phaseD(TB)
    for l in range(DEPTH):
        lam_init = 0.8 - 0.6 * math.exp(-0.3 * l)
        m = mod_all[l].reshape(6, D)
        mod6 = np.ascontiguousarray(np.stack([m[1], m[0], m[2], m[4], m[3], m[5]]))
        maps = [phaseA_inputs(i, SEQ, xs, mod6[0:2], f32(w_in[l]), f32(attn_sinks[l]), f32(lambda_q1[l]),
                              f32(lambda_k1[l]), f32(lambda_q2[l]), f32(lambda_k2[l]), f32(subln_g[l]), lam_init)
                for i in range(NCORE)]
        ra = _run(ncA, maps)
        o_cat = np.empty((SEQ, 3, 1024), np.float32)
        for i in range(NCORE):
            o_cat[:, :, 128 * i:128 * (i + 1)] = ra[i]["o"].reshape(SEQ, 3, 128)
        o_cat = o_cat.reshape(SEQ, 3072)
        del ra, maps
        wts = phaseB_weights(f32(w_branch_gate[l]), f32(b_branch_gate[l]), f32(w_branch[l]), f32(w_out[l]), f32(ln1_g[l]),
                             f32(ln1_b[l]), f32(w_router_group[l]), f32(b_router_group[l]), f32(w_router_expert[l]),
                             f32(b_router_expert[l]))
        maps = []
        for i in range(NCORE):
            mm = dict(wts)
            mm.update({"x": np.ascontiguousarray(xs[i * TB:(i + 1) * TB]),
                       "o": np.ascontiguousarray(o_cat[i * TB:(i + 1) * TB]), "mod": mod6})
            maps.append(mm)
        rb = _run(ncB, maps)
        x1 = [rb[i]["x1"] for i in range(NCORE)]
        h2T = np.ascontiguousarray(np.concatenate([rb[i]["h2T"] for i in range(NCORE)], axis=1))
        rw = np.concatenate([rb[i]["rw"] for i in range(NCORE)], axis=0)
        del rb, maps, wts, o_cat
        sizes = [int(np.count_nonzero(rw[:, 8 * g:8 * g + 8].max(axis=1) > 0)) for g in range(NCORE)]
        cap = max(1024, -(-max(sizes) // 512) * 512)
        print("[kernel] layer", l, "group sizes", sizes, "cap", cap, flush=True)
        maps = []
        if cap <= 3072:
            if cap not in ncC2:
                ncC2[cap] = build_phaseC2(SEQ, cap)
            x1_full = np.ascontiguousarray(np.concatenate(x1, axis=0))
            c2c = phaseC2_consts(SEQ)
            modC = np.ascontiguousarray(mod6[3:5])
            for g in range(NCORE):
                mm = phaseC_weights(f32(w_exp_gate[l, 8 * g:8 * g + 8]), f32(w_exp_up[l, 8 * g:8 * g + 8]),
                                    f32(w_exp_down[l, 8 * g:8 * g + 8]))
                mm.update(c2c)
                mm.update({"x1": x1_full, "rw": np.ascontiguousarray(rw[:, 8 * g:8 * g + 8]), "mod": modC})
                maps.append(mm)
            rc = _run(ncC2[cap], maps)
            del x1_full
        else:
            if ncC[0] is None:
                ncC[0] = build_phaseC(SEQ)
            for g in range(NCORE):
                mm = phaseC_weights(f32(w_exp_gate[l, 8 * g:8 * g + 8]), f32(w_exp_up[l, 8 * g:8 * g + 8]),
                                    f32(w_exp_down[l, 8 * g:8 * g + 8]))
                mm.update({"h2T": h2T, "rw": np.ascontiguousarray(rw[:, 8 * g:8 * g + 8])})
                maps.append(mm)
            rc = _run(ncC[0], maps)
        del maps
        ln2 = np.ascontiguousarray(np.stack([f32(ln2_g[l]), f32(ln2_b[l])]))
        maps = []
        for i in range(NCORE):
            yg = np.ascontiguousarray(np.stack([rc[g]["y"][i * TB:(i + 1) * TB] for g in range(NCORE)]))
            maps.append({"x1": x1[i], "yg": yg, "mod": mod6, "ln": ln2})
        del rc
        rd = _run(ncD, maps)
        xs = np.ascontiguousarray(np.concatenate([rd[i]["x2"] for i in range(NCORE)], axis=0))
        del rd, maps
    return xs[None].astype(np.float32)


def phaseA_consts2(core, S):
    NQ = S // 128
    sl_swa = [2.0 ** (-8.0 * (h + 1) / 16.0) for h in (2 * core, 2 * core + 1)]
    sl_d = 2.0 ** (-8.0 * (core + 1) / 8.0)
    p = np.arange(128)[:, None].astype(np.float64)
    t = np.arange(128)[None, :].astype(np.float64)
    swab = np.zeros((128, 4, 128), np.float32)
    for hh in range(2):
        d1 = t - p
        swab[:, 2 * hh, :] = np.where(d1 >= 0, -sl_swa[hh] * d1, NEG)
        d0 = 128 + t - p
        swab[:, 2 * hh + 1, :] = np.where(d0 < 128, -sl_swa[hh] * d0, NEG)
    nj = NQ + 3
    jj = np.arange(nj)[None, :] - 3
    dbias = (sl_d * p - sl_d * 128.0 * jj).astype(np.float32)
    ident = np.eye(128, dtype=np.float32)
    cf = np.concatenate([swab.reshape(128, 512), dbias, ident], axis=1)
    tri = (p >= t).astype(np.float32)
    ones = np.ones((128, 128), np.float32)
    strict = (p < t).astype(np.float32)
    incl = (p <= t).astype(np.float32)
    upper = (p < t).astype(np.float32)
    tl = np.arange(512).astype(np.float64)
    v = (-8.0 * sl_d * tl)
    hi = v.astype(np.float32).astype(ml_dtypes.bfloat16)
    lo = (v - hi.astype(np.float64)).astype(np.float32).astype(ml_dtypes.bfloat16)
    qaug = np.zeros((128, 512), np.float32)
    qaug[0] = hi.astype(np.float32)
    qaug[1] = lo.astype(np.float32)
    qaug[64] = hi.astype(np.float32)
    qaug[65] = lo.astype(np.float32)
    cb = _bf(np.concatenate([tri, ones, strict, incl, qaug, upper], axis=1))
    return np.ascontiguousarray(cf), np.ascontiguousarray(cb)


def build_phaseA2(S, parts=("swa", "sb", "df"), ndummy=0):
    NQ = S // 128
    NG = S // 512
    NJ = NQ + 3
    NCF = 512 + NJ + 128
    nc = bass.Bass("TRN2", target_bir_lowering=False)
    x = nc.dram_tensor("x", [S, D], F32, kind="ExternalInput").ap()
    mod = nc.dram_tensor("mod", [2, D], F32, kind="ExternalInput").ap()
    w_t = nc.dram_tensor("w_t", [D, A_WT], F32, kind="ExternalInput").ap()
    w_v = nc.dram_tensor("w_v", [D, A_WV], F32, kind="ExternalInput").ap()
    cf_d = nc.dram_tensor("cf", [128, NCF], F32, kind="ExternalInput").ap()
    cb_d = nc.dram_tensor("cb", [128, 1152], BF16, kind="ExternalInput").ap()
    sm_d = nc.dram_tensor("sm", [1, A_SM], F32, kind="ExternalInput").ap()
    o_d = nc.dram_tensor("o", [S, 384], F32, kind="ExternalOutput").ap()
    with ExitStack() as st:
        p = Prog(nc, st)
        sb = lambda name, shape, dt: _sb(nc, st, name, shape, dt)
        wT = sb("wT", [128, DC, A_WT], BF16)
        wV = sb("wV", [128, DC, A_WV], BF16)
        KA = sb("KA", [128, S], BF16)
        KB = sb("KB", [128, S], BF16)
        KC = sb("KC", [128, S], BF16)
        V = sb("V", [128, NQ, VW], BF16)
        xs = sb("xs", [128, D], F32)
        hT = sb("hT", [128, DC, 512], BF16)
        QA = sb("QA", [128, 512], BF16)
        QB = sb("QB", [128, 512], BF16)
        QC = sb("QC", [128, 512], BF16)
        cf = sb("cfs", [128, NCF], F32)
        cb = sb("cbs", [128, 1152], BF16)
        sm = sb("sms", [128, A_SM], F32)
        sclT = sb("sclT", [128, DC], F32)
        shT = sb("shT", [128, DC], F32)
        misc = sb("misc", [128, 16], F32)
        gsub = sb("gsub", [128, 128], F32)
        junk = sb("junk", [128, 128], F32)
        ost = [sb("ost%d" % i, [128, 4, 384], F32) for i in range(2)]
        t_ost = [Tk(), Tk()]
        banks = [_ps(nc, st, "bk%d" % i, [128, 512], F32) for i in range(8)]
        scr = Rot(banks[0:2])
        CARRY = banks[2:4]
        t_carry = [Tk(), Tk()]
        SBACC, t_sbacc = banks[4], Tk()
        DACC = banks[5:8]
        t_dacc = [Tk() for _ in range(3)]
        e_r = Rot([sb("e%d" % i, [128, 512], F32) for i in range(4)])
        L_r = Rot([sb("L%d" % i, [128, 512], BF16) for i in range(6)])
        W_r = Rot([sb("W%d" % i, [128, 512], BF16) for i in range(4)])
        E_r = Rot([sb("E%d" % i, [128, 512], BF16) for i in range(4)])
        X_r = Rot([sb("X%d" % i, [128, 512], BF16) for i in range(4)])
        sE_r = Rot([sb("sE%d" % i, [128, 512], BF16) for i in range(2)])
        fin_r = Rot([sb("fin%d" % i, [128, 8], F32) for i in range(2)])
        da_r = Rot([sb("da%d" % i, [128, 128], F32) for i in range(2)])
        dd_r = Rot([sb("dd%d" % i, [128, 128], F32) for i in range(2)])

        t_c = Tk()
        t_k = [Tk() for _ in range(NG)]
        t_xs, t_hT, t_q, t_misc = Tk(), Tk(), Tk(), Tk()
        t_junk = Tk()

        swab = cf[:, 0:512]
        dbias = lambda j: cf[:, 512 + j:512 + j + 1]
        ident = cf[:, 512 + NJ:512 + NJ + 128]
        tri = cb[:, 0:128]
        ones = cb[:, 128:256]
        strict = cb[:, 256:384]
        incl = cb[:, 384:512]
        qaug = cb[:, 512:1024]
        upper = cb[:, 1024:1152]

        p.dma("sp", cf[:], cf_d[:, :], writes=[t_c])
        p.dma("sp", cb[:], cb_d[:, :], writes=[t_c])
        p.dma("sp", sm[:], sm_d[0, :].partition_broadcast(128), writes=[t_c])
        p.dma("sp", sclT[:], mod[0, :].rearrange("(k p) -> p k", p=128), writes=[t_c], slow=True)
        p.dma("sp", shT[:], mod[1, :].rearrange("(k p) -> p k", p=128), writes=[t_c], slow=True)
        for k in range(DC):
            p.dma("pool", wT[:, k, :], w_t[k * 128:(k + 1) * 128, :], writes=[t_c])
            p.dma("pool", wV[:, k, :], w_v[k * 128:(k + 1) * 128, :], writes=[t_c])
        p.op("pool", lambda e: e.memset(V[:, :, 64:65], 1.0), writes=[t_c])
        p.op("pool", lambda e: e.memset(V[:, :, 321:322], 1.0), writes=[t_c])
        p.op("act", lambda e: e.activation(out=misc[:, 0:2], in_=sm[:, 0:2], func=AF.Exp), reads=[t_c], writes=[t_misc])
        p.op("dve", lambda e: e.tensor_tensor(out=junk[:, 0:64], in0=sm[:, 2:66], in1=sm[:, 66:130], op=ALU.mult),
             reads=[t_c], writes=[t_misc])
        p.op("dve", lambda e: e.reduce_sum(out=misc[:, 2:3], in_=junk[:, 0:64], axis=AX.X), reads=[t_misc], writes=[t_misc])
        p.op("dve", lambda e: e.tensor_tensor(out=junk[:, 64:128], in0=sm[:, 130:194], in1=sm[:, 194:258], op=ALU.mult),
             reads=[t_c], writes=[t_misc])
        p.op("dve", lambda e: e.reduce_sum(out=misc[:, 3:4], in_=junk[:, 64:128], axis=AX.X), reads=[t_misc], writes=[t_misc])
        p.op("act", lambda e: e.activation(out=misc[:, 5:7], in_=misc[:, 2:4], func=AF.Exp), reads=[t_misc], writes=[t_misc])
        p.op("dve", lambda e: e.tensor_tensor(out=misc[:, 7:8], in0=misc[:, 6:7], in1=misc[:, 5:6], op=ALU.subtract),
             reads=[t_misc], writes=[t_misc])
        p.op("dve", lambda e: e.tensor_tensor(out=misc[:, 4:5], in0=misc[:, 7:8], in1=sm[:, 386:387], op=ALU.subtract),
             reads=[t_misc, t_c], writes=[t_misc])
        p.op("dve", lambda e: e.tensor_scalar(out=gsub[:], in0=sm[:, 258:386], scalar1=sm[:, 387:388], scalar2=None,
                                              op0=ALU.mult), reads=[t_c], writes=[t_misc])

        def dacc(m, qi):
            j = m * 4 + qi
            return DACC[j // 3], t_dacc[j // 3], (j % 3) * 129, j // 3

        for g in range(NG):
            for tt in range(4):
                r0 = g * 512 + tt * 128
                p.dma("sp", xs[:], x[r0:r0 + 128, :], writes=[t_xs])
                for c4 in range(4):
                    bk, tk = scr.next()
                    for j in range(4):
                        c = c4 * 4 + j
                        p.op("pe", lambda e, bk=bk, j=j, c=c: e.transpose(
                            bk[:, j * 128:(j + 1) * 128], xs[:, c * 128:(c + 1) * 128], ident),
                            reads=[t_xs, t_c], writes=[tk])
                    for j in range(4):
                        c = c4 * 4 + j
                        p.op("dve", lambda e, bk=bk, j=j, c=c, tt=tt: e.tensor_scalar(
                            out=hT[:, c, tt * 128:(tt + 1) * 128], in0=bk[:, j * 128:(j + 1) * 128], scalar1=sclT[:, c:c + 1],
                            scalar2=shT[:, c:c + 1], op0=ALU.mult, op1=ALU.add), reads=[tk, t_c], writes=[t_hT])
            dests = [QA[:, :], KA[:, g * 512:(g + 1) * 512], QB[:, :], KB[:, g * 512:(g + 1) * 512],
                     QC[:, :], KC[:, g * 512:(g + 1) * 512]]
            for m in range(6):
                bk, tk = scr.next()
                for k in range(DC):
                    p.op("pe", lambda e, bk=bk, k=k, m=m: e.matmul(
                        bk[:, :], wT[:, k, m * 128:(m + 1) * 128], hT[:, k, :], start=(k == 0), stop=(k == DC - 1)),
                        reads=[t_c, t_hT], writes=[tk])
                wtk = t_q if m % 2 == 0 else t_k[g]
                p.op("act", lambda e, bk=bk, m=m, dests=dests: e.activation(out=dests[m], in_=bk[:, :], func=AF.Identity),
                     reads=[tk], writes=[wtk])
            for t in range(4):
                bk, tk = scr.next()
                for k in range(DC):
                    p.op("pe", lambda e, bk=bk, k=k, t=t: e.matmul(
                        bk[:, 0:A_WV], hT[:, k, t * 128:(t + 1) * 128], wV[:, k, :], start=(k == 0), stop=(k == DC - 1)),
                        reads=[t_c, t_hT], writes=[tk])
                qb = g * 4 + t
                p.op("dve", lambda e, bk=bk, qb=qb: e.tensor_copy(out=V[:, qb, 0:64], in_=bk[:, 0:64]),
                     reads=[tk], writes=[t_k[g]])
                p.op("dve", lambda e, bk=bk, qb=qb: e.tensor_copy(out=V[:, qb, 65:321], in_=bk[:, 64:320]),
                     reads=[tk], writes=[t_k[g]])

            o_t, o_tk = ost[g % 2], t_ost[g % 2]

            swst = {}

            def sw_a(qi):
                i = g * 4 + qi
                kbs = [i] + ([i - 1] if i > 0 else [])
                wd = 128 * len(kbs)
                tm, ttk = e_r.next()
                sE, setk = sE_r.next()
                segs = []
                for hh in range(2):
                    bk, tk = scr.next()
                    for n, kb in enumerate(kbs):
                        p.op("pe", lambda e, bk=bk, hh=hh, kb=kb, n=n: e.matmul(
                            bk[:, n * 128:(n + 1) * 128], KA[64 * hh:64 * hh + 64, kb * 128:(kb + 1) * 128],
                            QA[64 * hh:64 * hh + 64, qi * 128:(qi + 1) * 128], start=True, stop=True),
                            reads=[t_k[kb // 4], t_q], writes=[tk])
                        segs.append((hh, kb, 2 * hh + n))
                    p.op("dve", lambda e, bk=bk, hh=hh: e.scalar_tensor_tensor(
                        out=tm[:, 256 * hh:256 * hh + wd], in0=bk[:, 0:wd], scalar=SCALE, in1=swab[:, 256 * hh:256 * hh + wd],
                        op0=ALU.mult, op1=ALU.add), reads=[tk, t_c], writes=[ttk])
                    p.op("act", lambda e, hh=hh: e.activation(out=sE[:, 256 * hh:256 * hh + wd], in_=tm[:, 256 * hh:256 * hh + wd],
                                                              func=AF.Exp), reads=[ttk], writes=[setk])
                swst[qi] = (segs, sE, setk)

            def sw_b(qi):
                segs, sE, setk = swst.pop(qi)
                abk, atk = scr.next()
                first = True
                for hh in range(2):
                    mine = [s for s in segs if s[0] == hh]
                    for n, (_, kb, slot) in enumerate(mine):
                        p.op("pe", lambda e, hh=hh, kb=kb, slot=slot, first=first: e.matmul(
                            abk[:, hh * 65:hh * 65 + 65], sE[:, slot * 128:(slot + 1) * 128], V[:, kb, 0:65],
                            start=first, stop=True, skip_group_check=True),
                            reads=[setk, t_k[kb // 4], t_c], writes=[atk])
                        first = False
                fb, ftk = fin_r.next()
                for hh in range(2):
                    p.op("dve", lambda e, hh=hh: e.tensor_tensor(
                        out=fb[:, hh:hh + 1], in0=abk[:, hh * 65 + 64:hh * 65 + 65], in1=misc[:, hh:hh + 1], op=ALU.add),
                        reads=[atk, t_misc], writes=[ftk])
                p.op("dve", lambda e: e.reciprocal(out=fb[:, 2:4], in_=fb[:, 0:2]), reads=[ftk], writes=[ftk])
                for hh in range(2):
                    p.op("dve", lambda e, hh=hh, o_t=o_t: e.tensor_scalar(
                        out=o_t[:, qi, 64 * hh:64 * hh + 64], in0=abk[:, hh * 65:hh * 65 + 64], scalar1=fb[:, 2 + hh:3 + hh],
                        scalar2=None, op0=ALU.mult), reads=[atk, ftk], writes=[o_tk])

            if "swa" in parts:
                pipeline(4, [sw_a, sw_b])

            nst = 4 * g + 4
            sbst = {}
            dfst = {}
            dstarted = set()

            def sb_a(hh, s):
                kb = 4 * g + 3 - s
                qs = max(0, kb - 4 * g) * 128
                kg = t_k[kb // 4]
                bk, tk = scr.next()
                p.op("pe", lambda e: e.matmul(bk[:, qs:512], KB[64 * hh:64 * hh + 64, kb * 128:(kb + 1) * 128],
                                              QB[64 * hh:64 * hh + 64, qs:512], start=True, stop=True),
                     reads=[kg, t_q], writes=[tk])
                eb, etk = e_r.next()
                p.op("act", lambda e: e.activation(out=eb[:, qs:512], in_=bk[:, qs:512], func=AF.Exp, scale=SCALE),
                     reads=[tk], writes=[etk])
                Lb, Ltk = L_r.next()
                p.op("act", lambda e: e.activation(out=Lb[:, qs:512], in_=eb[:, qs:512], func=AF.Ln, bias=1.0, scale=1.0),
                     reads=[etk], writes=[Ltk])
                if kb >= 4 * g:
                    p.op("pool", lambda e: e.tensor_tensor(out=Lb[:, qs:qs + 128], in0=Lb[:, qs:qs + 128], in1=strict,
                                                           op=ALU.mult), reads=[Ltk, t_c], writes=[Ltk])
                sbst[(hh, s)] = dict(kb=kb, qs=qs, kg=kg, Lb=Lb, Ltk=Ltk, eb=eb, etk=etk)

            def sb_b(hh, s):
                d = sbst[(hh, s)]
                kb, qs, kg, Lb, Ltk, eb, etk = d["kb"], d["qs"], d["kg"], d["Lb"], d["Ltk"], d["eb"], d["etk"]
                cbk, ctk = CARRY[hh], t_carry[hh]
                p.op("pe", lambda e: e.matmul(cbk[:, qs:512], tri, Lb[:, qs:512], start=(s == 0), stop=True,
                                              skip_group_check=True), reads=[Ltk, t_c], writes=[ctk])
                Xb, Xtk = X_r.next()
                p.op("act", lambda e: e.activation(out=Xb[:, qs:512], in_=cbk[:, qs:512], func=AF.Exp, scale=-1.0),
                     reads=[ctk], writes=[Xtk])
                Wb, Wtk = W_r.next()
                p.op("dve", lambda e: e.tensor_tensor(out=Wb[:, qs:512], in0=eb[:, qs:512], in1=Xb[:, qs:512], op=ALU.mult),
                     reads=[etk, Xtk], writes=[Wtk])
                if kb >= 4 * g:
                    p.op("pool", lambda e: e.tensor_tensor(out=Wb[:, qs:qs + 128], in0=Wb[:, qs:qs + 128], in1=strict,
                                                           op=ALU.mult), reads=[Wtk, t_c], writes=[Wtk])
                d["Wb"], d["Wtk"] = Wb, Wtk

            def sb_c(hh, s):
                d = sbst.pop((hh, s))
                kb, qs, kg, Lb, Ltk, Wb, Wtk = d["kb"], d["qs"], d["kg"], d["Lb"], d["Ltk"], d["Wb"], d["Wtk"]
                cbk, ctk = CARRY[hh], t_carry[hh]
                if kb > 0:
                    p.op("pe", lambda e: e.matmul(cbk[:, qs:512], upper, Lb[:, qs:512], start=False, stop=True,
                                                  skip_group_check=True), reads=[Ltk, t_c], writes=[ctk])
                for qi in range(qs // 128, 4):
                    c0 = hh * 256 + qi * 64
                    p.op("pe", lambda e, qi=qi, c0=c0: e.matmul(
                        SBACC[:, c0:c0 + 64], Wb[:, qi * 128:(qi + 1) * 128], V[:, kb, 65 + 64 * hh:65 + 64 * hh + 64],
                        start=(s == 0 and hh == 0 and qi == 3), stop=(kb == 0), skip_group_check=True),
                        reads=[Wtk, kg], writes=[t_sbacc])
                if kb == 0:
                    p.op("dve", lambda e, o_t=o_t: e.tensor_copy(
                        out=o_t[:, :, 128 + 64 * hh:128 + 64 * hh + 64],
                        in_=SBACC[:, hh * 256:(hh + 1) * 256].rearrange("p (q d) -> p q d", d=64)),
                        reads=[t_sbacc], writes=[o_tk])

            def df_a(m, s):
                kb = s
                qs = max(0, kb - 4 * g) * 128
                kg = t_k[kb // 4]
                bk, tk = scr.next()
                p.op("pe", lambda e: e.matmul(bk[:, qs:512], KC[64 * m:64 * m + 64, kb * 128:(kb + 1) * 128],
                                              QC[64 * m:64 * m + 64, qs:512], start=True, stop=False),
                     reads=[kg, t_q], writes=[tk])
                p.op("pe", lambda e: e.matmul(bk[:, qs:512], ones[64 * m:64 * m + 2, :], qaug[64 * m:64 * m + 2, qs:512],
                                              start=False, stop=True), reads=[t_c], writes=[tk])
                Eb, Etk = E_r.next()
                jidx = (4 * g - kb) + 3
                p.op("act", lambda e: e.activation(out=Eb[:, qs:512], in_=bk[:, qs:512], func=AF.Exp, scale=SCALE,
                                                   bias=dbias(jidx)), reads=[tk, t_c], writes=[Etk])
                if kb >= 4 * g:
                    p.op("pool", lambda e: e.tensor_tensor(out=Eb[:, qs:qs + 128], in0=Eb[:, qs:qs + 128], in1=incl,
                                                           op=ALU.mult), reads=[Etk, t_c], writes=[Etk])
                dfst[(m, s)] = dict(kb=kb, qs=qs, kg=kg, Eb=Eb, Etk=Etk)

            def df_b(m, s):
                d = dfst.pop((m, s))
                kb, qs, kg, Eb, Etk = d["kb"], d["qs"], d["kg"], d["Eb"], d["Etk"]
                for qi in range(qs // 128, 4):
                    ab, atk, c0, bi = dacc(m, qi)
                    stt = bi not in dstarted
                    dstarted.add(bi)
                    p.op("pe", lambda e, qi=qi, ab=ab, c0=c0, stt=stt: e.matmul(
                        ab[:, c0:c0 + 129], Eb[:, qi * 128:(qi + 1) * 128], V[:, kb, 193:322],
                        start=stt, stop=(kb == 4 * g + qi), skip_group_check=True),
                        reads=[Etk, kg, t_c], writes=[atk])

            do_sb = "sb" in parts
            do_df = "df" in parts
            for step in range(nst + 2):
                if step < nst:
                    if do_sb:
                        sb_a(0, step)
                        sb_a(1, step)
                    if do_df:
                        df_a(0, step)
                        df_a(1, step)
                if 0 <= step - 2 < nst and do_sb:
                    sb_c(0, step - 2)
                    sb_c(1, step - 2)
                if 0 <= step - 1 < nst:
                    if do_sb:
                        sb_b(0, step - 1)
                        sb_b(1, step - 1)
                    if do_df:
                        df_b(0, step - 1)
                        df_b(1, step - 1)
                for _ in range(ndummy):
                    p.op("pe", lambda e: e.matmul(DACC[2][:, 260:508], tri, cb[:, 0:248], start=False, stop=True,
                                                  skip_group_check=True), reads=[t_c], writes=[t_junk])

            for qi in (range(4) if do_df else ()):
                a1, a1tk, c1, _ = dacc(0, qi)
                a2, a2tk, c2, _ = dacc(1, qi)
                fb, ftk = fin_r.next()
                p.op("dve", lambda e, fb=fb, a1=a1, c1=c1: e.reciprocal(out=fb[:, 0:1], in_=a1[:, c1 + 128:c1 + 129]),
                     reads=[a1tk], writes=[ftk])
                p.op("dve", lambda e, fb=fb, a2=a2, c2=c2: e.reciprocal(out=fb[:, 1:2], in_=a2[:, c2 + 128:c2 + 129]),
                     reads=[a2tk], writes=[ftk])
                p.op("dve", lambda e, fb=fb: e.tensor_tensor(out=fb[:, 2:3], in0=fb[:, 1:2], in1=misc[:, 4:5], op=ALU.mult),
                     reads=[ftk, t_misc], writes=[ftk])
                da, datk = da_r.next()
                p.op("dve", lambda e, fb=fb, a1=a1, c1=c1, da=da: e.tensor_scalar(
                    out=da[:], in0=a1[:, c1:c1 + 128], scalar1=fb[:, 0:1], scalar2=None, op0=ALU.mult),
                    reads=[a1tk, ftk], writes=[datk])
                dd, ddtk = dd_r.next()
                p.op("dve", lambda e, fb=fb, a2=a2, c2=c2, da=da, dd=dd: e.scalar_tensor_tensor(
                    out=dd[:], in0=a2[:, c2:c2 + 128], scalar=fb[:, 2:3], in1=da[:], op0=ALU.mult, op1=ALU.add),
                    reads=[a2tk, ftk, datk], writes=[ddtk])
                p.op("dve", lambda e, dd=dd, da=da: e.tensor_tensor(out=da[:], in0=dd[:], in1=dd[:], op=ALU.mult),
                     reads=[ddtk], writes=[datk])
                p.op("dve", lambda e, fb=fb, da=da: e.reduce_sum(out=fb[:, 3:4], in_=da[:], axis=AX.X),
                     reads=[datk], writes=[ftk])
                p.op("act", lambda e, fb=fb: e.activation(out=fb[:, 4:5], in_=fb[:, 3:4], func=AF.Ln, bias=RMS_EPS,
                                                          scale=1.0 / 128.0), reads=[ftk], writes=[ftk])
                p.op("act", lambda e, fb=fb: e.activation(out=fb[:, 5:6], in_=fb[:, 4:5], func=AF.Exp, scale=-0.5),
                     reads=[ftk], writes=[ftk])
                p.op("dve", lambda e, fb=fb, dd=dd, qi=qi, o_t=o_t: e.scalar_tensor_tensor(
                    out=o_t[:, qi, 256:384], in0=dd[:], scalar=fb[:, 5:6], in1=gsub[:], op0=ALU.mult, op1=ALU.mult),
                    reads=[ddtk, ftk, t_misc], writes=[o_tk])
            p.dma("sp", o_d[g * 512:(g + 1) * 512, :].rearrange("(q p) c -> p q c", p=128), o_t[:], reads=[o_tk],
                  is_out=True)
        p.finish()
    return nc


I32 = mybir.dt.int32
BIGPOS = 1.0e6


def _prog_dma_custom(p, q, fn, reads=(), writes=(), is_out=False):
    waits = p._deps(q, reads, writes, strict=True)
    n = getattr(p, "n_ind", 0)
    p.n_ind = n + 1
    sem = p.st.enter_context(p.nc.semaphore("ind%d" % n))
    ev = ("ind%d" % n, 16, sem)

    def run(eng, waits=waits, sem=sem, fn=fn):
        for s, v in waits:
            eng.wait_ge(s, v)
        fn(eng).then_inc(sem, 16)

    p.ops[q].append(run)
    p.meta[q].append(([(id(s_), v) for s_, v in waits], (id(sem), 16)))
    for t in reads:
        t.r.append(ev)
    for t in writes:
        t.w = ev
        t.r = []
    if is_out:
        p.outs.append(ev)


def build_phaseC2(S, CAP, NEX=8):
    J = S // 128
    NCH = -(-CAP // 1024)
    NK = CAP // 128
    nc = bass.Bass("TRN2", target_bir_lowering=False)
    x1_d = nc.dram_tensor("x1", [S, D], F32, kind="ExternalInput").ap()
    rw_d = nc.dram_tensor("rw", [S, 8], F32, kind="ExternalInput").ap()
    mod_d = nc.dram_tensor("mod", [2, D], F32, kind="ExternalInput").ap()
    wgu_d = nc.dram_tensor("wgu", [8, 2, 128, DC * FE], F32, kind="ExternalInput").ap()
    wd_d = nc.dram_tensor("wd", [8, 128, 3 * D], F32, kind="ExternalInput").ap()
    tid_d = nc.dram_tensor("tid", [128, J], F32, kind="ExternalInput").ap()
    cst_d = nc.dram_tensor("cst", [128, 384], F32, kind="ExternalInput").ap()
    y_d = nc.dram_tensor("y", [S, D], BF16, kind="ExternalOutput").ap()
    with ExitStack() as st:
        p = Prog(nc, st)
        sb = lambda name, shape, dt: _sb(nc, st, name, shape, dt)
        h2 = sb("h2", [128, DC, 1024], BF16)
        rwc = sb("rwc", [128, NK, 8], F32)
        idxc = sb("idxc", [128, NK], I32)
        RH = sb("RH", [128, J, 10], F32)
        tidf = sb("tidf", [128, J], F32)
        pk_r = Rot([sb("pk%d" % i, [128, J], F32) for i in range(2)])
        oh_r = Rot([sb("oh%d" % i, [128, 128], F32) for i in range(3)])
        Yacc = sb("Yacc", [128, 8, D], F32)
        wg_r = Rot([sb("wgt%d" % i, [128, DC * FE], BF16) for i in range(2)])
        wu_r = Rot([sb("wut%d" % i, [128, DC * FE], BF16) for i in range(2)])
        wd_r = Rot([sb("wdt%d" % i, [128, 3 * D], BF16) for i in range(2)])
        A = sb("A", [128, 3, 1024], BF16)
        sg_r = Rot([sb("sg%d" % i, [128, 512], F32) for i in range(2)])
        xg = sb("xg", [128, D], F32)
        R = sb("R", [128, J, 8], F32)
        sc = [sb("sc%d" % i, [128, J], F32) for i in range(2)]
        msk = sb("msk", [128, J], F32)
        posf = sb("posf", [128, J], F32)
        cst = sb("cst", [128, 384], F32)
        modT = sb("modT", [128, 2, DC], F32)
        offs = sb("offs", [128, 2], F32)
        zt = sb("zt", [128, D], BF16)
        banks = [_ps(nc, st, "bk%d" % i, [128, 512], F32) for i in range(8)]
        scr = Rot(banks)
        t_c, t_ix, t_h2, t_rw, t_A, t_xg, t_idl, t_ic, t_yd = [Tk() for _ in range(9)]
        t_y = [Tk() for _ in range(8)]
        ident = cst[:, 0:128]
        U2 = cst[:, 128:256]
        _bregs = {}

        def breg(e):
            if "r" not in _bregs:
                _bregs["r"] = e.to_reg(S - 1)
            return _bregs["r"]

        iota_s = cst[:, 256:384]

        p.dma("sp", cst[:], cst_d[:, :], writes=[t_c])
        p.dma("sp", tidf[:], tid_d[:, :], writes=[t_c])
        p.dma("sp", modT[:], mod_d.rearrange("m (k p) -> p m k", p=128), writes=[t_c], slow=True)
        p.dma("sp", R[:], rw_d.rearrange("(p j) e -> p j e", j=J), writes=[t_ix])
        p.op("dve", lambda e: e.memset(zt[:], 0.0), writes=[t_c])
        for k in range(J):
            p.dma("sp", y_d[k * 128:(k + 1) * 128, :], zt[:], reads=[t_c], writes=[t_yd], is_out=True)
        p.op("dve", lambda e: e.reduce_sum(out=sc[0][:], in_=R[:], axis=AX.X), reads=[t_ix], writes=[t_ix])
        p.op("dve", lambda e: e.tensor_scalar(out=msk[:], in0=sc[0][:], scalar1=0.0, scalar2=None, op0=ALU.is_gt),
             reads=[t_ix], writes=[t_ix])
        p.op("dve", lambda e: e.tensor_copy(out=sc[0][:], in_=msk[:]), reads=[t_ix], writes=[t_ix])
        cur = 0
        sh = 1
        while sh < J:
            a, b = sc[cur], sc[1 - cur]
            p.op("dve", lambda e, a=a, b=b, sh=sh: e.tensor_copy(out=b[:, 0:sh], in_=a[:, 0:sh]), reads=[t_ix], writes=[t_ix])
            p.op("dve", lambda e, a=a, b=b, sh=sh: e.tensor_tensor(out=b[:, sh:J], in0=a[:, sh:J], in1=a[:, 0:J - sh], op=ALU.add),
                 reads=[t_ix], writes=[t_ix])
            cur = 1 - cur
            sh *= 2
        incl = sc[cur]
        obk, otk = scr.next()
        p.op("pe", lambda e: e.matmul(obk[:, 0:1], U2, incl[:, J - 1:J], start=True, stop=True), reads=[t_ix, t_c], writes=[otk])
        p.op("dve", lambda e: e.tensor_scalar(out=offs[:, 0:1], in0=obk[:, 0:1], scalar1=-BIGPOS, scalar2=None, op0=ALU.add),
             reads=[otk], writes=[t_ix])
        p.op("dve", lambda e: e.tensor_tensor(out=posf[:], in0=incl[:], in1=msk[:], op=ALU.subtract), reads=[t_ix], writes=[t_ix])
        p.op("dve", lambda e: e.tensor_scalar(out=posf[:], in0=posf[:], scalar1=offs[:, 0:1], scalar2=None, op0=ALU.add),
             reads=[t_ix], writes=[t_ix])
        p.op("dve", lambda e: e.tensor_tensor(out=posf[:], in0=posf[:], in1=msk[:], op=ALU.mult), reads=[t_ix], writes=[t_ix])
        p.op("dve", lambda e: e.tensor_scalar(out=posf[:], in0=posf[:], scalar1=BIGPOS, scalar2=None, op0=ALU.add),
             reads=[t_ix], writes=[t_ix])
        p.op("dve", lambda e: e.memset(RH[:], 1.0), writes=[t_ix])
        p.op("dve", lambda e: e.tensor_copy(out=RH[:, :, 0], in_=tidf[:]), reads=[t_c], writes=[t_ix])
        p.op("dve", lambda e: e.tensor_copy(out=RH[:, :, 2:10], in_=R[:]), reads=[t_ix], writes=[t_ix])
        for k in range(NK):
            pk, pktk = pk_r.next()
            p.op("dve", lambda e, pk=pk, k=k: e.tensor_scalar(out=pk[:], in0=posf[:], scalar1=float(-128 * k), scalar2=None,
                                                              op0=ALU.add), reads=[t_ix], writes=[pktk])
            cbk, ctk = scr.next()
            for j in range(J):
                oh, ohtk = oh_r.next()
                p.op("dve", lambda e, oh=oh, pk=pk, j=j: e.tensor_scalar(out=oh[:], in0=iota_s, scalar1=pk[:, j:j + 1], scalar2=None,
                                                                         op0=ALU.is_equal), reads=[pktk, t_c], writes=[ohtk])
                p.op("pe", lambda e, oh=oh, cbk=cbk, j=j: e.matmul(cbk[:, 0:10], oh[:], RH[:, j, :], start=(j == 0), stop=(j == J - 1)),
                     reads=[ohtk, t_ix], writes=[ctk])
            p.op("dve", lambda e, cbk=cbk: e.tensor_scalar(out=offs[:, 1:2], in0=cbk[:, 1:2], scalar1=float(-S), scalar2=float(S),
                                                           op0=ALU.mult, op1=ALU.add), reads=[ctk], writes=[t_ic])
            p.op("dve", lambda e, cbk=cbk: e.tensor_tensor(out=offs[:, 1:2], in0=offs[:, 1:2], in1=cbk[:, 0:1], op=ALU.add),
                 reads=[ctk, t_ic], writes=[t_ic])
            p.op("dve", lambda e, k=k: e.tensor_copy(out=idxc[:, k:k + 1], in_=offs[:, 1:2]), reads=[t_ic], writes=[t_ic])
            p.op("dve", lambda e, cbk=cbk, k=k: e.tensor_copy(out=rwc[:, k, :], in_=cbk[:, 2:10]), reads=[ctk], writes=[t_rw])

        for ch in range(NCH):
            nslot = min(1024, CAP - ch * 1024)
            NTT = nslot // 128
            NHH = nslot // 512
            for t in range(NTT):
                k = ch * 8 + t
                p.op("pool", lambda e: e.memset(xg[:], 0.0), writes=[t_xg])
                _prog_dma_custom(p, "pool", lambda e, k=k: e.indirect_dma_start(
                    out=xg[:, :], out_offset=None, in_=x1_d[:, :],
                    in_offset=bass.IndirectOffsetOnAxis(ap=idxc[:, k:k + 1], axis=0), bounds_check=breg(e), oob_is_err=False),
                    reads=[t_ic], writes=[t_xg])
                for c4 in range(4):
                    bk, tk = scr.next()
                    for jj in range(4):
                        c = c4 * 4 + jj
                        p.op("pe", lambda e, bk=bk, jj=jj, c=c: e.transpose(
                            bk[:, jj * 128:(jj + 1) * 128], xg[:, c * 128:(c + 1) * 128], ident), reads=[t_xg, t_c], writes=[tk])
                    for jj in range(4):
                        c = c4 * 4 + jj
                        p.op("dve", lambda e, bk=bk, jj=jj, c=c, t=t: e.tensor_scalar(
                            out=h2[:, c, t * 128:(t + 1) * 128], in0=bk[:, jj * 128:(jj + 1) * 128], scalar1=modT[:, 0, c:c + 1],
                            scalar2=modT[:, 1, c:c + 1], op0=ALU.mult, op1=ALU.add), reads=[tk, t_c], writes=[t_h2])
            for ex in range(NEX):
                wgb, wgtk = wg_r.next()
                wub, wutk = wu_r.next()
                wdb, wdtk = wd_r.next()
                for j in range(3):
                    p.dma("pool", wgb[:, j * 2048:(j + 1) * 2048], wgu_d[ex, 0, :, j * 2048:(j + 1) * 2048], writes=[wgtk])
                    p.dma("pool", wub[:, j * 2048:(j + 1) * 2048], wgu_d[ex, 1, :, j * 2048:(j + 1) * 2048], writes=[wutk])
                for j in range(3):
                    p.dma("pool", wdb[:, j * 2048:(j + 1) * 2048], wd_d[ex, :, j * 2048:(j + 1) * 2048], writes=[wdtk])
                for f in range(3):
                    for nh in range(NHH):
                        gbk, gtk = scr.next()
                        for k in range(DC):
                            p.op("pe", lambda e, gbk=gbk, wgb=wgb, k=k, f=f, nh=nh: e.matmul(
                                gbk[:, :], wgb[:, k * FE + f * 128:k * FE + (f + 1) * 128], h2[:, k, nh * 512:(nh + 1) * 512],
                                start=(k == 0), stop=(k == DC - 1)), reads=[wgtk, t_h2], writes=[gtk])
                        ubk, utk = scr.next()
                        for k in range(DC):
                            p.op("pe", lambda e, ubk=ubk, wub=wub, k=k, f=f, nh=nh: e.matmul(
                                ubk[:, :], wub[:, k * FE + f * 128:k * FE + (f + 1) * 128], h2[:, k, nh * 512:(nh + 1) * 512],
                                start=(k == 0), stop=(k == DC - 1)), reads=[wutk, t_h2], writes=[utk])
                        sg, sgtk = sg_r.next()
                        p.op("act", lambda e, gbk=gbk, sg=sg: e.activation(out=sg[:], in_=gbk[:, :], func=AF.Silu),
                             reads=[gtk], writes=[sgtk])
                        p.op("dve", lambda e, ubk=ubk, sg=sg, f=f, nh=nh: e.tensor_tensor(
                            out=A[:, f, nh * 512:(nh + 1) * 512], in0=ubk[:, :], in1=sg[:], op=ALU.mult),
                            reads=[utk, sgtk], writes=[t_A])
                for t in range(NTT):
                    for db in range(4):
                        ybk, ytk = scr.next()
                        for f in range(3):
                            p.op("pe", lambda e, ybk=ybk, wdb=wdb, f=f, t=t, db=db: e.matmul(
                                ybk[:, :], A[:, f, t * 128:(t + 1) * 128], wdb[:, f * D + db * 512:f * D + (db + 1) * 512],
                                start=(f == 0), stop=(f == 2)), reads=[t_A, wdtk], writes=[ytk])
                        if ex == 0:
                            p.op("dve", lambda e, ybk=ybk, t=t, db=db, ex=ex, ch=ch: e.tensor_scalar(
                                out=Yacc[:, t, db * 512:(db + 1) * 512], in0=ybk[:, :], scalar1=rwc[:, ch * 8 + t, ex:ex + 1], scalar2=None,
                                op0=ALU.mult), reads=[ytk, t_rw], writes=[t_y[t]])
                        else:
                            p.op("dve", lambda e, ybk=ybk, t=t, db=db, ex=ex, ch=ch: e.scalar_tensor_tensor(
                                out=Yacc[:, t, db * 512:(db + 1) * 512], in0=ybk[:, :], scalar=rwc[:, ch * 8 + t, ex:ex + 1],
                                in1=Yacc[:, t, db * 512:(db + 1) * 512], op0=ALU.mult, op1=ALU.add),
                                reads=[ytk, t_rw, t_y[t]], writes=[t_y[t]])
            for t in range(NTT):
                _prog_dma_custom(p, "pool", lambda e, t=t, ch=ch: e.indirect_dma_start(
                    out=y_d[:, :], out_offset=bass.IndirectOffsetOnAxis(ap=idxc[:, ch * 8 + t:ch * 8 + t + 1], axis=0),
                    in_=Yacc[:, t, :], in_offset=None, bounds_check=breg(e), oob_is_err=False),
                    reads=[t_y[t], t_ic], writes=[t_yd], is_out=True)
        p.finish()
    return nc


def phaseC2_consts(S):
    J = S // 128
    tid = (np.arange(128)[:, None] * J + np.arange(J)[None, :]).astype(np.float32)
    pp = np.arange(128)
    U2 = (pp[:, None] < pp[None, :]).astype(np.float32)
    iota = np.broadcast_to(np.arange(128, dtype=np.float32)[None, :], (128, 128))
    cst = np.ascontiguousarray(np.concatenate([np.eye(128, dtype=np.float32), U2, iota], axis=1))
    return {"tid": np.ascontiguousarray(tid), "cst": cst}
```
